# Optimizing a Trainium2 kernel written in Bass

```python
import jax, jax.numpy as jnp
from jax import lax
import numpy as np

D_MODEL = 2048
BATCH = 4
SEQ = 2048
DEPTH = 1

HEAD_DIM = 64
ATTN_SCALE = HEAD_DIM ** -0.5
SWA_Q_HEADS = 16
SWA_KV_HEADS = 2
SWA_WINDOW = 128
SWA_BLOCK = 128
MOBA_HEADS = 16
MOBA_BLOCK = 256
MOBA_TOPK = 3
MOBA_Q_CHUNK = 16
N_GROUPS = 4
EXPERTS_PER_GROUP = 4
N_EXPERTS = N_GROUPS * EXPERTS_PER_GROUP
D_EXPERT = 512
TOPK_IN_GROUP = 2
EPS = 1e-6

SWA_Q_DIM = SWA_Q_HEADS * HEAD_DIM
SWA_KV_DIM = SWA_KV_HEADS * HEAD_DIM
MOBA_DIM = MOBA_HEADS * HEAD_DIM
IN_COLS = SWA_Q_DIM + 2 * SWA_KV_DIM + 3 * MOBA_DIM + 2 * D_MODEL

kernel_name = "hybrid_swa_moba_hier_moe"


def rms_norm(x, g):
    xf = x.astype(jnp.float32)
    y = xf * lax.rsqrt(jnp.mean(xf * xf, axis=-1, keepdims=True) + EPS)
    return (y * g.astype(jnp.float32)).astype(x.dtype)


def alibi_slopes(n):
    return jnp.exp2(-8.0 * jnp.arange(1, n + 1, dtype=jnp.float32) / n)


def swa_attention(q, k, v, sinks):
    B, S, Hq, dh = q.shape
    Hkv = k.shape[2]
    G = Hq // Hkv
    L = SWA_BLOCK
    nb = S // L
    qb = q.reshape(B, nb, L, Hkv, G, dh)
    kb = k.reshape(B, nb, L, Hkv, dh)
    vb = v.reshape(B, nb, L, Hkv, dh)
    zero = jnp.zeros_like(kb[:, :1])
    kw = jnp.concatenate([jnp.concatenate([zero, kb[:, :-1]], 1), kb], 2)
    vw = jnp.concatenate([jnp.concatenate([zero, vb[:, :-1]], 1), vb], 2)
    logits = jnp.einsum('bnqhgd,bnkhd->bnhgqk', qb, kw).astype(jnp.float32) * ATTN_SCALE
    qpos = jnp.arange(L)[:, None] + L
    kpos = jnp.arange(2 * L)[None, :]
    dist = qpos - kpos
    blk_ok = (jnp.arange(nb)[:, None, None] > 0) | (kpos[None] >= L)
    mask = (dist >= 0) & (dist < SWA_WINDOW) & blk_ok
    slopes = alibi_slopes(Hq).reshape(Hkv, G)[:, :, None, None]
    logits = logits - slopes * dist.astype(jnp.float32)
    logits = jnp.where(mask[None, :, None, None], logits, -jnp.inf)
    sink = jnp.broadcast_to(sinks.astype(jnp.float32).reshape(Hkv, G)[None, None, :, :, None, None],
                            logits.shape[:-1] + (1,))
    p = jax.nn.softmax(jnp.concatenate([logits, sink], -1), axis=-1)[..., :-1]
    out = jnp.einsum('bnhgqk,bnkhd->bnqhgd', p.astype(v.dtype), vw)
    return out.reshape(B, S, Hq, dh)


def moba_attention(q, k, v):
    B, S, H, dh = q.shape
    L = MOBA_BLOCK
    C = MOBA_Q_CHUNK
    nb = -(-S // L)
    Sp = nb * L
    padw = ((0, 0), (0, Sp - S), (0, 0), (0, 0))
    q, k, v = [jnp.pad(t, padw).transpose(0, 2, 1, 3) for t in (q, k, v)]
    kb = k.reshape(B, H, nb, L, dh)
    vb = v.reshape(B, H, nb, L, dh)
    t_blk = jnp.arange(Sp) // L
    slopes = alibi_slopes(H)[None, :, None, None]
    topk = min(MOBA_TOPK, nb - 1)
    if topk > 0:
        k_mean = jnp.mean(kb.astype(jnp.float32), axis=3)
        gate = jnp.einsum('bhtd,bhnd->bhtn', q.astype(jnp.float32), k_mean)
        past = jnp.arange(nb)[None, :] < t_blk[:, None]
        gate = jnp.where(past, gate, -jnp.inf)
        _, g_idx = lax.top_k(gate, topk)
        sel_ok = jnp.arange(topk)[None, :] < t_blk[:, None]
    gather = jax.vmap(jax.vmap(lambda blocks, ix: blocks[ix]))

    def chunk(c):
        start = c * C
        qc = lax.dynamic_slice_in_dim(q, start, C, axis=2)
        tq = start + jnp.arange(C)
        own = start // L
        k_own = lax.dynamic_index_in_dim(kb, own, axis=2, keepdims=False)
        v_own = lax.dynamic_index_in_dim(vb, own, axis=2, keepdims=False)
        d_own = (tq[:, None] - (own * L + jnp.arange(L))[None, :])
        lo = jnp.einsum('bhcd,bhkd->bhck', qc, k_own).astype(jnp.float32) * ATTN_SCALE
        lo = jnp.where(d_own >= 0, lo - slopes * d_own.astype(jnp.float32), -jnp.inf)
        if topk > 0:
            idx_c = lax.dynamic_slice_in_dim(g_idx, start, C, axis=2)
            ok_c = lax.dynamic_slice_in_dim(sel_ok, start, C, axis=0)
            kg = gather(kb, idx_c)
            vg = gather(vb, idx_c)
            d_g = tq[:, None, None] - (idx_c[..., None] * L + jnp.arange(L))
            lg = jnp.einsum('bhcd,bhcjkd->bhcjk', qc, kg).astype(jnp.float32) * ATTN_SCALE
            lg = lg - slopes[..., None] * d_g.astype(jnp.float32)
            lg = jnp.where(ok_c[:, :, None], lg, -jnp.inf).reshape(B, H, C, topk * L)
            p = jax.nn.softmax(jnp.concatenate([lg, lo], -1), axis=-1).astype(v.dtype)
            pg = p[..., :topk * L].reshape(B, H, C, topk, L)
            po = p[..., topk * L:]
            return (jnp.einsum('bhcjk,bhcjkd->bhcd', pg, vg)
                    + jnp.einsum('bhck,bhkd->bhcd', po, v_own))
        po = jax.nn.softmax(lo, axis=-1).astype(v.dtype)
        return jnp.einsum('bhck,bhkd->bhcd', po, v_own)

    outs = lax.map(chunk, jnp.arange(Sp // C))
    out = outs.transpose(1, 2, 0, 3, 4).reshape(B, H, Sp, dh)[:, :, :S]
    return out.transpose(0, 2, 1, 3)


def hierarchical_moe(h, w_router_group, w_router_expert, w_gate_e, w_up_e, w_down_e):
    B, S, D = h.shape
    t = h.reshape(B * S, D)
    grp_prob = jax.nn.softmax((t @ w_router_group).astype(jnp.float32), axis=-1)
    g_p, g_i = lax.top_k(grp_prob, 1)
    exp_logits = jnp.einsum('td,gde->tge', t, w_router_expert).astype(jnp.float32)
    sel_logits = jnp.take_along_axis(exp_logits, g_i[:, :, None], axis=1)[:, 0]
    e_p, e_i = lax.top_k(jax.nn.softmax(sel_logits, axis=-1), TOPK_IN_GROUP)
    w = g_p * (e_p / jnp.sum(e_p, axis=-1, keepdims=True))
    expert_id = g_i * EXPERTS_PER_GROUP + e_i
    combine = jnp.sum(jax.nn.one_hot(expert_id, N_EXPERTS, dtype=jnp.float32) * w[..., None], axis=1)
    hid = jax.nn.silu(jnp.einsum('td,edf->tef', t, w_gate_e)) * jnp.einsum('td,edf->tef', t, w_up_e)
    out = jnp.einsum('tef,efd->td', hid * combine[:, :, None].astype(hid.dtype), w_down_e)
    return out.reshape(B, S, D)


def setup_inputs(seed: int = 0) -> dict:
    key = jax.random.key(seed)
    ks = jax.random.split(key, 20)
    nrm = lambda k, shape, fan: jax.random.normal(k, shape, jnp.float32) * fan ** -0.5
    gain = lambda k, n: 1.0 + 0.1 * jax.random.normal(k, (n,), jnp.float32)
    return {
        "x": jax.random.normal(ks[0], (BATCH, SEQ, D_MODEL), jnp.float32),
        "g_mix": gain(ks[1], D_MODEL),
        "w_in": nrm(ks[2], (D_MODEL, IN_COLS), D_MODEL),
        "q_norm_swa": gain(ks[3], HEAD_DIM),
        "k_norm_swa": gain(ks[4], HEAD_DIM),
        "sinks": jax.random.normal(ks[5], (SWA_Q_HEADS,), jnp.float32),
        "q_norm_moba": gain(ks[6], HEAD_DIM),
        "k_norm_moba": gain(ks[7], HEAD_DIM),
        "w_up_swa": nrm(ks[8], (SWA_Q_DIM, D_MODEL), SWA_Q_DIM),
        "w_up_moba": nrm(ks[9], (MOBA_DIM, D_MODEL), MOBA_DIM),
        "w_out": nrm(ks[10], (D_MODEL, D_MODEL), D_MODEL),
        "g_ffn": gain(ks[11], D_MODEL),
        "w_router_group": nrm(ks[12], (D_MODEL, N_GROUPS), D_MODEL),
        "w_router_expert": nrm(ks[13], (N_GROUPS, D_MODEL, EXPERTS_PER_GROUP), D_MODEL),
        "w_gate_e": nrm(ks[14], (N_EXPERTS, D_MODEL, D_EXPERT), D_MODEL),
        "w_up_e": nrm(ks[15], (N_EXPERTS, D_MODEL, D_EXPERT), D_MODEL),
        "w_down_e": nrm(ks[16], (N_EXPERTS, D_EXPERT, D_MODEL), D_EXPERT),
    }


def reference(x, g_mix, w_in, q_norm_swa, k_norm_swa, sinks, q_norm_moba, k_norm_moba,
              w_up_swa, w_up_moba, w_out, g_ffn, w_router_group, w_router_expert,
              w_gate_e, w_up_e, w_down_e):
    B, S, D = x.shape
    for _ in range(DEPTH):
        h = rms_norm(x, g_mix)
        proj = h @ w_in
        cuts = np.cumsum([SWA_Q_DIM, SWA_KV_DIM, SWA_KV_DIM, MOBA_DIM, MOBA_DIM, MOBA_DIM, D_MODEL]).tolist()
        qa, ka, va, qb, kb, vb, gate_a, gate_b = jnp.split(proj, cuts, axis=-1)
        qa = rms_norm(qa.reshape(B, S, SWA_Q_HEADS, HEAD_DIM), q_norm_swa)
        ka = rms_norm(ka.reshape(B, S, SWA_KV_HEADS, HEAD_DIM), k_norm_swa)
        va = va.reshape(B, S, SWA_KV_HEADS, HEAD_DIM)
        qb = rms_norm(qb.reshape(B, S, MOBA_HEADS, HEAD_DIM), q_norm_moba)
        kb = rms_norm(kb.reshape(B, S, MOBA_HEADS, HEAD_DIM), k_norm_moba)
        vb = vb.reshape(B, S, MOBA_HEADS, HEAD_DIM)
        y_a = swa_attention(qa, ka, va, sinks).reshape(B, S, SWA_Q_DIM) @ w_up_swa
        y_b = moba_attention(qb, kb, vb).reshape(B, S, MOBA_DIM) @ w_up_moba
        merged = jax.nn.sigmoid(gate_a) * y_a + jax.nn.sigmoid(gate_b) * y_b
        x = x + merged @ w_out
        h = rms_norm(x, g_ffn)
        x = x + hierarchical_moe(h, w_router_group, w_router_expert, w_gate_e, w_up_e, w_down_e)
    return x
```

```python
import numpy as np
import ml_dtypes
from contextlib import ExitStack
import concourse.bass as bass
import concourse.mybir as mybir
from concourse.bass_utils import run_bass_kernel_spmd

F32 = mybir.dt.float32
BF16 = mybir.dt.bfloat16
U8 = mybir.dt.uint8
ALU = mybir.AluOpType
AF = mybir.ActivationFunctionType
AX = mybir.AxisListType
NPBF = ml_dtypes.bfloat16

D = 2048
NOWN = 1024
NKV = 2048
EPS = 1e-6
SCALE = 0.125
BIG = 32768.0
IN_COLS = 8448
C_QA, C_KA, C_VA, C_QB, C_KB, C_VB, C_GA, C_GB = 0, 1024, 1152, 1280, 2304, 3328, 4352, 6400
SLOPES = [2.0 ** (-(h + 1) / 2.0) for h in range(16)]
ARENA = 211968


class Buf:
    __slots__ = ("name", "lw", "lr", "excl")

    def __init__(self, name, excl=False):
        self.name = name
        self.lw = None
        self.lr = {}
        self.excl = excl


class DSem:
    def __init__(self, h):
        self.h = h
        self.count = 0


class Op:
    __slots__ = ("eng", "fn", "deps", "signal", "dsem", "val", "idx")


ENGS = ("pe", "act", "dve", "pool", "sp")


class Prog:
    def __init__(self):
        self.ops = []

    @staticmethod
    def _need(p, eng, is_dma, raw):
        if p.dsem is not None or is_dma:
            return True
        if p.eng != eng:
            return True
        if eng == "pe":
            return False
        return raw

    def add(self, eng, fn, reads=(), writes=(), dsem=None, ndma=1):
        idx = len(self.ops)
        op = Op()
        op.eng, op.fn, op.signal, op.dsem, op.idx, op.val = eng, fn, False, dsem, idx, None
        is_dma = dsem is not None
        key = ("d", idx) if is_dma else eng
        deps = set()
        for b in reads:
            w = b.lw
            if w is not None and self._need(self.ops[w], eng, is_dma, True):
                deps.add(w)
            if b.excl:
                for k2, r in b.lr.items():
                    if k2 != key:
                        deps.add(r)
        for b in writes:
            w = b.lw
            if w is not None and self._need(self.ops[w], eng, is_dma, False):
                deps.add(w)
            for r in b.lr.values():
                if self._need(self.ops[r], eng, is_dma, False):
                    deps.add(r)
        for b in reads:
            b.lr[key] = idx
        for b in writes:
            b.lw = idx
            b.lr = {}
        for d in deps:
            self.ops[d].signal = True
        op.deps = sorted(deps)
        if is_dma:
            dsem.count += 16 * ndma
            op.val = dsem.count
        self.ops.append(op)
        return op

    def emit(self, block, engsem):
        cnt = {}
        for op in self.ops:
            if op.dsem is None and op.signal:
                cnt[op.eng] = cnt.get(op.eng, 0) + 1
                op.val = cnt[op.eng]
        by = {e: [] for e in ENGS}
        for op in self.ops:
            by[op.eng].append(op)
        ops = self.ops

        def run(name):
            def body(e):
                waited = {}
                for op in by[name]:
                    for d in op.deps:
                        p = ops[d]
                        if p.dsem is not None:
                            sem, k = p.dsem.h, ("d", id(p.dsem))
                        else:
                            sem, k = engsem[p.eng], p.eng
                        if waited.get(k, 0) < p.val:
                            e.wait_ge(sem, p.val)
                            waited[k] = p.val
                    if op.fn is None:
                        continue
                    r = op.fn(e)
                    if op.dsem is not None:
                        for ins in (r if isinstance(r, (list, tuple)) else [r]):
                            ins.then_inc(op.dsem.h, 16)
                    elif op.signal:
                        r.then_inc(engsem[op.eng], 1)
            return body

        block.tensor(run("pe"))
        block.scalar(run("act"))
        block.vector(run("dve"))
        block.gpsimd(run("pool"))
        block.sync(run("sp"))


def _split3(a):
    a = a.astype(np.float64)
    hi = a.astype(NPBF)
    r = a - hi.astype(np.float64)
    mid = r.astype(NPBF)
    r = r - mid.astype(np.float64)
    lo = r.astype(NPBF)
    return hi, mid, lo


def _const_tables():
    c = {}
    c["ident_bf"] = np.eye(128, dtype=np.float32).astype(NPBF)
    c["ident_f"] = np.eye(128, dtype=np.float32)
    bd = np.zeros((128, 128), np.float32)
    bd[:64, :64] = 1.0 / 64
    bd[64:, 64:] = 1.0 / 64
    c["bd64"] = bd.astype(NPBF)
    k = np.arange(128)[:, None].astype(np.float64)
    q = np.arange(128)[None, :].astype(np.float64)
    da = q - k
    da = np.where(da >= 0, da, 1e9)
    db = q + 128 - k
    db = np.where(db < 128, db, 1e9)
    c["dsw"] = np.concatenate([db, da, db, da], axis=1).astype(np.float32)
    q2 = np.arange(256)[None, :]
    m0 = np.where(q2 >= np.arange(128)[:, None], 0.0, -1e9)
    m1 = np.where(q2 >= (np.arange(128)[:, None] + 128), 0.0, -1e9)
    c["mneg"] = np.concatenate([m0, m1], axis=1).astype(np.float32)
    tq = np.arange(NOWN).astype(np.float64)
    tk = (np.arange(NKV) - 1024).astype(np.float64)
    tabq = np.zeros((16, 6, NOWN), NPBF)
    tabk = np.zeros((16, 14, NKV), NPBF)
    ind = (np.arange(NKV)[None, :] // 256 == np.arange(8)[:, None]).astype(np.float32)
    for h in range(16):
        s = SLOPES[h]
        a = _split3(-s * tq / SCALE)
        b = _split3(s * tk / SCALE)
        for i in range(3):
            tabq[h, i] = a[i]
            tabq[h, 3 + i] = 1.0
            tabk[h, 8 + i] = 1.0
            tabk[h, 11 + i] = b[i]
        tabk[h, 0:8] = ind.astype(NPBF)
    c["tabq"] = tabq
    c["tabk"] = tabk
    return c


def _percore_tables(hf):
    pb = np.full((128, 8, 8), -1e30, np.float32)
    for t in range(8):
        own = 4 + t // 2
        for n in range(8):
            if n == own:
                pb[:, t, n] = 1e30
            elif n < own and (hf == 1 or n >= 4):
                pb[:, t, n] = 0.0
    pv = np.full((128, 1), float(hf), np.float32)
    return {"pastbias": pb.reshape(128, 64), "pvcol": pv}


def build_program(stage=99, debug=False):
    nc = bass.Bass("TRN2", target_bir_lowering=False)
    es = ExitStack()
    prog = Prog()
    dbg_outs = {}

    def din(name, shape, dt):
        return nc.dram_tensor(name, list(shape), dt, kind="ExternalInput").ap()

    xs = din("xs", [NKV, D], F32)
    w_in = din("w_in", [D, IN_COLS], F32)
    w_up_swa = din("w_up_swa", [1024, D], F32)
    w_up_moba = din("w_up_moba", [1024, D], F32)
    w_out = din("w_out", [D, D], F32)
    w_r = din("w_r", [D, 20], F32)
    w_gate_e = din("w_gate_e", [16, D, 512], F32)
    w_up_e = din("w_up_e", [16, D, 512], F32)
    w_down_e = din("w_down_e", [16, 512, D], F32)
    gmix_bc = din("gmix_bc", [128, D], F32)
    gffn_bc = din("gffn_bc", [128, D], F32)
    gcols_d = din("gcols", [128, 4], F32)
    sinks_bc = din("sinks_bc", [128, 16], F32)
    ident_bf_d = din("ident_bf", [128, 128], BF16)
    ident_f_d = din("ident_f", [128, 128], F32)
    bd64_d = din("bd64", [128, 128], BF16)
    dsw_d = din("dsw", [128, 512], F32)
    mneg_d = din("mneg", [128, 512], F32)
    tabq_d = din("tabq", [16, 6, NOWN], BF16)
    tabk_d = din("tabk", [16, 14, NKV], BF16)
    pastbias_d = din("pastbias", [128, 64], F32)
    pvcol_d = din("pvcol", [128, 1], F32)
    y = nc.dram_tensor("y", [NOWN, D], F32, kind="ExternalOutput").ap()

    arena = es.enter_context(nc.sbuf_tensor("arena", [128, ARENA], U8))
    psum = es.enter_context(nc.psum_tensor("psum", [128, 4096], F32))

    def PS(b, lo=0, hi=512):
        return psum[:, b * 512 + lo:b * 512 + hi]

    def PSB(b, lo=0, hi=1024):
        return psum[:, b * 512:(b + 1) * 512].bitcast(BF16)[:, lo:hi]

    B_ps = [Buf("ps%d" % i, excl=True) for i in range(8)]

    class Region:
        def __init__(self, base, size):
            self.base, self.size, self.off = base, size, 0

        def reset(self, off=0):
            self.off = off

        def alloc(self, shape, dt, at=None):
            nb = {F32: 4, BF16: 2}[dt]
            n = int(np.prod(shape[1:])) * nb
            n32 = (n + 31) // 32 * 32
            off = self.off if at is None else at
            assert off + n32 <= self.size, (off, n32, self.size)
            if at is None:
                self.off += n32
            v = arena[:, self.base + off:self.base + off + n].bitcast(dt)
            if len(shape) == 3:
                v = v.rearrange("p (a b) -> p a b", b=shape[2])
            return v

    CONST_SZ = 6144
    R1_SZ = 65536
    R2_SZ = 32768
    RC = Region(0, CONST_SZ)
    R1 = Region(CONST_SZ, R1_SZ)
    R2 = Region(CONST_SZ + R1_SZ, R2_SZ)
    R3 = Region(CONST_SZ + R1_SZ + R2_SZ, ARENA - CONST_SZ - R1_SZ - R2_SZ)

    sem_id = [0]

    def new_sem():
        sem_id[0] += 1
        return DSem(es.enter_context(nc.semaphore("s%d" % sem_id[0])))

    engsem = {e: es.enter_context(nc.semaphore("eng_" + e)) for e in ENGS}

    ident_bf = RC.alloc([128, 128], BF16)
    ident_f = RC.alloc([128, 128], F32)
    bd64 = RC.alloc([128, 128], BF16)
    gcols = RC.alloc([128, 4], F32)
    epscol = RC.alloc([128, 1], F32)
    expsink = RC.alloc([128, 16], F32)
    pvcol = RC.alloc([128, 1], F32)
    pastbias = RC.alloc([128, 64], F32)
    dsw = RC.alloc([128, 512], F32)
    mneg = RC.alloc([128, 512], F32)
    ss1 = RC.alloc([128, 16], F32)
    ln1 = RC.alloc([128, 16], F32)
    rs1 = RC.alloc([128, 16], F32)
    B_const = Buf("const")
    S_const = new_sem()
    const_loads = [(ident_bf, ident_bf_d), (ident_f, ident_f_d), (bd64, bd64_d), (gcols, gcols_d),
                   (expsink, sinks_bc), (pvcol, pvcol_d), (pastbias, pastbias_d), (dsw, dsw_d), (mneg, mneg_d)]

    def _ld_consts(e):
        return [e.dma_start(out=o, in_=i[:, :]) for o, i in const_loads]
    prog.add("sp", _ld_consts, writes=[B_const], dsem=S_const, ndma=len(const_loads))
    B_eps = Buf("eps")
    prog.add("dve", lambda e: e.memset(epscol, EPS), writes=[B_eps])
    B_ss1 = [Buf("ss1_%d" % t) for t in range(16)]
    prog.add("dve", lambda e: e.memset(ss1, 0.0), writes=B_ss1)
    B_expsink = Buf("expsink")
    prog.add("act", lambda e: e.activation(out=expsink, in_=expsink, func=AF.Exp), reads=[B_const], writes=[B_expsink])

    hTp = R1.alloc([128, 16, 1024], BF16)
    hTo = R1.alloc([128, 16, 1024], BF16)
    B_hTp, B_hTo = Buf("hTp"), Buf("hTo")
    attn_swa = R2.alloc([128, 8, 1024], BF16)
    attn_moba = R2.alloc([128, 8, 1024], BF16)
    B_attn_swa = [Buf("attn_swa%d" % i) for i in range(8)]
    B_attn_moba = [Buf("attn_moba%d" % i) for i in range(8)]

    NW = 6
    wslot = [R3.alloc([128, 16, 128], BF16) for _ in range(NW)]
    B_w = [Buf("w%d" % i) for i in range(NW)]
    S_w = [new_sem() for _ in range(NW)]
    QA = [[R3.alloc([128, 1024], BF16) for _ in range(2)] for _ in range(2)]
    B_QA = [[Buf("QA%d%d" % (i, j)) for j in range(2)] for i in range(2)]
    B_QAaug = [[Buf("QAaug%d%d" % (i, j)) for j in range(2)] for i in range(2)]
    S_QAaug = [[new_sem() for j in range(2)] for i in range(2)]
    off_KA = R3.off
    KA = [[R3.alloc([128, 2048], BF16) for _ in range(2)] for _ in range(2)]
    B_KA = [[Buf("KA%d%d" % (i, j)) for j in range(2)] for i in range(2)]
    B_KAaug = [[Buf("KAaug%d%d" % (i, j)) for j in range(2)] for i in range(2)]
    S_KAaug = [[new_sem() for j in range(2)] for i in range(2)]
    VA = [[R3.alloc([128, 16, 128], BF16) for _ in range(2)] for _ in range(2)]
    B_VA = [[Buf("VA%d%d" % (i, j)) for j in range(2)] for i in range(2)]
    KS = [R3.alloc([128, 1152], BF16) for _ in range(2)]
    B_KS = [Buf("KS%d" % i) for i in range(2)]
    VS = [R3.alloc([128, 9, 128], BF16) for _ in range(2)]
    B_VS = [Buf("VS%d" % i) for i in range(2)]
    sqb = [R3.alloc([128, 512], BF16) for _ in range(2)]
    B_sqb = [Buf("sqb%d" % i) for i in range(2)]
    lnb = [R3.alloc([128, 512], F32) for _ in range(2)]
    B_lnb = [Buf("lnb%d" % i) for i in range(2)]
    rsb = [R3.alloc([128, 512], F32) for _ in range(2)]
    B_rsb = [Buf("rsb%d" % i) for i in range(2)]
    tmpB = [R3.alloc([128, 512], BF16) for _ in range(2)]
    B_tmpB = [Buf("tmpB%d" % i) for i in range(2)]
    off_Sp = R3.off
    Sp = [R3.alloc([128, 512], F32) for _ in range(2)]
    B_Sp = [Buf("Sp%d" % i) for i in range(2)]
    NPT = 4
    Pt = [R3.alloc([128, 512], BF16) for _ in range(NPT)]
    B_Pt = [Buf("Pt%d" % i) for i in range(NPT)]
    Rt = [R3.alloc([128, 256], F32) for _ in range(2)]
    B_Rt = [Buf("Rt%d" % i) for i in range(2)]
    Rt2 = [R3.alloc([128, 256], F32) for _ in range(2)]
    tmpo = [R3.alloc([128, 256], BF16) for _ in range(2)]
    B_tmpo = [Buf("tmpo%d" % i) for i in range(2)]
    gm = R3.alloc([128, 8, 8], F32)
    m8 = R3.alloc([128, 8, 8], F32)
    selt = R3.alloc([128, 8, 8], F32)
    kmf = R3.alloc([128, 8], F32)
    kmb = R3.alloc([128, 8], BF16)
    B_gm, B_m8, B_selt, B_kmf, B_kmb = Buf("gm"), Buf("m8"), Buf("selt"), Buf("kmf"), Buf("kmb")
    stage_t = [R3.alloc([128, 8, 72], BF16) for _ in range(2)]
    B_stage = [Buf("stage%d" % i) for i in range(2)]
    attn_end = R3.off
    xt = [R3.alloc([128, D], F32, at=off_KA + i * 8192) for i in range(3)]
    B_xt = [Buf("xt%d" % i) for i in range(3)]
    S_xt = [new_sem() for _ in range(3)]
    gbc = R3.alloc([128, D], F32, at=off_KA + 3 * 8192)
    xn = [R3.alloc([128, D], BF16, at=off_KA + 4 * 8192 + i * 4096) for i in range(2)]
    B_xn = [Buf("xn%d" % i) for i in range(2)]
    junk = R3.alloc([128, D], BF16, at=off_Sp)
    B_junk = Buf("junk")
    B_gbc = Buf("gbc")
    S_gbc = new_sem()
    bar_scr = RC.alloc([128, 8], F32)

    def barrier(old, new):
        prog.add("dve", lambda e: e.memset(bar_scr, 0.0), writes=list(old) + list(new))

    prog.add("sp", lambda e: e.dma_start(out=gbc, in_=gmix_bc[:, :]), writes=[B_gbc], dsem=S_gbc)
    for t in range(16):
        sl = t % 3
        x2 = t % 2
        prog.add("sp", lambda e, t=t, sl=sl: e.dma_start(out=xt[sl], in_=xs[t * 128:(t + 1) * 128, :]),
                 writes=[B_xt[sl]], dsem=S_xt[sl])
        prog.add("act", lambda e, t=t, sl=sl: e.activation(out=junk, in_=xt[sl], func=AF.Square,
                                                           accum_out=ss1[:, t:t + 1]),
                 reads=[B_xt[sl], B_ss1[t]], writes=[B_junk, B_ss1[t]])
        prog.add("act", lambda e, t=t: e.activation(out=ln1[:, t:t + 1], in_=ss1[:, t:t + 1], func=AF.Ln,
                                                    scale=1.0 / D, bias=epscol[:, 0:1]),
                 reads=[B_ss1[t], B_eps], writes=[B_ss1[t]])
        prog.add("act", lambda e, t=t: e.activation(out=rs1[:, t:t + 1], in_=ln1[:, t:t + 1], func=AF.Exp, scale=-0.5),
                 reads=[B_ss1[t]], writes=[B_ss1[t]])
        prog.add("dve", lambda e, t=t, sl=sl, x2=x2: e.scalar_tensor_tensor(
            out=xn[x2], in0=xt[sl], scalar=rs1[:, t:t + 1], op0=ALU.mult, in1=gbc, op1=ALU.mult),
            reads=[B_xt[sl], B_ss1[t], B_gbc], writes=[B_xn[x2]])
        dst = hTp if t < 8 else hTo
        Bdst = B_hTp if t < 8 else B_hTo
        tc = (t % 8) * 128
        for k in range(16):
            bank = 6 + k // 8
            prog.add("pe", lambda e, k=k, x2=x2, bank=bank: e.transpose(
                out=PSB(bank, (k % 8) * 128, (k % 8 + 1) * 128), in_=xn[x2][:, k * 128:(k + 1) * 128],
                identity=ident_bf), reads=[B_xn[x2], B_const], writes=[B_ps[bank]])
        prog.add("act", lambda e, dst=dst, tc=tc: e.activation(
            out=dst[:, 0:8, tc:tc + 128], in_=PSB(6).rearrange("p (a b) -> p a b", b=128), func=AF.Copy),
            reads=[B_ps[6]], writes=[Bdst])
        prog.add("dve", lambda e, dst=dst, tc=tc: e.tensor_copy(
            out=dst[:, 8:16, tc:tc + 128], in_=PSB(7).rearrange("p (a b) -> p a b", b=128)),
            reads=[B_ps[7]], writes=[Bdst])

    if debug and stage == 1:
        dbg_outs["hTp"] = (hTp, [128, 16 * 1024], BF16, [B_hTp])
        dbg_outs["hTo"] = (hTo, [128, 16 * 1024], BF16, [B_hTo])

    w_in_v = w_in.rearrange("(k p) n -> p k n", p=128)
    wring = [0]

    def load_wcols(c0):
        s = wring[0] % NW
        wring[0] += 1
        prog.add("pool", lambda e: e.dma_start(out=wslot[s], in_=w_in_v[:, :, c0:c0 + 128]),
                 writes=[B_w[s]], dsem=S_w[s])
        return s

    pbank = [0]
    nrm = [0]

    def proj_fm(s, src, Bsrc, lo, n):
        bank = pbank[0] % 2
        pbank[0] += 1
        for k in range(16):
            prog.add("pe", lambda e, k=k: e.matmul(PS(bank, 0, n), lhsT=wslot[s][:, k, :], rhs=src[:, k, lo:lo + n],
                                                   start=(k == 0), stop=(k == 15)),
                     reads=[B_w[s], Bsrc], writes=[B_ps[bank]])
        return bank

    def headnorm(bank, n, gidx, dstA, BA, dstB, BB):
        i = nrm[0] % 2
        nrm[0] += 1
        prog.add("act", lambda e: e.activation(out=sqb[i][:, 0:n], in_=PS(bank, 0, n), func=AF.Square),
                 reads=[B_ps[bank]], writes=[B_sqb[i]])
        prog.add("pe", lambda e: e.matmul(PS(2, 0, n), lhsT=bd64, rhs=sqb[i][:, 0:n], start=True, stop=True),
                 reads=[B_sqb[i], B_const], writes=[B_ps[2]])
        prog.add("act", lambda e: e.activation(out=lnb[i][:, 0:n], in_=PS(2, 0, n), func=AF.Ln, bias=epscol[:, 0:1]),
                 reads=[B_ps[2], B_eps], writes=[B_lnb[i]])
        prog.add("act", lambda e: e.activation(out=rsb[i][:, 0:n], in_=lnb[i][:, 0:n], func=AF.Exp, scale=-0.5),
                 reads=[B_lnb[i]], writes=[B_rsb[i]])
        prog.add("dve", lambda e: e.scalar_tensor_tensor(
            out=dstA, in0=PS(bank, 0, n)[0:64, :], scalar=gcols[0:64, gidx:gidx + 1], op0=ALU.mult,
            in1=rsb[i][0:64, 0:n], op1=ALU.mult), reads=[B_ps[bank], B_rsb[i], B_const], writes=[BA])
        prog.add("dve", lambda e: e.scalar_tensor_tensor(
            out=tmpB[i][64:128, 0:n], in0=PS(bank, 0, n)[64:128, :], scalar=gcols[64:128, gidx:gidx + 1], op0=ALU.mult,
            in1=rsb[i][64:128, 0:n], op1=ALU.mult), reads=[B_ps[bank], B_rsb[i], B_const], writes=[B_tmpB[i]])
        prog.add("dve", lambda e: e.tensor_copy(out=dstB, in_=tmpB[i][64:128, 0:n]),
                 reads=[B_tmpB[i]], writes=[BB])

    sbank = [0]
    obank = [0]
    ptc = [0]
    rtc = [0]

    def finish_block(ob, nq, dst_pair, Bdst, parity, qlo, sink_h=None):
        r = rtc[0] % 2
        rtc[0] += 1
        if sink_h is not None:
            prog.add("act", lambda e: e.activation(out=Rt2[r][64:128, 0:nq], in_=PS(ob, 0, nq)[64:128, :], func=AF.Ln,
                                                   bias=expsink[64:128, sink_h:sink_h + 1]),
                     reads=[B_ps[ob], B_expsink], writes=[B_Rt[r]])
        else:
            prog.add("act", lambda e: e.activation(out=Rt2[r][64:128, 0:nq], in_=PS(ob, 0, nq)[64:128, :], func=AF.Ln),
                     reads=[B_ps[ob]], writes=[B_Rt[r]])
        prog.add("act", lambda e: e.activation(out=Rt2[r][64:128, 0:nq], in_=Rt2[r][64:128, 0:nq], func=AF.Exp, scale=-1.0),
                 reads=[B_Rt[r]], writes=[B_Rt[r]])
        prog.add("dve", lambda e: e.tensor_copy(out=Rt[r][0:64, 0:nq], in_=Rt2[r][64:128, 0:nq]),
                 reads=[B_Rt[r]], writes=[B_Rt[r]])
        if parity == 0:
            prog.add("dve", lambda e: e.tensor_tensor(out=dst_pair[0:64, qlo:qlo + nq], in0=PS(ob, 0, nq)[0:64, :],
                                                      in1=Rt[r][0:64, 0:nq], op=ALU.mult),
                     reads=[B_ps[ob], B_Rt[r]], writes=[Bdst])
        else:
            prog.add("dve", lambda e: e.tensor_tensor(out=tmpo[r][0:64, 0:nq], in0=PS(ob, 0, nq)[0:64, :],
                                                      in1=Rt[r][0:64, 0:nq], op=ALU.mult),
                     reads=[B_ps[ob], B_Rt[r]], writes=[B_tmpo[r]])
            prog.add("dve", lambda e: e.tensor_copy(out=dst_pair[64:128, qlo:qlo + nq], in_=tmpo[r][0:64, 0:nq]),
                     reads=[B_tmpo[r]], writes=[Bdst])

    def run_units(units):
        n = len(units)
        if not n:
            return
        units[0]["S"]()
        if n > 1:
            units[1]["S"]()
        units[0]["E"]()
        for i, u in enumerate(units):
            u["PV"]()
            if i + 2 < n:
                units[i + 2]["S"]()
            if i + 1 < n:
                units[i + 1]["E"]()
            if u.get("F") is not None:
                u["F"]()

    def swa_kv():
        for g in range(2):
            prog.add("dve", lambda e, g=g: e.memset(VS[g][:, :, 64:128], 1.0), writes=[B_VS[g]])
            prog.add("dve", lambda e, g=g: e.tensor_scalar(out=VS[g][:, 0:1, 64:128], in0=VS[g][:, 0:1, 64:128],
                                                           scalar1=pvcol[:, 0:1], scalar2=None, op0=ALU.mult),
                     reads=[B_const, B_VS[g]], writes=[B_VS[g]])
        sk = load_wcols(C_KA)
        sv = load_wcols(C_VA)
        for (src, Bsrc, lo, n, dlo) in ((hTp, B_hTp, 896, 128, 0), (hTo, B_hTo, 0, 512, 128), (hTo, B_hTo, 512, 512, 640)):
            bank = proj_fm(sk, src, Bsrc, lo, n)
            headnorm(bank, n, 1, KS[0][0:64, dlo:dlo + n], B_KS[0], KS[1][0:64, dlo:dlo + n], B_KS[1])
        for grp in ((7, 8, 9, 10), (11, 12, 13, 14), (15,)):
            bank = pbank[0] % 2
            pbank[0] += 1
            for j, t in enumerate(grp):
                src, Bsrc, tc = (hTp, B_hTp, t * 128) if t < 8 else (hTo, B_hTo, (t - 8) * 128)
                for k in range(16):
                    prog.add("pe", lambda e, k=k, j=j, src=src, tc=tc, bank=bank: e.matmul(
                        PS(bank, j * 128, (j + 1) * 128), lhsT=src[:, k, tc:tc + 128], rhs=wslot[sv][:, k, :],
                        start=(k == 0), stop=(k == 15)), reads=[B_w[sv], Bsrc], writes=[B_ps[bank]])
            for g in range(2):
                for j, t in enumerate(grp):
                    if t < 8:
                        prog.add("act", lambda e, g=g, j=j, t=t, bank=bank: e.activation(
                            out=VS[g][:, t - 7, 0:64], in_=PS(bank, j * 128 + g * 64, j * 128 + g * 64 + 64),
                            func=AF.Copy, scale=pvcol[:, 0:1]), reads=[B_ps[bank], B_const], writes=[B_VS[g]])
                    else:
                        prog.add("act", lambda e, g=g, j=j, t=t, bank=bank: e.activation(
                            out=VS[g][:, t - 7, 0:64], in_=PS(bank, j * 128 + g * 64, j * 128 + g * 64 + 64),
                            func=AF.Copy), reads=[B_ps[bank]], writes=[B_VS[g]])

    def swa_qproj(p, buf):
        s = load_wcols(C_QA + p * 128)
        for n in range(2):
            bank = proj_fm(s, hTo, B_hTo, n * 512, 512)
            headnorm(bank, 512, 0, QA[buf][0][0:64, n * 512:(n + 1) * 512], B_QA[buf][0],
                     QA[buf][1][0:64, n * 512:(n + 1) * 512], B_QA[buf][1])

    def swa_attn(p, buf):
        units = []
        for i in range(2):
            h = 2 * p + i
            g = h // 8
            q = QA[buf][i]
            Bq = B_QA[buf][i]
            for c in range(4):
                i0 = 8 + 2 * c - 7
                sb = 3 + sbank[0] % 2
                sbank[0] += 1
                ob = 5 + obank[0] % 2
                obank[0] += 1
                pi = ptc[0] % NPT
                ptc[0] += 1
                si = sb - 3

                def S(q=q, Bq=Bq, g=g, c=c, i0=i0, sb=sb):
                    qa = 2 * c * 128
                    for (olo, ohi, kt, qlo, qhi) in ((0, 128, i0 - 1, qa, qa + 128), (128, 384, i0, qa, qa + 256),
                                                     (384, 512, i0 + 1, qa + 128, qa + 256)):
                        prog.add("pe", lambda e, olo=olo, ohi=ohi, kt=kt, qlo=qlo, qhi=qhi: e.matmul(
                            PS(sb, olo, ohi), lhsT=KS[g][0:64, kt * 128:(kt + 1) * 128], rhs=q[0:64, qlo:qhi],
                            start=True, stop=True), reads=[B_KS[g], Bq], writes=[B_ps[sb]])

                def E(h=h, sb=sb, si=si, pi=pi):
                    prog.add("dve", lambda e: e.scalar_tensor_tensor(
                        out=Sp[si], in0=dsw, scalar=-SLOPES[h] / SCALE, op0=ALU.mult, in1=PS(sb), op1=ALU.add),
                        reads=[B_ps[sb], B_const], writes=[B_Sp[si]])
                    prog.add("act", lambda e: e.activation(out=Pt[pi], in_=Sp[si], func=AF.Exp, scale=SCALE),
                             reads=[B_Sp[si]], writes=[B_Pt[pi]])

                def PV(h=h, p=p, i=i, g=g, c=c, i0=i0, ob=ob, pi=pi):
                    for (olo, kt, plo, st, sp_) in ((0, i0 - 1, 0, True, False), (0, i0, 128, False, True),
                                                    (128, i0, 256, True, False), (128, i0 + 1, 384, False, True)):
                        prog.add("pe", lambda e, olo=olo, kt=kt, plo=plo, st=st, sp_=sp_: e.matmul(
                            PS(ob, olo, olo + 128), lhsT=VS[g][:, kt, :], rhs=Pt[pi][:, plo:plo + 128],
                            start=st, stop=sp_), reads=[B_VS[g], B_Pt[pi]], writes=[B_ps[ob]])

                def F(h=h, p=p, i=i, c=c, ob=ob):
                    finish_block(ob, 256, attn_swa[:, p, :], B_attn_swa[p], i, c * 256, sink_h=h)

                units.append({"S": S, "E": E, "PV": PV, "F": F})
        run_units(units)

    def moba_init():
        for b in range(2):
            for i in range(2):
                prog.add("dve", lambda e, b=b, i=i: e.memset(VA[b][i][:, :, 64:128], 1.0),
                         writes=[B_VA[b][i]])
                prog.add("dve", lambda e, b=b, i=i: e.tensor_scalar(
                    out=VA[b][i][:, 0:8, 64:128], in0=VA[b][i][:, 0:8, 64:128], scalar1=pvcol[:, 0:1], scalar2=None,
                    op0=ALU.mult), reads=[B_const, B_VA[b][i]], writes=[B_VA[b][i]])
            prog.add("dve", lambda e, b=b: e.memset(stage_t[b], 0.0), writes=[B_stage[b]])

    def moba_proj(p, buf):
        sk = load_wcols(C_KB + p * 128)
        sv = load_wcols(C_VB + p * 128)
        sq = load_wcols(C_QB + p * 128)
        for i in range(2):
            h = 2 * p + i
            prog.add("sp", lambda e, i=i, h=h: e.dma_start(out=KA[buf][i][64:78, :], in_=tabk_d[h]),
                     writes=[B_KAaug[buf][i]], dsem=S_KAaug[buf][i])
            prog.add("sp", lambda e, i=i, h=h: e.dma_start(out=QA[buf][i][72:78, :], in_=tabq_d[h]),
                     writes=[B_QAaug[buf][i]], dsem=S_QAaug[buf][i])
        for (src, Bsrc, lo, dlo) in ((hTp, B_hTp, 0, 0), (hTp, B_hTp, 512, 512), (hTo, B_hTo, 0, 1024), (hTo, B_hTo, 512, 1536)):
            bank = proj_fm(sk, src, Bsrc, lo, 512)
            headnorm(bank, 512, 3, KA[buf][0][0:64, dlo:dlo + 512], B_KA[buf][0], KA[buf][1][0:64, dlo:dlo + 512], B_KA[buf][1])
        for t0 in range(0, 16, 4):
            bank = pbank[0] % 2
            pbank[0] += 1
            for j in range(4):
                t = t0 + j
                src, Bsrc, tc = (hTp, B_hTp, t * 128) if t < 8 else (hTo, B_hTo, (t - 8) * 128)
                for k in range(16):
                    prog.add("pe", lambda e, k=k, j=j, src=src, tc=tc, bank=bank: e.matmul(
                        PS(bank, j * 128, (j + 1) * 128), lhsT=src[:, k, tc:tc + 128], rhs=wslot[sv][:, k, :],
                        start=(k == 0), stop=(k == 15)), reads=[B_w[sv], Bsrc], writes=[B_ps[bank]])
            for i in range(2):
                src_ps = PS(bank).rearrange("p (a b) -> p a b", b=128)[:, :, i * 64:(i + 1) * 64]
                if t0 < 8:
                    prog.add("act", lambda e, i=i, t0=t0, src_ps=src_ps: e.activation(
                        out=VA[buf][i][:, t0:t0 + 4, 0:64], in_=src_ps, func=AF.Copy, scale=pvcol[:, 0:1]),
                        reads=[B_ps[bank], B_const], writes=[B_VA[buf][i]])
                else:
                    prog.add("act", lambda e, i=i, t0=t0, src_ps=src_ps: e.activation(
                        out=VA[buf][i][:, t0:t0 + 4, 0:64], in_=src_ps, func=AF.Copy),
                        reads=[B_ps[bank]], writes=[B_VA[buf][i]])
        for n in range(2):
            bank = proj_fm(sq, hTo, B_hTo, n * 512, 512)
            headnorm(bank, 512, 2, QA[buf][0][0:64, n * 512:(n + 1) * 512], B_QA[buf][0],
                     QA[buf][1][0:64, n * 512:(n + 1) * 512], B_QA[buf][1])
        for i in range(2):
            K_, Q_ = KA[buf][i], QA[buf][i]
            prog.add("dve", lambda e, K_=K_: e.tensor_reduce(
                out=kmf[0:64, :], in_=K_[0:64, :].rearrange("p (n l) -> p n l", l=256), axis=AX.X, op=ALU.add),
                reads=[B_KA[buf][i]], writes=[B_kmf])
            prog.add("dve", lambda e: e.tensor_copy(out=kmb[0:64, :], in_=kmf[0:64, :]), reads=[B_kmf], writes=[B_kmb])
            for t in range(8):
                prog.add("pe", lambda e, t=t, Q_=Q_: e.matmul(PS(7, 256 + t * 8, 256 + t * 8 + 8),
                                                              lhsT=Q_[0:64, t * 128:(t + 1) * 128], rhs=kmb[0:64, :],
                                                              start=True, stop=True),
                         reads=[B_QA[buf][i], B_kmb], writes=[B_ps[7]])
            prog.add("dve", lambda e: e.tensor_tensor(out=gm.rearrange("p a b -> p (a b)"), in0=PS(7, 256, 320),
                                                      in1=pastbias, op=ALU.add),
                     reads=[B_ps[7], B_const], writes=[B_gm])
            for t in range(8):
                prog.add("dve", lambda e, t=t: e.max(out=m8[:, t, :], in_=gm[:, t, :]), reads=[B_gm], writes=[B_m8])
            prog.add("dve", lambda e: e.tensor_tensor(out=selt, in0=gm, in1=m8[:, :, 3:4].to_broadcast([128, 8, 8]),
                                                      op=ALU.is_ge), reads=[B_gm, B_m8], writes=[B_selt])
            prog.add("dve", lambda e: e.tensor_scalar(out=stage_t[buf][:, :, 64:72], in0=selt, scalar1=BIG,
                                                      scalar2=-BIG, op0=ALU.mult, op1=ALU.add),
                     reads=[B_selt], writes=[B_stage[buf]])
            for r in range(2):
                for t4 in range(4):
                    t = r * 4 + t4
                    prog.add("pe", lambda e, t=t, t4=t4: e.transpose(
                        out=PSB(7, t4 * 128, (t4 + 1) * 128)[0:72, :], in_=stage_t[buf][:, t, :], identity=ident_bf),
                        reads=[B_stage[buf], B_const], writes=[B_ps[7]])
                prog.add("dve", lambda e, r=r, Q_=Q_: e.tensor_copy(out=Q_[64:72, r * 512:(r + 1) * 512],
                                                                    in_=PSB(7, 0, 512)[64:72, :]),
                         reads=[B_ps[7]], writes=[B_QA[buf][i]])

    def moba_attn(p, buf):
        units = []
        for i in range(2):
            h = 2 * p + i
            K_, Q_, V_ = KA[buf][i], QA[buf][i], VA[buf][i]
            rd = [B_KA[buf][i], B_KAaug[buf][i], B_QA[buf][i], B_QAaug[buf][i]]
            for j in range(4):
                nkt = 8 + 2 * (j + 1)
                ob = 5 + obank[0] % 2
                obank[0] += 1
                for kt in range(0, nkt, 2):
                    sb = 3 + sbank[0] % 2
                    sbank[0] += 1
                    pi = ptc[0] % NPT
                    ptc[0] += 1
                    si = sb - 3
                    diag = (kt == 8 + 2 * j)
                    last = (kt == nkt - 2)

                    def S(K_=K_, Q_=Q_, rd=rd, j=j, kt=kt, sb=sb):
                        for u in range(2):
                            prog.add("pe", lambda e, u=u: e.matmul(
                                PS(sb, u * 256, (u + 1) * 256), lhsT=K_[0:78, (kt + u) * 128:(kt + u + 1) * 128],
                                rhs=Q_[0:78, j * 256:(j + 1) * 256], start=True, stop=True), reads=rd, writes=[B_ps[sb]])

                    def E(diag=diag, sb=sb, si=si, pi=pi):
                        if diag:
                            prog.add("dve", lambda e: e.tensor_tensor(out=Sp[si], in0=PS(sb), in1=mneg, op=ALU.add),
                                     reads=[B_ps[sb], B_const], writes=[B_Sp[si]])
                            prog.add("act", lambda e: e.activation(out=Pt[pi], in_=Sp[si], func=AF.Exp, scale=SCALE),
                                     reads=[B_Sp[si]], writes=[B_Pt[pi]])
                        else:
                            prog.add("act", lambda e: e.activation(out=Pt[pi], in_=PS(sb), func=AF.Exp, scale=SCALE),
                                     reads=[B_ps[sb]], writes=[B_Pt[pi]])

                    def PV(V_=V_, i=i, p=p, j=j, kt=kt, ob=ob, pi=pi, last=last, buf=buf):
                        for u in range(2):
                            prog.add("pe", lambda e, u=u: e.matmul(
                                PS(ob, 0, 256), lhsT=V_[:, kt + u, :], rhs=Pt[pi][:, u * 256:(u + 1) * 256],
                                start=(kt == 0 and u == 0), stop=(last and u == 1)),
                                reads=[B_VA[buf][i], B_Pt[pi]], writes=[B_ps[ob]])

                    F = None
                    if last:
                        def F(i=i, p=p, j=j, ob=ob):
                            finish_block(ob, 256, attn_moba[:, p, :], B_attn_moba[p], i, j * 256)

                    units.append({"S": S, "E": E, "PV": PV, "F": F})
        run_units(units)

    if stage >= 2:
        barrier(B_xt + B_xn + [B_gbc, B_junk],
                [b for bb in B_KA for b in bb] + [b for bb in B_KAaug for b in bb] + [b for bb in B_VA for b in bb] +
                B_KS + B_VS + B_Sp + B_Pt)
        swa_kv()
        moba_init()
        swa_qproj(0, 0)
        for p in range(8):
            if p + 1 < 8:
                swa_qproj(p + 1, (p + 1) % 2)
            elif stage >= 3:
                moba_proj(0, 0)
            swa_attn(p, p % 2)
        if debug and stage == 2:
            dbg_outs["KS0"] = (KS[0], [128, 1152], BF16, [B_KS[0]])
            dbg_outs["KS1"] = (KS[1], [128, 1152], BF16, [B_KS[1]])
            dbg_outs["VS0"] = (VS[0], [128, 9 * 128], BF16, [B_VS[0]])
            dbg_outs["attn_swa"] = (attn_swa, [128, 8 * 1024], BF16, B_attn_swa)
    if stage >= 3:
        for p in range(8):
            if p + 1 < 8:
                moba_proj(p + 1, (p + 1) % 2)
            moba_attn(p, p % 2)
        if debug and stage == 3:
            dbg_outs["attn_moba"] = (attn_moba, [128, 8 * 1024], BF16, B_attn_moba)
            dbg_outs["QA10"] = (QA[1][0], [128, 1024], BF16, [B_QA[1][0], B_QAaug[1][0]])
            dbg_outs["KA10"] = (KA[1][0], [128, 2048], BF16, [B_KA[1][0], B_KAaug[1][0]])
            dbg_outs["VA10"] = (VA[1][0], [128, 2048], BF16, [B_VA[1][0]])

    all_attn_bufs = (B_w + [b for bb in B_QA for b in bb] + [b for bb in B_QAaug for b in bb] +
                     [b for bb in B_KA for b in bb] + [b for bb in B_KAaug for b in bb] +
                     [b for bb in B_VA for b in bb] + B_KS + B_VS + B_sqb + B_lnb + B_rsb + B_tmpB + B_Sp + B_Pt +
                     B_Rt + B_tmpo + [B_gm, B_m8, B_selt, B_kmf, B_kmb] + B_stage + B_xt + B_xn + [B_junk, B_gbc])
    if stage >= 4:
        R3.reset()
        gslot = [[R3.alloc([128, 16, 128], BF16) for _ in range(2)] for _ in range(2)]
        uslot = [[R3.alloc([128, 8, 128], BF16) for _ in range(2)] for _ in range(2)]
        B_gs = [Buf("gs%d" % i) for i in range(2)]
        S_gs = [new_sem() for _ in range(2)]
        mergedT = R3.alloc([128, 16, 1024], BF16)
        B_merged = [Buf("merged%d" % i) for i in range(16)]
        sga = [R3.alloc([128, 512], F32) for _ in range(2)]
        sgb = [R3.alloc([128, 512], F32) for _ in range(2)]
        tmul = [R3.alloc([128, 512], F32) for _ in range(2)]
        B_sga = [Buf("sga%d" % i) for i in range(2)]
        B_sgb = [Buf("sgb%d" % i) for i in range(2)]
        B_tmul = [Buf("tmul%d" % i) for i in range(2)]
        NWO = 3
        woslot = [R3.alloc([128, 16, 256], BF16) for _ in range(NWO)]
        B_wo = [Buf("wo%d" % i) for i in range(NWO)]
        S_wo = [new_sem() for _ in range(NWO)]
        assert R3.off <= R3.size
        barrier(all_attn_bufs, B_gs + B_merged + B_sga + B_sgb + B_tmul + B_wo)
        first = [True]
        wus_v = w_up_swa.rearrange("(k p) n -> p k n", p=128)
        wum_v = w_up_moba.rearrange("(k p) n -> p k n", p=128)
        wo_v = w_out.rearrange("(k p) n -> p k n", p=128)
        cnt4 = [0]
        for c in range(16):
            st_ = c % 2
            extra = []

            def _ld(e, c=c, st_=st_):
                return [e.dma_start(out=gslot[st_][0], in_=w_in_v[:, :, C_GA + c * 128:C_GA + (c + 1) * 128]),
                        e.dma_start(out=gslot[st_][1], in_=w_in_v[:, :, C_GB + c * 128:C_GB + (c + 1) * 128]),
                        e.dma_start(out=uslot[st_][0], in_=wus_v[:, :, c * 128:(c + 1) * 128]),
                        e.dma_start(out=uslot[st_][1], in_=wum_v[:, :, c * 128:(c + 1) * 128])]
            prog.add("pool", _ld, writes=[B_gs[st_]] + extra, dsem=S_gs[st_], ndma=4)
            for n in range(2):
                alt = cnt4[0] % 2
                cnt4[0] += 1
                bga, bgb, bya, byb = ((0, 1, 3, 4), (2, 5, 6, 7))[alt]
                tl = n * 512
                for (bank, wt) in ((bga, gslot[st_][0]), (bgb, gslot[st_][1])):
                    for k in range(16):
                        prog.add("pe", lambda e, k=k, bank=bank, wt=wt, tl=tl: e.matmul(
                            PS(bank), lhsT=wt[:, k, :], rhs=hTo[:, k, tl:tl + 512], start=(k == 0), stop=(k == 15)),
                            reads=[B_gs[st_], B_hTo], writes=[B_ps[bank]])
                for (bank, wt, at_, Bat) in ((bya, uslot[st_][0], attn_swa, B_attn_swa), (byb, uslot[st_][1], attn_moba, B_attn_moba)):
                    for k in range(8):
                        prog.add("pe", lambda e, k=k, bank=bank, wt=wt, at_=at_, tl=tl: e.matmul(
                            PS(bank), lhsT=wt[:, k, :], rhs=at_[:, k, tl:tl + 512], start=(k == 0), stop=(k == 7)),
                            reads=[B_gs[st_]] + Bat, writes=[B_ps[bank]])
                prog.add("act", lambda e, bga=bga, alt=alt: e.activation(out=sga[alt], in_=PS(bga), func=AF.Sigmoid),
                         reads=[B_ps[bga]], writes=[B_sga[alt]])
                prog.add("act", lambda e, bgb=bgb, alt=alt: e.activation(out=sgb[alt], in_=PS(bgb), func=AF.Sigmoid),
                         reads=[B_ps[bgb]], writes=[B_sgb[alt]])
                prog.add("dve", lambda e, bya=bya, alt=alt: e.tensor_tensor(out=tmul[alt], in0=PS(bya), in1=sga[alt], op=ALU.mult),
                         reads=[B_ps[bya], B_sga[alt]], writes=[B_tmul[alt]])
                prog.add("dve", lambda e, byb=byb, alt=alt: e.tensor_tensor(out=sgb[alt], in0=PS(byb), in1=sgb[alt], op=ALU.mult),
                         reads=[B_ps[byb], B_sgb[alt]], writes=[B_sgb[alt]])
                prog.add("dve", lambda e, c=c, tl=tl, alt=alt: e.tensor_tensor(out=mergedT[:, c, tl:tl + 512], in0=tmul[alt],
                                                                               in1=sgb[alt], op=ALU.add),
                         reads=[B_tmul[alt], B_sgb[alt]], writes=[B_merged[c]])
        if debug and stage == 4:
            dbg_outs["mergedT"] = (mergedT, [128, 16 * 1024], BF16, B_merged)

    if stage >= 5:
        R1.reset()
        x1 = R1.alloc([128, 8, D], F32)
        B_x1 = [Buf("x1_%d" % t) for t in range(8)]
        S_x1 = new_sem()
        xs_own = xs[NOWN:NKV, :].rearrange("(t p) d -> p t d", p=128)

        def _ldx(e):
            return [e.dma_start(out=x1[:, t, :], in_=xs_own[:, t, :]) for t in range(8)]
        prog.add("sp", _ldx, writes=B_x1 + [B_hTp, B_hTo], dsem=S_x1, ndma=8)
        ob2 = [0]
        for cg in range(8):
            s = cg % NWO
            prog.add("pool", lambda e, cg=cg, s=s: e.dma_start(out=woslot[s], in_=wo_v[:, :, cg * 256:(cg + 1) * 256]),
                     writes=[B_wo[s]], dsem=S_wo[s])
            for t in range(8):
                bank = ob2[0] % 8
                ob2[0] += 1
                for k in range(16):
                    prog.add("pe", lambda e, k=k, t=t, s=s, bank=bank: e.matmul(
                        PS(bank, 0, 256), lhsT=mergedT[:, k, t * 128:(t + 1) * 128], rhs=woslot[s][:, k, :],
                        start=(k == 0), stop=(k == 15)), reads=[B_wo[s]] + B_merged, writes=[B_ps[bank]])
                prog.add("dve", lambda e, t=t, cg=cg, bank=bank: e.tensor_tensor(
                    out=x1[:, t, cg * 256:(cg + 1) * 256], in0=PS(bank, 0, 256), in1=x1[:, t, cg * 256:(cg + 1) * 256],
                    op=ALU.add), reads=[B_ps[bank], B_x1[t]], writes=[B_x1[t]])
        if debug and stage == 5:
            dbg_outs["x1"] = (x1, [128, 8 * D], F32, B_x1)

    if stage >= 6:
        R2.reset()
        h2T = R2.alloc([128, 16, 1024], BF16)
        B_h2T = Buf("h2T")
        phaseB_bufs = B_gs + B_merged + B_sga + B_sgb + B_tmul + B_wo
        R3.reset()
        NWE = 4
        ering = [R3.alloc([128, 8192], BF16) for _ in range(NWE)]
        B_er = [Buf("er%d" % i) for i in range(NWE)]
        S_er = [new_sem() for _ in range(NWE)]
        hidT = [R3.alloc([128, 4, 1024], BF16) for _ in range(2)]
        B_hid = [[Buf("hid%d_%d" % (i, n)) for n in range(2)] for i in range(2)]
        sg = [R3.alloc([128, 512], F32) for _ in range(2)]
        B_sg = [Buf("sg%d" % i) for i in range(2)]
        comb = R3.alloc([128, 8, 16], F32)
        B_comb = Buf("comb")
        wr_sb = R3.alloc([128, 16, 20], F32)
        B_wr = Buf("wr")
        S_wr = new_sem()
        L_all = R3.alloc([128, 8, 20], F32)
        B_L = Buf("L")
        ss2 = R3.alloc([128, 8], F32)
        ln2 = R3.alloc([128, 8], F32)
        rs2 = R3.alloc([128, 8], F32)
        B_ss2 = Buf("ss2")
        rt_small = [R3.alloc([128, 8, 16], F32) for _ in range(8)]
        B_rts = Buf("rts")
        moe_end = R3.off
        assert R3.off <= R3.size, (R3.off, R3.size)
        gbc2 = R3.alloc([128, D], F32, at=16384)
        h2f = [R3.alloc([128, D], F32, at=16384 + 8192 + i * 8192) for i in range(2)]
        B_h2f = [Buf("h2f%d" % i) for i in range(2)]
        h2Tf = [R3.alloc([128, 16, 128], F32, at=16384 + 3 * 8192 + i * 8192) for i in range(2)]
        B_h2Tf = [Buf("h2Tf%d" % i) for i in range(2)]
        junk2 = R3.alloc([128, D], BF16, at=16384 + 5 * 8192)
        B_junk2 = Buf("junk2")
        B_gbc2 = Buf("gbc2")
        S_gbc2 = new_sem()
        B_scrC = [B_er[1], B_er[2], B_er[3]]

        barrier(phaseB_bufs, B_er + [b for bb in B_hid for b in bb] + B_sg + [B_comb, B_wr, B_L, B_ss2, B_rts, B_gbc2, B_junk2] +
                B_h2f + B_h2Tf)
        prog.add("sp", lambda e: e.dma_start(out=gbc2, in_=gffn_bc[:, :]), writes=[B_gbc2], dsem=S_gbc2)
        prog.add("sp", lambda e: e.dma_start(out=wr_sb, in_=w_r.rearrange("(k p) n -> p k n", p=128)),
                 writes=[B_wr], dsem=S_wr)
        prog.add("dve", lambda e: e.memset(ss2, 0.0), writes=[B_ss2])
        for t in range(8):
            i2 = t % 2
            prog.add("act", lambda e, t=t: e.activation(out=junk2, in_=x1[:, t, :], func=AF.Square, accum_out=ss2[:, t:t + 1]),
                     reads=[B_x1[t], B_ss2, B_gbc2], writes=[B_junk2, B_ss2])
            prog.add("act", lambda e, t=t: e.activation(out=ln2[:, t:t + 1], in_=ss2[:, t:t + 1], func=AF.Ln, scale=1.0 / D,
                                                        bias=epscol[:, 0:1]), reads=[B_ss2, B_eps], writes=[B_ss2])
            prog.add("act", lambda e, t=t: e.activation(out=rs2[:, t:t + 1], in_=ln2[:, t:t + 1], func=AF.Exp, scale=-0.5),
                     reads=[B_ss2], writes=[B_ss2])
            prog.add("dve", lambda e, t=t, i2=i2: e.scalar_tensor_tensor(
                out=h2f[i2], in0=x1[:, t, :], scalar=rs2[:, t:t + 1], op0=ALU.mult, in1=gbc2, op1=ALU.mult),
                reads=[B_x1[t], B_ss2, B_gbc2], writes=[B_h2f[i2]])
            for q4 in range(4):
                bank = (t * 4 + q4) % 4
                for kk in range(4):
                    k = q4 * 4 + kk
                    prog.add("pe", lambda e, k=k, kk=kk, bank=bank, i2=i2: e.transpose(
                        out=PS(bank, kk * 128, (kk + 1) * 128), in_=h2f[i2][:, k * 128:(k + 1) * 128], identity=ident_f),
                        reads=[B_h2f[i2], B_const], writes=[B_ps[bank]])
                prog.add("act", lambda e, q4=q4, bank=bank, i2=i2: e.activation(
                    out=h2Tf[i2][:, q4 * 4:(q4 + 1) * 4, :], in_=PS(bank).rearrange("p (a b) -> p a b", b=128), func=AF.Copy),
                    reads=[B_ps[bank]], writes=[B_h2Tf[i2]])
                prog.add("dve", lambda e, q4=q4, i2=i2, t=t: e.tensor_copy(
                    out=h2T[:, q4 * 4:(q4 + 1) * 4, t * 128:(t + 1) * 128], in_=h2Tf[i2][:, q4 * 4:(q4 + 1) * 4, :]),
                    reads=[B_h2Tf[i2]], writes=[B_h2T] + (B_attn_swa + B_attn_moba if (t == 0 and q4 == 0) else []))
            for k in range(16):
                prog.add("pe", lambda e, k=k, t=t, i2=i2: e.matmul(PS(4 + t % 2, 0, 20), lhsT=h2Tf[i2][:, k, :], rhs=wr_sb[:, k, :],
                                                                   start=(k == 0), stop=(k == 15)),
                         reads=[B_h2Tf[i2], B_wr], writes=[B_ps[4 + t % 2]])
            prog.add("dve", lambda e, t=t: e.tensor_copy(out=L_all[:, t, :], in_=PS(4 + t % 2, 0, 20)),
                     reads=[B_ps[4 + t % 2]], writes=[B_L])
        lg = L_all[:, :, 0:4]
        le = L_all[:, :, 4:20]
        mg, ohg, tmp16, sl, l1, msk, l2, ex, exm, den, gp, sumg, wexp, junk8 = (None,) * 14
        mg = rt_small[0][:, :, 0:1]
        ohg = rt_small[0][:, :, 4:8]
        sumg = rt_small[0][:, :, 8:9]
        gp = rt_small[0][:, :, 9:10]
        l1 = rt_small[0][:, :, 10:11]
        l2 = rt_small[0][:, :, 11:12]
        den = rt_small[0][:, :, 12:13]
        fac = rt_small[0][:, :, 13:14]
        tmp16 = rt_small[1]
        sl = rt_small[2][:, :, 0:4]
        msk = rt_small[2][:, :, 4:8]
        sl2 = rt_small[2][:, :, 8:12]
        ex = rt_small[3][:, :, 0:4]
        exm = rt_small[3][:, :, 4:8]
        wexp = rt_small[3][:, :, 8:12]
        eg = rt_small[4][:, :, 0:4]
        dgl = rt_small[4][:, :, 4:8]
        dsl = rt_small[4][:, :, 8:12]

        def dv(fn, r=(B_L, B_rts), w=(B_rts,)):
            prog.add("dve", fn, reads=list(r), writes=list(w))
        dv(lambda e: e.tensor_reduce(out=mg, in_=lg, axis=AX.X, op=ALU.max))
        dv(lambda e: e.tensor_tensor(out=ohg, in0=lg, in1=mg.to_broadcast([128, 8, 4]), op=ALU.is_ge))
        dv(lambda e: e.tensor_tensor(out=dgl, in0=lg, in1=mg.to_broadcast([128, 8, 4]), op=ALU.subtract))
        prog.add("act", lambda e: e.activation(out=eg, in_=dgl, func=AF.Exp), reads=[B_rts], writes=[B_rts])
        dv(lambda e: e.tensor_reduce(out=sumg, in_=eg, axis=AX.X, op=ALU.add))
        dv(lambda e: e.reciprocal(out=gp, in_=sumg))
        dv(lambda e: e.tensor_tensor(out=tmp16.rearrange("p t (g e) -> p t g e", e=4),
                                     in0=le.rearrange("p t (g e) -> p t g e", e=4),
                                     in1=ohg.unsqueeze(3).to_broadcast([128, 8, 4, 4]), op=ALU.mult))
        dv(lambda e: e.tensor_reduce(out=sl, in_=tmp16.rearrange("p t (g e) -> p t e g", e=4), axis=AX.X, op=ALU.add))
        dv(lambda e: e.tensor_reduce(out=l1, in_=sl, axis=AX.X, op=ALU.max))
        dv(lambda e: e.tensor_tensor(out=msk, in0=sl, in1=l1.to_broadcast([128, 8, 4]), op=ALU.is_ge))
        dv(lambda e: e.scalar_tensor_tensor(out=sl2, in0=msk, scalar=-1e30, op0=ALU.mult, in1=sl, op1=ALU.add))
        dv(lambda e: e.tensor_reduce(out=l2, in_=sl2, axis=AX.X, op=ALU.max))
        dv(lambda e: e.tensor_tensor(out=msk, in0=sl, in1=l2.to_broadcast([128, 8, 4]), op=ALU.is_ge))
        dv(lambda e: e.tensor_tensor(out=dsl, in0=sl, in1=l1.to_broadcast([128, 8, 4]), op=ALU.subtract))
        prog.add("act", lambda e: e.activation(out=ex, in_=dsl, func=AF.Exp), reads=[B_rts], writes=[B_rts])
        dv(lambda e: e.tensor_tensor(out=exm, in0=ex, in1=msk, op=ALU.mult))
        dv(lambda e: e.tensor_reduce(out=den, in_=exm, axis=AX.X, op=ALU.add))
        dv(lambda e: e.reciprocal(out=fac, in_=den))
        dv(lambda e: e.tensor_tensor(out=fac, in0=fac, in1=gp, op=ALU.mult))
        dv(lambda e: e.tensor_tensor(out=wexp, in0=exm, in1=fac.to_broadcast([128, 8, 4]), op=ALU.mult))
        dv(lambda e: e.tensor_tensor(out=comb.rearrange("p t (g e) -> p t g e", e=4),
                                     in0=ohg.unsqueeze(3).to_broadcast([128, 8, 4, 4]),
                                     in1=wexp.unsqueeze(2).to_broadcast([128, 8, 4, 4]), op=ALU.mult),
           w=(B_rts, B_comb))
        if debug and stage == 6:
            dbg_outs["h2T"] = (h2T, [128, 16 * 1024], BF16, [B_h2T])
            dbg_outs["L_all"] = (L_all, [128, 8 * 20], F32, [B_L])
            dbg_outs["comb"] = (comb, [128, 8 * 16], F32, [B_comb])

    if stage >= 7:
        er = [0]

        def load_e(dram_ap, shape3):
            s = er[0] % NWE
            er[0] += 1
            view = ering[s].rearrange("p (a b) -> p a b", b=shape3[2])
            extra = B_scrC_users if s in (1, 2, 3) and er[0] <= NWE else []
            prog.add("pool", lambda e: e.dma_start(out=view, in_=dram_ap), writes=[B_er[s]] + extra, dsem=S_er[s])
            return s, view
        B_scrC_users = [B_gbc2, B_junk2] + B_h2f + B_h2Tf
        gu = [0]
        yb = [0]
        for ex_i in range(16):
            hb = ex_i % 2
            s_g, Wg = load_e(w_gate_e[ex_i].rearrange("(k p) f -> p k f", p=128), [128, 16, 512])
            s_u, Wu = load_e(w_up_e[ex_i].rearrange("(k p) f -> p k f", p=128), [128, 16, 512])
            s_d, Wd = load_e(w_down_e[ex_i].rearrange("(k p) d -> p k d", p=128), [128, 4, 2048])
            for n in range(2):
                for fc in range(4):
                    alt = gu[0] % 2
                    gu[0] += 1
                    bg, bu = (0, 1) if alt == 0 else (2, 3)
                    for (bank, W_, s_) in ((bg, Wg, s_g), (bu, Wu, s_u)):
                        for k in range(16):
                            prog.add("pe", lambda e, k=k, bank=bank, W_=W_, fc=fc, n=n: e.matmul(
                                PS(bank), lhsT=W_[:, k, fc * 128:(fc + 1) * 128], rhs=h2T[:, k, n * 512:(n + 1) * 512],
                                start=(k == 0), stop=(k == 15)), reads=[B_er[s_], B_h2T], writes=[B_ps[bank]])
                    prog.add("act", lambda e, bg=bg, alt=alt: e.activation(out=sg[alt], in_=PS(bg), func=AF.Silu),
                             reads=[B_ps[bg]], writes=[B_sg[alt]])
                    prog.add("dve", lambda e, bu=bu, alt=alt, hb=hb, fc=fc, n=n: e.tensor_tensor(
                        out=hidT[hb][:, fc, n * 512:(n + 1) * 512], in0=PS(bu), in1=sg[alt], op=ALU.mult),
                        reads=[B_ps[bu], B_sg[alt]], writes=[B_hid[hb][n]])
            for t in range(8):
                for half in range(2):
                    b0 = 4 + 2 * (yb[0] % 2)
                    yb[0] += 1
                    for bb in range(2):
                        col = half * 1024 + bb * 512
                        for fc in range(4):
                            prog.add("pe", lambda e, fc=fc, t=t, col=col, bank=b0 + bb, hb=hb, Wd=Wd: e.matmul(
                                PS(bank), lhsT=hidT[hb][:, fc, t * 128:(t + 1) * 128], rhs=Wd[:, fc, col:col + 512],
                                start=(fc == 0), stop=(fc == 3)), reads=[B_er[s_d], B_hid[hb][t // 4]],
                                writes=[B_ps[b0 + bb]])
                    prog.add("dve", lambda e, t=t, half=half, b0=b0, ex_i=ex_i: e.scalar_tensor_tensor(
                        out=x1[:, t, half * 1024:(half + 1) * 1024], in0=psum[:, b0 * 512:(b0 + 2) * 512],
                        scalar=comb[:, t, ex_i:ex_i + 1], op0=ALU.mult, in1=x1[:, t, half * 1024:(half + 1) * 1024],
                        op1=ALU.add), reads=[B_ps[b0], B_ps[b0 + 1], B_comb, B_x1[t]], writes=[B_x1[t]])
        B_y = Buf("y")
        S_y = new_sem()
        y_v = y.rearrange("(t p) d -> p t d", p=128)
        for t in range(8):
            prog.add("sp", lambda e, t=t: e.dma_start(out=y_v[:, t, :], in_=x1[:, t, :]), reads=[B_x1[t]], writes=[B_y],
                     dsem=S_y)
        prog.add("sp", None, reads=[B_y])

    if debug:
        B_dbg = Buf("dbg")
        S_dbg = new_sem()
        for name, (ap, shape, dt, bufs) in dbg_outs.items():
            o = nc.dram_tensor("dbg_" + name, list(shape), dt, kind="ExternalOutput").ap()
            src = ap
            if len(ap.shape) == 3:
                src = ap.rearrange("p a b -> p (a b)")
            prog.add("sp", lambda e, o=o, src=src: e.dma_start(out=o[:, :], in_=src), reads=bufs, writes=[B_dbg], dsem=S_dbg)
        prog.add("sp", None, reads=[B_dbg])
        if stage < 7:
            pass

    block = es.enter_context(nc.Block())
    prog.emit(block, engsem)
    es.close()
    return nc, list(dbg_outs.keys())


_CONSTS = None


def _prepare_inputs(inputs, cores):
    global _CONSTS
    if _CONSTS is None:
        _CONSTS = _const_tables()
    c = _CONSTS
    f = lambda a: np.ascontiguousarray(np.asarray(a, dtype=np.float32))
    x = f(inputs["x"])
    shared = {
        "w_in": f(inputs["w_in"]), "w_up_swa": f(inputs["w_up_swa"]), "w_up_moba": f(inputs["w_up_moba"]),
        "w_out": f(inputs["w_out"]),
        "w_r": np.ascontiguousarray(np.concatenate(
            [f(inputs["w_router_group"]), f(inputs["w_router_expert"]).transpose(1, 0, 2).reshape(D, 16)], axis=1)),
        "w_gate_e": f(inputs["w_gate_e"]), "w_up_e": f(inputs["w_up_e"]), "w_down_e": f(inputs["w_down_e"]),
        "gmix_bc": np.ascontiguousarray(np.broadcast_to(f(inputs["g_mix"])[None, :], (128, D))),
        "gffn_bc": np.ascontiguousarray(np.broadcast_to(f(inputs["g_ffn"])[None, :], (128, D))),
        "gcols": np.ascontiguousarray(np.stack(
            [np.tile(f(inputs[k]), 2) for k in ("q_norm_swa", "k_norm_swa", "q_norm_moba", "k_norm_moba")], axis=1)),
        "sinks_bc": np.ascontiguousarray(np.broadcast_to(f(inputs["sinks"])[None, :], (128, 16))),
        "ident_bf": c["ident_bf"], "ident_f": c["ident_f"], "bd64": c["bd64"], "dsw": c["dsw"], "mneg": c["mneg"],
        "tabq": c["tabq"], "tabk": c["tabk"],
    }
    in_maps = []
    for cid in cores:
        b, hf = cid // 2, cid % 2
        if hf == 1:
            xs_ = x[b]
        else:
            xs_ = np.concatenate([x[b, NOWN:], x[b, :NOWN]], axis=0)
        m = dict(shared)
        m["xs"] = np.ascontiguousarray(xs_)
        m.update(_percore_tables(hf))
        in_maps.append(m)
    return in_maps


_NC_CACHE = {}


def kernel(**inputs):
    if "full" not in _NC_CACHE:
        _NC_CACHE["full"] = build_program(stage=99, debug=False)[0]
    nc = _NC_CACHE["full"]
    cores = list(range(8))
    in_maps = _prepare_inputs(inputs, cores)
    res = run_bass_kernel_spmd(nc, in_maps, core_ids=cores)
    out = np.empty((4, 2048, D), np.float32)
    for cid in cores:
        b, hf = cid // 2, cid % 2
        out[b, hf * NOWN:(hf + 1) * NOWN, :] = res.results[cid]["y"]
    return out
```

```python
import numpy as np
import ml_dtypes
from contextlib import ExitStack
import concourse.bass as bass
import concourse.mybir as mybir
from concourse.bass_utils import run_bass_kernel_spmd

F32 = mybir.dt.float32
BF16 = mybir.dt.bfloat16
U8 = mybir.dt.uint8
ALU = mybir.AluOpType
AF = mybir.ActivationFunctionType
AX = mybir.AxisListType
NPBF = ml_dtypes.bfloat16

D = 2048
NOWN = 1024
NKV = 2048
EPS = 1e-6
SCALE = 0.125
BIG = 32768.0
IN_COLS = 8448
C_QA, C_KA, C_VA, C_QB, C_KB, C_VB, C_GA, C_GB = 0, 1024, 1152, 1280, 2304, 3328, 4352, 6400
SLOPES = [2.0 ** (-(h + 1) / 2.0) for h in range(16)]
ARENA = 211968


class Buf:
    __slots__ = ("name", "lw", "lr", "excl")

    def __init__(self, name, excl=False):
        self.name = name
        self.lw = None
        self.lr = {}
        self.excl = excl


class DSem:
    def __init__(self, h):
        self.h = h
        self.count = 0


class Op:
    __slots__ = ("eng", "fn", "deps", "signal", "dsem", "val", "idx")


ENGS = ("pe", "act", "dve", "pool", "sp")


class Prog:
    def __init__(self):
        self.ops = []

    @staticmethod
    def _need(p, eng, is_dma, raw):
        if p.dsem is not None or is_dma:
            return True
        if p.eng != eng:
            return True
        if eng == "pe":
            return False
        return raw

    def add(self, eng, fn, reads=(), writes=(), dsem=None, ndma=1):
        idx = len(self.ops)
        op = Op()
        op.eng, op.fn, op.signal, op.dsem, op.idx, op.val = eng, fn, False, dsem, idx, None
        is_dma = dsem is not None
        key = ("d", idx) if is_dma else eng
        deps = set()
        for b in reads:
            w = b.lw
            if w is not None and self._need(self.ops[w], eng, is_dma, True):
                deps.add(w)
            if b.excl:
                for k2, r in b.lr.items():
                    if k2 != key:
                        deps.add(r)
        for b in writes:
            w = b.lw
            if w is not None and self._need(self.ops[w], eng, is_dma, False):
                deps.add(w)
            for r in b.lr.values():
                if self._need(self.ops[r], eng, is_dma, False):
                    deps.add(r)
        for b in reads:
            b.lr[key] = idx
        for b in writes:
            b.lw = idx
            b.lr = {}
        for d in deps:
            self.ops[d].signal = True
        op.deps = sorted(deps)
        if is_dma:
            dsem.count += 16 * ndma
            op.val = dsem.count
        self.ops.append(op)
        return op

    def emit(self, block, engsem):
        cnt = {}
        for op in self.ops:
            if op.dsem is None and op.signal:
                cnt[op.eng] = cnt.get(op.eng, 0) + 1
                op.val = cnt[op.eng]
        by = {e: [] for e in ENGS}
        for op in self.ops:
            by[op.eng].append(op)
        ops = self.ops

        def run(name):
            def body(e):
                waited = {}
                for op in by[name]:
                    for d in op.deps:
                        p = ops[d]
                        if p.dsem is not None:
                            sem, k = p.dsem.h, ("d", id(p.dsem))
                        else:
                            sem, k = engsem[p.eng], p.eng
                        if waited.get(k, 0) < p.val:
                            e.wait_ge(sem, p.val)
                            waited[k] = p.val
                    if op.fn is None:
                        continue
                    r = op.fn(e)
                    if op.dsem is not None:
                        for ins in (r if isinstance(r, (list, tuple)) else [r]):
                            ins.then_inc(op.dsem.h, 16)
                    elif op.signal:
                        r.then_inc(engsem[op.eng], 1)
            return body

        block.tensor(run("pe"))
        block.scalar(run("act"))
        block.vector(run("dve"))
        block.gpsimd(run("pool"))
        block.sync(run("sp"))


def _split3(a):
    a = a.astype(np.float64)
    hi = a.astype(NPBF)
    r = a - hi.astype(np.float64)
    mid = r.astype(NPBF)
    r = r - mid.astype(np.float64)
    lo = r.astype(NPBF)
    return hi, mid, lo


def _const_tables():
    c = {}
    c["ident_bf"] = np.eye(128, dtype=np.float32).astype(NPBF)
    c["ident_f"] = np.eye(128, dtype=np.float32)
    bd = np.zeros((128, 128), np.float32)
    bd[:64, :64] = 1.0 / 64
    bd[64:, 64:] = 1.0 / 64
    c["bd64"] = bd.astype(NPBF)
    k = np.arange(128)[:, None].astype(np.float64)
    q = np.arange(128)[None, :].astype(np.float64)
    da = q - k
    da = np.where(da >= 0, da, 1e9)
    db = q + 128 - k
    db = np.where(db < 128, db, 1e9)
    c["dsw"] = np.concatenate([db, da, db, da], axis=1).astype(np.float32)
    q2 = np.arange(256)[None, :]
    m0 = np.where(q2 >= np.arange(128)[:, None], 0.0, -1e9)
    m1 = np.where(q2 >= (np.arange(128)[:, None] + 128), 0.0, -1e9)
    c["mneg"] = np.concatenate([m0, m1], axis=1).astype(np.float32)
    tq = np.arange(NOWN).astype(np.float64)
    tk = (np.arange(NKV) - 1024).astype(np.float64)
    tabq = np.zeros((16, 6, NOWN), NPBF)
    tabk = np.zeros((16, 14, NKV), NPBF)
    ind = (np.arange(NKV)[None, :] // 256 == np.arange(8)[:, None]).astype(np.float32)
    for h in range(16):
        s = SLOPES[h]
        a = _split3(-s * tq / SCALE)
        b = _split3(s * tk / SCALE)
        for i in range(3):
            tabq[h, i] = a[i]
            tabq[h, 3 + i] = 1.0
            tabk[h, 8 + i] = 1.0
            tabk[h, 11 + i] = b[i]
        tabk[h, 0:8] = ind.astype(NPBF)
    c["tabq"] = tabq
    c["tabk"] = tabk
    return c


def _percore_tables(hf):
    pb = np.full((128, 8, 8), -1e30, np.float32)
    for t in range(8):
        own = 4 + t // 2
        for n in range(8):
            if n == own:
                pb[:, t, n] = 1e30
            elif n < own and (hf == 1 or n >= 4):
                pb[:, t, n] = 0.0
    pv = np.full((128, 1), float(hf), np.float32)
    return {"pastbias": pb.reshape(128, 64), "pvcol": pv}


def build_program(stage=99, debug=False):
    nc = bass.Bass("TRN2", target_bir_lowering=False)
    es = ExitStack()
    prog = Prog()
    dbg_outs = {}

    def din(name, shape, dt):
        return nc.dram_tensor(name, list(shape), dt, kind="ExternalInput").ap()

    xs = din("xs", [NKV, D], F32)
    w_in = din("w_in", [D, IN_COLS], F32)
    w_up_swa = din("w_up_swa", [1024, D], F32)
    w_up_moba = din("w_up_moba", [1024, D], F32)
    w_out = din("w_out", [D, D], F32)
    w_r = din("w_r", [D, 20], F32)
    w_gate_e = din("w_gate_e", [16, D, 512], F32)
    w_up_e = din("w_up_e", [16, D, 512], F32)
    w_down_e = din("w_down_e", [16, 512, D], F32)
    gmix_bc = din("gmix_bc", [128, D], F32)
    gffn_bc = din("gffn_bc", [128, D], F32)
    gcols_d = din("gcols", [128, 4], F32)
    sinks_bc = din("sinks_bc", [128, 16], F32)
    ident_bf_d = din("ident_bf", [128, 128], BF16)
    ident_f_d = din("ident_f", [128, 128], F32)
    bd64_d = din("bd64", [128, 128], BF16)
    dsw_d = din("dsw", [128, 512], F32)
    mneg_d = din("mneg", [128, 512], F32)
    tabq_d = din("tabq", [16, 6, NOWN], BF16)
    tabk_d = din("tabk", [16, 14, NKV], BF16)
    pastbias_d = din("pastbias", [128, 64], F32)
    pvcol_d = din("pvcol", [128, 1], F32)
    y = nc.dram_tensor("y", [NOWN, D], F32, kind="ExternalOutput").ap()

    arena = es.enter_context(nc.sbuf_tensor("arena", [128, ARENA], U8))
    psum = es.enter_context(nc.psum_tensor("psum", [128, 4096], F32))

    def PS(b, lo=0, hi=512):
        return psum[:, b * 512 + lo:b * 512 + hi]

    def PSB(b, lo=0, hi=1024):
        return psum[:, b * 512:(b + 1) * 512].bitcast(BF16)[:, lo:hi]

    B_ps = [Buf("ps%d" % i, excl=True) for i in range(8)]

    class Region:
        def __init__(self, base, size):
            self.base, self.size, self.off = base, size, 0

        def reset(self, off=0):
            self.off = off

        def alloc(self, shape, dt, at=None):
            nb = {F32: 4, BF16: 2}[dt]
            n = int(np.prod(shape[1:])) * nb
            n32 = (n + 31) // 32 * 32
            off = self.off if at is None else at
            assert off + n32 <= self.size, (off, n32, self.size)
            if at is None:
                self.off += n32
            v = arena[:, self.base + off:self.base + off + n].bitcast(dt)
            if len(shape) == 3:
                v = v.rearrange("p (a b) -> p a b", b=shape[2])
            return v

    CONST_SZ = 6144
    R1_SZ = 65536
    R2_SZ = 32768
    RC = Region(0, CONST_SZ)
    R1 = Region(CONST_SZ, R1_SZ)
    R2 = Region(CONST_SZ + R1_SZ, R2_SZ)
    R3 = Region(CONST_SZ + R1_SZ + R2_SZ, ARENA - CONST_SZ - R1_SZ - R2_SZ)

    sem_id = [0]

    def new_sem():
        sem_id[0] += 1
        return DSem(es.enter_context(nc.semaphore("s%d" % sem_id[0])))

    engsem = {e: es.enter_context(nc.semaphore("eng_" + e)) for e in ENGS}

    ident_bf = RC.alloc([128, 128], BF16)
    ident_f = RC.alloc([128, 128], F32)
    bd64 = RC.alloc([128, 128], BF16)
    gcols = RC.alloc([128, 4], F32)
    epscol = RC.alloc([128, 1], F32)
    expsink = RC.alloc([128, 16], F32)
    pvcol = RC.alloc([128, 1], F32)
    pastbias = RC.alloc([128, 64], F32)
    dsw = RC.alloc([128, 512], F32)
    mneg = RC.alloc([128, 512], F32)
    ss1 = RC.alloc([128, 16], F32)
    ln1 = RC.alloc([128, 16], F32)
    rs1 = RC.alloc([128, 16], F32)
    B_const = Buf("const")
    S_const = new_sem()
    const_loads = [(ident_bf, ident_bf_d), (ident_f, ident_f_d), (bd64, bd64_d), (gcols, gcols_d),
                   (expsink, sinks_bc), (pvcol, pvcol_d), (pastbias, pastbias_d), (dsw, dsw_d), (mneg, mneg_d)]

    def _ld_consts(e):
        return [e.dma_start(out=o, in_=i[:, :]) for o, i in const_loads]
    prog.add("sp", _ld_consts, writes=[B_const], dsem=S_const, ndma=len(const_loads))
    B_eps = Buf("eps")
    prog.add("dve", lambda e: e.memset(epscol, EPS), writes=[B_eps])
    B_ss1 = [Buf("ss1_%d" % t) for t in range(16)]
    prog.add("dve", lambda e: e.memset(ss1, 0.0), writes=B_ss1)
    B_expsink = Buf("expsink")
    prog.add("act", lambda e: e.activation(out=expsink, in_=expsink, func=AF.Exp), reads=[B_const], writes=[B_expsink])

    hTp = R1.alloc([128, 16, 1024], BF16)
    hTo = R1.alloc([128, 16, 1024], BF16)
    B_hTp, B_hTo = Buf("hTp"), Buf("hTo")
    attn_swa = R2.alloc([128, 8, 1024], BF16)
    attn_moba = R2.alloc([128, 8, 1024], BF16)
    B_attn_swa = [Buf("attn_swa%d" % i) for i in range(8)]
    B_attn_moba = [Buf("attn_moba%d" % i) for i in range(8)]

    NW = 6
    wslot = [R3.alloc([128, 16, 128], BF16) for _ in range(NW)]
    B_w = [Buf("w%d" % i) for i in range(NW)]
    S_w = [new_sem() for _ in range(NW)]
    QA = [[R3.alloc([128, 1024], BF16) for _ in range(2)] for _ in range(2)]
    B_QA = [[Buf("QA%d%d" % (i, j)) for j in range(2)] for i in range(2)]
    B_QAaug = [[Buf("QAaug%d%d" % (i, j)) for j in range(2)] for i in range(2)]
    S_QAaug = [[new_sem() for j in range(2)] for i in range(2)]
    off_KA = R3.off
    KA = [[R3.alloc([128, 2048], BF16) for _ in range(2)] for _ in range(2)]
    B_KA = [[Buf("KA%d%d" % (i, j)) for j in range(2)] for i in range(2)]
    B_KAaug = [[Buf("KAaug%d%d" % (i, j)) for j in range(2)] for i in range(2)]
    S_KAaug = [[new_sem() for j in range(2)] for i in range(2)]
    VA = [[R3.alloc([128, 16, 128], BF16) for _ in range(2)] for _ in range(2)]
    B_VA = [[Buf("VA%d%d" % (i, j)) for j in range(2)] for i in range(2)]
    KS = [R3.alloc([128, 1152], BF16) for _ in range(2)]
    B_KS = [Buf("KS%d" % i) for i in range(2)]
    VS = [R3.alloc([128, 9, 128], BF16) for _ in range(2)]
    B_VS = [Buf("VS%d" % i) for i in range(2)]
    sqb = [R3.alloc([128, 512], BF16) for _ in range(2)]
    B_sqb = [Buf("sqb%d" % i) for i in range(2)]
    lnb = [R3.alloc([128, 512], F32) for _ in range(2)]
    B_lnb = [Buf("lnb%d" % i) for i in range(2)]
    rsb = [R3.alloc([128, 512], F32) for _ in range(2)]
    B_rsb = [Buf("rsb%d" % i) for i in range(2)]
    tmpB = [R3.alloc([128, 512], BF16) for _ in range(2)]
    B_tmpB = [Buf("tmpB%d" % i) for i in range(2)]
    off_Sp = R3.off
    Sp = [R3.alloc([128, 512], F32) for _ in range(2)]
    B_Sp = [Buf("Sp%d" % i) for i in range(2)]
    NPT = 4
    Pt = [R3.alloc([128, 512], BF16) for _ in range(NPT)]
    B_Pt = [Buf("Pt%d" % i) for i in range(NPT)]
    Rt = [R3.alloc([128, 256], F32) for _ in range(2)]
    B_Rt = [Buf("Rt%d" % i) for i in range(2)]
    Rt2 = [R3.alloc([128, 256], F32) for _ in range(2)]
    tmpo = [R3.alloc([128, 256], BF16) for _ in range(2)]
    B_tmpo = [Buf("tmpo%d" % i) for i in range(2)]
    gm = R3.alloc([128, 8, 8], F32)
    m8 = R3.alloc([128, 8, 8], F32)
    selt = R3.alloc([128, 8, 8], F32)
    kmf = R3.alloc([128, 8], F32)
    kmb = R3.alloc([128, 8], BF16)
    B_gm, B_m8, B_selt, B_kmf, B_kmb = Buf("gm"), Buf("m8"), Buf("selt"), Buf("kmf"), Buf("kmb")
    stage_t = [R3.alloc([128, 8, 72], BF16) for _ in range(2)]
    B_stage = [Buf("stage%d" % i) for i in range(2)]
    attn_end = R3.off
    xt = [R3.alloc([128, D], F32, at=off_KA + i * 8192) for i in range(3)]
    B_xt = [Buf("xt%d" % i) for i in range(3)]
    S_xt = [new_sem() for _ in range(3)]
    gbc = R3.alloc([128, D], F32, at=off_KA + 3 * 8192)
    xn = [R3.alloc([128, D], BF16, at=off_KA + 4 * 8192 + i * 4096) for i in range(2)]
    B_xn = [Buf("xn%d" % i) for i in range(2)]
    junk = R3.alloc([128, D], BF16, at=off_Sp)
    B_junk = Buf("junk")
    B_gbc = Buf("gbc")
    S_gbc = new_sem()
    bar_scr = RC.alloc([128, 8], F32)

    def barrier(old, new):
        prog.add("dve", lambda e: e.memset(bar_scr, 0.0), writes=list(old) + list(new))

    prog.add("sp", lambda e: e.dma_start(out=gbc, in_=gmix_bc[:, :]), writes=[B_gbc], dsem=S_gbc)
    for t in range(16):
        sl = t % 3
        x2 = t % 2
        prog.add("sp", lambda e, t=t, sl=sl: e.dma_start(out=xt[sl], in_=xs[t * 128:(t + 1) * 128, :]),
                 writes=[B_xt[sl]], dsem=S_xt[sl])
        prog.add("act", lambda e, t=t, sl=sl: e.activation(out=junk, in_=xt[sl], func=AF.Square,
                                                           accum_out=ss1[:, t:t + 1]),
                 reads=[B_xt[sl], B_ss1[t]], writes=[B_junk, B_ss1[t]])
        prog.add("act", lambda e, t=t: e.activation(out=ln1[:, t:t + 1], in_=ss1[:, t:t + 1], func=AF.Ln,
                                                    scale=1.0 / D, bias=epscol[:, 0:1]),
                 reads=[B_ss1[t], B_eps], writes=[B_ss1[t]])
        prog.add("act", lambda e, t=t: e.activation(out=rs1[:, t:t + 1], in_=ln1[:, t:t + 1], func=AF.Exp, scale=-0.5),
                 reads=[B_ss1[t]], writes=[B_ss1[t]])
        prog.add("dve", lambda e, t=t, sl=sl, x2=x2: e.scalar_tensor_tensor(
            out=xn[x2], in0=xt[sl], scalar=rs1[:, t:t + 1], op0=ALU.mult, in1=gbc, op1=ALU.mult),
            reads=[B_xt[sl], B_ss1[t], B_gbc], writes=[B_xn[x2]])
        dst = hTp if t < 8 else hTo
        Bdst = B_hTp if t < 8 else B_hTo
        tc = (t % 8) * 128
        for k in range(16):
            bank = 6 + k // 8
            prog.add("pe", lambda e, k=k, x2=x2, bank=bank: e.transpose(
                out=PSB(bank, (k % 8) * 128, (k % 8 + 1) * 128), in_=xn[x2][:, k * 128:(k + 1) * 128],
                identity=ident_bf), reads=[B_xn[x2], B_const], writes=[B_ps[bank]])
        prog.add("act", lambda e, dst=dst, tc=tc: e.activation(
            out=dst[:, 0:8, tc:tc + 128], in_=PSB(6).rearrange("p (a b) -> p a b", b=128), func=AF.Copy),
            reads=[B_ps[6]], writes=[Bdst])
        prog.add("dve", lambda e, dst=dst, tc=tc: e.tensor_copy(
            out=dst[:, 8:16, tc:tc + 128], in_=PSB(7).rearrange("p (a b) -> p a b", b=128)),
            reads=[B_ps[7]], writes=[Bdst])

    if debug and stage == 1:
        dbg_outs["hTp"] = (hTp, [128, 16 * 1024], BF16, [B_hTp])
        dbg_outs["hTo"] = (hTo, [128, 16 * 1024], BF16, [B_hTo])

    w_in_v = w_in.rearrange("(k p) n -> p k n", p=128)
    wring = [0]

    def load_wcols(c0):
        s = wring[0] % NW
        wring[0] += 1
        prog.add("pool", lambda e: e.dma_start(out=wslot[s], in_=w_in_v[:, :, c0:c0 + 128]),
                 writes=[B_w[s]], dsem=S_w[s])
        return s

    pbank = [0]
    nrm = [0]

    def proj_fm(s, src, Bsrc, lo, n):
        bank = pbank[0] % 2
        pbank[0] += 1
        for k in range(16):
            prog.add("pe", lambda e, k=k: e.matmul(PS(bank, 0, n), lhsT=wslot[s][:, k, :], rhs=src[:, k, lo:lo + n],
                                                   start=(k == 0), stop=(k == 15)),
                     reads=[B_w[s], Bsrc], writes=[B_ps[bank]])
        return bank

    def headnorm(bank, n, gidx, dstA, BA, dstB, BB):
        i = nrm[0] % 2
        nrm[0] += 1
        prog.add("act", lambda e: e.activation(out=sqb[i][:, 0:n], in_=PS(bank, 0, n), func=AF.Square),
                 reads=[B_ps[bank]], writes=[B_sqb[i]])
        prog.add("pe", lambda e: e.matmul(PS(2, 0, n), lhsT=bd64, rhs=sqb[i][:, 0:n], start=True, stop=True),
                 reads=[B_sqb[i], B_const], writes=[B_ps[2]])
        prog.add("act", lambda e: e.activation(out=lnb[i][:, 0:n], in_=PS(2, 0, n), func=AF.Ln, bias=epscol[:, 0:1]),
                 reads=[B_ps[2], B_eps], writes=[B_lnb[i]])
        prog.add("act", lambda e: e.activation(out=rsb[i][:, 0:n], in_=lnb[i][:, 0:n], func=AF.Exp, scale=-0.5),
                 reads=[B_lnb[i]], writes=[B_rsb[i]])
        prog.add("dve", lambda e: e.scalar_tensor_tensor(
            out=dstA, in0=PS(bank, 0, n)[0:64, :], scalar=gcols[0:64, gidx:gidx + 1], op0=ALU.mult,
            in1=rsb[i][0:64, 0:n], op1=ALU.mult), reads=[B_ps[bank], B_rsb[i], B_const], writes=[BA])
        prog.add("dve", lambda e: e.scalar_tensor_tensor(
            out=tmpB[i][64:128, 0:n], in0=PS(bank, 0, n)[64:128, :], scalar=gcols[64:128, gidx:gidx + 1], op0=ALU.mult,
            in1=rsb[i][64:128, 0:n], op1=ALU.mult), reads=[B_ps[bank], B_rsb[i], B_const], writes=[B_tmpB[i]])
        prog.add("dve", lambda e: e.tensor_copy(out=dstB, in_=tmpB[i][64:128, 0:n]),
                 reads=[B_tmpB[i]], writes=[BB])

    sbank = [0]
    obank = [0]
    ptc = [0]
    rtc = [0]

    def finish_block(ob, nq, dst_pair, Bdst, parity, qlo, sink_h=None):
        r = rtc[0] % 2
        rtc[0] += 1
        if sink_h is not None:
            prog.add("act", lambda e: e.activation(out=Rt2[r][64:128, 0:nq], in_=PS(ob, 0, nq)[64:128, :], func=AF.Ln,
                                                   bias=expsink[64:128, sink_h:sink_h + 1]),
                     reads=[B_ps[ob], B_expsink], writes=[B_Rt[r]])
        else:
            prog.add("act", lambda e: e.activation(out=Rt2[r][64:128, 0:nq], in_=PS(ob, 0, nq)[64:128, :], func=AF.Ln),
                     reads=[B_ps[ob]], writes=[B_Rt[r]])
        prog.add("act", lambda e: e.activation(out=Rt2[r][64:128, 0:nq], in_=Rt2[r][64:128, 0:nq], func=AF.Exp, scale=-1.0),
                 reads=[B_Rt[r]], writes=[B_Rt[r]])
        prog.add("dve", lambda e: e.tensor_copy(out=Rt[r][0:64, 0:nq], in_=Rt2[r][64:128, 0:nq]),
                 reads=[B_Rt[r]], writes=[B_Rt[r]])
        if parity == 0:
            prog.add("dve", lambda e: e.tensor_tensor(out=dst_pair[0:64, qlo:qlo + nq], in0=PS(ob, 0, nq)[0:64, :],
                                                      in1=Rt[r][0:64, 0:nq], op=ALU.mult),
                     reads=[B_ps[ob], B_Rt[r]], writes=[Bdst])
        else:
            prog.add("dve", lambda e: e.tensor_tensor(out=tmpo[r][0:64, 0:nq], in0=PS(ob, 0, nq)[0:64, :],
                                                      in1=Rt[r][0:64, 0:nq], op=ALU.mult),
                     reads=[B_ps[ob], B_Rt[r]], writes=[B_tmpo[r]])
            prog.add("dve", lambda e: e.tensor_copy(out=dst_pair[64:128, qlo:qlo + nq], in_=tmpo[r][0:64, 0:nq]),
                     reads=[B_tmpo[r]], writes=[Bdst])

    def drain(g):
        if g is not None:
            for _ in g:
                pass

    def run_units(units, filler=None, every=3):
        n = len(units)
        if not n:
            drain(filler)
            return
        units[0]["S"]()
        if n > 1:
            units[1]["S"]()
        units[0]["E"]()
        for i, u in enumerate(units):
            u["PV"]()
            if i + 2 < n:
                units[i + 2]["S"]()
            if filler is not None and i % every == every - 1:
                next(filler, None)
            if i + 1 < n:
                units[i + 1]["E"]()
            if u.get("F") is not None:
                u["F"]()
        drain(filler)

    def swa_kv():
        for g in range(2):
            prog.add("dve", lambda e, g=g: e.memset(VS[g][:, :, 64:128], 1.0), writes=[B_VS[g]])
            prog.add("dve", lambda e, g=g: e.tensor_scalar(out=VS[g][:, 0:1, 64:128], in0=VS[g][:, 0:1, 64:128],
                                                           scalar1=pvcol[:, 0:1], scalar2=None, op0=ALU.mult),
                     reads=[B_const, B_VS[g]], writes=[B_VS[g]])
        sk = load_wcols(C_KA)
        sv = load_wcols(C_VA)
        for (src, Bsrc, lo, n, dlo) in ((hTp, B_hTp, 896, 128, 0), (hTo, B_hTo, 0, 512, 128), (hTo, B_hTo, 512, 512, 640)):
            bank = proj_fm(sk, src, Bsrc, lo, n)
            headnorm(bank, n, 1, KS[0][0:64, dlo:dlo + n], B_KS[0], KS[1][0:64, dlo:dlo + n], B_KS[1])
        for grp in ((7, 8, 9, 10), (11, 12, 13, 14), (15,)):
            bank = pbank[0] % 2
            pbank[0] += 1
            for j, t in enumerate(grp):
                src, Bsrc, tc = (hTp, B_hTp, t * 128) if t < 8 else (hTo, B_hTo, (t - 8) * 128)
                for k in range(16):
                    prog.add("pe", lambda e, k=k, j=j, src=src, tc=tc, bank=bank: e.matmul(
                        PS(bank, j * 128, (j + 1) * 128), lhsT=src[:, k, tc:tc + 128], rhs=wslot[sv][:, k, :],
                        start=(k == 0), stop=(k == 15)), reads=[B_w[sv], Bsrc], writes=[B_ps[bank]])
            for g in range(2):
                for j, t in enumerate(grp):
                    if t < 8:
                        prog.add("act", lambda e, g=g, j=j, t=t, bank=bank: e.activation(
                            out=VS[g][:, t - 7, 0:64], in_=PS(bank, j * 128 + g * 64, j * 128 + g * 64 + 64),
                            func=AF.Copy, scale=pvcol[:, 0:1]), reads=[B_ps[bank], B_const], writes=[B_VS[g]])
                    else:
                        prog.add("act", lambda e, g=g, j=j, t=t, bank=bank: e.activation(
                            out=VS[g][:, t - 7, 0:64], in_=PS(bank, j * 128 + g * 64, j * 128 + g * 64 + 64),
                            func=AF.Copy), reads=[B_ps[bank]], writes=[B_VS[g]])

    def swa_qproj(p, buf):
        s = load_wcols(C_QA + p * 128)
        pend = None
        for n in range(2):
            if pend is not None:
                pend()
            bank = proj_fm(s, hTo, B_hTo, n * 512, 512)
            pend = (lambda bank=bank, n=n: headnorm(
                bank, 512, 0, QA[buf][0][0:64, n * 512:(n + 1) * 512], B_QA[buf][0],
                QA[buf][1][0:64, n * 512:(n + 1) * 512], B_QA[buf][1]))
            yield
        pend()
        yield

    def swa_attn(p, buf, filler=None, every=3):
        units = []
        for i in range(2):
            h = 2 * p + i
            g = h // 8
            q = QA[buf][i]
            Bq = B_QA[buf][i]
            for c in range(4):
                i0 = 8 + 2 * c - 7
                sb = 3 + sbank[0] % 2
                sbank[0] += 1
                ob = 5 + obank[0] % 2
                obank[0] += 1
                pi = ptc[0] % NPT
                ptc[0] += 1
                si = sb - 3

                def S(q=q, Bq=Bq, g=g, c=c, i0=i0, sb=sb):
                    qa = 2 * c * 128
                    for (olo, ohi, kt, qlo, qhi) in ((0, 128, i0 - 1, qa, qa + 128), (128, 384, i0, qa, qa + 256),
                                                     (384, 512, i0 + 1, qa + 128, qa + 256)):
                        prog.add("pe", lambda e, olo=olo, ohi=ohi, kt=kt, qlo=qlo, qhi=qhi: e.matmul(
                            PS(sb, olo, ohi), lhsT=KS[g][0:64, kt * 128:(kt + 1) * 128], rhs=q[0:64, qlo:qhi],
                            start=True, stop=True), reads=[B_KS[g], Bq], writes=[B_ps[sb]])

                def E(h=h, sb=sb, si=si, pi=pi):
                    prog.add("dve", lambda e: e.scalar_tensor_tensor(
                        out=Sp[si], in0=dsw, scalar=-SLOPES[h] / SCALE, op0=ALU.mult, in1=PS(sb), op1=ALU.add),
                        reads=[B_ps[sb], B_const], writes=[B_Sp[si]])
                    prog.add("act", lambda e: e.activation(out=Pt[pi], in_=Sp[si], func=AF.Exp, scale=SCALE),
                             reads=[B_Sp[si]], writes=[B_Pt[pi]])

                def PV(h=h, p=p, i=i, g=g, c=c, i0=i0, ob=ob, pi=pi):
                    for (olo, kt, plo, st, sp_) in ((0, i0 - 1, 0, True, False), (0, i0, 128, False, True),
                                                    (128, i0, 256, True, False), (128, i0 + 1, 384, False, True)):
                        prog.add("pe", lambda e, olo=olo, kt=kt, plo=plo, st=st, sp_=sp_: e.matmul(
                            PS(ob, olo, olo + 128), lhsT=VS[g][:, kt, :], rhs=Pt[pi][:, plo:plo + 128],
                            start=st, stop=sp_), reads=[B_VS[g], B_Pt[pi]], writes=[B_ps[ob]])

                def F(h=h, p=p, i=i, c=c, ob=ob):
                    finish_block(ob, 256, attn_swa[:, p, :], B_attn_swa[p], i, c * 256, sink_h=h)

                units.append({"S": S, "E": E, "PV": PV, "F": F})
        run_units(units, filler, every)

    def moba_init():
        for b in range(2):
            for i in range(2):
                prog.add("dve", lambda e, b=b, i=i: e.memset(VA[b][i][:, :, 64:128], 1.0),
                         writes=[B_VA[b][i]])
                prog.add("dve", lambda e, b=b, i=i: e.tensor_scalar(
                    out=VA[b][i][:, 0:8, 64:128], in0=VA[b][i][:, 0:8, 64:128], scalar1=pvcol[:, 0:1], scalar2=None,
                    op0=ALU.mult), reads=[B_const, B_VA[b][i]], writes=[B_VA[b][i]])
            prog.add("dve", lambda e, b=b: e.memset(stage_t[b], 0.0), writes=[B_stage[b]])

    def moba_proj(p, buf):
        sk = load_wcols(C_KB + p * 128)
        sv = load_wcols(C_VB + p * 128)
        sq = load_wcols(C_QB + p * 128)
        for i in range(2):
            h = 2 * p + i
            prog.add("sp", lambda e, i=i, h=h: e.dma_start(out=KA[buf][i][64:78, :], in_=tabk_d[h]),
                     writes=[B_KAaug[buf][i]], dsem=S_KAaug[buf][i])
            prog.add("sp", lambda e, i=i, h=h: e.dma_start(out=QA[buf][i][72:78, :], in_=tabq_d[h]),
                     writes=[B_QAaug[buf][i]], dsem=S_QAaug[buf][i])
        pend = None
        for (src, Bsrc, lo, dlo) in ((hTp, B_hTp, 0, 0), (hTp, B_hTp, 512, 512), (hTo, B_hTo, 0, 1024), (hTo, B_hTo, 512, 1536)):
            if pend is not None:
                pend()
            bank = proj_fm(sk, src, Bsrc, lo, 512)
            pend = (lambda bank=bank, dlo=dlo: headnorm(
                bank, 512, 3, KA[buf][0][0:64, dlo:dlo + 512], B_KA[buf][0], KA[buf][1][0:64, dlo:dlo + 512], B_KA[buf][1]))
            yield
        for t0 in range(0, 16, 4):
            if pend is not None:
                pend()
            bank = pbank[0] % 2
            pbank[0] += 1
            for j in range(4):
                t = t0 + j
                src, Bsrc, tc = (hTp, B_hTp, t * 128) if t < 8 else (hTo, B_hTo, (t - 8) * 128)
                for k in range(16):
                    prog.add("pe", lambda e, k=k, j=j, src=src, tc=tc, bank=bank: e.matmul(
                        PS(bank, j * 128, (j + 1) * 128), lhsT=src[:, k, tc:tc + 128], rhs=wslot[sv][:, k, :],
                        start=(k == 0), stop=(k == 15)), reads=[B_w[sv], Bsrc], writes=[B_ps[bank]])

            def vcopy(bank=bank, t0=t0):
                for i in range(2):
                    src_ps = PS(bank).rearrange("p (a b) -> p a b", b=128)[:, :, i * 64:(i + 1) * 64]
                    if t0 < 8:
                        prog.add("act", lambda e, i=i, src_ps=src_ps: e.activation(
                            out=VA[buf][i][:, t0:t0 + 4, 0:64], in_=src_ps, func=AF.Copy, scale=pvcol[:, 0:1]),
                            reads=[B_ps[bank], B_const], writes=[B_VA[buf][i]])
                    else:
                        prog.add("act", lambda e, i=i, src_ps=src_ps: e.activation(
                            out=VA[buf][i][:, t0:t0 + 4, 0:64], in_=src_ps, func=AF.Copy),
                            reads=[B_ps[bank]], writes=[B_VA[buf][i]])
            pend = vcopy
            yield
        for n in range(2):
            pend()
            bank = proj_fm(sq, hTo, B_hTo, n * 512, 512)
            pend = (lambda bank=bank, n=n: headnorm(
                bank, 512, 2, QA[buf][0][0:64, n * 512:(n + 1) * 512], B_QA[buf][0],
                QA[buf][1][0:64, n * 512:(n + 1) * 512], B_QA[buf][1]))
            yield
        pend()
        yield
        for i in range(2):
            K_, Q_ = KA[buf][i], QA[buf][i]
            prog.add("dve", lambda e, K_=K_: e.tensor_reduce(
                out=kmf[0:64, :], in_=K_[0:64, :].rearrange("p (n l) -> p n l", l=256), axis=AX.X, op=ALU.add),
                reads=[B_KA[buf][i]], writes=[B_kmf])
            prog.add("dve", lambda e: e.tensor_copy(out=kmb[0:64, :], in_=kmf[0:64, :]), reads=[B_kmf], writes=[B_kmb])
            for t in range(8):
                prog.add("pe", lambda e, t=t, Q_=Q_: e.matmul(PS(7, 256 + t * 8, 256 + t * 8 + 8),
                                                              lhsT=Q_[0:64, t * 128:(t + 1) * 128], rhs=kmb[0:64, :],
                                                              start=True, stop=True),
                         reads=[B_QA[buf][i], B_kmb], writes=[B_ps[7]])
            prog.add("dve", lambda e: e.tensor_tensor(out=gm.rearrange("p a b -> p (a b)"), in0=PS(7, 256, 320),
                                                      in1=pastbias, op=ALU.add),
                     reads=[B_ps[7], B_const], writes=[B_gm])
            for t in range(8):
                prog.add("dve", lambda e, t=t: e.max(out=m8[:, t, :], in_=gm[:, t, :]), reads=[B_gm], writes=[B_m8])
            prog.add("dve", lambda e: e.tensor_tensor(out=selt, in0=gm, in1=m8[:, :, 3:4].to_broadcast([128, 8, 8]),
                                                      op=ALU.is_ge), reads=[B_gm, B_m8], writes=[B_selt])
            prog.add("dve", lambda e: e.tensor_scalar(out=stage_t[buf][:, :, 64:72], in0=selt, scalar1=BIG,
                                                      scalar2=-BIG, op0=ALU.mult, op1=ALU.add),
                     reads=[B_selt], writes=[B_stage[buf]])
            yield
            for r in range(2):
                for t4 in range(4):
                    t = r * 4 + t4
                    prog.add("pe", lambda e, t=t, t4=t4: e.transpose(
                        out=PSB(7, t4 * 128, (t4 + 1) * 128)[0:72, :], in_=stage_t[buf][:, t, :], identity=ident_bf),
                        reads=[B_stage[buf], B_const], writes=[B_ps[7]])
                prog.add("dve", lambda e, r=r, Q_=Q_: e.tensor_copy(out=Q_[64:72, r * 512:(r + 1) * 512],
                                                                    in_=PSB(7, 0, 512)[64:72, :]),
                         reads=[B_ps[7]], writes=[B_QA[buf][i]])
            yield

    def moba_attn(p, buf, filler=None, every=3):
        units = []
        for i in range(2):
            h = 2 * p + i
            K_, Q_, V_ = KA[buf][i], QA[buf][i], VA[buf][i]
            rd = [B_KA[buf][i], B_KAaug[buf][i], B_QA[buf][i], B_QAaug[buf][i]]
            for j in range(4):
                nkt = 8 + 2 * (j + 1)
                ob = 5 + obank[0] % 2
                obank[0] += 1
                for kt in range(0, nkt, 2):
                    sb = 3 + sbank[0] % 2
                    sbank[0] += 1
                    pi = ptc[0] % NPT
                    ptc[0] += 1
                    si = sb - 3
                    diag = (kt == 8 + 2 * j)
                    last = (kt == nkt - 2)

                    def S(K_=K_, Q_=Q_, rd=rd, j=j, kt=kt, sb=sb):
                        for u in range(2):
                            prog.add("pe", lambda e, u=u: e.matmul(
                                PS(sb, u * 256, (u + 1) * 256), lhsT=K_[0:78, (kt + u) * 128:(kt + u + 1) * 128],
                                rhs=Q_[0:78, j * 256:(j + 1) * 256], start=True, stop=True), reads=rd, writes=[B_ps[sb]])

                    def E(diag=diag, sb=sb, si=si, pi=pi):
                        if diag:
                            prog.add("dve", lambda e: e.tensor_tensor(out=Sp[si], in0=PS(sb), in1=mneg, op=ALU.add),
                                     reads=[B_ps[sb], B_const], writes=[B_Sp[si]])
                            prog.add("act", lambda e: e.activation(out=Pt[pi], in_=Sp[si], func=AF.Exp, scale=SCALE),
                                     reads=[B_Sp[si]], writes=[B_Pt[pi]])
                        else:
                            prog.add("act", lambda e: e.activation(out=Pt[pi], in_=PS(sb), func=AF.Exp, scale=SCALE),
                                     reads=[B_ps[sb]], writes=[B_Pt[pi]])

                    def PV(V_=V_, i=i, p=p, j=j, kt=kt, ob=ob, pi=pi, last=last, buf=buf):
                        for u in range(2):
                            prog.add("pe", lambda e, u=u: e.matmul(
                                PS(ob, 0, 256), lhsT=V_[:, kt + u, :], rhs=Pt[pi][:, u * 256:(u + 1) * 256],
                                start=(kt == 0 and u == 0), stop=(last and u == 1)),
                                reads=[B_VA[buf][i], B_Pt[pi]], writes=[B_ps[ob]])

                    F = None
                    if last:
                        def F(i=i, p=p, j=j, ob=ob):
                            finish_block(ob, 256, attn_moba[:, p, :], B_attn_moba[p], i, j * 256)

                    units.append({"S": S, "E": E, "PV": PV, "F": F})
        run_units(units, filler, every)

    if stage >= 2:
        barrier(B_xt + B_xn + [B_gbc, B_junk],
                [b for bb in B_KA for b in bb] + [b for bb in B_KAaug for b in bb] + [b for bb in B_VA for b in bb] +
                B_KS + B_VS + B_Sp + B_Pt)
        swa_kv()
        moba_init()
        drain(swa_qproj(0, 0))
        for p in range(8):
            if p + 1 < 8:
                filler, every = swa_qproj(p + 1, (p + 1) % 2), 2
            elif stage >= 3:
                filler, every = moba_proj(0, 0), 1
            else:
                filler, every = None, 1
            swa_attn(p, p % 2, filler, every)
        if debug and stage == 2:
            dbg_outs["KS0"] = (KS[0], [128, 1152], BF16, [B_KS[0]])
            dbg_outs["KS1"] = (KS[1], [128, 1152], BF16, [B_KS[1]])
            dbg_outs["VS0"] = (VS[0], [128, 9 * 128], BF16, [B_VS[0]])
            dbg_outs["attn_swa"] = (attn_swa, [128, 8 * 1024], BF16, B_attn_swa)
    if stage >= 3:
        for p in range(8):
            filler = moba_proj(p + 1, (p + 1) % 2) if p + 1 < 8 else None
            moba_attn(p, p % 2, filler, 3)
        if debug and stage == 3:
            dbg_outs["attn_moba"] = (attn_moba, [128, 8 * 1024], BF16, B_attn_moba)
            dbg_outs["QA10"] = (QA[1][0], [128, 1024], BF16, [B_QA[1][0], B_QAaug[1][0]])
            dbg_outs["KA10"] = (KA[1][0], [128, 2048], BF16, [B_KA[1][0], B_KAaug[1][0]])
            dbg_outs["VA10"] = (VA[1][0], [128, 2048], BF16, [B_VA[1][0]])

    all_attn_bufs = (B_w + [b for bb in B_QA for b in bb] + [b for bb in B_QAaug for b in bb] +
                     [b for bb in B_KA for b in bb] + [b for bb in B_KAaug for b in bb] +
                     [b for bb in B_VA for b in bb] + B_KS + B_VS + B_sqb + B_lnb + B_rsb + B_tmpB + B_Sp + B_Pt +
                     B_Rt + B_tmpo + [B_gm, B_m8, B_selt, B_kmf, B_kmb] + B_stage + B_xt + B_xn + [B_junk, B_gbc])
    if stage >= 4:
        R3.reset()
        gslot = [[R3.alloc([128, 16, 128], BF16) for _ in range(2)] for _ in range(2)]
        uslot = [[R3.alloc([128, 8, 128], BF16) for _ in range(2)] for _ in range(2)]
        B_gs = [Buf("gs%d" % i) for i in range(2)]
        S_gs = [new_sem() for _ in range(2)]
        mergedT = R3.alloc([128, 16, 1024], BF16)
        B_merged = [Buf("merged%d" % i) for i in range(16)]
        sga = [R3.alloc([128, 512], F32) for _ in range(2)]
        sgb = [R3.alloc([128, 512], F32) for _ in range(2)]
        tmul = [R3.alloc([128, 512], F32) for _ in range(2)]
        B_sga = [Buf("sga%d" % i) for i in range(2)]
        B_sgb = [Buf("sgb%d" % i) for i in range(2)]
        B_tmul = [Buf("tmul%d" % i) for i in range(2)]
        NWO = 3
        woslot = [R3.alloc([128, 16, 256], BF16) for _ in range(NWO)]
        B_wo = [Buf("wo%d" % i) for i in range(NWO)]
        S_wo = [new_sem() for _ in range(NWO)]
        assert R3.off <= R3.size
        barrier(all_attn_bufs, B_gs + B_merged + B_sga + B_sgb + B_tmul + B_wo)
        first = [True]
        wus_v = w_up_swa.rearrange("(k p) n -> p k n", p=128)
        wum_v = w_up_moba.rearrange("(k p) n -> p k n", p=128)
        wo_v = w_out.rearrange("(k p) n -> p k n", p=128)
        cnt4 = [0]
        for c in range(16):
            st_ = c % 2
            extra = []

            def _ld(e, c=c, st_=st_):
                return [e.dma_start(out=gslot[st_][0], in_=w_in_v[:, :, C_GA + c * 128:C_GA + (c + 1) * 128]),
                        e.dma_start(out=gslot[st_][1], in_=w_in_v[:, :, C_GB + c * 128:C_GB + (c + 1) * 128]),
                        e.dma_start(out=uslot[st_][0], in_=wus_v[:, :, c * 128:(c + 1) * 128]),
                        e.dma_start(out=uslot[st_][1], in_=wum_v[:, :, c * 128:(c + 1) * 128])]
            prog.add("pool", _ld, writes=[B_gs[st_]] + extra, dsem=S_gs[st_], ndma=4)
            for n in range(2):
                alt = cnt4[0] % 2
                cnt4[0] += 1
                bga, bgb, bya, byb = ((0, 1, 3, 4), (2, 5, 6, 7))[alt]
                tl = n * 512
                for (bank, wt) in ((bga, gslot[st_][0]), (bgb, gslot[st_][1])):
                    for k in range(16):
                        prog.add("pe", lambda e, k=k, bank=bank, wt=wt, tl=tl: e.matmul(
                            PS(bank), lhsT=wt[:, k, :], rhs=hTo[:, k, tl:tl + 512], start=(k == 0), stop=(k == 15)),
                            reads=[B_gs[st_], B_hTo], writes=[B_ps[bank]])
                for (bank, wt, at_, Bat) in ((bya, uslot[st_][0], attn_swa, B_attn_swa), (byb, uslot[st_][1], attn_moba, B_attn_moba)):
                    for k in range(8):
                        prog.add("pe", lambda e, k=k, bank=bank, wt=wt, at_=at_, tl=tl: e.matmul(
                            PS(bank), lhsT=wt[:, k, :], rhs=at_[:, k, tl:tl + 512], start=(k == 0), stop=(k == 7)),
                            reads=[B_gs[st_]] + Bat, writes=[B_ps[bank]])
                prog.add("act", lambda e, bga=bga, alt=alt: e.activation(out=sga[alt], in_=PS(bga), func=AF.Sigmoid),
                         reads=[B_ps[bga]], writes=[B_sga[alt]])
                prog.add("act", lambda e, bgb=bgb, alt=alt: e.activation(out=sgb[alt], in_=PS(bgb), func=AF.Sigmoid),
                         reads=[B_ps[bgb]], writes=[B_sgb[alt]])
                prog.add("dve", lambda e, bya=bya, alt=alt: e.tensor_tensor(out=tmul[alt], in0=PS(bya), in1=sga[alt], op=ALU.mult),
                         reads=[B_ps[bya], B_sga[alt]], writes=[B_tmul[alt]])
                prog.add("dve", lambda e, byb=byb, alt=alt: e.tensor_tensor(out=sgb[alt], in0=PS(byb), in1=sgb[alt], op=ALU.mult),
                         reads=[B_ps[byb], B_sgb[alt]], writes=[B_sgb[alt]])
                prog.add("dve", lambda e, c=c, tl=tl, alt=alt: e.tensor_tensor(out=mergedT[:, c, tl:tl + 512], in0=tmul[alt],
                                                                               in1=sgb[alt], op=ALU.add),
                         reads=[B_tmul[alt], B_sgb[alt]], writes=[B_merged[c]])
        if debug and stage == 4:
            dbg_outs["mergedT"] = (mergedT, [128, 16 * 1024], BF16, B_merged)

    if stage >= 5:
        R1.reset()
        x1 = R1.alloc([128, 8, D], F32)
        B_x1 = [Buf("x1_%d" % t) for t in range(8)]
        S_x1 = new_sem()
        xs_own = xs[NOWN:NKV, :].rearrange("(t p) d -> p t d", p=128)

        def _ldx(e):
            return [e.dma_start(out=x1[:, t, :], in_=xs_own[:, t, :]) for t in range(8)]
        prog.add("sp", _ldx, writes=B_x1 + [B_hTp, B_hTo], dsem=S_x1, ndma=8)
        ob2 = [0]
        for cg in range(8):
            s = cg % NWO
            prog.add("pool", lambda e, cg=cg, s=s: e.dma_start(out=woslot[s], in_=wo_v[:, :, cg * 256:(cg + 1) * 256]),
                     writes=[B_wo[s]], dsem=S_wo[s])
            for t in range(8):
                bank = ob2[0] % 8
                ob2[0] += 1
                for k in range(16):
                    prog.add("pe", lambda e, k=k, t=t, s=s, bank=bank: e.matmul(
                        PS(bank, 0, 256), lhsT=mergedT[:, k, t * 128:(t + 1) * 128], rhs=woslot[s][:, k, :],
                        start=(k == 0), stop=(k == 15)), reads=[B_wo[s]] + B_merged, writes=[B_ps[bank]])
                prog.add("dve", lambda e, t=t, cg=cg, bank=bank: e.tensor_tensor(
                    out=x1[:, t, cg * 256:(cg + 1) * 256], in0=PS(bank, 0, 256), in1=x1[:, t, cg * 256:(cg + 1) * 256],
                    op=ALU.add), reads=[B_ps[bank], B_x1[t]], writes=[B_x1[t]])
        if debug and stage == 5:
            dbg_outs["x1"] = (x1, [128, 8 * D], F32, B_x1)

    if stage >= 6:
        R2.reset()
        h2T = R2.alloc([128, 16, 1024], BF16)
        B_h2T = Buf("h2T")
        phaseB_bufs = B_gs + B_merged + B_sga + B_sgb + B_tmul + B_wo
        R3.reset()
        NWE = 4
        ering = [R3.alloc([128, 8192], BF16) for _ in range(NWE)]
        B_er = [Buf("er%d" % i) for i in range(NWE)]
        S_er = [new_sem() for _ in range(NWE)]
        hidT = [R3.alloc([128, 4, 1024], BF16) for _ in range(2)]
        B_hid = [[Buf("hid%d_%d" % (i, n)) for n in range(2)] for i in range(2)]
        sg = [R3.alloc([128, 512], F32) for _ in range(2)]
        B_sg = [Buf("sg%d" % i) for i in range(2)]
        comb = R3.alloc([128, 8, 16], F32)
        B_comb = Buf("comb")
        wr_sb = R3.alloc([128, 16, 20], F32)
        B_wr = Buf("wr")
        S_wr = new_sem()
        L_all = R3.alloc([128, 8, 20], F32)
        B_L = Buf("L")
        ss2 = R3.alloc([128, 8], F32)
        ln2 = R3.alloc([128, 8], F32)
        rs2 = R3.alloc([128, 8], F32)
        B_ss2 = Buf("ss2")
        rt_small = [R3.alloc([128, 8, 16], F32) for _ in range(8)]
        B_rts = Buf("rts")
        moe_end = R3.off
        assert R3.off <= R3.size, (R3.off, R3.size)
        gbc2 = R3.alloc([128, D], F32, at=16384)
        h2f = [R3.alloc([128, D], F32, at=16384 + 8192 + i * 8192) for i in range(2)]
        B_h2f = [Buf("h2f%d" % i) for i in range(2)]
        h2Tf = [R3.alloc([128, 16, 128], F32, at=16384 + 3 * 8192 + i * 8192) for i in range(2)]
        B_h2Tf = [Buf("h2Tf%d" % i) for i in range(2)]
        junk2 = R3.alloc([128, D], BF16, at=16384 + 5 * 8192)
        B_junk2 = Buf("junk2")
        B_gbc2 = Buf("gbc2")
        S_gbc2 = new_sem()
        B_scrC = [B_er[1], B_er[2], B_er[3]]

        barrier(phaseB_bufs, B_er + [b for bb in B_hid for b in bb] + B_sg + [B_comb, B_wr, B_L, B_ss2, B_rts, B_gbc2, B_junk2] +
                B_h2f + B_h2Tf)
        prog.add("sp", lambda e: e.dma_start(out=gbc2, in_=gffn_bc[:, :]), writes=[B_gbc2], dsem=S_gbc2)
        prog.add("sp", lambda e: e.dma_start(out=wr_sb, in_=w_r.rearrange("(k p) n -> p k n", p=128)),
                 writes=[B_wr], dsem=S_wr)
        prog.add("dve", lambda e: e.memset(ss2, 0.0), writes=[B_ss2])
        for t in range(8):
            i2 = t % 2
            prog.add("act", lambda e, t=t: e.activation(out=junk2, in_=x1[:, t, :], func=AF.Square, accum_out=ss2[:, t:t + 1]),
                     reads=[B_x1[t], B_ss2, B_gbc2], writes=[B_junk2, B_ss2])
            prog.add("act", lambda e, t=t: e.activation(out=ln2[:, t:t + 1], in_=ss2[:, t:t + 1], func=AF.Ln, scale=1.0 / D,
                                                        bias=epscol[:, 0:1]), reads=[B_ss2, B_eps], writes=[B_ss2])
            prog.add("act", lambda e, t=t: e.activation(out=rs2[:, t:t + 1], in_=ln2[:, t:t + 1], func=AF.Exp, scale=-0.5),
                     reads=[B_ss2], writes=[B_ss2])
            prog.add("dve", lambda e, t=t, i2=i2: e.scalar_tensor_tensor(
                out=h2f[i2], in0=x1[:, t, :], scalar=rs2[:, t:t + 1], op0=ALU.mult, in1=gbc2, op1=ALU.mult),
                reads=[B_x1[t], B_ss2, B_gbc2], writes=[B_h2f[i2]])
            for q4 in range(4):
                bank = (t * 4 + q4) % 4
                for kk in range(4):
                    k = q4 * 4 + kk
                    prog.add("pe", lambda e, k=k, kk=kk, bank=bank, i2=i2: e.transpose(
                        out=PS(bank, kk * 128, (kk + 1) * 128), in_=h2f[i2][:, k * 128:(k + 1) * 128], identity=ident_f),
                        reads=[B_h2f[i2], B_const], writes=[B_ps[bank]])
                prog.add("act", lambda e, q4=q4, bank=bank, i2=i2: e.activation(
                    out=h2Tf[i2][:, q4 * 4:(q4 + 1) * 4, :], in_=PS(bank).rearrange("p (a b) -> p a b", b=128), func=AF.Copy),
                    reads=[B_ps[bank]], writes=[B_h2Tf[i2]])
                prog.add("dve", lambda e, q4=q4, i2=i2, t=t: e.tensor_copy(
                    out=h2T[:, q4 * 4:(q4 + 1) * 4, t * 128:(t + 1) * 128], in_=h2Tf[i2][:, q4 * 4:(q4 + 1) * 4, :]),
                    reads=[B_h2Tf[i2]], writes=[B_h2T] + (B_attn_swa + B_attn_moba if (t == 0 and q4 == 0) else []))
            for k in range(16):
                prog.add("pe", lambda e, k=k, t=t, i2=i2: e.matmul(PS(4 + t % 2, 0, 20), lhsT=h2Tf[i2][:, k, :], rhs=wr_sb[:, k, :],
                                                                   start=(k == 0), stop=(k == 15)),
                         reads=[B_h2Tf[i2], B_wr], writes=[B_ps[4 + t % 2]])
            prog.add("dve", lambda e, t=t: e.tensor_copy(out=L_all[:, t, :], in_=PS(4 + t % 2, 0, 20)),
                     reads=[B_ps[4 + t % 2]], writes=[B_L])
        lg = L_all[:, :, 0:4]
        le = L_all[:, :, 4:20]
        mg, ohg, tmp16, sl, l1, msk, l2, ex, exm, den, gp, sumg, wexp, junk8 = (None,) * 14
        mg = rt_small[0][:, :, 0:1]
        ohg = rt_small[0][:, :, 4:8]
        sumg = rt_small[0][:, :, 8:9]
        gp = rt_small[0][:, :, 9:10]
        l1 = rt_small[0][:, :, 10:11]
        l2 = rt_small[0][:, :, 11:12]
        den = rt_small[0][:, :, 12:13]
        fac = rt_small[0][:, :, 13:14]
        tmp16 = rt_small[1]
        sl = rt_small[2][:, :, 0:4]
        msk = rt_small[2][:, :, 4:8]
        sl2 = rt_small[2][:, :, 8:12]
        ex = rt_small[3][:, :, 0:4]
        exm = rt_small[3][:, :, 4:8]
        wexp = rt_small[3][:, :, 8:12]
        eg = rt_small[4][:, :, 0:4]
        dgl = rt_small[4][:, :, 4:8]
        dsl = rt_small[4][:, :, 8:12]

        def dv(fn, r=(B_L, B_rts), w=(B_rts,)):
            prog.add("dve", fn, reads=list(r), writes=list(w))
        dv(lambda e: e.tensor_reduce(out=mg, in_=lg, axis=AX.X, op=ALU.max))
        dv(lambda e: e.tensor_tensor(out=ohg, in0=lg, in1=mg.to_broadcast([128, 8, 4]), op=ALU.is_ge))
        dv(lambda e: e.tensor_tensor(out=dgl, in0=lg, in1=mg.to_broadcast([128, 8, 4]), op=ALU.subtract))
        prog.add("act", lambda e: e.activation(out=eg, in_=dgl, func=AF.Exp), reads=[B_rts], writes=[B_rts])
        dv(lambda e: e.tensor_reduce(out=sumg, in_=eg, axis=AX.X, op=ALU.add))
        dv(lambda e: e.reciprocal(out=gp, in_=sumg))
        dv(lambda e: e.tensor_tensor(out=tmp16.rearrange("p t (g e) -> p t g e", e=4),
                                     in0=le.rearrange("p t (g e) -> p t g e", e=4),
                                     in1=ohg.unsqueeze(3).to_broadcast([128, 8, 4, 4]), op=ALU.mult))
        dv(lambda e: e.tensor_reduce(out=sl, in_=tmp16.rearrange("p t (g e) -> p t e g", e=4), axis=AX.X, op=ALU.add))
        dv(lambda e: e.tensor_reduce(out=l1, in_=sl, axis=AX.X, op=ALU.max))
        dv(lambda e: e.tensor_tensor(out=msk, in0=sl, in1=l1.to_broadcast([128, 8, 4]), op=ALU.is_ge))
        dv(lambda e: e.scalar_tensor_tensor(out=sl2, in0=msk, scalar=-1e30, op0=ALU.mult, in1=sl, op1=ALU.add))
        dv(lambda e: e.tensor_reduce(out=l2, in_=sl2, axis=AX.X, op=ALU.max))
        dv(lambda e: e.tensor_tensor(out=msk, in0=sl, in1=l2.to_broadcast([128, 8, 4]), op=ALU.is_ge))
        dv(lambda e: e.tensor_tensor(out=dsl, in0=sl, in1=l1.to_broadcast([128, 8, 4]), op=ALU.subtract))
        prog.add("act", lambda e: e.activation(out=ex, in_=dsl, func=AF.Exp), reads=[B_rts], writes=[B_rts])
        dv(lambda e: e.tensor_tensor(out=exm, in0=ex, in1=msk, op=ALU.mult))
        dv(lambda e: e.tensor_reduce(out=den, in_=exm, axis=AX.X, op=ALU.add))
        dv(lambda e: e.reciprocal(out=fac, in_=den))
        dv(lambda e: e.tensor_tensor(out=fac, in0=fac, in1=gp, op=ALU.mult))
        dv(lambda e: e.tensor_tensor(out=wexp, in0=exm, in1=fac.to_broadcast([128, 8, 4]), op=ALU.mult))
        dv(lambda e: e.tensor_tensor(out=comb.rearrange("p t (g e) -> p t g e", e=4),
                                     in0=ohg.unsqueeze(3).to_broadcast([128, 8, 4, 4]),
                                     in1=wexp.unsqueeze(2).to_broadcast([128, 8, 4, 4]), op=ALU.mult),
           w=(B_rts, B_comb))
        if debug and stage == 6:
            dbg_outs["h2T"] = (h2T, [128, 16 * 1024], BF16, [B_h2T])
            dbg_outs["L_all"] = (L_all, [128, 8 * 20], F32, [B_L])
            dbg_outs["comb"] = (comb, [128, 8 * 16], F32, [B_comb])

    if stage >= 7:
        er = [0]

        def load_e(dram_ap, shape3):
            s = er[0] % NWE
            er[0] += 1
            view = ering[s].rearrange("p (a b) -> p a b", b=shape3[2])
            extra = B_scrC_users if s in (1, 2, 3) and er[0] <= NWE else []
            prog.add("pool", lambda e: e.dma_start(out=view, in_=dram_ap), writes=[B_er[s]] + extra, dsem=S_er[s])
            return s, view
        B_scrC_users = [B_gbc2, B_junk2] + B_h2f + B_h2Tf
        gu = [0]
        yb = [0]
        for ex_i in range(16):
            hb = ex_i % 2
            s_g, Wg = load_e(w_gate_e[ex_i].rearrange("(k p) f -> p k f", p=128), [128, 16, 512])
            s_u, Wu = load_e(w_up_e[ex_i].rearrange("(k p) f -> p k f", p=128), [128, 16, 512])
            s_d, Wd = load_e(w_down_e[ex_i].rearrange("(k p) d -> p k d", p=128), [128, 4, 2048])
            for n in range(2):
                for fc in range(4):
                    alt = gu[0] % 2
                    gu[0] += 1
                    bg, bu = (0, 1) if alt == 0 else (2, 3)
                    for (bank, W_, s_) in ((bg, Wg, s_g), (bu, Wu, s_u)):
                        for k in range(16):
                            prog.add("pe", lambda e, k=k, bank=bank, W_=W_, fc=fc, n=n: e.matmul(
                                PS(bank), lhsT=W_[:, k, fc * 128:(fc + 1) * 128], rhs=h2T[:, k, n * 512:(n + 1) * 512],
                                start=(k == 0), stop=(k == 15)), reads=[B_er[s_], B_h2T], writes=[B_ps[bank]])
                    prog.add("act", lambda e, bg=bg, alt=alt: e.activation(out=sg[alt], in_=PS(bg), func=AF.Silu),
                             reads=[B_ps[bg]], writes=[B_sg[alt]])
                    prog.add("dve", lambda e, bu=bu, alt=alt, hb=hb, fc=fc, n=n: e.tensor_tensor(
                        out=hidT[hb][:, fc, n * 512:(n + 1) * 512], in0=PS(bu), in1=sg[alt], op=ALU.mult),
                        reads=[B_ps[bu], B_sg[alt]], writes=[B_hid[hb][n]])
            for t in range(8):
                for half in range(2):
                    b0 = 4 + 2 * (yb[0] % 2)
                    yb[0] += 1
                    for bb in range(2):
                        col = half * 1024 + bb * 512
                        for fc in range(4):
                            prog.add("pe", lambda e, fc=fc, t=t, col=col, bank=b0 + bb, hb=hb, Wd=Wd: e.matmul(
                                PS(bank), lhsT=hidT[hb][:, fc, t * 128:(t + 1) * 128], rhs=Wd[:, fc, col:col + 512],
                                start=(fc == 0), stop=(fc == 3)), reads=[B_er[s_d], B_hid[hb][t // 4]],
                                writes=[B_ps[b0 + bb]])
                    prog.add("dve", lambda e, t=t, half=half, b0=b0, ex_i=ex_i: e.scalar_tensor_tensor(
                        out=x1[:, t, half * 1024:(half + 1) * 1024], in0=psum[:, b0 * 512:(b0 + 2) * 512],
                        scalar=comb[:, t, ex_i:ex_i + 1], op0=ALU.mult, in1=x1[:, t, half * 1024:(half + 1) * 1024],
                        op1=ALU.add), reads=[B_ps[b0], B_ps[b0 + 1], B_comb, B_x1[t]], writes=[B_x1[t]])
        B_y = Buf("y")
        S_y = new_sem()
        y_v = y.rearrange("(t p) d -> p t d", p=128)
        for t in range(8):
            prog.add("sp", lambda e, t=t: e.dma_start(out=y_v[:, t, :], in_=x1[:, t, :]), reads=[B_x1[t]], writes=[B_y],
                     dsem=S_y)
        prog.add("sp", None, reads=[B_y])

    if debug:
        B_dbg = Buf("dbg")
        S_dbg = new_sem()
        for name, (ap, shape, dt, bufs) in dbg_outs.items():
            o = nc.dram_tensor("dbg_" + name, list(shape), dt, kind="ExternalOutput").ap()
            src = ap
            if len(ap.shape) == 3:
                src = ap.rearrange("p a b -> p (a b)")
            prog.add("sp", lambda e, o=o, src=src: e.dma_start(out=o[:, :], in_=src), reads=bufs, writes=[B_dbg], dsem=S_dbg)
        prog.add("sp", None, reads=[B_dbg])
        if stage < 7:
            pass

    block = es.enter_context(nc.Block())
    prog.emit(block, engsem)
    es.close()
    return nc, list(dbg_outs.keys())


_CONSTS = None


def _prepare_inputs(inputs, cores):
    global _CONSTS
    if _CONSTS is None:
        _CONSTS = _const_tables()
    c = _CONSTS
    f = lambda a: np.ascontiguousarray(np.asarray(a, dtype=np.float32))
    x = f(inputs["x"])
    shared = {
        "w_in": f(inputs["w_in"]), "w_up_swa": f(inputs["w_up_swa"]), "w_up_moba": f(inputs["w_up_moba"]),
        "w_out": f(inputs["w_out"]),
        "w_r": np.ascontiguousarray(np.concatenate(
            [f(inputs["w_router_group"]), f(inputs["w_router_expert"]).transpose(1, 0, 2).reshape(D, 16)], axis=1)),
        "w_gate_e": f(inputs["w_gate_e"]), "w_up_e": f(inputs["w_up_e"]), "w_down_e": f(inputs["w_down_e"]),
        "gmix_bc": np.ascontiguousarray(np.broadcast_to(f(inputs["g_mix"])[None, :], (128, D))),
        "gffn_bc": np.ascontiguousarray(np.broadcast_to(f(inputs["g_ffn"])[None, :], (128, D))),
        "gcols": np.ascontiguousarray(np.stack(
            [np.tile(f(inputs[k]), 2) for k in ("q_norm_swa", "k_norm_swa", "q_norm_moba", "k_norm_moba")], axis=1)),
        "sinks_bc": np.ascontiguousarray(np.broadcast_to(f(inputs["sinks"])[None, :], (128, 16))),
        "ident_bf": c["ident_bf"], "ident_f": c["ident_f"], "bd64": c["bd64"], "dsw": c["dsw"], "mneg": c["mneg"],
        "tabq": c["tabq"], "tabk": c["tabk"],
    }
    in_maps = []
    for cid in cores:
        b, hf = cid // 2, cid % 2
        if hf == 1:
            xs_ = x[b]
        else:
            xs_ = np.concatenate([x[b, NOWN:], x[b, :NOWN]], axis=0)
        m = dict(shared)
        m["xs"] = np.ascontiguousarray(xs_)
        m.update(_percore_tables(hf))
        in_maps.append(m)
    return in_maps


_NC_CACHE = {}


def kernel(**inputs):
    if "full" not in _NC_CACHE:
        _NC_CACHE["full"] = build_program(stage=99, debug=False)[0]
    nc = _NC_CACHE["full"]
    cores = list(range(8))
    in_maps = _prepare_inputs(inputs, cores)
    res = run_bass_kernel_spmd(nc, in_maps, core_ids=cores)
    out = np.empty((4, 2048, D), np.float32)
    for cid in cores:
        b, hf = cid // 2, cid % 2
        out[b, hf * NOWN:(hf + 1) * NOWN, :] = res.results[cid]["y"]
    return out
```

```python
import numpy as np
import ml_dtypes
from contextlib import ExitStack
import concourse.bass as bass
import concourse.mybir as mybir
from concourse.bass_utils import run_bass_kernel_spmd

F32 = mybir.dt.float32
BF16 = mybir.dt.bfloat16
U8 = mybir.dt.uint8
ALU = mybir.AluOpType
AF = mybir.ActivationFunctionType
AX = mybir.AxisListType
NPBF = ml_dtypes.bfloat16

D = 2048
NOWN = 1024
NKV = 2048
EPS = 1e-6
SCALE = 0.125
BIG = 32768.0
IN_COLS = 8448
C_QA, C_KA, C_VA, C_QB, C_KB, C_VB, C_GA, C_GB = 0, 1024, 1152, 1280, 2304, 3328, 4352, 6400
SLOPES = [2.0 ** (-(h + 1) / 2.0) for h in range(16)]
ARENA = 211968


class Buf:
    __slots__ = ("name", "lw", "lr", "excl")

    def __init__(self, name, excl=False):
        self.name = name
        self.lw = None
        self.lr = {}
        self.excl = excl


class DSem:
    def __init__(self, h):
        self.h = h
        self.count = 0


class Op:
    __slots__ = ("eng", "fn", "deps", "signal", "dsem", "val", "idx")


ENGS = ("pe", "act", "dve", "pool", "sp")


class Prog:
    def __init__(self):
        self.ops = []

    @staticmethod
    def _need(p, eng, is_dma, raw):
        if p.dsem is not None or is_dma:
            return True
        if p.eng != eng:
            return True
        if eng == "pe":
            return False
        return raw

    def add(self, eng, fn, reads=(), writes=(), dsem=None, ndma=1):
        idx = len(self.ops)
        op = Op()
        op.eng, op.fn, op.signal, op.dsem, op.idx, op.val = eng, fn, False, dsem, idx, None
        is_dma = dsem is not None
        key = ("d", idx) if is_dma else eng
        deps = set()
        for b in reads:
            w = b.lw
            if w is not None and self._need(self.ops[w], eng, is_dma, True):
                deps.add(w)
            if b.excl:
                for k2, r in b.lr.items():
                    if k2 != key:
                        deps.add(r)
        for b in writes:
            w = b.lw
            if w is not None and self._need(self.ops[w], eng, is_dma, False):
                deps.add(w)
            for r in b.lr.values():
                if self._need(self.ops[r], eng, is_dma, False):
                    deps.add(r)
        for b in reads:
            b.lr[key] = idx
        for b in writes:
            b.lw = idx
            b.lr = {}
        for d in deps:
            self.ops[d].signal = True
        op.deps = sorted(deps)
        if is_dma:
            dsem.count += 16 * ndma
            op.val = dsem.count
        self.ops.append(op)
        return op

    def emit(self, block, engsem):
        cnt = {}
        for op in self.ops:
            if op.dsem is None and op.signal:
                cnt[op.eng] = cnt.get(op.eng, 0) + 1
                op.val = cnt[op.eng]
        by = {e: [] for e in ENGS}
        for op in self.ops:
            by[op.eng].append(op)
        ops = self.ops

        def run(name):
            def body(e):
                waited = {}
                for op in by[name]:
                    for d in op.deps:
                        p = ops[d]
                        if p.dsem is not None:
                            sem, k = p.dsem.h, ("d", id(p.dsem))
                        else:
                            sem, k = engsem[p.eng], p.eng
                        if waited.get(k, 0) < p.val:
                            e.wait_ge(sem, p.val)
                            waited[k] = p.val
                    if op.fn is None:
                        continue
                    r = op.fn(e)
                    if op.dsem is not None:
                        for ins in (r if isinstance(r, (list, tuple)) else [r]):
                            ins.then_inc(op.dsem.h, 16)
                    elif op.signal:
                        r.then_inc(engsem[op.eng], 1)
            return body

        block.tensor(run("pe"))
        block.scalar(run("act"))
        block.vector(run("dve"))
        block.gpsimd(run("pool"))
        block.sync(run("sp"))


def _split3(a):
    a = a.astype(np.float64)
    hi = a.astype(NPBF)
    r = a - hi.astype(np.float64)
    mid = r.astype(NPBF)
    r = r - mid.astype(np.float64)
    lo = r.astype(NPBF)
    return hi, mid, lo


def _const_tables():
    c = {}
    c["ident_bf"] = np.eye(128, dtype=np.float32).astype(NPBF)
    c["ident_f"] = np.eye(128, dtype=np.float32)
    bd = np.zeros((128, 128), np.float32)
    bd[:64, :64] = 1.0 / 64
    bd[64:, 64:] = 1.0 / 64
    c["bd64"] = bd.astype(NPBF)
    k = np.arange(128)[:, None].astype(np.float64)
    q = np.arange(128)[None, :].astype(np.float64)
    da = q - k
    da = np.where(da >= 0, da, 1e9)
    db = q + 128 - k
    db = np.where(db < 128, db, 1e9)
    c["dsw"] = np.concatenate([db, da, db, da], axis=1).astype(np.float32)
    q2 = np.arange(256)[None, :]
    m0 = np.where(q2 >= np.arange(128)[:, None], 0.0, -1e9)
    m1 = np.where(q2 >= (np.arange(128)[:, None] + 128), 0.0, -1e9)
    c["mneg"] = np.concatenate([m0, m1], axis=1).astype(np.float32)
    tq = np.arange(NOWN).astype(np.float64)
    tk = (np.arange(NKV) - 1024).astype(np.float64)
    tabq = np.zeros((16, 6, NOWN), NPBF)
    tabk = np.zeros((16, 14, NKV), NPBF)
    ind = (np.arange(NKV)[None, :] // 256 == np.arange(8)[:, None]).astype(np.float32)
    for h in range(16):
        s = SLOPES[h]
        a = _split3(-s * tq / SCALE)
        b = _split3(s * tk / SCALE)
        for i in range(3):
            tabq[h, i] = a[i]
            tabq[h, 3 + i] = 1.0
            tabk[h, 8 + i] = 1.0
            tabk[h, 11 + i] = b[i]
        tabk[h, 0:8] = ind.astype(NPBF)
    c["tabq"] = tabq
    c["tabk"] = tabk
    return c


def _percore_tables(hf):
    pb = np.full((128, 8, 8), -1e30, np.float32)
    for t in range(8):
        own = 4 + t // 2
        for n in range(8):
            if n == own:
                pb[:, t, n] = 1e30
            elif n < own and (hf == 1 or n >= 4):
                pb[:, t, n] = 0.0
    pv = np.full((128, 1), float(hf), np.float32)
    return {"pastbias": pb.reshape(128, 64), "pvcol": pv}


def build_program(stage=99, debug=False):
    nc = bass.Bass("TRN2", target_bir_lowering=False)
    es = ExitStack()
    prog = Prog()
    dbg_outs = {}

    def din(name, shape, dt):
        return nc.dram_tensor(name, list(shape), dt, kind="ExternalInput").ap()

    xs = din("xs", [NKV, D], F32)
    w_in = din("w_in", [D, IN_COLS], F32)
    w_up_swa = din("w_up_swa", [1024, D], F32)
    w_up_moba = din("w_up_moba", [1024, D], F32)
    w_out = din("w_out", [D, D], F32)
    w_r = din("w_r", [D, 20], F32)
    w_gate_e = din("w_gate_e", [16, D, 512], F32)
    w_up_e = din("w_up_e", [16, D, 512], F32)
    w_down_e = din("w_down_e", [16, 512, D], F32)
    gmix_bc = din("gmix_bc", [128, D], F32)
    gffn_bc = din("gffn_bc", [128, D], F32)
    gcols_d = din("gcols", [128, 4], F32)
    sinks_bc = din("sinks_bc", [128, 16], F32)
    ident_bf_d = din("ident_bf", [128, 128], BF16)
    ident_f_d = din("ident_f", [128, 128], F32)
    bd64_d = din("bd64", [128, 128], BF16)
    dsw_d = din("dsw", [128, 512], F32)
    mneg_d = din("mneg", [128, 512], F32)
    tabq_d = din("tabq", [16, 6, NOWN], BF16)
    tabk_d = din("tabk", [16, 14, NKV], BF16)
    pastbias_d = din("pastbias", [128, 64], F32)
    pvcol_d = din("pvcol", [128, 1], F32)
    y = nc.dram_tensor("y", [NOWN, D], F32, kind="ExternalOutput").ap()

    arena = es.enter_context(nc.sbuf_tensor("arena", [128, ARENA], U8))
    psum = es.enter_context(nc.psum_tensor("psum", [128, 4096], F32))

    def PS(b, lo=0, hi=512):
        return psum[:, b * 512 + lo:b * 512 + hi]

    def PSB(b, lo=0, hi=1024):
        return psum[:, b * 512:(b + 1) * 512].bitcast(BF16)[:, lo:hi]

    B_ps = [Buf("ps%d" % i, excl=True) for i in range(8)]

    class Region:
        def __init__(self, base, size):
            self.base, self.size, self.off = base, size, 0

        def reset(self, off=0):
            self.off = off

        def alloc(self, shape, dt, at=None):
            nb = {F32: 4, BF16: 2}[dt]
            n = int(np.prod(shape[1:])) * nb
            n32 = (n + 31) // 32 * 32
            off = self.off if at is None else at
            assert off + n32 <= self.size, (off, n32, self.size)
            if at is None:
                self.off += n32
            v = arena[:, self.base + off:self.base + off + n].bitcast(dt)
            if len(shape) == 3:
                v = v.rearrange("p (a b) -> p a b", b=shape[2])
            return v

    CONST_SZ = 6144
    R1_SZ = 65536
    R2_SZ = 32768
    RC = Region(0, CONST_SZ)
    R1 = Region(CONST_SZ, R1_SZ)
    R2 = Region(CONST_SZ + R1_SZ, R2_SZ)
    R3 = Region(CONST_SZ + R1_SZ + R2_SZ, ARENA - CONST_SZ - R1_SZ - R2_SZ)

    sem_id = [0]

    def new_sem():
        sem_id[0] += 1
        return DSem(es.enter_context(nc.semaphore("s%d" % sem_id[0])))

    engsem = {e: es.enter_context(nc.semaphore("eng_" + e)) for e in ENGS}

    ident_bf = RC.alloc([128, 128], BF16)
    ident_f = RC.alloc([128, 128], F32)
    bd64 = RC.alloc([128, 128], BF16)
    gcols = RC.alloc([128, 4], F32)
    epscol = RC.alloc([128, 1], F32)
    expsink = RC.alloc([128, 16], F32)
    pvcol = RC.alloc([128, 1], F32)
    pastbias = RC.alloc([128, 64], F32)
    dsw = RC.alloc([128, 512], F32)
    mneg = RC.alloc([128, 512], F32)
    ss1 = RC.alloc([128, 16], F32)
    ln1 = RC.alloc([128, 16], F32)
    rs1 = RC.alloc([128, 16], F32)
    B_const = Buf("const")
    S_const = new_sem()
    const_loads = [(ident_bf, ident_bf_d), (ident_f, ident_f_d), (bd64, bd64_d), (gcols, gcols_d),
                   (expsink, sinks_bc), (pvcol, pvcol_d), (pastbias, pastbias_d), (dsw, dsw_d), (mneg, mneg_d)]

    def _ld_consts(e):
        return [e.dma_start(out=o, in_=i[:, :]) for o, i in const_loads]
    prog.add("sp", _ld_consts, writes=[B_const], dsem=S_const, ndma=len(const_loads))
    B_eps = Buf("eps")
    prog.add("dve", lambda e: e.memset(epscol, EPS), writes=[B_eps])
    B_ss1 = [Buf("ss1_%d" % t) for t in range(16)]
    prog.add("dve", lambda e: e.memset(ss1, 0.0), writes=B_ss1)
    B_expsink = Buf("expsink")
    prog.add("act", lambda e: e.activation(out=expsink, in_=expsink, func=AF.Exp), reads=[B_const], writes=[B_expsink])

    hTp = R1.alloc([128, 16, 1024], BF16)
    hTo = R1.alloc([128, 16, 1024], BF16)
    B_hTp, B_hTo = Buf("hTp"), Buf("hTo")
    attn_swa = R2.alloc([128, 8, 1024], BF16)
    attn_moba = R2.alloc([128, 8, 1024], BF16)
    B_attn_swa = [Buf("attn_swa%d" % i) for i in range(8)]
    B_attn_moba = [Buf("attn_moba%d" % i) for i in range(8)]

    NW = 6
    wslot = [R3.alloc([128, 16, 128], BF16) for _ in range(NW)]
    B_w = [Buf("w%d" % i) for i in range(NW)]
    S_w = [new_sem() for _ in range(NW)]
    QA = [[R3.alloc([128, 1024], BF16) for _ in range(2)] for _ in range(2)]
    B_QA = [[Buf("QA%d%d" % (i, j)) for j in range(2)] for i in range(2)]
    B_QAaug = [[Buf("QAaug%d%d" % (i, j)) for j in range(2)] for i in range(2)]
    S_QAaug = [[new_sem() for j in range(2)] for i in range(2)]
    off_KA = R3.off
    KA = [[R3.alloc([128, 2048], BF16) for _ in range(2)] for _ in range(2)]
    B_KA = [[Buf("KA%d%d" % (i, j)) for j in range(2)] for i in range(2)]
    B_KAaug = [[Buf("KAaug%d%d" % (i, j)) for j in range(2)] for i in range(2)]
    S_KAaug = [[new_sem() for j in range(2)] for i in range(2)]
    VA = [[R3.alloc([128, 16, 128], BF16) for _ in range(2)] for _ in range(2)]
    B_VA = [[Buf("VA%d%d" % (i, j)) for j in range(2)] for i in range(2)]
    KS = [R3.alloc([128, 1152], BF16) for _ in range(2)]
    B_KS = [Buf("KS%d" % i) for i in range(2)]
    VS = [R3.alloc([128, 9, 128], BF16) for _ in range(2)]
    B_VS = [Buf("VS%d" % i) for i in range(2)]
    sqb = [R3.alloc([128, 512], BF16) for _ in range(2)]
    B_sqb = [Buf("sqb%d" % i) for i in range(2)]
    lnb = [R3.alloc([128, 512], F32) for _ in range(2)]
    B_lnb = [Buf("lnb%d" % i) for i in range(2)]
    rsb = [R3.alloc([128, 512], F32) for _ in range(2)]
    B_rsb = [Buf("rsb%d" % i) for i in range(2)]
    tmpB = [R3.alloc([128, 512], BF16) for _ in range(2)]
    B_tmpB = [Buf("tmpB%d" % i) for i in range(2)]
    off_Sp = R3.off
    Sp = [R3.alloc([128, 512], F32) for _ in range(3)]
    B_Sp = [Buf("Sp%d" % i) for i in range(3)]
    NPT = 4
    Pt = [R3.alloc([128, 512], BF16) for _ in range(NPT)]
    B_Pt = [Buf("Pt%d" % i) for i in range(NPT)]
    Rt = [R3.alloc([128, 256], F32) for _ in range(2)]
    B_Rt = [Buf("Rt%d" % i) for i in range(2)]
    Rt2 = [R3.alloc([128, 256], F32) for _ in range(2)]
    tmpo = [R3.alloc([128, 256], BF16) for _ in range(2)]
    B_tmpo = [Buf("tmpo%d" % i) for i in range(2)]
    gm = R3.alloc([128, 8, 8], F32)
    m8 = R3.alloc([128, 8, 8], F32)
    selt = R3.alloc([128, 8, 8], F32)
    kmf = R3.alloc([128, 8], F32)
    kmb = R3.alloc([128, 8], BF16)
    B_gm, B_m8, B_selt, B_kmf, B_kmb = Buf("gm"), Buf("m8"), Buf("selt"), Buf("kmf"), Buf("kmb")
    stage_t = [R3.alloc([128, 8, 72], BF16) for _ in range(2)]
    B_stage = [Buf("stage%d" % i) for i in range(2)]
    attn_end = R3.off
    xt = [R3.alloc([128, D], F32, at=off_KA + i * 8192) for i in range(3)]
    B_xt = [Buf("xt%d" % i) for i in range(3)]
    S_xt = [new_sem() for _ in range(3)]
    gbc = R3.alloc([128, D], F32, at=off_KA + 3 * 8192)
    xn = [R3.alloc([128, D], BF16, at=off_KA + 4 * 8192 + i * 4096) for i in range(2)]
    B_xn = [Buf("xn%d" % i) for i in range(2)]
    junk = R3.alloc([128, D], BF16, at=off_Sp)
    B_junk = Buf("junk")
    B_gbc = Buf("gbc")
    S_gbc = new_sem()
    bar_scr = RC.alloc([128, 8], F32)

    def barrier(old, new):
        prog.add("dve", lambda e: e.memset(bar_scr, 0.0), writes=list(old) + list(new))

    prog.add("sp", lambda e: e.dma_start(out=gbc, in_=gmix_bc[:, :]), writes=[B_gbc], dsem=S_gbc)
    for t in range(16):
        sl = t % 3
        x2 = t % 2
        prog.add("sp", lambda e, t=t, sl=sl: e.dma_start(out=xt[sl], in_=xs[t * 128:(t + 1) * 128, :]),
                 writes=[B_xt[sl]], dsem=S_xt[sl])
        prog.add("act", lambda e, t=t, sl=sl: e.activation(out=junk, in_=xt[sl], func=AF.Square,
                                                           accum_out=ss1[:, t:t + 1]),
                 reads=[B_xt[sl], B_ss1[t]], writes=[B_junk, B_ss1[t]])
        prog.add("act", lambda e, t=t: e.activation(out=ln1[:, t:t + 1], in_=ss1[:, t:t + 1], func=AF.Ln,
                                                    scale=1.0 / D, bias=epscol[:, 0:1]),
                 reads=[B_ss1[t], B_eps], writes=[B_ss1[t]])
        prog.add("act", lambda e, t=t: e.activation(out=rs1[:, t:t + 1], in_=ln1[:, t:t + 1], func=AF.Exp, scale=-0.5),
                 reads=[B_ss1[t]], writes=[B_ss1[t]])
        prog.add("dve", lambda e, t=t, sl=sl, x2=x2: e.scalar_tensor_tensor(
            out=xn[x2], in0=xt[sl], scalar=rs1[:, t:t + 1], op0=ALU.mult, in1=gbc, op1=ALU.mult),
            reads=[B_xt[sl], B_ss1[t], B_gbc], writes=[B_xn[x2]])
        dst = hTp if t < 8 else hTo
        Bdst = B_hTp if t < 8 else B_hTo
        tc = (t % 8) * 128
        for k in range(16):
            bank = 6 + k // 8
            prog.add("pe", lambda e, k=k, x2=x2, bank=bank: e.transpose(
                out=PSB(bank, (k % 8) * 128, (k % 8 + 1) * 128), in_=xn[x2][:, k * 128:(k + 1) * 128],
                identity=ident_bf), reads=[B_xn[x2], B_const], writes=[B_ps[bank]])
        prog.add("act", lambda e, dst=dst, tc=tc: e.activation(
            out=dst[:, 0:8, tc:tc + 128], in_=PSB(6).rearrange("p (a b) -> p a b", b=128), func=AF.Copy),
            reads=[B_ps[6]], writes=[Bdst])
        prog.add("dve", lambda e, dst=dst, tc=tc: e.tensor_copy(
            out=dst[:, 8:16, tc:tc + 128], in_=PSB(7).rearrange("p (a b) -> p a b", b=128)),
            reads=[B_ps[7]], writes=[Bdst])

    if debug and stage == 1:
        dbg_outs["hTp"] = (hTp, [128, 16 * 1024], BF16, [B_hTp])
        dbg_outs["hTo"] = (hTo, [128, 16 * 1024], BF16, [B_hTo])

    w_in_v = w_in.rearrange("(k p) n -> p k n", p=128)
    wring = [0]

    def load_wcols(c0):
        s = wring[0] % NW
        wring[0] += 1
        prog.add("pool", lambda e: e.dma_start(out=wslot[s], in_=w_in_v[:, :, c0:c0 + 128]),
                 writes=[B_w[s]], dsem=S_w[s])
        return s

    pbank = [0]
    nrm = [0]

    def proj_fm(s, src, Bsrc, lo, n):
        bank = pbank[0] % 2
        pbank[0] += 1
        for k in range(16):
            prog.add("pe", lambda e, k=k: e.matmul(PS(bank, 0, n), lhsT=wslot[s][:, k, :], rhs=src[:, k, lo:lo + n],
                                                   start=(k == 0), stop=(k == 15)),
                     reads=[B_w[s], Bsrc], writes=[B_ps[bank]])
        return bank

    def headnorm(bank, n, gidx, dstA, BA, dstB, BB):
        i = nrm[0] % 2
        nrm[0] += 1
        prog.add("act", lambda e: e.activation(out=sqb[i][:, 0:n], in_=PS(bank, 0, n), func=AF.Square),
                 reads=[B_ps[bank]], writes=[B_sqb[i]])
        prog.add("pe", lambda e: e.matmul(PS(2, 0, n), lhsT=bd64, rhs=sqb[i][:, 0:n], start=True, stop=True),
                 reads=[B_sqb[i], B_const], writes=[B_ps[2]])
        prog.add("act", lambda e: e.activation(out=lnb[i][:, 0:n], in_=PS(2, 0, n), func=AF.Ln, bias=epscol[:, 0:1]),
                 reads=[B_ps[2], B_eps], writes=[B_lnb[i]])
        prog.add("act", lambda e: e.activation(out=rsb[i][:, 0:n], in_=lnb[i][:, 0:n], func=AF.Exp, scale=-0.5),
                 reads=[B_lnb[i]], writes=[B_rsb[i]])
        prog.add("dve", lambda e: e.scalar_tensor_tensor(
            out=dstA, in0=PS(bank, 0, n)[0:64, :], scalar=gcols[0:64, gidx:gidx + 1], op0=ALU.mult,
            in1=rsb[i][0:64, 0:n], op1=ALU.mult), reads=[B_ps[bank], B_rsb[i], B_const], writes=[BA])
        prog.add("dve", lambda e: e.scalar_tensor_tensor(
            out=tmpB[i][64:128, 0:n], in0=PS(bank, 0, n)[64:128, :], scalar=gcols[64:128, gidx:gidx + 1], op0=ALU.mult,
            in1=rsb[i][64:128, 0:n], op1=ALU.mult), reads=[B_ps[bank], B_rsb[i], B_const], writes=[B_tmpB[i]])
        prog.add("dve", lambda e: e.tensor_copy(out=dstB, in_=tmpB[i][64:128, 0:n]),
                 reads=[B_tmpB[i]], writes=[BB])

    sbank = [0]
    swa_sb = [0]
    obank = [0]
    ptc = [0]
    rtc = [0]

    def finish_block(ob, nq, dst_pair, Bdst, parity, qlo, sink_h=None):
        r = rtc[0] % 2
        rtc[0] += 1
        if sink_h is not None:
            prog.add("act", lambda e: e.activation(out=Rt2[r][64:128, 0:nq], in_=PS(ob, 0, nq)[64:128, :], func=AF.Ln,
                                                   bias=expsink[64:128, sink_h:sink_h + 1]),
                     reads=[B_ps[ob], B_expsink], writes=[B_Rt[r]])
        else:
            prog.add("act", lambda e: e.activation(out=Rt2[r][64:128, 0:nq], in_=PS(ob, 0, nq)[64:128, :], func=AF.Ln),
                     reads=[B_ps[ob]], writes=[B_Rt[r]])
        prog.add("act", lambda e: e.activation(out=Rt2[r][64:128, 0:nq], in_=Rt2[r][64:128, 0:nq], func=AF.Exp, scale=-1.0),
                 reads=[B_Rt[r]], writes=[B_Rt[r]])
        prog.add("dve", lambda e: e.tensor_copy(out=Rt[r][0:64, 0:nq], in_=Rt2[r][64:128, 0:nq]),
                 reads=[B_Rt[r]], writes=[B_Rt[r]])
        if parity == 0:
            prog.add("dve", lambda e: e.tensor_tensor(out=dst_pair[0:64, qlo:qlo + nq], in0=PS(ob, 0, nq)[0:64, :],
                                                      in1=Rt[r][0:64, 0:nq], op=ALU.mult),
                     reads=[B_ps[ob], B_Rt[r]], writes=[Bdst])
        else:
            prog.add("dve", lambda e: e.tensor_tensor(out=tmpo[r][0:64, 0:nq], in0=PS(ob, 0, nq)[0:64, :],
                                                      in1=Rt[r][0:64, 0:nq], op=ALU.mult),
                     reads=[B_ps[ob], B_Rt[r]], writes=[B_tmpo[r]])
            prog.add("dve", lambda e: e.tensor_copy(out=dst_pair[64:128, qlo:qlo + nq], in_=tmpo[r][0:64, 0:nq]),
                     reads=[B_tmpo[r]], writes=[Bdst])

    def drain(g):
        if g is not None:
            for _ in g:
                pass

    def run_units(units, filler=None, every=3, depth=2):
        n = len(units)
        if not n:
            drain(filler)
            return
        for j in range(min(depth, n)):
            units[j]["S"]()
        for j in range(min(depth - 1, n)):
            units[j]["E"]()
        for i, u in enumerate(units):
            u["PV"]()
            if i + depth < n:
                units[i + depth]["S"]()
            if filler is not None and i % every == every - 1:
                next(filler, None)
            if i + depth - 1 < n:
                units[i + depth - 1]["E"]()
            if u.get("F") is not None:
                u["F"]()
        drain(filler)

    def swa_kv():
        for g in range(2):
            prog.add("dve", lambda e, g=g: e.memset(VS[g][:, :, 64:128], 1.0), writes=[B_VS[g]])
            prog.add("dve", lambda e, g=g: e.tensor_scalar(out=VS[g][:, 0:1, 64:128], in0=VS[g][:, 0:1, 64:128],
                                                           scalar1=pvcol[:, 0:1], scalar2=None, op0=ALU.mult),
                     reads=[B_const, B_VS[g]], writes=[B_VS[g]])
        sk = load_wcols(C_KA)
        sv = load_wcols(C_VA)
        for (src, Bsrc, lo, n, dlo) in ((hTp, B_hTp, 896, 128, 0), (hTo, B_hTo, 0, 512, 128), (hTo, B_hTo, 512, 512, 640)):
            bank = proj_fm(sk, src, Bsrc, lo, n)
            headnorm(bank, n, 1, KS[0][0:64, dlo:dlo + n], B_KS[0], KS[1][0:64, dlo:dlo + n], B_KS[1])
        for grp in ((7, 8, 9, 10), (11, 12, 13, 14), (15,)):
            bank = pbank[0] % 2
            pbank[0] += 1
            for j, t in enumerate(grp):
                src, Bsrc, tc = (hTp, B_hTp, t * 128) if t < 8 else (hTo, B_hTo, (t - 8) * 128)
                for k in range(16):
                    prog.add("pe", lambda e, k=k, j=j, src=src, tc=tc, bank=bank: e.matmul(
                        PS(bank, j * 128, (j + 1) * 128), lhsT=src[:, k, tc:tc + 128], rhs=wslot[sv][:, k, :],
                        start=(k == 0), stop=(k == 15)), reads=[B_w[sv], Bsrc], writes=[B_ps[bank]])
            for g in range(2):
                for j, t in enumerate(grp):
                    if t < 8:
                        prog.add("act", lambda e, g=g, j=j, t=t, bank=bank: e.activation(
                            out=VS[g][:, t - 7, 0:64], in_=PS(bank, j * 128 + g * 64, j * 128 + g * 64 + 64),
                            func=AF.Copy, scale=pvcol[:, 0:1]), reads=[B_ps[bank], B_const], writes=[B_VS[g]])
                    else:
                        prog.add("act", lambda e, g=g, j=j, t=t, bank=bank: e.activation(
                            out=VS[g][:, t - 7, 0:64], in_=PS(bank, j * 128 + g * 64, j * 128 + g * 64 + 64),
                            func=AF.Copy), reads=[B_ps[bank]], writes=[B_VS[g]])

    def swa_qproj(p, buf):
        s = load_wcols(C_QA + p * 128)
        pend = None
        for n in range(2):
            if pend is not None:
                pend()
            bank = proj_fm(s, hTo, B_hTo, n * 512, 512)
            pend = (lambda bank=bank, n=n: headnorm(
                bank, 512, 0, QA[buf][0][0:64, n * 512:(n + 1) * 512], B_QA[buf][0],
                QA[buf][1][0:64, n * 512:(n + 1) * 512], B_QA[buf][1]))
            yield
        pend()
        yield

    def swa_attn(p, buf, filler=None, every=3, depth=3):
        banks = (3, 4, 7) if depth == 3 else (3, 4)
        units = []
        for i in range(2):
            h = 2 * p + i
            g = h // 8
            q = QA[buf][i]
            Bq = B_QA[buf][i]
            for c in range(4):
                i0 = 8 + 2 * c - 7
                sb = banks[swa_sb[0] % len(banks)]
                swa_sb[0] += 1
                ob = 5 + obank[0] % 2
                obank[0] += 1
                pi = ptc[0] % NPT
                ptc[0] += 1
                si = (swa_sb[0] - 1) % len(banks)

                def S(q=q, Bq=Bq, g=g, c=c, i0=i0, sb=sb):
                    qa = 2 * c * 128
                    for (olo, ohi, kt, qlo, qhi) in ((0, 128, i0 - 1, qa, qa + 128), (128, 384, i0, qa, qa + 256),
                                                     (384, 512, i0 + 1, qa + 128, qa + 256)):
                        prog.add("pe", lambda e, olo=olo, ohi=ohi, kt=kt, qlo=qlo, qhi=qhi: e.matmul(
                            PS(sb, olo, ohi), lhsT=KS[g][0:64, kt * 128:(kt + 1) * 128], rhs=q[0:64, qlo:qhi],
                            start=True, stop=True), reads=[B_KS[g], Bq], writes=[B_ps[sb]])

                def E(h=h, sb=sb, si=si, pi=pi):
                    prog.add("dve", lambda e: e.scalar_tensor_tensor(
                        out=Sp[si], in0=dsw, scalar=-SLOPES[h] / SCALE, op0=ALU.mult, in1=PS(sb), op1=ALU.add),
                        reads=[B_ps[sb], B_const], writes=[B_Sp[si]])
                    prog.add("act", lambda e: e.activation(out=Pt[pi], in_=Sp[si], func=AF.Exp, scale=SCALE),
                             reads=[B_Sp[si]], writes=[B_Pt[pi]])

                def PV(h=h, p=p, i=i, g=g, c=c, i0=i0, ob=ob, pi=pi):
                    for (olo, kt, plo, st, sp_) in ((0, i0 - 1, 0, True, False), (0, i0, 128, False, True),
                                                    (128, i0, 256, True, False), (128, i0 + 1, 384, False, True)):
                        prog.add("pe", lambda e, olo=olo, kt=kt, plo=plo, st=st, sp_=sp_: e.matmul(
                            PS(ob, olo, olo + 128), lhsT=VS[g][:, kt, :], rhs=Pt[pi][:, plo:plo + 128],
                            start=st, stop=sp_), reads=[B_VS[g], B_Pt[pi]], writes=[B_ps[ob]])

                def F(h=h, p=p, i=i, c=c, ob=ob):
                    finish_block(ob, 256, attn_swa[:, p, :], B_attn_swa[p], i, c * 256, sink_h=h)

                units.append({"S": S, "E": E, "PV": PV, "F": F})
        run_units(units, filler, every, depth=depth)

    def moba_init():
        for b in range(2):
            for i in range(2):
                prog.add("dve", lambda e, b=b, i=i: e.memset(VA[b][i][:, :, 64:128], 1.0),
                         writes=[B_VA[b][i]])
                prog.add("dve", lambda e, b=b, i=i: e.tensor_scalar(
                    out=VA[b][i][:, 0:8, 64:128], in0=VA[b][i][:, 0:8, 64:128], scalar1=pvcol[:, 0:1], scalar2=None,
                    op0=ALU.mult), reads=[B_const, B_VA[b][i]], writes=[B_VA[b][i]])
            prog.add("dve", lambda e, b=b: e.memset(stage_t[b], 0.0), writes=[B_stage[b]])

    def moba_proj(p, buf):
        sk = load_wcols(C_KB + p * 128)
        sv = load_wcols(C_VB + p * 128)
        sq = load_wcols(C_QB + p * 128)
        for i in range(2):
            h = 2 * p + i
            prog.add("sp", lambda e, i=i, h=h: e.dma_start(out=KA[buf][i][64:78, :], in_=tabk_d[h]),
                     writes=[B_KAaug[buf][i]], dsem=S_KAaug[buf][i])
            prog.add("sp", lambda e, i=i, h=h: e.dma_start(out=QA[buf][i][72:78, :], in_=tabq_d[h]),
                     writes=[B_QAaug[buf][i]], dsem=S_QAaug[buf][i])
        pend = None
        for (src, Bsrc, lo, dlo) in ((hTp, B_hTp, 0, 0), (hTp, B_hTp, 512, 512), (hTo, B_hTo, 0, 1024), (hTo, B_hTo, 512, 1536)):
            if pend is not None:
                pend()
            bank = proj_fm(sk, src, Bsrc, lo, 512)
            pend = (lambda bank=bank, dlo=dlo: headnorm(
                bank, 512, 3, KA[buf][0][0:64, dlo:dlo + 512], B_KA[buf][0], KA[buf][1][0:64, dlo:dlo + 512], B_KA[buf][1]))
            cmd = yield
            if cmd == "flush" and pend is not None:
                pend()
                pend = None
                yield
        for t0 in range(0, 16, 4):
            if pend is not None:
                pend()
            bank = pbank[0] % 2
            pbank[0] += 1
            for j in range(4):
                t = t0 + j
                src, Bsrc, tc = (hTp, B_hTp, t * 128) if t < 8 else (hTo, B_hTo, (t - 8) * 128)
                for k in range(16):
                    prog.add("pe", lambda e, k=k, j=j, src=src, tc=tc, bank=bank: e.matmul(
                        PS(bank, j * 128, (j + 1) * 128), lhsT=src[:, k, tc:tc + 128], rhs=wslot[sv][:, k, :],
                        start=(k == 0), stop=(k == 15)), reads=[B_w[sv], Bsrc], writes=[B_ps[bank]])

            def vcopy(bank=bank, t0=t0):
                for i in range(2):
                    src_ps = PS(bank).rearrange("p (a b) -> p a b", b=128)[:, :, i * 64:(i + 1) * 64]
                    if t0 < 8:
                        prog.add("act", lambda e, i=i, src_ps=src_ps: e.activation(
                            out=VA[buf][i][:, t0:t0 + 4, 0:64], in_=src_ps, func=AF.Copy, scale=pvcol[:, 0:1]),
                            reads=[B_ps[bank], B_const], writes=[B_VA[buf][i]])
                    else:
                        prog.add("act", lambda e, i=i, src_ps=src_ps: e.activation(
                            out=VA[buf][i][:, t0:t0 + 4, 0:64], in_=src_ps, func=AF.Copy),
                            reads=[B_ps[bank]], writes=[B_VA[buf][i]])
            pend = vcopy
            cmd = yield
            if cmd == "flush" and pend is not None:
                pend()
                pend = None
                yield
        for n in range(2):
            if pend is not None:
                pend()
            bank = proj_fm(sq, hTo, B_hTo, n * 512, 512)
            pend = (lambda bank=bank, n=n: headnorm(
                bank, 512, 2, QA[buf][0][0:64, n * 512:(n + 1) * 512], B_QA[buf][0],
                QA[buf][1][0:64, n * 512:(n + 1) * 512], B_QA[buf][1]))
            cmd = yield
            if cmd == "flush" and pend is not None:
                pend()
                pend = None
                yield
        if pend is not None:
            pend()
            pend = None
        yield
        for i in range(2):
            K_, Q_ = KA[buf][i], QA[buf][i]
            prog.add("dve", lambda e, K_=K_: e.tensor_reduce(
                out=kmf[0:64, :], in_=K_[0:64, :].rearrange("p (n l) -> p n l", l=256), axis=AX.X, op=ALU.add),
                reads=[B_KA[buf][i]], writes=[B_kmf])
            prog.add("dve", lambda e: e.tensor_copy(out=kmb[0:64, :], in_=kmf[0:64, :]), reads=[B_kmf], writes=[B_kmb])
            for t in range(8):
                prog.add("pe", lambda e, t=t, Q_=Q_: e.matmul(PS(7, 256 + t * 8, 256 + t * 8 + 8),
                                                              lhsT=Q_[0:64, t * 128:(t + 1) * 128], rhs=kmb[0:64, :],
                                                              start=True, stop=True),
                         reads=[B_QA[buf][i], B_kmb], writes=[B_ps[7]])
            prog.add("dve", lambda e: e.tensor_tensor(out=gm.rearrange("p a b -> p (a b)"), in0=PS(7, 256, 320),
                                                      in1=pastbias, op=ALU.add),
                     reads=[B_ps[7], B_const], writes=[B_gm])
            for t in range(8):
                prog.add("dve", lambda e, t=t: e.max(out=m8[:, t, :], in_=gm[:, t, :]), reads=[B_gm], writes=[B_m8])
            prog.add("dve", lambda e: e.tensor_tensor(out=selt, in0=gm, in1=m8[:, :, 3:4].to_broadcast([128, 8, 8]),
                                                      op=ALU.is_ge), reads=[B_gm, B_m8], writes=[B_selt])
            prog.add("dve", lambda e: e.tensor_scalar(out=stage_t[buf][:, :, 64:72], in0=selt, scalar1=BIG,
                                                      scalar2=-BIG, op0=ALU.mult, op1=ALU.add),
                     reads=[B_selt], writes=[B_stage[buf]])
            yield
            for r in range(2):
                for t4 in range(4):
                    t = r * 4 + t4
                    prog.add("pe", lambda e, t=t, t4=t4: e.transpose(
                        out=PSB(7, t4 * 128, (t4 + 1) * 128)[0:72, :], in_=stage_t[buf][:, t, :], identity=ident_bf),
                        reads=[B_stage[buf], B_const], writes=[B_ps[7]])
                prog.add("dve", lambda e, r=r, Q_=Q_: e.tensor_copy(out=Q_[64:72, r * 512:(r + 1) * 512],
                                                                    in_=PSB(7, 0, 512)[64:72, :]),
                         reads=[B_ps[7]], writes=[B_QA[buf][i]])
            yield

    def moba_attn(p, buf, filler=None, every=3):
        units = []
        for i in range(2):
            h = 2 * p + i
            K_, Q_, V_ = KA[buf][i], QA[buf][i], VA[buf][i]
            rd = [B_KA[buf][i], B_KAaug[buf][i], B_QA[buf][i], B_QAaug[buf][i]]
            for j in range(4):
                nkt = 8 + 2 * (j + 1)
                ob = 5 + obank[0] % 2
                obank[0] += 1
                for kt in range(0, nkt, 2):
                    sb = 3 + sbank[0] % 2
                    sbank[0] += 1
                    pi = ptc[0] % NPT
                    ptc[0] += 1
                    si = sb - 3
                    diag = (kt == 8 + 2 * j)
                    last = (kt == nkt - 2)

                    def S(K_=K_, Q_=Q_, rd=rd, j=j, kt=kt, sb=sb):
                        for u in range(2):
                            prog.add("pe", lambda e, u=u: e.matmul(
                                PS(sb, u * 256, (u + 1) * 256), lhsT=K_[0:78, (kt + u) * 128:(kt + u + 1) * 128],
                                rhs=Q_[0:78, j * 256:(j + 1) * 256], start=True, stop=True), reads=rd, writes=[B_ps[sb]])

                    def E(diag=diag, sb=sb, si=si, pi=pi):
                        if diag:
                            prog.add("dve", lambda e: e.tensor_tensor(out=Sp[si], in0=PS(sb), in1=mneg, op=ALU.add),
                                     reads=[B_ps[sb], B_const], writes=[B_Sp[si]])
                            prog.add("act", lambda e: e.activation(out=Pt[pi], in_=Sp[si], func=AF.Exp, scale=SCALE),
                                     reads=[B_Sp[si]], writes=[B_Pt[pi]])
                        else:
                            prog.add("act", lambda e: e.activation(out=Pt[pi], in_=PS(sb), func=AF.Exp, scale=SCALE),
                                     reads=[B_ps[sb]], writes=[B_Pt[pi]])

                    def PV(V_=V_, i=i, p=p, j=j, kt=kt, ob=ob, pi=pi, last=last, buf=buf):
                        for u in range(2):
                            prog.add("pe", lambda e, u=u: e.matmul(
                                PS(ob, 0, 256), lhsT=V_[:, kt + u, :], rhs=Pt[pi][:, u * 256:(u + 1) * 256],
                                start=(kt == 0 and u == 0), stop=(last and u == 1)),
                                reads=[B_VA[buf][i], B_Pt[pi]], writes=[B_ps[ob]])

                    F = None
                    if last:
                        def F(i=i, p=p, j=j, ob=ob):
                            finish_block(ob, 256, attn_moba[:, p, :], B_attn_moba[p], i, j * 256)

                    units.append({"S": S, "E": E, "PV": PV, "F": F})
        run_units(units, filler, every)

    if stage >= 2:
        barrier(B_xt + B_xn + [B_gbc, B_junk],
                [b for bb in B_KA for b in bb] + [b for bb in B_KAaug for b in bb] + [b for bb in B_VA for b in bb] +
                B_KS + B_VS + B_Sp + B_Pt)
        swa_kv()
        moba_init()
        drain(swa_qproj(0, 0))
        moba0 = moba_proj(0, 0) if stage >= 3 else None

        def chain2(a, b, nb):
            for _ in a:
                yield
            for _ in range(nb):
                if next(b, "end") == "end":
                    return
                yield
            try:
                b.send("flush")
            except StopIteration:
                pass

        for p in range(8):
            if p + 1 < 8:
                if moba0 is not None and p >= 4:
                    filler, every = chain2(swa_qproj(p + 1, (p + 1) % 2), moba0, 3), 1
                else:
                    filler, every = swa_qproj(p + 1, (p + 1) % 2), 2
            elif moba0 is not None:
                filler, every = moba0, 1
            else:
                filler, every = None, 1
            swa_attn(p, p % 2, filler, every, depth=(2 if p == 7 else 3))
        if debug and stage == 2:
            dbg_outs["KS0"] = (KS[0], [128, 1152], BF16, [B_KS[0]])
            dbg_outs["KS1"] = (KS[1], [128, 1152], BF16, [B_KS[1]])
            dbg_outs["VS0"] = (VS[0], [128, 9 * 128], BF16, [B_VS[0]])
            dbg_outs["attn_swa"] = (attn_swa, [128, 8 * 1024], BF16, B_attn_swa)
    if stage >= 3:
        for p in range(8):
            filler = moba_proj(p + 1, (p + 1) % 2) if p + 1 < 8 else None
            moba_attn(p, p % 2, filler, 3)
        if debug and stage == 3:
            dbg_outs["attn_moba"] = (attn_moba, [128, 8 * 1024], BF16, B_attn_moba)
            dbg_outs["QA10"] = (QA[1][0], [128, 1024], BF16, [B_QA[1][0], B_QAaug[1][0]])
            dbg_outs["KA10"] = (KA[1][0], [128, 2048], BF16, [B_KA[1][0], B_KAaug[1][0]])
            dbg_outs["VA10"] = (VA[1][0], [128, 2048], BF16, [B_VA[1][0]])

    all_attn_bufs = (B_w + [b for bb in B_QA for b in bb] + [b for bb in B_QAaug for b in bb] +
                     [b for bb in B_KA for b in bb] + [b for bb in B_KAaug for b in bb] +
                     [b for bb in B_VA for b in bb] + B_KS + B_VS + B_sqb + B_lnb + B_rsb + B_tmpB + B_Sp + B_Pt +
                     B_Rt + B_tmpo + [B_gm, B_m8, B_selt, B_kmf, B_kmb] + B_stage + B_xt + B_xn + [B_junk, B_gbc])
    if stage >= 4:
        R3.reset()
        gslot = [[R3.alloc([128, 16, 128], BF16) for _ in range(2)] for _ in range(2)]
        uslot = [[R3.alloc([128, 8, 128], BF16) for _ in range(2)] for _ in range(2)]
        B_gs = [Buf("gs%d" % i) for i in range(2)]
        S_gs = [new_sem() for _ in range(2)]
        mergedT = R3.alloc([128, 16, 1024], BF16)
        B_merged = [Buf("merged%d" % i) for i in range(16)]
        sga = [R3.alloc([128, 512], F32) for _ in range(2)]
        sgb = [R3.alloc([128, 512], F32) for _ in range(2)]
        tmul = [R3.alloc([128, 512], F32) for _ in range(2)]
        B_sga = [Buf("sga%d" % i) for i in range(2)]
        B_sgb = [Buf("sgb%d" % i) for i in range(2)]
        B_tmul = [Buf("tmul%d" % i) for i in range(2)]
        NWO = 3
        woslot = [R3.alloc([128, 16, 256], BF16) for _ in range(NWO)]
        B_wo = [Buf("wo%d" % i) for i in range(NWO)]
        S_wo = [new_sem() for _ in range(NWO)]
        assert R3.off <= R3.size
        barrier(all_attn_bufs, B_gs + B_merged + B_sga + B_sgb + B_tmul + B_wo)
        first = [True]
        wus_v = w_up_swa.rearrange("(k p) n -> p k n", p=128)
        wum_v = w_up_moba.rearrange("(k p) n -> p k n", p=128)
        wo_v = w_out.rearrange("(k p) n -> p k n", p=128)
        cnt4 = [0]
        for c in range(16):
            st_ = c % 2
            extra = []

            def _ld(e, c=c, st_=st_):
                return [e.dma_start(out=gslot[st_][0], in_=w_in_v[:, :, C_GA + c * 128:C_GA + (c + 1) * 128]),
                        e.dma_start(out=gslot[st_][1], in_=w_in_v[:, :, C_GB + c * 128:C_GB + (c + 1) * 128]),
                        e.dma_start(out=uslot[st_][0], in_=wus_v[:, :, c * 128:(c + 1) * 128]),
                        e.dma_start(out=uslot[st_][1], in_=wum_v[:, :, c * 128:(c + 1) * 128])]
            prog.add("pool", _ld, writes=[B_gs[st_]] + extra, dsem=S_gs[st_], ndma=4)
            for n in range(2):
                alt = cnt4[0] % 2
                cnt4[0] += 1
                bga, bgb, bya, byb = ((0, 1, 3, 4), (2, 5, 6, 7))[alt]
                tl = n * 512
                for (bank, wt) in ((bga, gslot[st_][0]), (bgb, gslot[st_][1])):
                    for k in range(16):
                        prog.add("pe", lambda e, k=k, bank=bank, wt=wt, tl=tl: e.matmul(
                            PS(bank), lhsT=wt[:, k, :], rhs=hTo[:, k, tl:tl + 512], start=(k == 0), stop=(k == 15)),
                            reads=[B_gs[st_], B_hTo], writes=[B_ps[bank]])
                for (bank, wt, at_, Bat) in ((bya, uslot[st_][0], attn_swa, B_attn_swa), (byb, uslot[st_][1], attn_moba, B_attn_moba)):
                    for k in range(8):
                        prog.add("pe", lambda e, k=k, bank=bank, wt=wt, at_=at_, tl=tl: e.matmul(
                            PS(bank), lhsT=wt[:, k, :], rhs=at_[:, k, tl:tl + 512], start=(k == 0), stop=(k == 7)),
                            reads=[B_gs[st_]] + Bat, writes=[B_ps[bank]])
                prog.add("act", lambda e, bga=bga, alt=alt: e.activation(out=sga[alt], in_=PS(bga), func=AF.Sigmoid),
                         reads=[B_ps[bga]], writes=[B_sga[alt]])
                prog.add("act", lambda e, bgb=bgb, alt=alt: e.activation(out=sgb[alt], in_=PS(bgb), func=AF.Sigmoid),
                         reads=[B_ps[bgb]], writes=[B_sgb[alt]])
                prog.add("dve", lambda e, bya=bya, alt=alt: e.tensor_tensor(out=tmul[alt], in0=PS(bya), in1=sga[alt], op=ALU.mult),
                         reads=[B_ps[bya], B_sga[alt]], writes=[B_tmul[alt]])
                prog.add("dve", lambda e, byb=byb, alt=alt: e.tensor_tensor(out=sgb[alt], in0=PS(byb), in1=sgb[alt], op=ALU.mult),
                         reads=[B_ps[byb], B_sgb[alt]], writes=[B_sgb[alt]])
                prog.add("dve", lambda e, c=c, tl=tl, alt=alt: e.tensor_tensor(out=mergedT[:, c, tl:tl + 512], in0=tmul[alt],
                                                                               in1=sgb[alt], op=ALU.add),
                         reads=[B_tmul[alt], B_sgb[alt]], writes=[B_merged[c]])
        if debug and stage == 4:
            dbg_outs["mergedT"] = (mergedT, [128, 16 * 1024], BF16, B_merged)

    if stage >= 5:
        R1.reset()
        x1 = R1.alloc([128, 8, D], F32)
        B_x1 = [Buf("x1_%d" % t) for t in range(8)]
        S_x1 = new_sem()
        xs_own = xs[NOWN:NKV, :].rearrange("(t p) d -> p t d", p=128)

        def _ldx(e):
            return [e.dma_start(out=x1[:, t, :], in_=xs_own[:, t, :]) for t in range(8)]
        prog.add("sp", _ldx, writes=B_x1 + [B_hTp, B_hTo], dsem=S_x1, ndma=8)
        ob2 = [0]
        for cg in range(8):
            s = cg % NWO
            prog.add("pool", lambda e, cg=cg, s=s: e.dma_start(out=woslot[s], in_=wo_v[:, :, cg * 256:(cg + 1) * 256]),
                     writes=[B_wo[s]], dsem=S_wo[s])
            for t in range(8):
                bank = ob2[0] % 8
                ob2[0] += 1
                for k in range(16):
                    prog.add("pe", lambda e, k=k, t=t, s=s, bank=bank: e.matmul(
                        PS(bank, 0, 256), lhsT=mergedT[:, k, t * 128:(t + 1) * 128], rhs=woslot[s][:, k, :],
                        start=(k == 0), stop=(k == 15)), reads=[B_wo[s]] + B_merged, writes=[B_ps[bank]])
                prog.add("dve", lambda e, t=t, cg=cg, bank=bank: e.tensor_tensor(
                    out=x1[:, t, cg * 256:(cg + 1) * 256], in0=PS(bank, 0, 256), in1=x1[:, t, cg * 256:(cg + 1) * 256],
                    op=ALU.add), reads=[B_ps[bank], B_x1[t]], writes=[B_x1[t]])
        if debug and stage == 5:
            dbg_outs["x1"] = (x1, [128, 8 * D], F32, B_x1)

    if stage >= 6:
        R2.reset()
        h2T = R2.alloc([128, 16, 1024], BF16)
        B_h2T = Buf("h2T")
        phaseB_bufs = B_gs + B_merged + B_sga + B_sgb + B_tmul + B_wo
        R3.reset()
        NWE = 4
        ering = [R3.alloc([128, 8192], BF16) for _ in range(NWE)]
        B_er = [Buf("er%d" % i) for i in range(NWE)]
        S_er = [new_sem() for _ in range(NWE)]
        hidT = [R3.alloc([128, 4, 1024], BF16) for _ in range(2)]
        B_hid = [[Buf("hid%d_%d" % (i, n)) for n in range(2)] for i in range(2)]
        sg = [R3.alloc([128, 512], F32) for _ in range(2)]
        B_sg = [Buf("sg%d" % i) for i in range(2)]
        comb = R3.alloc([128, 8, 16], F32)
        B_comb = Buf("comb")
        wr_sb = R3.alloc([128, 16, 20], F32)
        B_wr = Buf("wr")
        S_wr = new_sem()
        L_all = R3.alloc([128, 8, 20], F32)
        B_L = Buf("L")
        ss2 = R3.alloc([128, 8], F32)
        ln2 = R3.alloc([128, 8], F32)
        rs2 = R3.alloc([128, 8], F32)
        B_ss2l = [Buf("ss2_%d" % t) for t in range(8)]
        B_ss2 = Buf("ss2")
        rt_small = [R3.alloc([128, 8, 16], F32) for _ in range(8)]
        B_rts = Buf("rts")
        moe_end = R3.off
        assert R3.off <= R3.size, (R3.off, R3.size)
        gbc2 = R3.alloc([128, D], F32, at=16384)
        h2f = [R3.alloc([128, D], F32, at=16384 + 8192 + i * 8192) for i in range(2)]
        B_h2f = [Buf("h2f%d" % i) for i in range(2)]
        h2Tf = [R3.alloc([128, 16, 128], F32, at=16384 + 3 * 8192 + i * 8192) for i in range(2)]
        B_h2Tf = [Buf("h2Tf%d" % i) for i in range(2)]
        junk2 = R3.alloc([128, D], BF16, at=16384 + 5 * 8192)
        B_junk2 = Buf("junk2")
        B_gbc2 = Buf("gbc2")
        S_gbc2 = new_sem()
        B_scrC = [B_er[1], B_er[2], B_er[3]]

        barrier(phaseB_bufs, B_er + [b for bb in B_hid for b in bb] + B_sg + [B_comb, B_wr, B_L, B_ss2, B_rts, B_gbc2, B_junk2] +
                B_h2f + B_h2Tf)
        prog.add("sp", lambda e: e.dma_start(out=gbc2, in_=gffn_bc[:, :]), writes=[B_gbc2], dsem=S_gbc2)
        prog.add("sp", lambda e: e.dma_start(out=wr_sb, in_=w_r.rearrange("(k p) n -> p k n", p=128)),
                 writes=[B_wr], dsem=S_wr)
        prog.add("dve", lambda e: e.memset(ss2, 0.0), writes=[B_ss2] + B_ss2l)
        pend_router = [None]
        for t in range(8):
            i2 = t % 2
            prog.add("act", lambda e, t=t: e.activation(out=junk2, in_=x1[:, t, :], func=AF.Square, accum_out=ss2[:, t:t + 1]),
                     reads=[B_x1[t], B_ss2l[t], B_gbc2], writes=[B_junk2, B_ss2l[t]])
            prog.add("act", lambda e, t=t: e.activation(out=ln2[:, t:t + 1], in_=ss2[:, t:t + 1], func=AF.Ln, scale=1.0 / D,
                                                        bias=epscol[:, 0:1]), reads=[B_ss2l[t], B_eps], writes=[B_ss2l[t]])
            prog.add("act", lambda e, t=t: e.activation(out=rs2[:, t:t + 1], in_=ln2[:, t:t + 1], func=AF.Exp, scale=-0.5),
                     reads=[B_ss2l[t]], writes=[B_ss2l[t]])
            prog.add("dve", lambda e, t=t, i2=i2: e.scalar_tensor_tensor(
                out=h2f[i2], in0=x1[:, t, :], scalar=rs2[:, t:t + 1], op0=ALU.mult, in1=gbc2, op1=ALU.mult),
                reads=[B_x1[t], B_ss2l[t], B_gbc2], writes=[B_h2f[i2]])
            for q4 in range(4):
                bank = (t * 4 + q4) % 4
                for kk in range(4):
                    k = q4 * 4 + kk
                    prog.add("pe", lambda e, k=k, kk=kk, bank=bank, i2=i2: e.transpose(
                        out=PS(bank, kk * 128, (kk + 1) * 128), in_=h2f[i2][:, k * 128:(k + 1) * 128], identity=ident_f),
                        reads=[B_h2f[i2], B_const], writes=[B_ps[bank]])
                prog.add("act", lambda e, q4=q4, bank=bank, i2=i2: e.activation(
                    out=h2Tf[i2][:, q4 * 4:(q4 + 1) * 4, :], in_=PS(bank).rearrange("p (a b) -> p a b", b=128), func=AF.Copy),
                    reads=[B_ps[bank]], writes=[B_h2Tf[i2]])
                prog.add("dve", lambda e, q4=q4, i2=i2, t=t: e.tensor_copy(
                    out=h2T[:, q4 * 4:(q4 + 1) * 4, t * 128:(t + 1) * 128], in_=h2Tf[i2][:, q4 * 4:(q4 + 1) * 4, :]),
                    reads=[B_h2Tf[i2]], writes=[B_h2T] + (B_attn_swa + B_attn_moba if (t == 0 and q4 == 0) else []))
            def router(t=t, i2=i2):
                for k in range(16):
                    prog.add("pe", lambda e, k=k: e.matmul(PS(4 + t % 2, 0, 20), lhsT=h2Tf[i2][:, k, :], rhs=wr_sb[:, k, :],
                                                           start=(k == 0), stop=(k == 15)),
                             reads=[B_h2Tf[i2], B_wr], writes=[B_ps[4 + t % 2]])
                prog.add("dve", lambda e: e.tensor_copy(out=L_all[:, t, :], in_=PS(4 + t % 2, 0, 20)),
                         reads=[B_ps[4 + t % 2]], writes=[B_L])
            if pend_router[0] is not None:
                pend_router[0]()
            pend_router[0] = router
        pend_router[0]()
        lg = L_all[:, :, 0:4]
        le = L_all[:, :, 4:20]
        mg, ohg, tmp16, sl, l1, msk, l2, ex, exm, den, gp, sumg, wexp, junk8 = (None,) * 14
        mg = rt_small[0][:, :, 0:1]
        ohg = rt_small[0][:, :, 4:8]
        sumg = rt_small[0][:, :, 8:9]
        gp = rt_small[0][:, :, 9:10]
        l1 = rt_small[0][:, :, 10:11]
        l2 = rt_small[0][:, :, 11:12]
        den = rt_small[0][:, :, 12:13]
        fac = rt_small[0][:, :, 13:14]
        tmp16 = rt_small[1]
        sl = rt_small[2][:, :, 0:4]
        msk = rt_small[2][:, :, 4:8]
        sl2 = rt_small[2][:, :, 8:12]
        ex = rt_small[3][:, :, 0:4]
        exm = rt_small[3][:, :, 4:8]
        wexp = rt_small[3][:, :, 8:12]
        eg = rt_small[4][:, :, 0:4]
        dgl = rt_small[4][:, :, 4:8]
        dsl = rt_small[4][:, :, 8:12]

        def dv(fn, r=(B_L, B_rts), w=(B_rts,)):
            prog.add("dve", fn, reads=list(r), writes=list(w))
        dv(lambda e: e.tensor_reduce(out=mg, in_=lg, axis=AX.X, op=ALU.max))
        dv(lambda e: e.tensor_tensor(out=ohg, in0=lg, in1=mg.to_broadcast([128, 8, 4]), op=ALU.is_ge))
        dv(lambda e: e.tensor_tensor(out=dgl, in0=lg, in1=mg.to_broadcast([128, 8, 4]), op=ALU.subtract))
        prog.add("act", lambda e: e.activation(out=eg, in_=dgl, func=AF.Exp), reads=[B_rts], writes=[B_rts])
        dv(lambda e: e.tensor_reduce(out=sumg, in_=eg, axis=AX.X, op=ALU.add))
        dv(lambda e: e.reciprocal(out=gp, in_=sumg))
        dv(lambda e: e.tensor_tensor(out=tmp16.rearrange("p t (g e) -> p t g e", e=4),
                                     in0=le.rearrange("p t (g e) -> p t g e", e=4),
                                     in1=ohg.unsqueeze(3).to_broadcast([128, 8, 4, 4]), op=ALU.mult))
        dv(lambda e: e.tensor_reduce(out=sl, in_=tmp16.rearrange("p t (g e) -> p t e g", e=4), axis=AX.X, op=ALU.add))
        dv(lambda e: e.tensor_reduce(out=l1, in_=sl, axis=AX.X, op=ALU.max))
        dv(lambda e: e.tensor_tensor(out=msk, in0=sl, in1=l1.to_broadcast([128, 8, 4]), op=ALU.is_ge))
        dv(lambda e: e.scalar_tensor_tensor(out=sl2, in0=msk, scalar=-1e30, op0=ALU.mult, in1=sl, op1=ALU.add))
        dv(lambda e: e.tensor_reduce(out=l2, in_=sl2, axis=AX.X, op=ALU.max))
        dv(lambda e: e.tensor_tensor(out=msk, in0=sl, in1=l2.to_broadcast([128, 8, 4]), op=ALU.is_ge))
        dv(lambda e: e.tensor_tensor(out=dsl, in0=sl, in1=l1.to_broadcast([128, 8, 4]), op=ALU.subtract))
        prog.add("act", lambda e: e.activation(out=ex, in_=dsl, func=AF.Exp), reads=[B_rts], writes=[B_rts])
        dv(lambda e: e.tensor_tensor(out=exm, in0=ex, in1=msk, op=ALU.mult))
        dv(lambda e: e.tensor_reduce(out=den, in_=exm, axis=AX.X, op=ALU.add))
        dv(lambda e: e.reciprocal(out=fac, in_=den))
        dv(lambda e: e.tensor_tensor(out=fac, in0=fac, in1=gp, op=ALU.mult))
        dv(lambda e: e.tensor_tensor(out=wexp, in0=exm, in1=fac.to_broadcast([128, 8, 4]), op=ALU.mult))
        dv(lambda e: e.tensor_tensor(out=comb.rearrange("p t (g e) -> p t g e", e=4),
                                     in0=ohg.unsqueeze(3).to_broadcast([128, 8, 4, 4]),
                                     in1=wexp.unsqueeze(2).to_broadcast([128, 8, 4, 4]), op=ALU.mult),
           w=(B_rts, B_comb))
        if debug and stage == 6:
            dbg_outs["h2T"] = (h2T, [128, 16 * 1024], BF16, [B_h2T])
            dbg_outs["L_all"] = (L_all, [128, 8 * 20], F32, [B_L])
            dbg_outs["comb"] = (comb, [128, 8 * 16], F32, [B_comb])

    if stage >= 7:
        er = [0]

        def load_e(dram_ap, shape3):
            s = er[0] % NWE
            er[0] += 1
            view = ering[s].rearrange("p (a b) -> p a b", b=shape3[2])
            extra = B_scrC_users if s in (1, 2, 3) and er[0] <= NWE else []
            prog.add("pool", lambda e: e.dma_start(out=view, in_=dram_ap), writes=[B_er[s]] + extra, dsem=S_er[s])
            return s, view
        B_scrC_users = [B_gbc2, B_junk2] + B_h2f + B_h2Tf
        gu = [0]
        yb = [0]
        for ex_i in range(16):
            hb = ex_i % 2
            s_g, Wg = load_e(w_gate_e[ex_i].rearrange("(k p) f -> p k f", p=128), [128, 16, 512])
            s_u, Wu = load_e(w_up_e[ex_i].rearrange("(k p) f -> p k f", p=128), [128, 16, 512])
            s_d, Wd = load_e(w_down_e[ex_i].rearrange("(k p) d -> p k d", p=128), [128, 4, 2048])
            for n in range(2):
                for fc in range(4):
                    alt = gu[0] % 2
                    gu[0] += 1
                    bg, bu = (0, 1) if alt == 0 else (2, 3)
                    for (bank, W_, s_) in ((bg, Wg, s_g), (bu, Wu, s_u)):
                        for k in range(16):
                            prog.add("pe", lambda e, k=k, bank=bank, W_=W_, fc=fc, n=n: e.matmul(
                                PS(bank), lhsT=W_[:, k, fc * 128:(fc + 1) * 128], rhs=h2T[:, k, n * 512:(n + 1) * 512],
                                start=(k == 0), stop=(k == 15)), reads=[B_er[s_], B_h2T], writes=[B_ps[bank]])
                    prog.add("act", lambda e, bg=bg, alt=alt: e.activation(out=sg[alt], in_=PS(bg), func=AF.Silu),
                             reads=[B_ps[bg]], writes=[B_sg[alt]])
                    prog.add("dve", lambda e, bu=bu, alt=alt, hb=hb, fc=fc, n=n: e.tensor_tensor(
                        out=hidT[hb][:, fc, n * 512:(n + 1) * 512], in0=PS(bu), in1=sg[alt], op=ALU.mult),
                        reads=[B_ps[bu], B_sg[alt]], writes=[B_hid[hb][n]])
            for t in range(8):
                for half in range(2):
                    b0 = 4 + 2 * (yb[0] % 2)
                    yb[0] += 1
                    for bb in range(2):
                        col = half * 1024 + bb * 512
                        for fc in range(4):
                            prog.add("pe", lambda e, fc=fc, t=t, col=col, bank=b0 + bb, hb=hb, Wd=Wd: e.matmul(
                                PS(bank), lhsT=hidT[hb][:, fc, t * 128:(t + 1) * 128], rhs=Wd[:, fc, col:col + 512],
                                start=(fc == 0), stop=(fc == 3)), reads=[B_er[s_d], B_hid[hb][t // 4]],
                                writes=[B_ps[b0 + bb]])
                    prog.add("dve", lambda e, t=t, half=half, b0=b0, ex_i=ex_i: e.scalar_tensor_tensor(
                        out=x1[:, t, half * 1024:(half + 1) * 1024], in0=psum[:, b0 * 512:(b0 + 2) * 512],
                        scalar=comb[:, t, ex_i:ex_i + 1], op0=ALU.mult, in1=x1[:, t, half * 1024:(half + 1) * 1024],
                        op1=ALU.add), reads=[B_ps[b0], B_ps[b0 + 1], B_comb, B_x1[t]], writes=[B_x1[t]])
        B_y = Buf("y")
        S_y = new_sem()
        y_v = y.rearrange("(t p) d -> p t d", p=128)
        for t in range(8):
            prog.add("sp", lambda e, t=t: e.dma_start(out=y_v[:, t, :], in_=x1[:, t, :]), reads=[B_x1[t]], writes=[B_y],
                     dsem=S_y)
        prog.add("sp", None, reads=[B_y])

    if debug:
        B_dbg = Buf("dbg")
        S_dbg = new_sem()
        for name, (ap, shape, dt, bufs) in dbg_outs.items():
            o = nc.dram_tensor("dbg_" + name, list(shape), dt, kind="ExternalOutput").ap()
            src = ap
            if len(ap.shape) == 3:
                src = ap.rearrange("p a b -> p (a b)")
            prog.add("sp", lambda e, o=o, src=src: e.dma_start(out=o[:, :], in_=src), reads=bufs, writes=[B_dbg], dsem=S_dbg)
        prog.add("sp", None, reads=[B_dbg])
        if stage < 7:
            pass

    block = es.enter_context(nc.Block())
    prog.emit(block, engsem)
    es.close()
    return nc, list(dbg_outs.keys())


_CONSTS = None


def _prepare_inputs(inputs, cores):
    global _CONSTS
    if _CONSTS is None:
        _CONSTS = _const_tables()
    c = _CONSTS
    f = lambda a: np.ascontiguousarray(np.asarray(a, dtype=np.float32))
    x = f(inputs["x"])
    shared = {
        "w_in": f(inputs["w_in"]), "w_up_swa": f(inputs["w_up_swa"]), "w_up_moba": f(inputs["w_up_moba"]),
        "w_out": f(inputs["w_out"]),
        "w_r": np.ascontiguousarray(np.concatenate(
            [f(inputs["w_router_group"]), f(inputs["w_router_expert"]).transpose(1, 0, 2).reshape(D, 16)], axis=1)),
        "w_gate_e": f(inputs["w_gate_e"]), "w_up_e": f(inputs["w_up_e"]), "w_down_e": f(inputs["w_down_e"]),
        "gmix_bc": np.ascontiguousarray(np.broadcast_to(f(inputs["g_mix"])[None, :], (128, D))),
        "gffn_bc": np.ascontiguousarray(np.broadcast_to(f(inputs["g_ffn"])[None, :], (128, D))),
        "gcols": np.ascontiguousarray(np.stack(
            [np.tile(f(inputs[k]), 2) for k in ("q_norm_swa", "k_norm_swa", "q_norm_moba", "k_norm_moba")], axis=1)),
        "sinks_bc": np.ascontiguousarray(np.broadcast_to(f(inputs["sinks"])[None, :], (128, 16))),
        "ident_bf": c["ident_bf"], "ident_f": c["ident_f"], "bd64": c["bd64"], "dsw": c["dsw"], "mneg": c["mneg"],
        "tabq": c["tabq"], "tabk": c["tabk"],
    }
    in_maps = []
    for cid in cores:
        b, hf = cid // 2, cid % 2
        if hf == 1:
            xs_ = x[b]
        else:
            xs_ = np.concatenate([x[b, NOWN:], x[b, :NOWN]], axis=0)
        m = dict(shared)
        m["xs"] = np.ascontiguousarray(xs_)
        m.update(_percore_tables(hf))
        in_maps.append(m)
    return in_maps


_NC_CACHE = {}


def kernel(**inputs):
    if "full" not in _NC_CACHE:
        _NC_CACHE["full"] = build_program(stage=99, debug=False)[0]
    nc = _NC_CACHE["full"]
    cores = list(range(8))
    in_maps = _prepare_inputs(inputs, cores)
    res = run_bass_kernel_spmd(nc, in_maps, core_ids=cores)
    out = np.empty((4, 2048, D), np.float32)
    for cid in cores:
        b, hf = cid // 2, cid % 2
        out[b, hf * NOWN:(hf + 1) * NOWN, :] = res.results[cid]["y"]
    return out
```

```python
import numpy as np
import ml_dtypes
from contextlib import ExitStack
import concourse.bass as bass
import concourse.mybir as mybir
from concourse.bass_utils import run_bass_kernel_spmd

F32 = mybir.dt.float32
BF16 = mybir.dt.bfloat16
U8 = mybir.dt.uint8
ALU = mybir.AluOpType
AF = mybir.ActivationFunctionType
AX = mybir.AxisListType
NPBF = ml_dtypes.bfloat16

D = 2048
NOWN = 1024
NKV = 2048
EPS = 1e-6
SCALE = 0.125
BIG = 32768.0
IN_COLS = 8448
C_QA, C_KA, C_VA, C_QB, C_KB, C_VB, C_GA, C_GB = 0, 1024, 1152, 1280, 2304, 3328, 4352, 6400
SLOPES = [2.0 ** (-(h + 1) / 2.0) for h in range(16)]
ARENA = 211968


class Buf:
    __slots__ = ("name", "lw", "lr", "excl")

    def __init__(self, name, excl=False):
        self.name = name
        self.lw = None
        self.lr = {}
        self.excl = excl


class DSem:
    def __init__(self, h):
        self.h = h
        self.count = 0


class Op:
    __slots__ = ("eng", "fn", "deps", "signal", "dsem", "val", "idx")


ENGS = ("pe", "act", "dve", "pool", "sp")


class Prog:
    def __init__(self):
        self.ops = []

    @staticmethod
    def _need(p, eng, is_dma, raw):
        if p.dsem is not None or is_dma:
            return True
        if p.eng != eng:
            return True
        if eng == "pe":
            return False
        return raw

    def add(self, eng, fn, reads=(), writes=(), dsem=None, ndma=1):
        idx = len(self.ops)
        op = Op()
        op.eng, op.fn, op.signal, op.dsem, op.idx, op.val = eng, fn, False, dsem, idx, None
        is_dma = dsem is not None
        key = ("d", idx) if is_dma else eng
        deps = set()
        for b in reads:
            w = b.lw
            if w is not None and self._need(self.ops[w], eng, is_dma, True):
                deps.add(w)
            if b.excl:
                for k2, r in b.lr.items():
                    if k2 != key:
                        deps.add(r)
        for b in writes:
            w = b.lw
            if w is not None and self._need(self.ops[w], eng, is_dma, False):
                deps.add(w)
            for r in b.lr.values():
                if self._need(self.ops[r], eng, is_dma, False):
                    deps.add(r)
        for b in reads:
            b.lr[key] = idx
        for b in writes:
            b.lw = idx
            b.lr = {}
        for d in deps:
            self.ops[d].signal = True
        op.deps = sorted(deps)
        if is_dma:
            dsem.count += 16 * ndma
            op.val = dsem.count
        self.ops.append(op)
        return op

    def emit(self, block, engsem):
        cnt = {}
        for op in self.ops:
            if op.dsem is None and op.signal:
                cnt[op.eng] = cnt.get(op.eng, 0) + 1
                op.val = cnt[op.eng]
        by = {e: [] for e in ENGS}
        for op in self.ops:
            by[op.eng].append(op)
        ops = self.ops

        def run(name):
            def body(e):
                waited = {}
                for op in by[name]:
                    for d in op.deps:
                        p = ops[d]
                        if p.dsem is not None:
                            sem, k = p.dsem.h, ("d", id(p.dsem))
                        else:
                            sem, k = engsem[p.eng], p.eng
                        if waited.get(k, 0) < p.val:
                            e.wait_ge(sem, p.val)
                            waited[k] = p.val
                    if op.fn is None:
                        continue
                    r = op.fn(e)
                    if op.dsem is not None:
                        for ins in (r if isinstance(r, (list, tuple)) else [r]):
                            ins.then_inc(op.dsem.h, 16)
                    elif op.signal:
                        r.then_inc(engsem[op.eng], 1)
            return body

        block.tensor(run("pe"))
        block.scalar(run("act"))
        block.vector(run("dve"))
        block.gpsimd(run("pool"))
        block.sync(run("sp"))


def _split3(a):
    a = a.astype(np.float64)
    hi = a.astype(NPBF)
    r = a - hi.astype(np.float64)
    mid = r.astype(NPBF)
    r = r - mid.astype(np.float64)
    lo = r.astype(NPBF)
    return hi, mid, lo


def _const_tables():
    c = {}
    c["ident_bf"] = np.eye(128, dtype=np.float32).astype(NPBF)
    c["ident_f"] = np.eye(128, dtype=np.float32)
    bd = np.zeros((128, 128), np.float32)
    bd[:64, :64] = 1.0 / 64
    bd[64:, 64:] = 1.0 / 64
    c["bd64"] = bd.astype(NPBF)
    k = np.arange(128)[:, None].astype(np.float64)
    q = np.arange(128)[None, :].astype(np.float64)
    da = q - k
    da = np.where(da >= 0, da, 1e9)
    db = q + 128 - k
    db = np.where(db < 128, db, 1e9)
    c["dsw"] = np.concatenate([db, da, db, da], axis=1).astype(np.float32)
    q2 = np.arange(256)[None, :]
    m0 = np.where(q2 >= np.arange(128)[:, None], 0.0, -1e9)
    m1 = np.where(q2 >= (np.arange(128)[:, None] + 128), 0.0, -1e9)
    c["mneg"] = np.concatenate([m0, m1], axis=1).astype(np.float32)
    tq = np.arange(NOWN).astype(np.float64)
    tk = (np.arange(NKV) - 1024).astype(np.float64)
    tabq = np.zeros((16, 6, NOWN), NPBF)
    tabk = np.zeros((16, 14, NKV), NPBF)
    ind = (np.arange(NKV)[None, :] // 256 == np.arange(8)[:, None]).astype(np.float32)
    for h in range(16):
        s = SLOPES[h]
        a = _split3(-s * tq / SCALE)
        b = _split3(s * tk / SCALE)
        for i in range(3):
            tabq[h, i] = a[i]
            tabq[h, 3 + i] = 1.0
            tabk[h, 8 + i] = 1.0
            tabk[h, 11 + i] = b[i]
        tabk[h, 0:8] = ind.astype(NPBF)
    c["tabq"] = tabq
    c["tabk"] = tabk
    return c


def _percore_tables(hf):
    pb = np.full((128, 8, 8), -1e30, np.float32)
    for t in range(8):
        own = 4 + t // 2
        for n in range(8):
            if n == own:
                pb[:, t, n] = 1e30
            elif n < own and (hf == 1 or n >= 4):
                pb[:, t, n] = 0.0
    pv = np.full((128, 1), float(hf), np.float32)
    return {"pastbias": pb.reshape(128, 64), "pvcol": pv}


def build_program(stage=99, debug=False):
    nc = bass.Bass("TRN2", target_bir_lowering=False)
    es = ExitStack()
    prog = Prog()
    dbg_outs = {}

    def din(name, shape, dt):
        return nc.dram_tensor(name, list(shape), dt, kind="ExternalInput").ap()

    xs = din("xs", [NKV, D], F32)
    w_in = din("w_in", [D, IN_COLS], F32)
    w_up_swa = din("w_up_swa", [1024, D], F32)
    w_up_moba = din("w_up_moba", [1024, D], F32)
    w_out = din("w_out", [D, D], F32)
    w_r = din("w_r", [D, 20], F32)
    w_gate_e = din("w_gate_e", [16, D, 512], F32)
    w_up_e = din("w_up_e", [16, D, 512], F32)
    w_down_e = din("w_down_e", [16, 512, D], F32)
    gmix_bc = din("gmix_bc", [128, D], F32)
    gffn_bc = din("gffn_bc", [128, D], F32)
    gcols_d = din("gcols", [128, 4], F32)
    sinks_bc = din("sinks_bc", [128, 16], F32)
    ident_bf_d = din("ident_bf", [128, 128], BF16)
    ident_f_d = din("ident_f", [128, 128], F32)
    bd64_d = din("bd64", [128, 128], BF16)
    dsw_d = din("dsw", [128, 512], F32)
    mneg_d = din("mneg", [128, 512], F32)
    tabq_d = din("tabq", [16, 6, NOWN], BF16)
    tabk_d = din("tabk", [16, 14, NKV], BF16)
    pastbias_d = din("pastbias", [128, 64], F32)
    pvcol_d = din("pvcol", [128, 1], F32)
    y = nc.dram_tensor("y", [NOWN, D], F32, kind="ExternalOutput").ap()

    arena = es.enter_context(nc.sbuf_tensor("arena", [128, ARENA], U8))
    psum = es.enter_context(nc.psum_tensor("psum", [128, 4096], F32))

    def PS(b, lo=0, hi=512):
        return psum[:, b * 512 + lo:b * 512 + hi]

    def PSB(b, lo=0, hi=1024):
        return psum[:, b * 512:(b + 1) * 512].bitcast(BF16)[:, lo:hi]

    B_ps = [Buf("ps%d" % i, excl=True) for i in range(8)]

    class Region:
        def __init__(self, base, size):
            self.base, self.size, self.off = base, size, 0

        def reset(self, off=0):
            self.off = off

        def alloc(self, shape, dt, at=None):
            nb = {F32: 4, BF16: 2}[dt]
            n = int(np.prod(shape[1:])) * nb
            n32 = (n + 31) // 32 * 32
            off = self.off if at is None else at
            assert off + n32 <= self.size, (off, n32, self.size)
            if at is None:
                self.off += n32
            v = arena[:, self.base + off:self.base + off + n].bitcast(dt)
            if len(shape) == 3:
                v = v.rearrange("p (a b) -> p a b", b=shape[2])
            return v

    CONST_SZ = 6144
    R1_SZ = 65536
    R2_SZ = 32768
    RC = Region(0, CONST_SZ)
    R1 = Region(CONST_SZ, R1_SZ)
    R2 = Region(CONST_SZ + R1_SZ, R2_SZ)
    R3 = Region(CONST_SZ + R1_SZ + R2_SZ, ARENA - CONST_SZ - R1_SZ - R2_SZ)

    sem_id = [0]

    def new_sem():
        sem_id[0] += 1
        return DSem(es.enter_context(nc.semaphore("s%d" % sem_id[0])))

    engsem = {e: es.enter_context(nc.semaphore("eng_" + e)) for e in ENGS}

    ident_bf = RC.alloc([128, 128], BF16)
    ident_f = RC.alloc([128, 128], F32)
    bd64 = RC.alloc([128, 128], BF16)
    gcols = RC.alloc([128, 4], F32)
    epscol = RC.alloc([128, 1], F32)
    expsink = RC.alloc([128, 16], F32)
    pvcol = RC.alloc([128, 1], F32)
    pastbias = RC.alloc([128, 64], F32)
    dsw = RC.alloc([128, 512], F32)
    mneg = RC.alloc([128, 512], F32)
    ss1 = RC.alloc([128, 16], F32)
    ln1 = RC.alloc([128, 16], F32)
    rs1 = RC.alloc([128, 16], F32)
    B_const = Buf("const")
    S_const = new_sem()
    const_loads = [(ident_bf, ident_bf_d), (ident_f, ident_f_d), (bd64, bd64_d), (gcols, gcols_d),
                   (expsink, sinks_bc), (pvcol, pvcol_d), (pastbias, pastbias_d), (dsw, dsw_d), (mneg, mneg_d)]

    def _ld_consts(e):
        return [e.dma_start(out=o, in_=i[:, :]) for o, i in const_loads]
    prog.add("sp", _ld_consts, writes=[B_const], dsem=S_const, ndma=len(const_loads))
    B_eps = Buf("eps")
    prog.add("dve", lambda e: e.memset(epscol, EPS), writes=[B_eps])
    B_ss1 = [Buf("ss1_%d" % t) for t in range(16)]
    prog.add("dve", lambda e: e.memset(ss1, 0.0), writes=B_ss1)
    B_expsink = Buf("expsink")
    prog.add("act", lambda e: e.activation(out=expsink, in_=expsink, func=AF.Exp), reads=[B_const], writes=[B_expsink])

    hTp = R1.alloc([128, 16, 1024], BF16)
    hTo = R1.alloc([128, 16, 1024], BF16)
    B_hTp, B_hTo = Buf("hTp"), Buf("hTo")
    attn_swa = R2.alloc([128, 8, 1024], BF16)
    attn_moba = R2.alloc([128, 8, 1024], BF16)
    B_attn_swa = [Buf("attn_swa%d" % i) for i in range(8)]
    B_attn_moba = [Buf("attn_moba%d" % i) for i in range(8)]

    NW = 6
    wslot = [R3.alloc([128, 16, 128], BF16) for _ in range(NW)]
    B_w = [Buf("w%d" % i) for i in range(NW)]
    S_w = [new_sem() for _ in range(NW)]
    QA = [[R3.alloc([128, 1024], BF16) for _ in range(2)] for _ in range(2)]
    B_QA = [[Buf("QA%d%d" % (i, j)) for j in range(2)] for i in range(2)]
    B_QAaug = [[Buf("QAaug%d%d" % (i, j)) for j in range(2)] for i in range(2)]
    S_QAaug = [[new_sem() for j in range(2)] for i in range(2)]
    off_KA = R3.off
    KA = [[R3.alloc([128, 2048], BF16) for _ in range(2)] for _ in range(2)]
    B_KA = [[Buf("KA%d%d" % (i, j)) for j in range(2)] for i in range(2)]
    B_KAaug = [[Buf("KAaug%d%d" % (i, j)) for j in range(2)] for i in range(2)]
    S_KAaug = [[new_sem() for j in range(2)] for i in range(2)]
    VA = [[R3.alloc([128, 16, 128], BF16) for _ in range(2)] for _ in range(2)]
    B_VA = [[Buf("VA%d%d" % (i, j)) for j in range(2)] for i in range(2)]
    KS = [R3.alloc([128, 1152], BF16) for _ in range(2)]
    B_KS = [Buf("KS%d" % i) for i in range(2)]
    VS = [R3.alloc([128, 9, 128], BF16) for _ in range(2)]
    B_VS = [Buf("VS%d" % i) for i in range(2)]
    sqb = [R3.alloc([128, 512], BF16) for _ in range(2)]
    B_sqb = [Buf("sqb%d" % i) for i in range(2)]
    lnb = [R3.alloc([128, 512], F32) for _ in range(2)]
    B_lnb = [Buf("lnb%d" % i) for i in range(2)]
    rsb = [R3.alloc([128, 512], F32) for _ in range(2)]
    B_rsb = [Buf("rsb%d" % i) for i in range(2)]
    tmpB = [R3.alloc([128, 512], BF16) for _ in range(2)]
    B_tmpB = [Buf("tmpB%d" % i) for i in range(2)]
    off_Sp = R3.off
    Sp = [R3.alloc([128, 512], F32) for _ in range(3)]
    B_Sp = [Buf("Sp%d" % i) for i in range(3)]
    NPT = 4
    Pt = [R3.alloc([128, 512], BF16) for _ in range(NPT)]
    B_Pt = [Buf("Pt%d" % i) for i in range(NPT)]
    Rt = [R3.alloc([128, 256], F32) for _ in range(2)]
    B_Rt = [Buf("Rt%d" % i) for i in range(2)]
    Rt2 = [R3.alloc([128, 256], F32) for _ in range(2)]
    tmpo = [R3.alloc([128, 256], BF16) for _ in range(2)]
    B_tmpo = [Buf("tmpo%d" % i) for i in range(2)]
    gm = R3.alloc([128, 8, 8], F32)
    m8 = R3.alloc([128, 8, 8], F32)
    selt = R3.alloc([128, 8, 8], F32)
    kmf = R3.alloc([128, 8], F32)
    kmb = R3.alloc([128, 8], BF16)
    B_gm, B_m8, B_selt, B_kmf, B_kmb = Buf("gm"), Buf("m8"), Buf("selt"), Buf("kmf"), Buf("kmb")
    stage_t = [R3.alloc([128, 8, 72], BF16) for _ in range(2)]
    B_stage = [Buf("stage%d" % i) for i in range(2)]
    sId = [R3.alloc([128, 128], F32) for _ in range(2)]
    B_sId = [Buf("sId%d" % i) for i in range(2)]
    attn_end = R3.off
    xt = [R3.alloc([128, D], F32, at=off_KA + i * 8192) for i in range(3)]
    B_xt = [Buf("xt%d" % i) for i in range(3)]
    S_xt = [new_sem() for _ in range(3)]
    gbc = R3.alloc([128, D], F32, at=off_KA + 3 * 8192)
    xn = [R3.alloc([128, D], BF16, at=off_KA + 4 * 8192 + i * 4096) for i in range(2)]
    B_xn = [Buf("xn%d" % i) for i in range(2)]
    junk = R3.alloc([128, D], BF16, at=off_Sp)
    B_junk = Buf("junk")
    B_gbc = Buf("gbc")
    S_gbc = new_sem()
    bar_scr = RC.alloc([128, 8], F32)

    def barrier(old, new):
        prog.add("dve", lambda e: e.memset(bar_scr, 0.0), writes=list(old) + list(new))

    prog.add("sp", lambda e: e.dma_start(out=gbc, in_=gmix_bc[:, :]), writes=[B_gbc], dsem=S_gbc)
    for t in range(16):
        sl = t % 3
        x2 = t % 2
        prog.add("sp", lambda e, t=t, sl=sl: e.dma_start(out=xt[sl], in_=xs[t * 128:(t + 1) * 128, :]),
                 writes=[B_xt[sl]], dsem=S_xt[sl])
        prog.add("act", lambda e, t=t, sl=sl: e.activation(out=junk, in_=xt[sl], func=AF.Square,
                                                           accum_out=ss1[:, t:t + 1]),
                 reads=[B_xt[sl], B_ss1[t]], writes=[B_junk, B_ss1[t]])
        prog.add("act", lambda e, t=t: e.activation(out=ln1[:, t:t + 1], in_=ss1[:, t:t + 1], func=AF.Ln,
                                                    scale=1.0 / D, bias=epscol[:, 0:1]),
                 reads=[B_ss1[t], B_eps], writes=[B_ss1[t]])
        prog.add("act", lambda e, t=t: e.activation(out=rs1[:, t:t + 1], in_=ln1[:, t:t + 1], func=AF.Exp, scale=-0.5),
                 reads=[B_ss1[t]], writes=[B_ss1[t]])
        prog.add("dve", lambda e, t=t, sl=sl, x2=x2: e.scalar_tensor_tensor(
            out=xn[x2], in0=xt[sl], scalar=rs1[:, t:t + 1], op0=ALU.mult, in1=gbc, op1=ALU.mult),
            reads=[B_xt[sl], B_ss1[t], B_gbc], writes=[B_xn[x2]])
        dst = hTp if t < 8 else hTo
        Bdst = B_hTp if t < 8 else B_hTo
        tc = (t % 8) * 128
        for k in range(16):
            bank = 6 + k // 8
            prog.add("pe", lambda e, k=k, x2=x2, bank=bank: e.transpose(
                out=PSB(bank, (k % 8) * 128, (k % 8 + 1) * 128), in_=xn[x2][:, k * 128:(k + 1) * 128],
                identity=ident_bf), reads=[B_xn[x2], B_const], writes=[B_ps[bank]])
        prog.add("act", lambda e, dst=dst, tc=tc: e.activation(
            out=dst[:, 0:8, tc:tc + 128], in_=PSB(6).rearrange("p (a b) -> p a b", b=128), func=AF.Copy),
            reads=[B_ps[6]], writes=[Bdst])
        prog.add("dve", lambda e, dst=dst, tc=tc: e.tensor_copy(
            out=dst[:, 8:16, tc:tc + 128], in_=PSB(7).rearrange("p (a b) -> p a b", b=128)),
            reads=[B_ps[7]], writes=[Bdst])

    if debug and stage == 1:
        dbg_outs["hTp"] = (hTp, [128, 16 * 1024], BF16, [B_hTp])
        dbg_outs["hTo"] = (hTo, [128, 16 * 1024], BF16, [B_hTo])

    w_in_v = w_in.rearrange("(k p) n -> p k n", p=128)
    wring = [0]

    def load_wcols(c0):
        s = wring[0] % NW
        wring[0] += 1
        prog.add("pool", lambda e: e.dma_start(out=wslot[s], in_=w_in_v[:, :, c0:c0 + 128]),
                 writes=[B_w[s]], dsem=S_w[s])
        return s

    pbank = [0]
    nrm = [0]

    def proj_fm(s, src, Bsrc, lo, n):
        bank = pbank[0] % 2
        pbank[0] += 1
        for k in range(16):
            prog.add("pe", lambda e, k=k: e.matmul(PS(bank, 0, n), lhsT=wslot[s][:, k, :], rhs=src[:, k, lo:lo + n],
                                                   start=(k == 0), stop=(k == 15)),
                     reads=[B_w[s], Bsrc], writes=[B_ps[bank]])
        return bank

    def headnorm(bank, n, gidx, dstA, BA, dstB, BB):
        i = nrm[0] % 2
        nrm[0] += 1
        prog.add("act", lambda e: e.activation(out=sqb[i][:, 0:n], in_=PS(bank, 0, n), func=AF.Square),
                 reads=[B_ps[bank]], writes=[B_sqb[i]])
        prog.add("pe", lambda e: e.matmul(PS(2, 0, n), lhsT=bd64, rhs=sqb[i][:, 0:n], start=True, stop=True),
                 reads=[B_sqb[i], B_const], writes=[B_ps[2]])
        prog.add("act", lambda e: e.activation(out=lnb[i][:, 0:n], in_=PS(2, 0, n), func=AF.Ln, bias=epscol[:, 0:1]),
                 reads=[B_ps[2], B_eps], writes=[B_lnb[i]])
        prog.add("act", lambda e: e.activation(out=rsb[i][:, 0:n], in_=lnb[i][:, 0:n], func=AF.Exp, scale=-0.5),
                 reads=[B_lnb[i]], writes=[B_rsb[i]])
        prog.add("dve", lambda e: e.scalar_tensor_tensor(
            out=dstA, in0=PS(bank, 0, n)[0:64, :], scalar=gcols[0:64, gidx:gidx + 1], op0=ALU.mult,
            in1=rsb[i][0:64, 0:n], op1=ALU.mult), reads=[B_ps[bank], B_rsb[i], B_const], writes=[BA])
        prog.add("dve", lambda e: e.scalar_tensor_tensor(
            out=tmpB[i][64:128, 0:n], in0=PS(bank, 0, n)[64:128, :], scalar=gcols[64:128, gidx:gidx + 1], op0=ALU.mult,
            in1=rsb[i][64:128, 0:n], op1=ALU.mult), reads=[B_ps[bank], B_rsb[i], B_const], writes=[B_tmpB[i]])
        prog.add("dve", lambda e: e.tensor_copy(out=dstB, in_=tmpB[i][64:128, 0:n]),
                 reads=[B_tmpB[i]], writes=[BB])

    sbank = [0]
    swa_sb = [0]
    obank = [0]
    ptc = [0]
    rtc = [0]

    def finish_block(ob, nq, dst_pair, Bdst, parity, qlo, sink_h=None):
        r = rtc[0] % 2
        rtc[0] += 1
        if sink_h is not None:
            prog.add("act", lambda e: e.activation(out=Rt2[r][64:128, 0:nq], in_=PS(ob, 0, nq)[64:128, :], func=AF.Ln,
                                                   bias=expsink[64:128, sink_h:sink_h + 1]),
                     reads=[B_ps[ob], B_expsink], writes=[B_Rt[r]])
        else:
            prog.add("act", lambda e: e.activation(out=Rt2[r][64:128, 0:nq], in_=PS(ob, 0, nq)[64:128, :], func=AF.Ln),
                     reads=[B_ps[ob]], writes=[B_Rt[r]])
        prog.add("act", lambda e: e.activation(out=Rt2[r][64:128, 0:nq], in_=Rt2[r][64:128, 0:nq], func=AF.Exp, scale=-1.0),
                 reads=[B_Rt[r]], writes=[B_Rt[r]])
        prog.add("dve", lambda e: e.tensor_copy(out=Rt[r][0:64, 0:nq], in_=Rt2[r][64:128, 0:nq]),
                 reads=[B_Rt[r]], writes=[B_Rt[r]])
        if parity == 0:
            prog.add("dve", lambda e: e.tensor_tensor(out=dst_pair[0:64, qlo:qlo + nq], in0=PS(ob, 0, nq)[0:64, :],
                                                      in1=Rt[r][0:64, 0:nq], op=ALU.mult),
                     reads=[B_ps[ob], B_Rt[r]], writes=[Bdst])
        else:
            prog.add("dve", lambda e: e.tensor_tensor(out=tmpo[r][0:64, 0:nq], in0=PS(ob, 0, nq)[0:64, :],
                                                      in1=Rt[r][0:64, 0:nq], op=ALU.mult),
                     reads=[B_ps[ob], B_Rt[r]], writes=[B_tmpo[r]])
            prog.add("dve", lambda e: e.tensor_copy(out=dst_pair[64:128, qlo:qlo + nq], in_=tmpo[r][0:64, 0:nq]),
                     reads=[B_tmpo[r]], writes=[Bdst])

    def drain(g):
        if g is not None:
            for _ in g:
                pass

    def run_units(units, filler=None, every=3, depth=2):
        n = len(units)
        if not n:
            drain(filler)
            return
        for j in range(min(depth, n)):
            units[j]["S"]()
        for j in range(min(depth - 1, n)):
            units[j]["E"]()
        for i, u in enumerate(units):
            u["PV"]()
            if i + depth < n:
                units[i + depth]["S"]()
            if filler is not None and i % every == every - 1:
                next(filler, None)
            if i + depth - 1 < n:
                units[i + depth - 1]["E"]()
            if u.get("F") is not None:
                u["F"]()
        drain(filler)

    def swa_kv():
        for g in range(2):
            prog.add("dve", lambda e, g=g: e.memset(VS[g][:, :, 64:128], 1.0), writes=[B_VS[g]])
            prog.add("dve", lambda e, g=g: e.tensor_scalar(out=VS[g][:, 0:1, 64:128], in0=VS[g][:, 0:1, 64:128],
                                                           scalar1=pvcol[:, 0:1], scalar2=None, op0=ALU.mult),
                     reads=[B_const, B_VS[g]], writes=[B_VS[g]])
        sk = load_wcols(C_KA)
        sv = load_wcols(C_VA)
        for (src, Bsrc, lo, n, dlo) in ((hTp, B_hTp, 896, 128, 0), (hTo, B_hTo, 0, 512, 128), (hTo, B_hTo, 512, 512, 640)):
            bank = proj_fm(sk, src, Bsrc, lo, n)
            headnorm(bank, n, 1, KS[0][0:64, dlo:dlo + n], B_KS[0], KS[1][0:64, dlo:dlo + n], B_KS[1])
        for grp in ((7, 8, 9, 10), (11, 12, 13, 14), (15,)):
            bank = pbank[0] % 2
            pbank[0] += 1
            for j, t in enumerate(grp):
                src, Bsrc, tc = (hTp, B_hTp, t * 128) if t < 8 else (hTo, B_hTo, (t - 8) * 128)
                for k in range(16):
                    prog.add("pe", lambda e, k=k, j=j, src=src, tc=tc, bank=bank: e.matmul(
                        PS(bank, j * 128, (j + 1) * 128), lhsT=src[:, k, tc:tc + 128], rhs=wslot[sv][:, k, :],
                        start=(k == 0), stop=(k == 15)), reads=[B_w[sv], Bsrc], writes=[B_ps[bank]])
            for g in range(2):
                for j, t in enumerate(grp):
                    if t < 8:
                        prog.add("act", lambda e, g=g, j=j, t=t, bank=bank: e.activation(
                            out=VS[g][:, t - 7, 0:64], in_=PS(bank, j * 128 + g * 64, j * 128 + g * 64 + 64),
                            func=AF.Copy, scale=pvcol[:, 0:1]), reads=[B_ps[bank], B_const], writes=[B_VS[g]])
                    else:
                        prog.add("act", lambda e, g=g, j=j, t=t, bank=bank: e.activation(
                            out=VS[g][:, t - 7, 0:64], in_=PS(bank, j * 128 + g * 64, j * 128 + g * 64 + 64),
                            func=AF.Copy), reads=[B_ps[bank]], writes=[B_VS[g]])

    def swa_qproj(p, buf):
        s = load_wcols(C_QA + p * 128)
        pend = None
        for n in range(2):
            if pend is not None:
                pend()
            bank = proj_fm(s, hTo, B_hTo, n * 512, 512)
            pend = (lambda bank=bank, n=n: headnorm(
                bank, 512, 0, QA[buf][0][0:64, n * 512:(n + 1) * 512], B_QA[buf][0],
                QA[buf][1][0:64, n * 512:(n + 1) * 512], B_QA[buf][1]))
            yield
        pend()
        yield

    def swa_attn(p, buf, filler=None, every=3, depth=3):
        banks = (3, 4, 7) if depth == 3 else (3, 4)
        units = []
        for i in range(2):
            h = 2 * p + i
            g = h // 8
            q = QA[buf][i]
            Bq = B_QA[buf][i]
            xi = h % 2
            prog.add("act", lambda e, h=h, xi=xi: e.activation(out=sId[xi], in_=ident_f, func=AF.Copy,
                                                               scale=-SLOPES[h] / SCALE),
                     reads=[B_const], writes=[B_sId[xi]])
            for c in range(4):
                i0 = 8 + 2 * c - 7
                sb = banks[swa_sb[0] % len(banks)]
                swa_sb[0] += 1
                ob = 5 + obank[0] % 2
                obank[0] += 1
                pi = ptc[0] % NPT
                ptc[0] += 1

                def S(q=q, Bq=Bq, g=g, c=c, i0=i0, sb=sb, xi=xi):
                    qa = 2 * c * 128
                    prog.add("pe", lambda e: e.matmul(PS(sb), lhsT=sId[xi], rhs=dsw, start=True, stop=False),
                             reads=[B_sId[xi], B_const], writes=[B_ps[sb]])
                    for (olo, ohi, kt, qlo, qhi, lastmm) in ((0, 128, i0 - 1, qa, qa + 128, False),
                                                             (128, 384, i0, qa, qa + 256, False),
                                                             (384, 512, i0 + 1, qa + 128, qa + 256, True)):
                        prog.add("pe", lambda e, olo=olo, ohi=ohi, kt=kt, qlo=qlo, qhi=qhi, lastmm=lastmm: e.matmul(
                            PS(sb, olo, ohi), lhsT=KS[g][0:64, kt * 128:(kt + 1) * 128], rhs=q[0:64, qlo:qhi],
                            start=False, stop=lastmm), reads=[B_KS[g], Bq], writes=[B_ps[sb]])

                def E(h=h, sb=sb, pi=pi):
                    prog.add("act", lambda e: e.activation(out=Pt[pi], in_=PS(sb), func=AF.Exp, scale=SCALE),
                             reads=[B_ps[sb]], writes=[B_Pt[pi]])

                def PV(h=h, p=p, i=i, g=g, c=c, i0=i0, ob=ob, pi=pi):
                    for (olo, kt, plo, st, sp_) in ((0, i0 - 1, 0, True, False), (0, i0, 128, False, True),
                                                    (128, i0, 256, True, False), (128, i0 + 1, 384, False, True)):
                        prog.add("pe", lambda e, olo=olo, kt=kt, plo=plo, st=st, sp_=sp_: e.matmul(
                            PS(ob, olo, olo + 128), lhsT=VS[g][:, kt, :], rhs=Pt[pi][:, plo:plo + 128],
                            start=st, stop=sp_), reads=[B_VS[g], B_Pt[pi]], writes=[B_ps[ob]])

                def F(h=h, p=p, i=i, c=c, ob=ob):
                    finish_block(ob, 256, attn_swa[:, p, :], B_attn_swa[p], i, c * 256, sink_h=h)

                units.append({"S": S, "E": E, "PV": PV, "F": F})
        run_units(units, filler, every, depth=depth)

    def moba_init():
        for b in range(2):
            for i in range(2):
                prog.add("dve", lambda e, b=b, i=i: e.memset(VA[b][i][:, :, 64:128], 1.0),
                         writes=[B_VA[b][i]])
                prog.add("dve", lambda e, b=b, i=i: e.tensor_scalar(
                    out=VA[b][i][:, 0:8, 64:128], in0=VA[b][i][:, 0:8, 64:128], scalar1=pvcol[:, 0:1], scalar2=None,
                    op0=ALU.mult), reads=[B_const, B_VA[b][i]], writes=[B_VA[b][i]])
            prog.add("dve", lambda e, b=b: e.memset(stage_t[b], 0.0), writes=[B_stage[b]])

    def moba_proj(p, buf):
        sk = load_wcols(C_KB + p * 128)
        sv = load_wcols(C_VB + p * 128)
        sq = load_wcols(C_QB + p * 128)
        for i in range(2):
            h = 2 * p + i
            prog.add("sp", lambda e, i=i, h=h: e.dma_start(out=KA[buf][i][64:78, :], in_=tabk_d[h]),
                     writes=[B_KAaug[buf][i]], dsem=S_KAaug[buf][i])
            prog.add("sp", lambda e, i=i, h=h: e.dma_start(out=QA[buf][i][72:78, :], in_=tabq_d[h]),
                     writes=[B_QAaug[buf][i]], dsem=S_QAaug[buf][i])
        pend = None
        for (src, Bsrc, lo, dlo) in ((hTp, B_hTp, 0, 0), (hTp, B_hTp, 512, 512), (hTo, B_hTo, 0, 1024), (hTo, B_hTo, 512, 1536)):
            if pend is not None:
                pend()
            bank = proj_fm(sk, src, Bsrc, lo, 512)
            pend = (lambda bank=bank, dlo=dlo: headnorm(
                bank, 512, 3, KA[buf][0][0:64, dlo:dlo + 512], B_KA[buf][0], KA[buf][1][0:64, dlo:dlo + 512], B_KA[buf][1]))
            cmd = yield
            if cmd == "flush" and pend is not None:
                pend()
                pend = None
                yield
        for t0 in range(0, 16, 4):
            if pend is not None:
                pend()
            bank = pbank[0] % 2
            pbank[0] += 1
            for j in range(4):
                t = t0 + j
                src, Bsrc, tc = (hTp, B_hTp, t * 128) if t < 8 else (hTo, B_hTo, (t - 8) * 128)
                for k in range(16):
                    prog.add("pe", lambda e, k=k, j=j, src=src, tc=tc, bank=bank: e.matmul(
                        PS(bank, j * 128, (j + 1) * 128), lhsT=src[:, k, tc:tc + 128], rhs=wslot[sv][:, k, :],
                        start=(k == 0), stop=(k == 15)), reads=[B_w[sv], Bsrc], writes=[B_ps[bank]])

            def vcopy(bank=bank, t0=t0):
                for i in range(2):
                    src_ps = PS(bank).rearrange("p (a b) -> p a b", b=128)[:, :, i * 64:(i + 1) * 64]
                    if t0 < 8:
                        prog.add("act", lambda e, i=i, src_ps=src_ps: e.activation(
                            out=VA[buf][i][:, t0:t0 + 4, 0:64], in_=src_ps, func=AF.Copy, scale=pvcol[:, 0:1]),
                            reads=[B_ps[bank], B_const], writes=[B_VA[buf][i]])
                    else:
                        prog.add("act", lambda e, i=i, src_ps=src_ps: e.activation(
                            out=VA[buf][i][:, t0:t0 + 4, 0:64], in_=src_ps, func=AF.Copy),
                            reads=[B_ps[bank]], writes=[B_VA[buf][i]])
            pend = vcopy
            cmd = yield
            if cmd == "flush" and pend is not None:
                pend()
                pend = None
                yield
        for n in range(2):
            if pend is not None:
                pend()
            bank = proj_fm(sq, hTo, B_hTo, n * 512, 512)
            pend = (lambda bank=bank, n=n: headnorm(
                bank, 512, 2, QA[buf][0][0:64, n * 512:(n + 1) * 512], B_QA[buf][0],
                QA[buf][1][0:64, n * 512:(n + 1) * 512], B_QA[buf][1]))
            cmd = yield
            if cmd == "flush" and pend is not None:
                pend()
                pend = None
                yield
        if pend is not None:
            pend()
            pend = None
        yield
        for i in range(2):
            K_, Q_ = KA[buf][i], QA[buf][i]
            prog.add("dve", lambda e, K_=K_: e.tensor_reduce(
                out=kmf[0:64, :], in_=K_[0:64, :].rearrange("p (n l) -> p n l", l=256), axis=AX.X, op=ALU.add),
                reads=[B_KA[buf][i]], writes=[B_kmf])
            prog.add("dve", lambda e: e.tensor_copy(out=kmb[0:64, :], in_=kmf[0:64, :]), reads=[B_kmf], writes=[B_kmb])
            for t in range(8):
                prog.add("pe", lambda e, t=t, Q_=Q_: e.matmul(PS(2, 256 + t * 8, 256 + t * 8 + 8),
                                                              lhsT=Q_[0:64, t * 128:(t + 1) * 128], rhs=kmb[0:64, :],
                                                              start=True, stop=True),
                         reads=[B_QA[buf][i], B_kmb], writes=[B_ps[2]])
            prog.add("dve", lambda e: e.tensor_tensor(out=gm.rearrange("p a b -> p (a b)"), in0=PS(2, 256, 320),
                                                      in1=pastbias, op=ALU.add),
                     reads=[B_ps[2], B_const], writes=[B_gm])
            for t in range(8):
                prog.add("dve", lambda e, t=t: e.max(out=m8[:, t, :], in_=gm[:, t, :]), reads=[B_gm], writes=[B_m8])
            prog.add("dve", lambda e: e.tensor_tensor(out=selt, in0=gm, in1=m8[:, :, 3:4].to_broadcast([128, 8, 8]),
                                                      op=ALU.is_ge), reads=[B_gm, B_m8], writes=[B_selt])
            prog.add("dve", lambda e: e.tensor_scalar(out=stage_t[buf][:, :, 64:72], in0=selt, scalar1=BIG,
                                                      scalar2=-BIG, op0=ALU.mult, op1=ALU.add),
                     reads=[B_selt], writes=[B_stage[buf]])
            yield
            for r in range(2):
                for t4 in range(4):
                    t = r * 4 + t4
                    prog.add("pe", lambda e, t=t, t4=t4: e.transpose(
                        out=PSB(2, t4 * 128, (t4 + 1) * 128)[0:72, :], in_=stage_t[buf][:, t, :], identity=ident_bf),
                        reads=[B_stage[buf], B_const], writes=[B_ps[2]])
                prog.add("dve", lambda e, r=r, Q_=Q_: e.tensor_copy(out=Q_[64:72, r * 512:(r + 1) * 512],
                                                                    in_=PSB(2, 0, 512)[64:72, :]),
                         reads=[B_ps[2]], writes=[B_QA[buf][i]])
            yield

    def moba_attn(p, buf, filler=None, every=3):
        units = []
        for i in range(2):
            h = 2 * p + i
            K_, Q_, V_ = KA[buf][i], QA[buf][i], VA[buf][i]
            rd = [B_KA[buf][i], B_KAaug[buf][i], B_QA[buf][i], B_QAaug[buf][i]]
            for j in range(4):
                nkt = 8 + 2 * (j + 1)
                ob = 5 + obank[0] % 2
                obank[0] += 1
                for kt in range(0, nkt, 2):
                    sb = (3, 4, 7)[sbank[0] % 3]
                    si = sbank[0] % 3
                    sbank[0] += 1
                    pi = ptc[0] % NPT
                    ptc[0] += 1
                    diag = (kt == 8 + 2 * j)
                    last = (kt == nkt - 2)

                    def S(K_=K_, Q_=Q_, rd=rd, j=j, kt=kt, sb=sb):
                        for u in range(2):
                            prog.add("pe", lambda e, u=u: e.matmul(
                                PS(sb, u * 256, (u + 1) * 256), lhsT=K_[0:78, (kt + u) * 128:(kt + u + 1) * 128],
                                rhs=Q_[0:78, j * 256:(j + 1) * 256], start=True, stop=True), reads=rd, writes=[B_ps[sb]])

                    def E(diag=diag, sb=sb, si=si, pi=pi):
                        if diag:
                            prog.add("dve", lambda e: e.tensor_tensor(out=Sp[si], in0=PS(sb), in1=mneg, op=ALU.add),
                                     reads=[B_ps[sb], B_const], writes=[B_Sp[si]])
                            prog.add("act", lambda e: e.activation(out=Pt[pi], in_=Sp[si], func=AF.Exp, scale=SCALE),
                                     reads=[B_Sp[si]], writes=[B_Pt[pi]])
                        else:
                            prog.add("act", lambda e: e.activation(out=Pt[pi], in_=PS(sb), func=AF.Exp, scale=SCALE),
                                     reads=[B_ps[sb]], writes=[B_Pt[pi]])

                    def PV(V_=V_, i=i, p=p, j=j, kt=kt, ob=ob, pi=pi, last=last, buf=buf):
                        for u in range(2):
                            prog.add("pe", lambda e, u=u: e.matmul(
                                PS(ob, 0, 256), lhsT=V_[:, kt + u, :], rhs=Pt[pi][:, u * 256:(u + 1) * 256],
                                start=(kt == 0 and u == 0), stop=(last and u == 1)),
                                reads=[B_VA[buf][i], B_Pt[pi]], writes=[B_ps[ob]])

                    F = None
                    if last:
                        def F(i=i, p=p, j=j, ob=ob):
                            finish_block(ob, 256, attn_moba[:, p, :], B_attn_moba[p], i, j * 256)

                    units.append({"S": S, "E": E, "PV": PV, "F": F})
        run_units(units, filler, every, depth=3)

    if stage >= 2:
        barrier(B_xt + B_xn + [B_gbc, B_junk],
                [b for bb in B_KA for b in bb] + [b for bb in B_KAaug for b in bb] + [b for bb in B_VA for b in bb] +
                B_KS + B_VS + B_Sp + B_Pt)
        swa_kv()
        moba_init()
        drain(swa_qproj(0, 0))
        moba0 = moba_proj(0, 0) if stage >= 3 else None

        def chain2(a, b, nb):
            for _ in a:
                yield
            for _ in range(nb):
                if next(b, "end") == "end":
                    return
                yield
            try:
                b.send("flush")
            except StopIteration:
                pass

        for p in range(8):
            if p + 1 < 8:
                if moba0 is not None and p >= 4:
                    filler, every = chain2(swa_qproj(p + 1, (p + 1) % 2), moba0, 3), 1
                else:
                    filler, every = swa_qproj(p + 1, (p + 1) % 2), 2
            elif moba0 is not None:
                filler, every = moba0, 1
            else:
                filler, every = None, 1
            swa_attn(p, p % 2, filler, every, depth=3)
        if debug and stage == 2:
            dbg_outs["KS0"] = (KS[0], [128, 1152], BF16, [B_KS[0]])
            dbg_outs["KS1"] = (KS[1], [128, 1152], BF16, [B_KS[1]])
            dbg_outs["VS0"] = (VS[0], [128, 9 * 128], BF16, [B_VS[0]])
            dbg_outs["attn_swa"] = (attn_swa, [128, 8 * 1024], BF16, B_attn_swa)
    if stage >= 3:
        for p in range(8):
            filler = moba_proj(p + 1, (p + 1) % 2) if p + 1 < 8 else None
            moba_attn(p, p % 2, filler, 3)
        if debug and stage == 3:
            dbg_outs["attn_moba"] = (attn_moba, [128, 8 * 1024], BF16, B_attn_moba)
            dbg_outs["QA10"] = (QA[1][0], [128, 1024], BF16, [B_QA[1][0], B_QAaug[1][0]])
            dbg_outs["KA10"] = (KA[1][0], [128, 2048], BF16, [B_KA[1][0], B_KAaug[1][0]])
            dbg_outs["VA10"] = (VA[1][0], [128, 2048], BF16, [B_VA[1][0]])

    all_attn_bufs = (B_w + [b for bb in B_QA for b in bb] + [b for bb in B_QAaug for b in bb] +
                     [b for bb in B_KA for b in bb] + [b for bb in B_KAaug for b in bb] +
                     [b for bb in B_VA for b in bb] + B_KS + B_VS + B_sqb + B_lnb + B_rsb + B_tmpB + B_Sp + B_Pt +
                     B_Rt + B_tmpo + [B_gm, B_m8, B_selt, B_kmf, B_kmb] + B_stage + B_xt + B_xn + [B_junk, B_gbc])
    if stage >= 4:
        R3.reset()
        gslot = [[R3.alloc([128, 16, 128], BF16) for _ in range(2)] for _ in range(2)]
        uslot = [[R3.alloc([128, 8, 128], BF16) for _ in range(2)] for _ in range(2)]
        B_gs = [Buf("gs%d" % i) for i in range(2)]
        S_gs = [new_sem() for _ in range(2)]
        mergedT = R3.alloc([128, 16, 1024], BF16)
        B_merged = [Buf("merged%d" % i) for i in range(16)]
        sga = [R3.alloc([128, 512], F32) for _ in range(2)]
        sgb = [R3.alloc([128, 512], F32) for _ in range(2)]
        tmul = [R3.alloc([128, 512], F32) for _ in range(2)]
        B_sga = [Buf("sga%d" % i) for i in range(2)]
        B_sgb = [Buf("sgb%d" % i) for i in range(2)]
        B_tmul = [Buf("tmul%d" % i) for i in range(2)]
        NWO = 3
        woslot = [R3.alloc([128, 16, 256], BF16) for _ in range(NWO)]
        B_wo = [Buf("wo%d" % i) for i in range(NWO)]
        S_wo = [new_sem() for _ in range(NWO)]
        assert R3.off <= R3.size
        barrier(all_attn_bufs, B_gs + B_merged + B_sga + B_sgb + B_tmul + B_wo)
        first = [True]
        wus_v = w_up_swa.rearrange("(k p) n -> p k n", p=128)
        wum_v = w_up_moba.rearrange("(k p) n -> p k n", p=128)
        wo_v = w_out.rearrange("(k p) n -> p k n", p=128)
        cnt4 = [0]
        for c in range(16):
            st_ = c % 2
            extra = []

            def _ld(e, c=c, st_=st_):
                return [e.dma_start(out=gslot[st_][0], in_=w_in_v[:, :, C_GA + c * 128:C_GA + (c + 1) * 128]),
                        e.dma_start(out=gslot[st_][1], in_=w_in_v[:, :, C_GB + c * 128:C_GB + (c + 1) * 128]),
                        e.dma_start(out=uslot[st_][0], in_=wus_v[:, :, c * 128:(c + 1) * 128]),
                        e.dma_start(out=uslot[st_][1], in_=wum_v[:, :, c * 128:(c + 1) * 128])]
            prog.add("pool", _ld, writes=[B_gs[st_]] + extra, dsem=S_gs[st_], ndma=4)
            for n in range(2):
                alt = cnt4[0] % 2
                cnt4[0] += 1
                bga, bgb, bya, byb = ((0, 1, 3, 4), (2, 5, 6, 7))[alt]
                tl = n * 512
                for (bank, wt) in ((bga, gslot[st_][0]), (bgb, gslot[st_][1])):
                    for k in range(16):
                        prog.add("pe", lambda e, k=k, bank=bank, wt=wt, tl=tl: e.matmul(
                            PS(bank), lhsT=wt[:, k, :], rhs=hTo[:, k, tl:tl + 512], start=(k == 0), stop=(k == 15)),
                            reads=[B_gs[st_], B_hTo], writes=[B_ps[bank]])
                for (bank, wt, at_, Bat) in ((bya, uslot[st_][0], attn_swa, B_attn_swa), (byb, uslot[st_][1], attn_moba, B_attn_moba)):
                    for k in range(8):
                        prog.add("pe", lambda e, k=k, bank=bank, wt=wt, at_=at_, tl=tl: e.matmul(
                            PS(bank), lhsT=wt[:, k, :], rhs=at_[:, k, tl:tl + 512], start=(k == 0), stop=(k == 7)),
                            reads=[B_gs[st_]] + Bat, writes=[B_ps[bank]])
                prog.add("act", lambda e, bga=bga, alt=alt: e.activation(out=sga[alt], in_=PS(bga), func=AF.Sigmoid),
                         reads=[B_ps[bga]], writes=[B_sga[alt]])
                prog.add("act", lambda e, bgb=bgb, alt=alt: e.activation(out=sgb[alt], in_=PS(bgb), func=AF.Sigmoid),
                         reads=[B_ps[bgb]], writes=[B_sgb[alt]])
                prog.add("dve", lambda e, bya=bya, alt=alt: e.tensor_tensor(out=tmul[alt], in0=PS(bya), in1=sga[alt], op=ALU.mult),
                         reads=[B_ps[bya], B_sga[alt]], writes=[B_tmul[alt]])
                prog.add("dve", lambda e, byb=byb, alt=alt: e.tensor_tensor(out=sgb[alt], in0=PS(byb), in1=sgb[alt], op=ALU.mult),
                         reads=[B_ps[byb], B_sgb[alt]], writes=[B_sgb[alt]])
                prog.add("dve", lambda e, c=c, tl=tl, alt=alt: e.tensor_tensor(out=mergedT[:, c, tl:tl + 512], in0=tmul[alt],
                                                                               in1=sgb[alt], op=ALU.add),
                         reads=[B_tmul[alt], B_sgb[alt]], writes=[B_merged[c]])
        if debug and stage == 4:
            dbg_outs["mergedT"] = (mergedT, [128, 16 * 1024], BF16, B_merged)

    if stage >= 5:
        R1.reset()
        x1 = R1.alloc([128, 8, D], F32)
        B_x1 = [Buf("x1_%d" % t) for t in range(8)]
        S_x1 = new_sem()
        xs_own = xs[NOWN:NKV, :].rearrange("(t p) d -> p t d", p=128)

        def _ldx(e):
            return [e.dma_start(out=x1[:, t, :], in_=xs_own[:, t, :]) for t in range(8)]
        prog.add("sp", _ldx, writes=B_x1 + [B_hTp, B_hTo], dsem=S_x1, ndma=8)
        ob2 = [0]
        for cg in range(8):
            s = cg % NWO
            prog.add("pool", lambda e, cg=cg, s=s: e.dma_start(out=woslot[s], in_=wo_v[:, :, cg * 256:(cg + 1) * 256]),
                     writes=[B_wo[s]], dsem=S_wo[s])
            for t in range(8):
                bank = ob2[0] % 8
                ob2[0] += 1
                for k in range(16):
                    prog.add("pe", lambda e, k=k, t=t, s=s, bank=bank: e.matmul(
                        PS(bank, 0, 256), lhsT=mergedT[:, k, t * 128:(t + 1) * 128], rhs=woslot[s][:, k, :],
                        start=(k == 0), stop=(k == 15)), reads=[B_wo[s]] + B_merged, writes=[B_ps[bank]])
                prog.add("dve", lambda e, t=t, cg=cg, bank=bank: e.tensor_tensor(
                    out=x1[:, t, cg * 256:(cg + 1) * 256], in0=PS(bank, 0, 256), in1=x1[:, t, cg * 256:(cg + 1) * 256],
                    op=ALU.add), reads=[B_ps[bank], B_x1[t]], writes=[B_x1[t]])
        if debug and stage == 5:
            dbg_outs["x1"] = (x1, [128, 8 * D], F32, B_x1)

    if stage >= 6:
        R2.reset()
        h2T = R2.alloc([128, 16, 1024], BF16)
        B_h2T = Buf("h2T")
        phaseB_bufs = B_gs + B_merged + B_sga + B_sgb + B_tmul + B_wo
        R3.reset()
        NWE = 4
        ering = [R3.alloc([128, 8192], BF16) for _ in range(NWE)]
        B_er = [Buf("er%d" % i) for i in range(NWE)]
        S_er = [new_sem() for _ in range(NWE)]
        hidT = [R3.alloc([128, 4, 1024], BF16) for _ in range(2)]
        B_hid = [[Buf("hid%d_%d" % (i, n)) for n in range(2)] for i in range(2)]
        sg = [R3.alloc([128, 512], F32) for _ in range(2)]
        B_sg = [Buf("sg%d" % i) for i in range(2)]
        comb = R3.alloc([128, 8, 16], F32)
        B_comb = Buf("comb")
        wr_sb = R3.alloc([128, 16, 20], F32)
        B_wr = Buf("wr")
        S_wr = new_sem()
        L_all = R3.alloc([128, 8, 20], F32)
        B_L = Buf("L")
        ss2 = R3.alloc([128, 8], F32)
        ln2 = R3.alloc([128, 8], F32)
        rs2 = R3.alloc([128, 8], F32)
        B_ss2l = [Buf("ss2_%d" % t) for t in range(8)]
        B_ss2 = Buf("ss2")
        rt_small = [R3.alloc([128, 8, 16], F32) for _ in range(8)]
        B_rts = Buf("rts")
        moe_end = R3.off
        assert R3.off <= R3.size, (R3.off, R3.size)
        gbc2 = R3.alloc([128, D], F32, at=16384)
        h2f = [R3.alloc([128, D], F32, at=16384 + 8192 + i * 8192) for i in range(2)]
        B_h2f = [Buf("h2f%d" % i) for i in range(2)]
        h2Tf = [R3.alloc([128, 16, 128], F32, at=16384 + 3 * 8192 + i * 8192) for i in range(2)]
        B_h2Tf = [Buf("h2Tf%d" % i) for i in range(2)]
        junk2 = R3.alloc([128, D], BF16, at=16384 + 5 * 8192)
        B_junk2 = Buf("junk2")
        B_gbc2 = Buf("gbc2")
        S_gbc2 = new_sem()
        B_scrC = [B_er[1], B_er[2], B_er[3]]

        barrier(phaseB_bufs, B_er + [b for bb in B_hid for b in bb] + B_sg + [B_comb, B_wr, B_L, B_ss2, B_rts, B_gbc2, B_junk2] +
                B_h2f + B_h2Tf)
        prog.add("sp", lambda e: e.dma_start(out=gbc2, in_=gffn_bc[:, :]), writes=[B_gbc2], dsem=S_gbc2)
        prog.add("sp", lambda e: e.dma_start(out=wr_sb, in_=w_r.rearrange("(k p) n -> p k n", p=128)),
                 writes=[B_wr], dsem=S_wr)
        prog.add("dve", lambda e: e.memset(ss2, 0.0), writes=[B_ss2] + B_ss2l)
        pend_router = [None]
        for t in range(8):
            i2 = t % 2
            prog.add("act", lambda e, t=t: e.activation(out=junk2, in_=x1[:, t, :], func=AF.Square, accum_out=ss2[:, t:t + 1]),
                     reads=[B_x1[t], B_ss2l[t], B_gbc2], writes=[B_junk2, B_ss2l[t]])
            prog.add("act", lambda e, t=t: e.activation(out=ln2[:, t:t + 1], in_=ss2[:, t:t + 1], func=AF.Ln, scale=1.0 / D,
                                                        bias=epscol[:, 0:1]), reads=[B_ss2l[t], B_eps], writes=[B_ss2l[t]])
            prog.add("act", lambda e, t=t: e.activation(out=rs2[:, t:t + 1], in_=ln2[:, t:t + 1], func=AF.Exp, scale=-0.5),
                     reads=[B_ss2l[t]], writes=[B_ss2l[t]])
            prog.add("dve", lambda e, t=t, i2=i2: e.scalar_tensor_tensor(
                out=h2f[i2], in0=x1[:, t, :], scalar=rs2[:, t:t + 1], op0=ALU.mult, in1=gbc2, op1=ALU.mult),
                reads=[B_x1[t], B_ss2l[t], B_gbc2], writes=[B_h2f[i2]])
            for q4 in range(4):
                bank = (t * 4 + q4) % 4
                for kk in range(4):
                    k = q4 * 4 + kk
                    prog.add("pe", lambda e, k=k, kk=kk, bank=bank, i2=i2: e.transpose(
                        out=PS(bank, kk * 128, (kk + 1) * 128), in_=h2f[i2][:, k * 128:(k + 1) * 128], identity=ident_f),
                        reads=[B_h2f[i2], B_const], writes=[B_ps[bank]])
                prog.add("act", lambda e, q4=q4, bank=bank, i2=i2: e.activation(
                    out=h2Tf[i2][:, q4 * 4:(q4 + 1) * 4, :], in_=PS(bank).rearrange("p (a b) -> p a b", b=128), func=AF.Copy),
                    reads=[B_ps[bank]], writes=[B_h2Tf[i2]])
                prog.add("dve", lambda e, q4=q4, i2=i2, t=t: e.tensor_copy(
                    out=h2T[:, q4 * 4:(q4 + 1) * 4, t * 128:(t + 1) * 128], in_=h2Tf[i2][:, q4 * 4:(q4 + 1) * 4, :]),
                    reads=[B_h2Tf[i2]], writes=[B_h2T] + (B_attn_swa + B_attn_moba if (t == 0 and q4 == 0) else []))
            def router(t=t, i2=i2):
                for k in range(16):
                    prog.add("pe", lambda e, k=k: e.matmul(PS(4 + t % 2, 0, 20), lhsT=h2Tf[i2][:, k, :], rhs=wr_sb[:, k, :],
                                                           start=(k == 0), stop=(k == 15)),
                             reads=[B_h2Tf[i2], B_wr], writes=[B_ps[4 + t % 2]])
                prog.add("dve", lambda e: e.tensor_copy(out=L_all[:, t, :], in_=PS(4 + t % 2, 0, 20)),
                         reads=[B_ps[4 + t % 2]], writes=[B_L])
            if pend_router[0] is not None:
                pend_router[0]()
            pend_router[0] = router
        pend_router[0]()
        lg = L_all[:, :, 0:4]
        le = L_all[:, :, 4:20]
        mg, ohg, tmp16, sl, l1, msk, l2, ex, exm, den, gp, sumg, wexp, junk8 = (None,) * 14
        mg = rt_small[0][:, :, 0:1]
        ohg = rt_small[0][:, :, 4:8]
        sumg = rt_small[0][:, :, 8:9]
        gp = rt_small[0][:, :, 9:10]
        l1 = rt_small[0][:, :, 10:11]
        l2 = rt_small[0][:, :, 11:12]
        den = rt_small[0][:, :, 12:13]
        fac = rt_small[0][:, :, 13:14]
        tmp16 = rt_small[1]
        sl = rt_small[2][:, :, 0:4]
        msk = rt_small[2][:, :, 4:8]
        sl2 = rt_small[2][:, :, 8:12]
        ex = rt_small[3][:, :, 0:4]
        exm = rt_small[3][:, :, 4:8]
        wexp = rt_small[3][:, :, 8:12]
        eg = rt_small[4][:, :, 0:4]
        dgl = rt_small[4][:, :, 4:8]
        dsl = rt_small[4][:, :, 8:12]

        def dv(fn, r=(B_L, B_rts), w=(B_rts,)):
            prog.add("dve", fn, reads=list(r), writes=list(w))
        dv(lambda e: e.tensor_reduce(out=mg, in_=lg, axis=AX.X, op=ALU.max))
        dv(lambda e: e.tensor_tensor(out=ohg, in0=lg, in1=mg.to_broadcast([128, 8, 4]), op=ALU.is_ge))
        dv(lambda e: e.tensor_tensor(out=dgl, in0=lg, in1=mg.to_broadcast([128, 8, 4]), op=ALU.subtract))
        prog.add("act", lambda e: e.activation(out=eg, in_=dgl, func=AF.Exp), reads=[B_rts], writes=[B_rts])
        dv(lambda e: e.tensor_reduce(out=sumg, in_=eg, axis=AX.X, op=ALU.add))
        dv(lambda e: e.reciprocal(out=gp, in_=sumg))
        dv(lambda e: e.tensor_tensor(out=tmp16.rearrange("p t (g e) -> p t g e", e=4),
                                     in0=le.rearrange("p t (g e) -> p t g e", e=4),
                                     in1=ohg.unsqueeze(3).to_broadcast([128, 8, 4, 4]), op=ALU.mult))
        dv(lambda e: e.tensor_reduce(out=sl, in_=tmp16.rearrange("p t (g e) -> p t e g", e=4), axis=AX.X, op=ALU.add))
        dv(lambda e: e.tensor_reduce(out=l1, in_=sl, axis=AX.X, op=ALU.max))
        dv(lambda e: e.tensor_tensor(out=msk, in0=sl, in1=l1.to_broadcast([128, 8, 4]), op=ALU.is_ge))
        dv(lambda e: e.scalar_tensor_tensor(out=sl2, in0=msk, scalar=-1e30, op0=ALU.mult, in1=sl, op1=ALU.add))
        dv(lambda e: e.tensor_reduce(out=l2, in_=sl2, axis=AX.X, op=ALU.max))
        dv(lambda e: e.tensor_tensor(out=msk, in0=sl, in1=l2.to_broadcast([128, 8, 4]), op=ALU.is_ge))
        dv(lambda e: e.tensor_tensor(out=dsl, in0=sl, in1=l1.to_broadcast([128, 8, 4]), op=ALU.subtract))
        prog.add("act", lambda e: e.activation(out=ex, in_=dsl, func=AF.Exp), reads=[B_rts], writes=[B_rts])
        dv(lambda e: e.tensor_tensor(out=exm, in0=ex, in1=msk, op=ALU.mult))
        dv(lambda e: e.tensor_reduce(out=den, in_=exm, axis=AX.X, op=ALU.add))
        dv(lambda e: e.reciprocal(out=fac, in_=den))
        dv(lambda e: e.tensor_tensor(out=fac, in0=fac, in1=gp, op=ALU.mult))
        dv(lambda e: e.tensor_tensor(out=wexp, in0=exm, in1=fac.to_broadcast([128, 8, 4]), op=ALU.mult))
        dv(lambda e: e.tensor_tensor(out=comb.rearrange("p t (g e) -> p t g e", e=4),
                                     in0=ohg.unsqueeze(3).to_broadcast([128, 8, 4, 4]),
                                     in1=wexp.unsqueeze(2).to_broadcast([128, 8, 4, 4]), op=ALU.mult),
           w=(B_rts, B_comb))
        if debug and stage == 6:
            dbg_outs["h2T"] = (h2T, [128, 16 * 1024], BF16, [B_h2T])
            dbg_outs["L_all"] = (L_all, [128, 8 * 20], F32, [B_L])
            dbg_outs["comb"] = (comb, [128, 8 * 16], F32, [B_comb])

    if stage >= 7:
        er = [0]

        def load_e(dram_ap, shape3):
            s = er[0] % NWE
            er[0] += 1
            view = ering[s].rearrange("p (a b) -> p a b", b=shape3[2])
            extra = B_scrC_users if s in (1, 2, 3) and er[0] <= NWE else []
            prog.add("pool", lambda e: e.dma_start(out=view, in_=dram_ap), writes=[B_er[s]] + extra, dsem=S_er[s])
            return s, view
        B_scrC_users = [B_gbc2, B_junk2] + B_h2f + B_h2Tf
        gu = [0]
        yb = [0]
        for ex_i in range(16):
            hb = ex_i % 2
            s_g, Wg = load_e(w_gate_e[ex_i].rearrange("(k p) f -> p k f", p=128), [128, 16, 512])
            s_u, Wu = load_e(w_up_e[ex_i].rearrange("(k p) f -> p k f", p=128), [128, 16, 512])
            s_d, Wd = load_e(w_down_e[ex_i].rearrange("(k p) d -> p k d", p=128), [128, 4, 2048])
            for n in range(2):
                for fc in range(4):
                    alt = gu[0] % 2
                    gu[0] += 1
                    bg, bu = (0, 1) if alt == 0 else (2, 3)
                    for (bank, W_, s_) in ((bg, Wg, s_g), (bu, Wu, s_u)):
                        for k in range(16):
                            prog.add("pe", lambda e, k=k, bank=bank, W_=W_, fc=fc, n=n: e.matmul(
                                PS(bank), lhsT=W_[:, k, fc * 128:(fc + 1) * 128], rhs=h2T[:, k, n * 512:(n + 1) * 512],
                                start=(k == 0), stop=(k == 15)), reads=[B_er[s_], B_h2T], writes=[B_ps[bank]])
                    prog.add("act", lambda e, bg=bg, alt=alt: e.activation(out=sg[alt], in_=PS(bg), func=AF.Silu),
                             reads=[B_ps[bg]], writes=[B_sg[alt]])
                    prog.add("dve", lambda e, bu=bu, alt=alt, hb=hb, fc=fc, n=n: e.tensor_tensor(
                        out=hidT[hb][:, fc, n * 512:(n + 1) * 512], in0=PS(bu), in1=sg[alt], op=ALU.mult),
                        reads=[B_ps[bu], B_sg[alt]], writes=[B_hid[hb][n]])
            for t in range(8):
                for half in range(2):
                    b0 = 4 + 2 * (yb[0] % 2)
                    yb[0] += 1
                    for bb in range(2):
                        col = half * 1024 + bb * 512
                        for fc in range(4):
                            prog.add("pe", lambda e, fc=fc, t=t, col=col, bank=b0 + bb, hb=hb, Wd=Wd: e.matmul(
                                PS(bank), lhsT=hidT[hb][:, fc, t * 128:(t + 1) * 128], rhs=Wd[:, fc, col:col + 512],
                                start=(fc == 0), stop=(fc == 3)), reads=[B_er[s_d], B_hid[hb][t // 4]],
                                writes=[B_ps[b0 + bb]])
                    prog.add("dve", lambda e, t=t, half=half, b0=b0, ex_i=ex_i: e.scalar_tensor_tensor(
                        out=x1[:, t, half * 1024:(half + 1) * 1024], in0=psum[:, b0 * 512:(b0 + 2) * 512],
                        scalar=comb[:, t, ex_i:ex_i + 1], op0=ALU.mult, in1=x1[:, t, half * 1024:(half + 1) * 1024],
                        op1=ALU.add), reads=[B_ps[b0], B_ps[b0 + 1], B_comb, B_x1[t]], writes=[B_x1[t]])
        B_y = Buf("y")
        S_y = new_sem()
        y_v = y.rearrange("(t p) d -> p t d", p=128)
        for t in range(8):
            prog.add("sp", lambda e, t=t: e.dma_start(out=y_v[:, t, :], in_=x1[:, t, :]), reads=[B_x1[t]], writes=[B_y],
                     dsem=S_y)
        prog.add("sp", None, reads=[B_y])

    if debug:
        B_dbg = Buf("dbg")
        S_dbg = new_sem()
        for name, (ap, shape, dt, bufs) in dbg_outs.items():
            o = nc.dram_tensor("dbg_" + name, list(shape), dt, kind="ExternalOutput").ap()
            src = ap
            if len(ap.shape) == 3:
                src = ap.rearrange("p a b -> p (a b)")
            prog.add("sp", lambda e, o=o, src=src: e.dma_start(out=o[:, :], in_=src), reads=bufs, writes=[B_dbg], dsem=S_dbg)
        prog.add("sp", None, reads=[B_dbg])
        if stage < 7:
            pass

    block = es.enter_context(nc.Block())
    prog.emit(block, engsem)
    es.close()
    return nc, list(dbg_outs.keys())


_CONSTS = None


def _prepare_inputs(inputs, cores):
    global _CONSTS
    if _CONSTS is None:
        _CONSTS = _const_tables()
    c = _CONSTS
    f = lambda a: np.ascontiguousarray(np.asarray(a, dtype=np.float32))
    x = f(inputs["x"])
    shared = {
        "w_in": f(inputs["w_in"]), "w_up_swa": f(inputs["w_up_swa"]), "w_up_moba": f(inputs["w_up_moba"]),
        "w_out": f(inputs["w_out"]),
        "w_r": np.ascontiguousarray(np.concatenate(
            [f(inputs["w_router_group"]), f(inputs["w_router_expert"]).transpose(1, 0, 2).reshape(D, 16)], axis=1)),
        "w_gate_e": f(inputs["w_gate_e"]), "w_up_e": f(inputs["w_up_e"]), "w_down_e": f(inputs["w_down_e"]),
        "gmix_bc": np.ascontiguousarray(np.broadcast_to(f(inputs["g_mix"])[None, :], (128, D))),
        "gffn_bc": np.ascontiguousarray(np.broadcast_to(f(inputs["g_ffn"])[None, :], (128, D))),
        "gcols": np.ascontiguousarray(np.stack(
            [np.tile(f(inputs[k]), 2) for k in ("q_norm_swa", "k_norm_swa", "q_norm_moba", "k_norm_moba")], axis=1)),
        "sinks_bc": np.ascontiguousarray(np.broadcast_to(f(inputs["sinks"])[None, :], (128, 16))),
        "ident_bf": c["ident_bf"], "ident_f": c["ident_f"], "bd64": c["bd64"], "dsw": c["dsw"], "mneg": c["mneg"],
        "tabq": c["tabq"], "tabk": c["tabk"],
    }
    in_maps = []
    for cid in cores:
        b, hf = cid // 2, cid % 2
        if hf == 1:
            xs_ = x[b]
        else:
            xs_ = np.concatenate([x[b, NOWN:], x[b, :NOWN]], axis=0)
        m = dict(shared)
        m["xs"] = np.ascontiguousarray(xs_)
        m.update(_percore_tables(hf))
        in_maps.append(m)
    return in_maps


_NC_CACHE = {}


def kernel(**inputs):
    if "full" not in _NC_CACHE:
        _NC_CACHE["full"] = build_program(stage=99, debug=False)[0]
    nc = _NC_CACHE["full"]
    cores = list(range(8))
    in_maps = _prepare_inputs(inputs, cores)
    res = run_bass_kernel_spmd(nc, in_maps, core_ids=cores)
    out = np.empty((4, 2048, D), np.float32)
    for cid in cores:
        b, hf = cid // 2, cid % 2
        out[b, hf * NOWN:(hf + 1) * NOWN, :] = res.results[cid]["y"]
    return out
```

```python
import numpy as np
import ml_dtypes
from contextlib import ExitStack
import concourse.bass as bass
import concourse.mybir as mybir
from concourse.bass_utils import run_bass_kernel_spmd

F32 = mybir.dt.float32
BF16 = mybir.dt.bfloat16
U8 = mybir.dt.uint8
ALU = mybir.AluOpType
AF = mybir.ActivationFunctionType
AX = mybir.AxisListType
NPBF = ml_dtypes.bfloat16

D = 2048
NOWN = 1024
NKV = 2048
EPS = 1e-6
SCALE = 0.125
BIG = 32768.0
IN_COLS = 8448
C_QA, C_KA, C_VA, C_QB, C_KB, C_VB, C_GA, C_GB = 0, 1024, 1152, 1280, 2304, 3328, 4352, 6400
SLOPES = [2.0 ** (-(h + 1) / 2.0) for h in range(16)]
ARENA = 211968


class Buf:
    __slots__ = ("name", "lw", "lr", "excl")

    def __init__(self, name, excl=False):
        self.name = name
        self.lw = None
        self.lr = {}
        self.excl = excl


class DSem:
    def __init__(self, h):
        self.h = h
        self.count = 0


class Op:
    __slots__ = ("eng", "fn", "deps", "signal", "dsem", "val", "idx")


ENGS = ("pe", "act", "dve", "pool", "sp")


class Prog:
    def __init__(self):
        self.ops = []

    @staticmethod
    def _need(p, eng, is_dma, raw):
        if p.dsem is not None or is_dma:
            return True
        if p.eng != eng:
            return True
        if eng == "pe":
            return False
        return raw

    def add(self, eng, fn, reads=(), writes=(), dsem=None, ndma=1):
        idx = len(self.ops)
        op = Op()
        op.eng, op.fn, op.signal, op.dsem, op.idx, op.val = eng, fn, False, dsem, idx, None
        is_dma = dsem is not None
        key = ("d", idx) if is_dma else eng
        deps = set()
        for b in reads:
            w = b.lw
            if w is not None and self._need(self.ops[w], eng, is_dma, True):
                deps.add(w)
            if b.excl:
                for k2, r in b.lr.items():
                    if k2 != key:
                        deps.add(r)
        for b in writes:
            w = b.lw
            if w is not None and self._need(self.ops[w], eng, is_dma, False):
                deps.add(w)
            for r in b.lr.values():
                if self._need(self.ops[r], eng, is_dma, False):
                    deps.add(r)
        for b in reads:
            b.lr[key] = idx
        for b in writes:
            b.lw = idx
            b.lr = {}
        for d in deps:
            self.ops[d].signal = True
        op.deps = sorted(deps)
        if is_dma:
            dsem.count += 16 * ndma
            op.val = dsem.count
        self.ops.append(op)
        return op

    def emit(self, block, engsem):
        cnt = {}
        for op in self.ops:
            if op.dsem is None and op.signal:
                cnt[op.eng] = cnt.get(op.eng, 0) + 1
                op.val = cnt[op.eng]
        by = {e: [] for e in ENGS}
        for op in self.ops:
            by[op.eng].append(op)
        ops = self.ops

        def run(name):
            def body(e):
                waited = {}
                for op in by[name]:
                    for d in op.deps:
                        p = ops[d]
                        if p.dsem is not None:
                            sem, k = p.dsem.h, ("d", id(p.dsem))
                        else:
                            sem, k = engsem[p.eng], p.eng
                        if waited.get(k, 0) < p.val:
                            e.wait_ge(sem, p.val)
                            waited[k] = p.val
                    if op.fn is None:
                        continue
                    r = op.fn(e)
                    if op.dsem is not None:
                        for ins in (r if isinstance(r, (list, tuple)) else [r]):
                            ins.then_inc(op.dsem.h, 16)
                    elif op.signal:
                        r.then_inc(engsem[op.eng], 1)
            return body

        block.tensor(run("pe"))
        block.scalar(run("act"))
        block.vector(run("dve"))
        block.gpsimd(run("pool"))
        block.sync(run("sp"))


def _split3(a):
    a = a.astype(np.float64)
    hi = a.astype(NPBF)
    r = a - hi.astype(np.float64)
    mid = r.astype(NPBF)
    r = r - mid.astype(np.float64)
    lo = r.astype(NPBF)
    return hi, mid, lo


def _const_tables():
    c = {}
    c["ident_bf"] = np.eye(128, dtype=np.float32).astype(NPBF)
    c["ident_f"] = np.eye(128, dtype=np.float32)
    bd = np.zeros((128, 128), np.float32)
    bd[:64, :64] = 1.0 / 64
    bd[64:, 64:] = 1.0 / 64
    c["bd64"] = bd.astype(NPBF)
    k = np.arange(128)[:, None].astype(np.float64)
    q = np.arange(128)[None, :].astype(np.float64)
    da = q - k
    da = np.where(da >= 0, da, 1e9)
    db = q + 128 - k
    db = np.where(db < 128, db, 1e9)
    c["dsw"] = np.concatenate([db, da, db, da], axis=1).astype(np.float32)
    q2 = np.arange(256)[None, :]
    m0 = np.where(q2 >= np.arange(128)[:, None], 0.0, -1e9)
    m1 = np.where(q2 >= (np.arange(128)[:, None] + 128), 0.0, -1e9)
    c["mneg"] = np.concatenate([m0, m1], axis=1).astype(np.float32)
    tq = np.arange(NOWN).astype(np.float64)
    tk = (np.arange(NKV) - 1024).astype(np.float64)
    tabq = np.zeros((16, 6, NOWN), NPBF)
    tabk = np.zeros((16, 14, NKV), NPBF)
    ind = (np.arange(NKV)[None, :] // 256 == np.arange(8)[:, None]).astype(np.float32)
    for h in range(16):
        s = SLOPES[h]
        a = _split3(-s * tq / SCALE)
        b = _split3(s * tk / SCALE)
        for i in range(3):
            tabq[h, i] = a[i]
            tabq[h, 3 + i] = 1.0
            tabk[h, 8 + i] = 1.0
            tabk[h, 11 + i] = b[i]
        tabk[h, 0:8] = ind.astype(NPBF)
    c["tabq"] = tabq
    c["tabk"] = tabk
    return c


def _percore_tables(hf):
    pb = np.full((128, 8, 8), -1e30, np.float32)
    for t in range(8):
        own = 4 + t // 2
        for n in range(8):
            if n == own:
                pb[:, t, n] = 1e30
            elif n < own and (hf == 1 or n >= 4):
                pb[:, t, n] = 0.0
    pv = np.full((128, 1), float(hf), np.float32)
    return {"pastbias": pb.reshape(128, 64), "pvcol": pv}


def build_program(stage=99, debug=False):
    nc = bass.Bass("TRN2", target_bir_lowering=False)
    es = ExitStack()
    prog = Prog()
    dbg_outs = {}

    def din(name, shape, dt):
        return nc.dram_tensor(name, list(shape), dt, kind="ExternalInput").ap()

    xs = din("xs", [NKV, D], F32)
    w_in = din("w_in", [D, IN_COLS], F32)
    w_up_swa = din("w_up_swa", [1024, D], F32)
    w_up_moba = din("w_up_moba", [1024, D], F32)
    w_out = din("w_out", [D, D], F32)
    w_r = din("w_r", [D, 20], F32)
    w_gate_e = din("w_gate_e", [16, D, 512], F32)
    w_up_e = din("w_up_e", [16, D, 512], F32)
    w_down_e = din("w_down_e", [16, 512, D], F32)
    gmix_bc = din("gmix_bc", [128, D], F32)
    gffn_bc = din("gffn_bc", [128, D], F32)
    gcols_d = din("gcols", [128, 4], F32)
    sinks_bc = din("sinks_bc", [128, 16], F32)
    ident_bf_d = din("ident_bf", [128, 128], BF16)
    ident_f_d = din("ident_f", [128, 128], F32)
    bd64_d = din("bd64", [128, 128], BF16)
    dsw_d = din("dsw", [128, 512], F32)
    mneg_d = din("mneg", [128, 512], F32)
    tabq_d = din("tabq", [16, 6, NOWN], BF16)
    tabk_d = din("tabk", [16, 14, NKV], BF16)
    pastbias_d = din("pastbias", [128, 64], F32)
    pvcol_d = din("pvcol", [128, 1], F32)
    y = nc.dram_tensor("y", [NOWN, D], F32, kind="ExternalOutput").ap()

    arena = es.enter_context(nc.sbuf_tensor("arena", [128, ARENA], U8))
    psum = es.enter_context(nc.psum_tensor("psum", [128, 4096], F32))

    def PS(b, lo=0, hi=512):
        return psum[:, b * 512 + lo:b * 512 + hi]

    def PSB(b, lo=0, hi=1024):
        return psum[:, b * 512:(b + 1) * 512].bitcast(BF16)[:, lo:hi]

    B_ps = [Buf("ps%d" % i, excl=True) for i in range(8)]

    class Region:
        def __init__(self, base, size):
            self.base, self.size, self.off = base, size, 0

        def reset(self, off=0):
            self.off = off

        def alloc(self, shape, dt, at=None):
            nb = {F32: 4, BF16: 2}[dt]
            n = int(np.prod(shape[1:])) * nb
            n32 = (n + 31) // 32 * 32
            off = self.off if at is None else at
            assert off + n32 <= self.size, (off, n32, self.size)
            if at is None:
                self.off += n32
            v = arena[:, self.base + off:self.base + off + n].bitcast(dt)
            if len(shape) == 3:
                v = v.rearrange("p (a b) -> p a b", b=shape[2])
            return v

    CONST_SZ = 6144
    R1_SZ = 65536
    R2_SZ = 32768
    RC = Region(0, CONST_SZ)
    R1 = Region(CONST_SZ, R1_SZ)
    R2 = Region(CONST_SZ + R1_SZ, R2_SZ)
    R3 = Region(CONST_SZ + R1_SZ + R2_SZ, ARENA - CONST_SZ - R1_SZ - R2_SZ)

    sem_id = [0]

    def new_sem():
        sem_id[0] += 1
        return DSem(es.enter_context(nc.semaphore("s%d" % sem_id[0])))

    engsem = {e: es.enter_context(nc.semaphore("eng_" + e)) for e in ENGS}

    ident_bf = RC.alloc([128, 128], BF16)
    ident_f = RC.alloc([128, 128], F32)
    bd64 = RC.alloc([128, 128], BF16)
    gcols = RC.alloc([128, 4], F32)
    epscol = RC.alloc([128, 1], F32)
    expsink = RC.alloc([128, 16], F32)
    pvcol = RC.alloc([128, 1], F32)
    pastbias = RC.alloc([128, 64], F32)
    dsw = RC.alloc([128, 512], F32)
    mneg = RC.alloc([128, 512], F32)
    ss1 = RC.alloc([128, 16], F32)
    ln1 = RC.alloc([128, 16], F32)
    rs1 = RC.alloc([128, 16], F32)
    B_const = Buf("const")
    S_const = new_sem()
    const_loads = [(ident_bf, ident_bf_d), (ident_f, ident_f_d), (bd64, bd64_d), (gcols, gcols_d),
                   (expsink, sinks_bc), (pvcol, pvcol_d), (pastbias, pastbias_d), (dsw, dsw_d), (mneg, mneg_d)]

    def _ld_consts(e):
        return [e.dma_start(out=o, in_=i[:, :]) for o, i in const_loads]
    prog.add("sp", _ld_consts, writes=[B_const], dsem=S_const, ndma=len(const_loads))
    B_eps = Buf("eps")
    prog.add("dve", lambda e: e.memset(epscol, EPS), writes=[B_eps])
    B_ss1 = [Buf("ss1_%d" % t) for t in range(16)]
    prog.add("dve", lambda e: e.memset(ss1, 0.0), writes=B_ss1)
    B_expsink = Buf("expsink")
    prog.add("act", lambda e: e.activation(out=expsink, in_=expsink, func=AF.Exp), reads=[B_const], writes=[B_expsink])

    hTp = R1.alloc([128, 16, 1024], BF16)
    hTo = R1.alloc([128, 16, 1024], BF16)
    B_hTp, B_hTo = Buf("hTp"), Buf("hTo")
    attn_swa = R2.alloc([128, 8, 1024], BF16)
    attn_moba = R2.alloc([128, 8, 1024], BF16)
    B_attn_swa = [Buf("attn_swa%d" % i) for i in range(8)]
    B_attn_moba = [Buf("attn_moba%d" % i) for i in range(8)]

    NW = 6
    wslot = [R3.alloc([128, 16, 128], BF16) for _ in range(NW)]
    B_w = [Buf("w%d" % i) for i in range(NW)]
    S_w = [new_sem() for _ in range(NW)]
    QA = [[R3.alloc([128, 1024], BF16) for _ in range(2)] for _ in range(2)]
    B_QA = [[Buf("QA%d%d" % (i, j)) for j in range(2)] for i in range(2)]
    B_QAaug = [[Buf("QAaug%d%d" % (i, j)) for j in range(2)] for i in range(2)]
    S_QAaug = [[new_sem() for j in range(2)] for i in range(2)]
    off_KA = R3.off
    KA = [[R3.alloc([128, 2048], BF16) for _ in range(2)] for _ in range(2)]
    B_KA = [[Buf("KA%d%d" % (i, j)) for j in range(2)] for i in range(2)]
    B_KAaug = [[Buf("KAaug%d%d" % (i, j)) for j in range(2)] for i in range(2)]
    S_KAaug = [[new_sem() for j in range(2)] for i in range(2)]
    VA = [[R3.alloc([128, 16, 128], BF16) for _ in range(2)] for _ in range(2)]
    B_VA = [[Buf("VA%d%d" % (i, j)) for j in range(2)] for i in range(2)]
    KS = [R3.alloc([128, 1152], BF16) for _ in range(2)]
    B_KS = [Buf("KS%d" % i) for i in range(2)]
    VS = [R3.alloc([128, 9, 128], BF16) for _ in range(2)]
    B_VS = [Buf("VS%d" % i) for i in range(2)]
    sqb = [R3.alloc([128, 512], BF16) for _ in range(2)]
    B_sqb = [Buf("sqb%d" % i) for i in range(2)]
    lnb = [R3.alloc([128, 512], F32) for _ in range(2)]
    B_lnb = [Buf("lnb%d" % i) for i in range(2)]
    rsb = [R3.alloc([128, 512], F32) for _ in range(2)]
    B_rsb = [Buf("rsb%d" % i) for i in range(2)]
    tmpB = [R3.alloc([128, 512], BF16) for _ in range(2)]
    B_tmpB = [Buf("tmpB%d" % i) for i in range(2)]
    off_Sp = R3.off
    Sp = [R3.alloc([128, 512], F32) for _ in range(3)]
    B_Sp = [Buf("Sp%d" % i) for i in range(3)]
    NPT = 4
    Pt = [R3.alloc([128, 512], BF16) for _ in range(NPT)]
    B_Pt = [Buf("Pt%d" % i) for i in range(NPT)]
    Rt = [R3.alloc([128, 256], F32) for _ in range(2)]
    B_Rt = [Buf("Rt%d" % i) for i in range(2)]
    Rt2 = [R3.alloc([128, 256], F32) for _ in range(2)]
    tmpo = [R3.alloc([128, 256], BF16) for _ in range(2)]
    B_tmpo = [Buf("tmpo%d" % i) for i in range(2)]
    gm = R3.alloc([128, 8, 8], F32)
    m8 = R3.alloc([128, 8, 8], F32)
    selt = R3.alloc([128, 8, 8], F32)
    kmf = R3.alloc([128, 8], F32)
    kmb = R3.alloc([128, 8], BF16)
    B_gm, B_m8, B_selt, B_kmf, B_kmb = Buf("gm"), Buf("m8"), Buf("selt"), Buf("kmf"), Buf("kmb")
    stage_t = [R3.alloc([128, 8, 72], BF16) for _ in range(2)]
    B_stage = [Buf("stage%d" % i) for i in range(2)]
    sId = [R3.alloc([128, 128], F32) for _ in range(2)]
    B_sId = [Buf("sId%d" % i) for i in range(2)]
    attn_end = R3.off
    xt = [R3.alloc([128, D], F32, at=off_KA + i * 8192) for i in range(3)]
    B_xt = [Buf("xt%d" % i) for i in range(3)]
    S_xt = [new_sem() for _ in range(3)]
    gbc = R3.alloc([128, D], F32, at=off_KA + 3 * 8192)
    xn = [R3.alloc([128, D], BF16, at=off_KA + 4 * 8192 + i * 4096) for i in range(2)]
    B_xn = [Buf("xn%d" % i) for i in range(2)]
    junk = R3.alloc([128, D], BF16, at=off_Sp)
    B_junk = Buf("junk")
    B_gbc = Buf("gbc")
    S_gbc = new_sem()
    bar_scr = RC.alloc([128, 8], F32)

    def barrier(old, new):
        prog.add("dve", lambda e: e.memset(bar_scr, 0.0), writes=list(old) + list(new))

    prog.add("sp", lambda e: e.dma_start(out=gbc, in_=gmix_bc[:, :]), writes=[B_gbc], dsem=S_gbc)
    for t in range(16):
        sl = t % 3
        x2 = t % 2
        prog.add("sp", lambda e, t=t, sl=sl: e.dma_start(out=xt[sl], in_=xs[t * 128:(t + 1) * 128, :]),
                 writes=[B_xt[sl]], dsem=S_xt[sl])
        prog.add("act", lambda e, t=t, sl=sl: e.activation(out=junk, in_=xt[sl], func=AF.Square,
                                                           accum_out=ss1[:, t:t + 1]),
                 reads=[B_xt[sl], B_ss1[t]], writes=[B_junk, B_ss1[t]])
        prog.add("act", lambda e, t=t: e.activation(out=ln1[:, t:t + 1], in_=ss1[:, t:t + 1], func=AF.Ln,
                                                    scale=1.0 / D, bias=epscol[:, 0:1]),
                 reads=[B_ss1[t], B_eps], writes=[B_ss1[t]])
        prog.add("act", lambda e, t=t: e.activation(out=rs1[:, t:t + 1], in_=ln1[:, t:t + 1], func=AF.Exp, scale=-0.5),
                 reads=[B_ss1[t]], writes=[B_ss1[t]])
        prog.add("dve", lambda e, t=t, sl=sl, x2=x2: e.scalar_tensor_tensor(
            out=xn[x2], in0=xt[sl], scalar=rs1[:, t:t + 1], op0=ALU.mult, in1=gbc, op1=ALU.mult),
            reads=[B_xt[sl], B_ss1[t], B_gbc], writes=[B_xn[x2]])
        dst = hTp if t < 8 else hTo
        Bdst = B_hTp if t < 8 else B_hTo
        tc = (t % 8) * 128
        for k in range(16):
            bank = 6 + k // 8
            prog.add("pe", lambda e, k=k, x2=x2, bank=bank: e.transpose(
                out=PSB(bank, (k % 8) * 128, (k % 8 + 1) * 128), in_=xn[x2][:, k * 128:(k + 1) * 128],
                identity=ident_bf), reads=[B_xn[x2], B_const], writes=[B_ps[bank]])
        prog.add("act", lambda e, dst=dst, tc=tc: e.activation(
            out=dst[:, 0:8, tc:tc + 128], in_=PSB(6).rearrange("p (a b) -> p a b", b=128), func=AF.Copy),
            reads=[B_ps[6]], writes=[Bdst])
        prog.add("dve", lambda e, dst=dst, tc=tc: e.tensor_copy(
            out=dst[:, 8:16, tc:tc + 128], in_=PSB(7).rearrange("p (a b) -> p a b", b=128)),
            reads=[B_ps[7]], writes=[Bdst])

    if debug and stage == 1:
        dbg_outs["hTp"] = (hTp, [128, 16 * 1024], BF16, [B_hTp])
        dbg_outs["hTo"] = (hTo, [128, 16 * 1024], BF16, [B_hTo])

    w_in_v = w_in.rearrange("(k p) n -> p k n", p=128)
    wring = [0]

    def load_wcols(c0):
        s = wring[0] % NW
        wring[0] += 1
        prog.add("pool", lambda e: e.dma_start(out=wslot[s], in_=w_in_v[:, :, c0:c0 + 128]),
                 writes=[B_w[s]], dsem=S_w[s])
        return s

    pbank = [0]
    nrm = [0]

    def proj_fm(s, src, Bsrc, lo, n):
        bank = pbank[0] % 2
        pbank[0] += 1
        for k in range(16):
            prog.add("pe", lambda e, k=k: e.matmul(PS(bank, 0, n), lhsT=wslot[s][:, k, :], rhs=src[:, k, lo:lo + n],
                                                   start=(k == 0), stop=(k == 15)),
                     reads=[B_w[s], Bsrc], writes=[B_ps[bank]])
        return bank

    def headnorm(bank, n, gidx, dstA, BA, dstB, BB):
        i = nrm[0] % 2
        nrm[0] += 1
        prog.add("act", lambda e: e.activation(out=sqb[i][:, 0:n], in_=PS(bank, 0, n), func=AF.Square),
                 reads=[B_ps[bank]], writes=[B_sqb[i]])
        prog.add("pe", lambda e: e.matmul(PS(2, 0, n), lhsT=bd64, rhs=sqb[i][:, 0:n], start=True, stop=True),
                 reads=[B_sqb[i], B_const], writes=[B_ps[2]])
        prog.add("act", lambda e: e.activation(out=lnb[i][:, 0:n], in_=PS(2, 0, n), func=AF.Ln, bias=epscol[:, 0:1]),
                 reads=[B_ps[2], B_eps], writes=[B_lnb[i]])
        prog.add("act", lambda e: e.activation(out=rsb[i][:, 0:n], in_=lnb[i][:, 0:n], func=AF.Exp, scale=-0.5),
                 reads=[B_lnb[i]], writes=[B_rsb[i]])
        prog.add("dve", lambda e: e.scalar_tensor_tensor(
            out=dstA, in0=PS(bank, 0, n)[0:64, :], scalar=gcols[0:64, gidx:gidx + 1], op0=ALU.mult,
            in1=rsb[i][0:64, 0:n], op1=ALU.mult), reads=[B_ps[bank], B_rsb[i], B_const], writes=[BA])
        prog.add("dve", lambda e: e.scalar_tensor_tensor(
            out=tmpB[i][64:128, 0:n], in0=PS(bank, 0, n)[64:128, :], scalar=gcols[64:128, gidx:gidx + 1], op0=ALU.mult,
            in1=rsb[i][64:128, 0:n], op1=ALU.mult), reads=[B_ps[bank], B_rsb[i], B_const], writes=[B_tmpB[i]])
        prog.add("dve", lambda e: e.tensor_copy(out=dstB, in_=tmpB[i][64:128, 0:n]),
                 reads=[B_tmpB[i]], writes=[BB])

    sbank = [0]
    swa_sb = [0]
    obank = [0]
    ptc = [0]
    rtc = [0]

    def finish_block(ob, nq, dst_pair, Bdst, parity, qlo, sink_h=None):
        r = rtc[0] % 2
        rtc[0] += 1
        if sink_h is not None:
            prog.add("act", lambda e: e.activation(out=Rt2[r][64:128, 0:nq], in_=PS(ob, 0, nq)[64:128, :], func=AF.Ln,
                                                   bias=expsink[64:128, sink_h:sink_h + 1]),
                     reads=[B_ps[ob], B_expsink], writes=[B_Rt[r]])
        else:
            prog.add("act", lambda e: e.activation(out=Rt2[r][64:128, 0:nq], in_=PS(ob, 0, nq)[64:128, :], func=AF.Ln),
                     reads=[B_ps[ob]], writes=[B_Rt[r]])
        prog.add("act", lambda e: e.activation(out=Rt2[r][64:128, 0:nq], in_=Rt2[r][64:128, 0:nq], func=AF.Exp, scale=-1.0),
                 reads=[B_Rt[r]], writes=[B_Rt[r]])
        prog.add("dve", lambda e: e.tensor_copy(out=Rt[r][0:64, 0:nq], in_=Rt2[r][64:128, 0:nq]),
                 reads=[B_Rt[r]], writes=[B_Rt[r]])
        if parity == 0:
            prog.add("dve", lambda e: e.tensor_tensor(out=dst_pair[0:64, qlo:qlo + nq], in0=PS(ob, 0, nq)[0:64, :],
                                                      in1=Rt[r][0:64, 0:nq], op=ALU.mult),
                     reads=[B_ps[ob], B_Rt[r]], writes=[Bdst])
        else:
            prog.add("dve", lambda e: e.tensor_tensor(out=tmpo[r][0:64, 0:nq], in0=PS(ob, 0, nq)[0:64, :],
                                                      in1=Rt[r][0:64, 0:nq], op=ALU.mult),
                     reads=[B_ps[ob], B_Rt[r]], writes=[B_tmpo[r]])
            prog.add("dve", lambda e: e.tensor_copy(out=dst_pair[64:128, qlo:qlo + nq], in_=tmpo[r][0:64, 0:nq]),
                     reads=[B_tmpo[r]], writes=[Bdst])

    def drain(g):
        if g is not None:
            for _ in g:
                pass

    def run_units(units, filler=None, every=3, depth=2):
        n = len(units)
        if not n:
            drain(filler)
            return
        for j in range(min(depth, n)):
            units[j]["S"]()
        for j in range(min(depth - 1, n)):
            units[j]["E"]()
        for i, u in enumerate(units):
            u["PV"]()
            if i + depth < n:
                units[i + depth]["S"]()
            if filler is not None and i % every == every - 1:
                next(filler, None)
            if i + depth - 1 < n:
                units[i + depth - 1]["E"]()
            if u.get("F") is not None:
                u["F"]()
        drain(filler)

    def swa_kv():
        for g in range(2):
            prog.add("dve", lambda e, g=g: e.memset(VS[g][:, :, 64:128], 1.0), writes=[B_VS[g]])
            prog.add("dve", lambda e, g=g: e.tensor_scalar(out=VS[g][:, 0:1, 64:128], in0=VS[g][:, 0:1, 64:128],
                                                           scalar1=pvcol[:, 0:1], scalar2=None, op0=ALU.mult),
                     reads=[B_const, B_VS[g]], writes=[B_VS[g]])
        sk = load_wcols(C_KA)
        sv = load_wcols(C_VA)
        for (src, Bsrc, lo, n, dlo) in ((hTp, B_hTp, 896, 128, 0), (hTo, B_hTo, 0, 512, 128), (hTo, B_hTo, 512, 512, 640)):
            bank = proj_fm(sk, src, Bsrc, lo, n)
            headnorm(bank, n, 1, KS[0][0:64, dlo:dlo + n], B_KS[0], KS[1][0:64, dlo:dlo + n], B_KS[1])
        for grp in ((7, 8, 9, 10), (11, 12, 13, 14), (15,)):
            bank = pbank[0] % 2
            pbank[0] += 1
            for j, t in enumerate(grp):
                src, Bsrc, tc = (hTp, B_hTp, t * 128) if t < 8 else (hTo, B_hTo, (t - 8) * 128)
                for k in range(16):
                    prog.add("pe", lambda e, k=k, j=j, src=src, tc=tc, bank=bank: e.matmul(
                        PS(bank, j * 128, (j + 1) * 128), lhsT=src[:, k, tc:tc + 128], rhs=wslot[sv][:, k, :],
                        start=(k == 0), stop=(k == 15)), reads=[B_w[sv], Bsrc], writes=[B_ps[bank]])
            for g in range(2):
                for j, t in enumerate(grp):
                    if t < 8:
                        prog.add("act", lambda e, g=g, j=j, t=t, bank=bank: e.activation(
                            out=VS[g][:, t - 7, 0:64], in_=PS(bank, j * 128 + g * 64, j * 128 + g * 64 + 64),
                            func=AF.Copy, scale=pvcol[:, 0:1]), reads=[B_ps[bank], B_const], writes=[B_VS[g]])
                    else:
                        prog.add("act", lambda e, g=g, j=j, t=t, bank=bank: e.activation(
                            out=VS[g][:, t - 7, 0:64], in_=PS(bank, j * 128 + g * 64, j * 128 + g * 64 + 64),
                            func=AF.Copy), reads=[B_ps[bank]], writes=[B_VS[g]])

    def swa_qproj(p, buf):
        s = load_wcols(C_QA + p * 128)
        pend = None
        for n in range(2):
            if pend is not None:
                pend()
            bank = proj_fm(s, hTo, B_hTo, n * 512, 512)
            pend = (lambda bank=bank, n=n: headnorm(
                bank, 512, 0, QA[buf][0][0:64, n * 512:(n + 1) * 512], B_QA[buf][0],
                QA[buf][1][0:64, n * 512:(n + 1) * 512], B_QA[buf][1]))
            yield
        pend()
        yield

    def swa_attn(p, buf, filler=None, every=3, depth=3):
        banks = (3, 4, 7) if depth == 3 else (3, 4)
        units = []
        for i in range(2):
            h = 2 * p + i
            g = h // 8
            q = QA[buf][i]
            Bq = B_QA[buf][i]
            xi = h % 2
            prog.add("act", lambda e, h=h, xi=xi: e.activation(out=sId[xi], in_=ident_f, func=AF.Copy,
                                                               scale=-SLOPES[h] / SCALE),
                     reads=[B_const], writes=[B_sId[xi]])
            for c in range(4):
                i0 = 8 + 2 * c - 7
                sb = banks[swa_sb[0] % len(banks)]
                swa_sb[0] += 1
                ob = 5 + obank[0] % 2
                obank[0] += 1
                pi = ptc[0] % NPT
                ptc[0] += 1

                def S(q=q, Bq=Bq, g=g, c=c, i0=i0, sb=sb, xi=xi):
                    qa = 2 * c * 128
                    prog.add("pe", lambda e: e.matmul(PS(sb), lhsT=sId[xi], rhs=dsw, start=True, stop=False),
                             reads=[B_sId[xi], B_const], writes=[B_ps[sb]])
                    for (olo, ohi, kt, qlo, qhi, lastmm) in ((0, 128, i0 - 1, qa, qa + 128, False),
                                                             (128, 384, i0, qa, qa + 256, False),
                                                             (384, 512, i0 + 1, qa + 128, qa + 256, True)):
                        prog.add("pe", lambda e, olo=olo, ohi=ohi, kt=kt, qlo=qlo, qhi=qhi, lastmm=lastmm: e.matmul(
                            PS(sb, olo, ohi), lhsT=KS[g][0:64, kt * 128:(kt + 1) * 128], rhs=q[0:64, qlo:qhi],
                            start=False, stop=lastmm), reads=[B_KS[g], Bq], writes=[B_ps[sb]])

                def E(h=h, sb=sb, pi=pi):
                    prog.add("act", lambda e: e.activation(out=Pt[pi], in_=PS(sb), func=AF.Exp, scale=SCALE),
                             reads=[B_ps[sb]], writes=[B_Pt[pi]])

                def PV(h=h, p=p, i=i, g=g, c=c, i0=i0, ob=ob, pi=pi):
                    for (olo, kt, plo, st, sp_) in ((0, i0 - 1, 0, True, False), (0, i0, 128, False, True),
                                                    (128, i0, 256, True, False), (128, i0 + 1, 384, False, True)):
                        prog.add("pe", lambda e, olo=olo, kt=kt, plo=plo, st=st, sp_=sp_: e.matmul(
                            PS(ob, olo, olo + 128), lhsT=VS[g][:, kt, :], rhs=Pt[pi][:, plo:plo + 128],
                            start=st, stop=sp_), reads=[B_VS[g], B_Pt[pi]], writes=[B_ps[ob]])

                def F(h=h, p=p, i=i, c=c, ob=ob):
                    finish_block(ob, 256, attn_swa[:, p, :], B_attn_swa[p], i, c * 256, sink_h=h)

                units.append({"S": S, "E": E, "PV": PV, "F": F})
        run_units(units, filler, every, depth=depth)

    def moba_init():
        for b in range(2):
            for i in range(2):
                prog.add("dve", lambda e, b=b, i=i: e.memset(VA[b][i][:, :, 64:128], 1.0),
                         writes=[B_VA[b][i]])
                prog.add("dve", lambda e, b=b, i=i: e.tensor_scalar(
                    out=VA[b][i][:, 0:8, 64:128], in0=VA[b][i][:, 0:8, 64:128], scalar1=pvcol[:, 0:1], scalar2=None,
                    op0=ALU.mult), reads=[B_const, B_VA[b][i]], writes=[B_VA[b][i]])
            prog.add("dve", lambda e, b=b: e.memset(stage_t[b], 0.0), writes=[B_stage[b]])

    def moba_proj(p, buf):
        sk = load_wcols(C_KB + p * 128)
        sv = load_wcols(C_VB + p * 128)
        sq = load_wcols(C_QB + p * 128)
        for i in range(2):
            h = 2 * p + i
            prog.add("sp", lambda e, i=i, h=h: e.dma_start(out=KA[buf][i][64:78, :], in_=tabk_d[h]),
                     writes=[B_KAaug[buf][i]], dsem=S_KAaug[buf][i])
            prog.add("sp", lambda e, i=i, h=h: e.dma_start(out=QA[buf][i][72:78, :], in_=tabq_d[h]),
                     writes=[B_QAaug[buf][i]], dsem=S_QAaug[buf][i])
        pend = None
        for (src, Bsrc, lo, dlo) in ((hTp, B_hTp, 0, 0), (hTp, B_hTp, 512, 512), (hTo, B_hTo, 0, 1024), (hTo, B_hTo, 512, 1536)):
            if pend is not None:
                pend()
            bank = proj_fm(sk, src, Bsrc, lo, 512)
            pend = (lambda bank=bank, dlo=dlo: headnorm(
                bank, 512, 3, KA[buf][0][0:64, dlo:dlo + 512], B_KA[buf][0], KA[buf][1][0:64, dlo:dlo + 512], B_KA[buf][1]))
            cmd = yield
            if cmd == "flush" and pend is not None:
                pend()
                pend = None
                yield
        for t0 in range(0, 16, 4):
            if pend is not None:
                pend()
            bank = pbank[0] % 2
            pbank[0] += 1
            for j in range(4):
                t = t0 + j
                src, Bsrc, tc = (hTp, B_hTp, t * 128) if t < 8 else (hTo, B_hTo, (t - 8) * 128)
                for k in range(16):
                    prog.add("pe", lambda e, k=k, j=j, src=src, tc=tc, bank=bank: e.matmul(
                        PS(bank, j * 128, (j + 1) * 128), lhsT=src[:, k, tc:tc + 128], rhs=wslot[sv][:, k, :],
                        start=(k == 0), stop=(k == 15)), reads=[B_w[sv], Bsrc], writes=[B_ps[bank]])

            def vcopy(bank=bank, t0=t0):
                for i in range(2):
                    src_ps = PS(bank).rearrange("p (a b) -> p a b", b=128)[:, :, i * 64:(i + 1) * 64]
                    if t0 < 8:
                        prog.add("act", lambda e, i=i, src_ps=src_ps: e.activation(
                            out=VA[buf][i][:, t0:t0 + 4, 0:64], in_=src_ps, func=AF.Copy, scale=pvcol[:, 0:1]),
                            reads=[B_ps[bank], B_const], writes=[B_VA[buf][i]])
                    else:
                        prog.add("act", lambda e, i=i, src_ps=src_ps: e.activation(
                            out=VA[buf][i][:, t0:t0 + 4, 0:64], in_=src_ps, func=AF.Copy),
                            reads=[B_ps[bank]], writes=[B_VA[buf][i]])
            pend = vcopy
            cmd = yield
            if cmd == "flush" and pend is not None:
                pend()
                pend = None
                yield
        for n in range(2):
            if pend is not None:
                pend()
            bank = proj_fm(sq, hTo, B_hTo, n * 512, 512)
            pend = (lambda bank=bank, n=n: headnorm(
                bank, 512, 2, QA[buf][0][0:64, n * 512:(n + 1) * 512], B_QA[buf][0],
                QA[buf][1][0:64, n * 512:(n + 1) * 512], B_QA[buf][1]))
            cmd = yield
            if cmd == "flush" and pend is not None:
                pend()
                pend = None
                yield
        if pend is not None:
            pend()
            pend = None
        yield
        for i in range(2):
            K_, Q_ = KA[buf][i], QA[buf][i]
            prog.add("dve", lambda e, K_=K_: e.tensor_reduce(
                out=kmf[0:64, :], in_=K_[0:64, :].rearrange("p (n l) -> p n l", l=256), axis=AX.X, op=ALU.add),
                reads=[B_KA[buf][i]], writes=[B_kmf])
            yield
            prog.add("dve", lambda e: e.tensor_copy(out=kmb[0:64, :], in_=kmf[0:64, :]), reads=[B_kmf], writes=[B_kmb])
            for t in range(8):
                prog.add("pe", lambda e, t=t, Q_=Q_: e.matmul(PS(2, 256 + t * 8, 256 + t * 8 + 8),
                                                              lhsT=Q_[0:64, t * 128:(t + 1) * 128], rhs=kmb[0:64, :],
                                                              start=True, stop=True),
                         reads=[B_QA[buf][i], B_kmb], writes=[B_ps[2]])
            prog.add("dve", lambda e: e.tensor_tensor(out=gm.rearrange("p a b -> p (a b)"), in0=PS(2, 256, 320),
                                                      in1=pastbias, op=ALU.add),
                     reads=[B_ps[2], B_const], writes=[B_gm])
            yield
            for t in range(8):
                prog.add("dve", lambda e, t=t: e.max(out=m8[:, t, :], in_=gm[:, t, :]), reads=[B_gm], writes=[B_m8])
            prog.add("dve", lambda e: e.tensor_tensor(out=selt, in0=gm, in1=m8[:, :, 3:4].to_broadcast([128, 8, 8]),
                                                      op=ALU.is_ge), reads=[B_gm, B_m8], writes=[B_selt])
            prog.add("dve", lambda e: e.tensor_scalar(out=stage_t[buf][:, :, 64:72], in0=selt, scalar1=BIG,
                                                      scalar2=-BIG, op0=ALU.mult, op1=ALU.add),
                     reads=[B_selt], writes=[B_stage[buf]])
            yield
            for r in range(2):
                for t4 in range(4):
                    t = r * 4 + t4
                    prog.add("pe", lambda e, t=t, t4=t4: e.transpose(
                        out=PSB(2, t4 * 128, (t4 + 1) * 128)[0:72, :], in_=stage_t[buf][:, t, :], identity=ident_bf),
                        reads=[B_stage[buf], B_const], writes=[B_ps[2]])
                prog.add("dve", lambda e, r=r, Q_=Q_: e.tensor_copy(out=Q_[64:72, r * 512:(r + 1) * 512],
                                                                    in_=PSB(2, 0, 512)[64:72, :]),
                         reads=[B_ps[2]], writes=[B_QA[buf][i]])
            yield

    def moba_attn(p, buf, filler=None, every=3):
        units = []
        for i in range(2):
            h = 2 * p + i
            K_, Q_, V_ = KA[buf][i], QA[buf][i], VA[buf][i]
            rd = [B_KA[buf][i], B_KAaug[buf][i], B_QA[buf][i], B_QAaug[buf][i]]
            for j in range(4):
                nkt = 8 + 2 * (j + 1)
                ob = 5 + obank[0] % 2
                obank[0] += 1
                for kt in range(0, nkt, 2):
                    sb = (3, 4, 7)[sbank[0] % 3]
                    si = sbank[0] % 3
                    sbank[0] += 1
                    pi = ptc[0] % NPT
                    ptc[0] += 1
                    diag = (kt == 8 + 2 * j)
                    last = (kt == nkt - 2)

                    def S(K_=K_, Q_=Q_, rd=rd, j=j, kt=kt, sb=sb):
                        for u in range(2):
                            prog.add("pe", lambda e, u=u: e.matmul(
                                PS(sb, u * 256, (u + 1) * 256), lhsT=K_[0:78, (kt + u) * 128:(kt + u + 1) * 128],
                                rhs=Q_[0:78, j * 256:(j + 1) * 256], start=True, stop=True), reads=rd, writes=[B_ps[sb]])

                    def E(diag=diag, sb=sb, si=si, pi=pi):
                        if diag:
                            prog.add("dve", lambda e: e.tensor_tensor(out=Sp[si], in0=PS(sb), in1=mneg, op=ALU.add),
                                     reads=[B_ps[sb], B_const], writes=[B_Sp[si]])
                            prog.add("act", lambda e: e.activation(out=Pt[pi], in_=Sp[si], func=AF.Exp, scale=SCALE),
                                     reads=[B_Sp[si]], writes=[B_Pt[pi]])
                        else:
                            prog.add("act", lambda e: e.activation(out=Pt[pi], in_=PS(sb), func=AF.Exp, scale=SCALE),
                                     reads=[B_ps[sb]], writes=[B_Pt[pi]])

                    def PV(V_=V_, i=i, p=p, j=j, kt=kt, ob=ob, pi=pi, last=last, buf=buf):
                        for u in range(2):
                            prog.add("pe", lambda e, u=u: e.matmul(
                                PS(ob, 0, 256), lhsT=V_[:, kt + u, :], rhs=Pt[pi][:, u * 256:(u + 1) * 256],
                                start=(kt == 0 and u == 0), stop=(last and u == 1)),
                                reads=[B_VA[buf][i], B_Pt[pi]], writes=[B_ps[ob]])

                    F = None
                    if last:
                        def F(i=i, p=p, j=j, ob=ob):
                            finish_block(ob, 256, attn_moba[:, p, :], B_attn_moba[p], i, j * 256)

                    units.append({"S": S, "E": E, "PV": PV, "F": F})
        run_units(units, filler, every, depth=3)

    if stage >= 2:
        barrier(B_xt + B_xn + [B_gbc, B_junk],
                [b for bb in B_KA for b in bb] + [b for bb in B_KAaug for b in bb] + [b for bb in B_VA for b in bb] +
                B_KS + B_VS + B_Sp + B_Pt)
        swa_kv()
        moba_init()
        drain(swa_qproj(0, 0))
        moba0 = moba_proj(0, 0) if stage >= 3 else None

        def chain2(a, b, nb):
            for _ in a:
                yield
            for _ in range(nb):
                if next(b, "end") == "end":
                    return
                yield
            try:
                b.send("flush")
            except StopIteration:
                pass

        for p in range(8):
            if p + 1 < 8:
                if moba0 is not None and p >= 4:
                    filler, every = chain2(swa_qproj(p + 1, (p + 1) % 2), moba0, 3), 1
                else:
                    filler, every = swa_qproj(p + 1, (p + 1) % 2), 2
            elif moba0 is not None:
                filler, every = moba0, 1
            else:
                filler, every = None, 1
            swa_attn(p, p % 2, filler, every, depth=3)
        if debug and stage == 2:
            dbg_outs["KS0"] = (KS[0], [128, 1152], BF16, [B_KS[0]])
            dbg_outs["KS1"] = (KS[1], [128, 1152], BF16, [B_KS[1]])
            dbg_outs["VS0"] = (VS[0], [128, 9 * 128], BF16, [B_VS[0]])
            dbg_outs["attn_swa"] = (attn_swa, [128, 8 * 1024], BF16, B_attn_swa)
    if stage >= 3:
        for p in range(8):
            filler = moba_proj(p + 1, (p + 1) % 2) if p + 1 < 8 else None
            moba_attn(p, p % 2, filler, 2)
        if debug and stage == 3:
            dbg_outs["attn_moba"] = (attn_moba, [128, 8 * 1024], BF16, B_attn_moba)
            dbg_outs["QA10"] = (QA[1][0], [128, 1024], BF16, [B_QA[1][0], B_QAaug[1][0]])
            dbg_outs["KA10"] = (KA[1][0], [128, 2048], BF16, [B_KA[1][0], B_KAaug[1][0]])
            dbg_outs["VA10"] = (VA[1][0], [128, 2048], BF16, [B_VA[1][0]])

    all_attn_bufs = (B_w + [b for bb in B_QA for b in bb] + [b for bb in B_QAaug for b in bb] +
                     [b for bb in B_KA for b in bb] + [b for bb in B_KAaug for b in bb] +
                     [b for bb in B_VA for b in bb] + B_KS + B_VS + B_sqb + B_lnb + B_rsb + B_tmpB + B_Sp + B_Pt +
                     B_Rt + B_tmpo + [B_gm, B_m8, B_selt, B_kmf, B_kmb] + B_stage + B_xt + B_xn + [B_junk, B_gbc])
    if stage >= 4:
        R3.reset()
        gslot = [[R3.alloc([128, 16, 128], BF16) for _ in range(2)] for _ in range(2)]
        uslot = [[R3.alloc([128, 8, 128], BF16) for _ in range(2)] for _ in range(2)]
        B_gs = [Buf("gs%d" % i) for i in range(2)]
        S_gs = [new_sem() for _ in range(2)]
        mergedT = R3.alloc([128, 16, 1024], BF16)
        B_merged = [Buf("merged%d" % i) for i in range(16)]
        sga = [R3.alloc([128, 512], F32) for _ in range(2)]
        sgb = [R3.alloc([128, 512], F32) for _ in range(2)]
        tmul = [R3.alloc([128, 512], F32) for _ in range(2)]
        B_sga = [Buf("sga%d" % i) for i in range(2)]
        B_sgb = [Buf("sgb%d" % i) for i in range(2)]
        B_tmul = [Buf("tmul%d" % i) for i in range(2)]
        NWO = 3
        woslot = [R3.alloc([128, 16, 256], BF16) for _ in range(NWO)]
        B_wo = [Buf("wo%d" % i) for i in range(NWO)]
        S_wo = [new_sem() for _ in range(NWO)]
        assert R3.off <= R3.size
        barrier(all_attn_bufs, B_gs + B_merged + B_sga + B_sgb + B_tmul + B_wo)
        first = [True]
        wus_v = w_up_swa.rearrange("(k p) n -> p k n", p=128)
        wum_v = w_up_moba.rearrange("(k p) n -> p k n", p=128)
        wo_v = w_out.rearrange("(k p) n -> p k n", p=128)
        cnt4 = [0]
        for c in range(16):
            st_ = c % 2
            extra = []

            def _ld(e, c=c, st_=st_):
                return [e.dma_start(out=gslot[st_][0], in_=w_in_v[:, :, C_GA + c * 128:C_GA + (c + 1) * 128]),
                        e.dma_start(out=gslot[st_][1], in_=w_in_v[:, :, C_GB + c * 128:C_GB + (c + 1) * 128]),
                        e.dma_start(out=uslot[st_][0], in_=wus_v[:, :, c * 128:(c + 1) * 128]),
                        e.dma_start(out=uslot[st_][1], in_=wum_v[:, :, c * 128:(c + 1) * 128])]
            prog.add("pool", _ld, writes=[B_gs[st_]] + extra, dsem=S_gs[st_], ndma=4)
            for n in range(2):
                alt = cnt4[0] % 2
                cnt4[0] += 1
                bga, bgb, bya, byb = ((0, 1, 3, 4), (2, 5, 6, 7))[alt]
                tl = n * 512
                for (bank, wt) in ((bga, gslot[st_][0]), (bgb, gslot[st_][1])):
                    for k in range(16):
                        prog.add("pe", lambda e, k=k, bank=bank, wt=wt, tl=tl: e.matmul(
                            PS(bank), lhsT=wt[:, k, :], rhs=hTo[:, k, tl:tl + 512], start=(k == 0), stop=(k == 15)),
                            reads=[B_gs[st_], B_hTo], writes=[B_ps[bank]])
                for (bank, wt, at_, Bat) in ((bya, uslot[st_][0], attn_swa, B_attn_swa), (byb, uslot[st_][1], attn_moba, B_attn_moba)):
                    for k in range(8):
                        prog.add("pe", lambda e, k=k, bank=bank, wt=wt, at_=at_, tl=tl: e.matmul(
                            PS(bank), lhsT=wt[:, k, :], rhs=at_[:, k, tl:tl + 512], start=(k == 0), stop=(k == 7)),
                            reads=[B_gs[st_]] + Bat, writes=[B_ps[bank]])
                prog.add("act", lambda e, bga=bga, alt=alt: e.activation(out=sga[alt], in_=PS(bga), func=AF.Sigmoid),
                         reads=[B_ps[bga]], writes=[B_sga[alt]])
                prog.add("act", lambda e, bgb=bgb, alt=alt: e.activation(out=sgb[alt], in_=PS(bgb), func=AF.Sigmoid),
                         reads=[B_ps[bgb]], writes=[B_sgb[alt]])
                prog.add("dve", lambda e, bya=bya, alt=alt: e.tensor_tensor(out=tmul[alt], in0=PS(bya), in1=sga[alt], op=ALU.mult),
                         reads=[B_ps[bya], B_sga[alt]], writes=[B_tmul[alt]])
                prog.add("dve", lambda e, byb=byb, alt=alt: e.tensor_tensor(out=sgb[alt], in0=PS(byb), in1=sgb[alt], op=ALU.mult),
                         reads=[B_ps[byb], B_sgb[alt]], writes=[B_sgb[alt]])
                prog.add("dve", lambda e, c=c, tl=tl, alt=alt: e.tensor_tensor(out=mergedT[:, c, tl:tl + 512], in0=tmul[alt],
                                                                               in1=sgb[alt], op=ALU.add),
                         reads=[B_tmul[alt], B_sgb[alt]], writes=[B_merged[c]])
        if debug and stage == 4:
            dbg_outs["mergedT"] = (mergedT, [128, 16 * 1024], BF16, B_merged)

    if stage >= 5:
        R1.reset()
        x1 = R1.alloc([128, 8, D], F32)
        B_x1 = [Buf("x1_%d" % t) for t in range(8)]
        S_x1 = new_sem()
        xs_own = xs[NOWN:NKV, :].rearrange("(t p) d -> p t d", p=128)

        def _ldx(e):
            return [e.dma_start(out=x1[:, t, :], in_=xs_own[:, t, :]) for t in range(8)]
        prog.add("sp", _ldx, writes=B_x1 + [B_hTp, B_hTo], dsem=S_x1, ndma=8)
        ob2 = [0]
        for cg in range(8):
            s = cg % NWO
            prog.add("pool", lambda e, cg=cg, s=s: e.dma_start(out=woslot[s], in_=wo_v[:, :, cg * 256:(cg + 1) * 256]),
                     writes=[B_wo[s]], dsem=S_wo[s])
            for t in range(8):
                bank = ob2[0] % 8
                ob2[0] += 1
                for k in range(16):
                    prog.add("pe", lambda e, k=k, t=t, s=s, bank=bank: e.matmul(
                        PS(bank, 0, 256), lhsT=mergedT[:, k, t * 128:(t + 1) * 128], rhs=woslot[s][:, k, :],
                        start=(k == 0), stop=(k == 15)), reads=[B_wo[s]] + B_merged, writes=[B_ps[bank]])
                prog.add("dve", lambda e, t=t, cg=cg, bank=bank: e.tensor_tensor(
                    out=x1[:, t, cg * 256:(cg + 1) * 256], in0=PS(bank, 0, 256), in1=x1[:, t, cg * 256:(cg + 1) * 256],
                    op=ALU.add), reads=[B_ps[bank], B_x1[t]], writes=[B_x1[t]])
        if debug and stage == 5:
            dbg_outs["x1"] = (x1, [128, 8 * D], F32, B_x1)

    if stage >= 6:
        R2.reset()
        h2T = R2.alloc([128, 16, 1024], BF16)
        B_h2T = Buf("h2T")
        phaseB_bufs = B_gs + B_merged + B_sga + B_sgb + B_tmul + B_wo
        R3.reset()
        NWE = 4
        ering = [R3.alloc([128, 8192], BF16) for _ in range(NWE)]
        B_er = [Buf("er%d" % i) for i in range(NWE)]
        S_er = [new_sem() for _ in range(NWE)]
        hidT = [R3.alloc([128, 4, 1024], BF16) for _ in range(2)]
        B_hid = [[Buf("hid%d_%d" % (i, n)) for n in range(2)] for i in range(2)]
        sg = [R3.alloc([128, 512], F32) for _ in range(2)]
        B_sg = [Buf("sg%d" % i) for i in range(2)]
        comb = R3.alloc([128, 8, 16], F32)
        B_comb = Buf("comb")
        wr_sb = R3.alloc([128, 16, 20], F32)
        B_wr = Buf("wr")
        S_wr = new_sem()
        L_all = R3.alloc([128, 8, 20], F32)
        B_L = Buf("L")
        ss2 = R3.alloc([128, 8], F32)
        ln2 = R3.alloc([128, 8], F32)
        rs2 = R3.alloc([128, 8], F32)
        B_ss2l = [Buf("ss2_%d" % t) for t in range(8)]
        B_ss2 = Buf("ss2")
        rt_small = [R3.alloc([128, 8, 16], F32) for _ in range(8)]
        B_rts = Buf("rts")
        moe_end = R3.off
        assert R3.off <= R3.size, (R3.off, R3.size)
        gbc2 = R3.alloc([128, D], F32, at=16384)
        h2f = [R3.alloc([128, D], F32, at=16384 + 8192 + i * 8192) for i in range(2)]
        B_h2f = [Buf("h2f%d" % i) for i in range(2)]
        h2Tf = [R3.alloc([128, 16, 128], F32, at=16384 + 3 * 8192 + i * 8192) for i in range(2)]
        B_h2Tf = [Buf("h2Tf%d" % i) for i in range(2)]
        junk2 = R3.alloc([128, D], BF16, at=16384 + 5 * 8192)
        B_junk2 = Buf("junk2")
        B_gbc2 = Buf("gbc2")
        S_gbc2 = new_sem()
        B_scrC = [B_er[1], B_er[2], B_er[3]]

        barrier(phaseB_bufs, B_er + [b for bb in B_hid for b in bb] + B_sg + [B_comb, B_wr, B_L, B_ss2, B_rts, B_gbc2, B_junk2] +
                B_h2f + B_h2Tf)
        prog.add("sp", lambda e: e.dma_start(out=gbc2, in_=gffn_bc[:, :]), writes=[B_gbc2], dsem=S_gbc2)
        prog.add("sp", lambda e: e.dma_start(out=wr_sb, in_=w_r.rearrange("(k p) n -> p k n", p=128)),
                 writes=[B_wr], dsem=S_wr)
        prog.add("dve", lambda e: e.memset(ss2, 0.0), writes=[B_ss2] + B_ss2l)
        pend_router = [None]
        for t in range(8):
            i2 = t % 2
            prog.add("act", lambda e, t=t: e.activation(out=junk2, in_=x1[:, t, :], func=AF.Square, accum_out=ss2[:, t:t + 1]),
                     reads=[B_x1[t], B_ss2l[t], B_gbc2], writes=[B_junk2, B_ss2l[t]])
            prog.add("act", lambda e, t=t: e.activation(out=ln2[:, t:t + 1], in_=ss2[:, t:t + 1], func=AF.Ln, scale=1.0 / D,
                                                        bias=epscol[:, 0:1]), reads=[B_ss2l[t], B_eps], writes=[B_ss2l[t]])
            prog.add("act", lambda e, t=t: e.activation(out=rs2[:, t:t + 1], in_=ln2[:, t:t + 1], func=AF.Exp, scale=-0.5),
                     reads=[B_ss2l[t]], writes=[B_ss2l[t]])
            prog.add("dve", lambda e, t=t, i2=i2: e.scalar_tensor_tensor(
                out=h2f[i2], in0=x1[:, t, :], scalar=rs2[:, t:t + 1], op0=ALU.mult, in1=gbc2, op1=ALU.mult),
                reads=[B_x1[t], B_ss2l[t], B_gbc2], writes=[B_h2f[i2]])
            for q4 in range(4):
                bank = (t * 4 + q4) % 4
                for kk in range(4):
                    k = q4 * 4 + kk
                    prog.add("pe", lambda e, k=k, kk=kk, bank=bank, i2=i2: e.transpose(
                        out=PS(bank, kk * 128, (kk + 1) * 128), in_=h2f[i2][:, k * 128:(k + 1) * 128], identity=ident_f),
                        reads=[B_h2f[i2], B_const], writes=[B_ps[bank]])
                prog.add("act", lambda e, q4=q4, bank=bank, i2=i2: e.activation(
                    out=h2Tf[i2][:, q4 * 4:(q4 + 1) * 4, :], in_=PS(bank).rearrange("p (a b) -> p a b", b=128), func=AF.Copy),
                    reads=[B_ps[bank]], writes=[B_h2Tf[i2]])
                prog.add("dve", lambda e, q4=q4, i2=i2, t=t: e.tensor_copy(
                    out=h2T[:, q4 * 4:(q4 + 1) * 4, t * 128:(t + 1) * 128], in_=h2Tf[i2][:, q4 * 4:(q4 + 1) * 4, :]),
                    reads=[B_h2Tf[i2]], writes=[B_h2T] + (B_attn_swa + B_attn_moba if (t == 0 and q4 == 0) else []))
            def router(t=t, i2=i2):
                for k in range(16):
                    prog.add("pe", lambda e, k=k: e.matmul(PS(4 + t % 2, 0, 20), lhsT=h2Tf[i2][:, k, :], rhs=wr_sb[:, k, :],
                                                           start=(k == 0), stop=(k == 15)),
                             reads=[B_h2Tf[i2], B_wr], writes=[B_ps[4 + t % 2]])
                prog.add("dve", lambda e: e.tensor_copy(out=L_all[:, t, :], in_=PS(4 + t % 2, 0, 20)),
                         reads=[B_ps[4 + t % 2]], writes=[B_L])
            if pend_router[0] is not None:
                pend_router[0]()
            pend_router[0] = router
        pend_router[0]()
        lg = L_all[:, :, 0:4]
        le = L_all[:, :, 4:20]
        mg, ohg, tmp16, sl, l1, msk, l2, ex, exm, den, gp, sumg, wexp, junk8 = (None,) * 14
        mg = rt_small[0][:, :, 0:1]
        ohg = rt_small[0][:, :, 4:8]
        sumg = rt_small[0][:, :, 8:9]
        gp = rt_small[0][:, :, 9:10]
        l1 = rt_small[0][:, :, 10:11]
        l2 = rt_small[0][:, :, 11:12]
        den = rt_small[0][:, :, 12:13]
        fac = rt_small[0][:, :, 13:14]
        tmp16 = rt_small[1]
        sl = rt_small[2][:, :, 0:4]
        msk = rt_small[2][:, :, 4:8]
        sl2 = rt_small[2][:, :, 8:12]
        ex = rt_small[3][:, :, 0:4]
        exm = rt_small[3][:, :, 4:8]
        wexp = rt_small[3][:, :, 8:12]
        eg = rt_small[4][:, :, 0:4]
        dgl = rt_small[4][:, :, 4:8]
        dsl = rt_small[4][:, :, 8:12]

        def dv(fn, r=(B_L, B_rts), w=(B_rts,)):
            prog.add("dve", fn, reads=list(r), writes=list(w))
        dv(lambda e: e.tensor_reduce(out=mg, in_=lg, axis=AX.X, op=ALU.max))
        dv(lambda e: e.tensor_tensor(out=ohg, in0=lg, in1=mg.to_broadcast([128, 8, 4]), op=ALU.is_ge))
        dv(lambda e: e.tensor_tensor(out=dgl, in0=lg, in1=mg.to_broadcast([128, 8, 4]), op=ALU.subtract))
        prog.add("act", lambda e: e.activation(out=eg, in_=dgl, func=AF.Exp), reads=[B_rts], writes=[B_rts])
        dv(lambda e: e.tensor_reduce(out=sumg, in_=eg, axis=AX.X, op=ALU.add))
        dv(lambda e: e.reciprocal(out=gp, in_=sumg))
        dv(lambda e: e.tensor_tensor(out=tmp16.rearrange("p t (g e) -> p t g e", e=4),
                                     in0=le.rearrange("p t (g e) -> p t g e", e=4),
                                     in1=ohg.unsqueeze(3).to_broadcast([128, 8, 4, 4]), op=ALU.mult))
        dv(lambda e: e.tensor_reduce(out=sl, in_=tmp16.rearrange("p t (g e) -> p t e g", e=4), axis=AX.X, op=ALU.add))
        dv(lambda e: e.tensor_reduce(out=l1, in_=sl, axis=AX.X, op=ALU.max))
        dv(lambda e: e.tensor_tensor(out=msk, in0=sl, in1=l1.to_broadcast([128, 8, 4]), op=ALU.is_ge))
        dv(lambda e: e.scalar_tensor_tensor(out=sl2, in0=msk, scalar=-1e30, op0=ALU.mult, in1=sl, op1=ALU.add))
        dv(lambda e: e.tensor_reduce(out=l2, in_=sl2, axis=AX.X, op=ALU.max))
        dv(lambda e: e.tensor_tensor(out=msk, in0=sl, in1=l2.to_broadcast([128, 8, 4]), op=ALU.is_ge))
        dv(lambda e: e.tensor_tensor(out=dsl, in0=sl, in1=l1.to_broadcast([128, 8, 4]), op=ALU.subtract))
        prog.add("act", lambda e: e.activation(out=ex, in_=dsl, func=AF.Exp), reads=[B_rts], writes=[B_rts])
        dv(lambda e: e.tensor_tensor(out=exm, in0=ex, in1=msk, op=ALU.mult))
        dv(lambda e: e.tensor_reduce(out=den, in_=exm, axis=AX.X, op=ALU.add))
        dv(lambda e: e.reciprocal(out=fac, in_=den))
        dv(lambda e: e.tensor_tensor(out=fac, in0=fac, in1=gp, op=ALU.mult))
        dv(lambda e: e.tensor_tensor(out=wexp, in0=exm, in1=fac.to_broadcast([128, 8, 4]), op=ALU.mult))
        dv(lambda e: e.tensor_tensor(out=comb.rearrange("p t (g e) -> p t g e", e=4),
                                     in0=ohg.unsqueeze(3).to_broadcast([128, 8, 4, 4]),
                                     in1=wexp.unsqueeze(2).to_broadcast([128, 8, 4, 4]), op=ALU.mult),
           w=(B_rts, B_comb))
        if debug and stage == 6:
            dbg_outs["h2T"] = (h2T, [128, 16 * 1024], BF16, [B_h2T])
            dbg_outs["L_all"] = (L_all, [128, 8 * 20], F32, [B_L])
            dbg_outs["comb"] = (comb, [128, 8 * 16], F32, [B_comb])

    if stage >= 7:
        er = [0]

        def load_e(dram_ap, shape3):
            s = er[0] % NWE
            er[0] += 1
            view = ering[s].rearrange("p (a b) -> p a b", b=shape3[2])
            extra = B_scrC_users if s in (1, 2, 3) and er[0] <= NWE else []
            prog.add("pool", lambda e: e.dma_start(out=view, in_=dram_ap), writes=[B_er[s]] + extra, dsem=S_er[s])
            return s, view
        B_scrC_users = [B_gbc2, B_junk2] + B_h2f + B_h2Tf
        gu = [0]
        yb = [0]
        for ex_i in range(16):
            hb = ex_i % 2
            s_g, Wg = load_e(w_gate_e[ex_i].rearrange("(k p) f -> p k f", p=128), [128, 16, 512])
            s_u, Wu = load_e(w_up_e[ex_i].rearrange("(k p) f -> p k f", p=128), [128, 16, 512])
            s_d, Wd = load_e(w_down_e[ex_i].rearrange("(k p) d -> p k d", p=128), [128, 4, 2048])
            for n in range(2):
                for fc in range(4):
                    alt = gu[0] % 2
                    gu[0] += 1
                    bg, bu = (0, 1) if alt == 0 else (2, 3)
                    for (bank, W_, s_) in ((bg, Wg, s_g), (bu, Wu, s_u)):
                        for k in range(16):
                            prog.add("pe", lambda e, k=k, bank=bank, W_=W_, fc=fc, n=n: e.matmul(
                                PS(bank), lhsT=W_[:, k, fc * 128:(fc + 1) * 128], rhs=h2T[:, k, n * 512:(n + 1) * 512],
                                start=(k == 0), stop=(k == 15)), reads=[B_er[s_], B_h2T], writes=[B_ps[bank]])
                    prog.add("act", lambda e, bg=bg, alt=alt: e.activation(out=sg[alt], in_=PS(bg), func=AF.Silu),
                             reads=[B_ps[bg]], writes=[B_sg[alt]])
                    prog.add("dve", lambda e, bu=bu, alt=alt, hb=hb, fc=fc, n=n: e.tensor_tensor(
                        out=hidT[hb][:, fc, n * 512:(n + 1) * 512], in0=PS(bu), in1=sg[alt], op=ALU.mult),
                        reads=[B_ps[bu], B_sg[alt]], writes=[B_hid[hb][n]])
            for t in range(8):
                for half in range(2):
                    b0 = 4 + 2 * (yb[0] % 2)
                    yb[0] += 1
                    for bb in range(2):
                        col = half * 1024 + bb * 512
                        for fc in range(4):
                            prog.add("pe", lambda e, fc=fc, t=t, col=col, bank=b0 + bb, hb=hb, Wd=Wd: e.matmul(
                                PS(bank), lhsT=hidT[hb][:, fc, t * 128:(t + 1) * 128], rhs=Wd[:, fc, col:col + 512],
                                start=(fc == 0), stop=(fc == 3)), reads=[B_er[s_d], B_hid[hb][t // 4]],
                                writes=[B_ps[b0 + bb]])
                    prog.add("dve", lambda e, t=t, half=half, b0=b0, ex_i=ex_i: e.scalar_tensor_tensor(
                        out=x1[:, t, half * 1024:(half + 1) * 1024], in0=psum[:, b0 * 512:(b0 + 2) * 512],
                        scalar=comb[:, t, ex_i:ex_i + 1], op0=ALU.mult, in1=x1[:, t, half * 1024:(half + 1) * 1024],
                        op1=ALU.add), reads=[B_ps[b0], B_ps[b0 + 1], B_comb, B_x1[t]], writes=[B_x1[t]])
        B_y = Buf("y")
        S_y = new_sem()
        y_v = y.rearrange("(t p) d -> p t d", p=128)
        for t in range(8):
            prog.add("sp", lambda e, t=t: e.dma_start(out=y_v[:, t, :], in_=x1[:, t, :]), reads=[B_x1[t]], writes=[B_y],
                     dsem=S_y)
        prog.add("sp", None, reads=[B_y])

    if debug:
        B_dbg = Buf("dbg")
        S_dbg = new_sem()
        for name, (ap, shape, dt, bufs) in dbg_outs.items():
            o = nc.dram_tensor("dbg_" + name, list(shape), dt, kind="ExternalOutput").ap()
            src = ap
            if len(ap.shape) == 3:
                src = ap.rearrange("p a b -> p (a b)")
            prog.add("sp", lambda e, o=o, src=src: e.dma_start(out=o[:, :], in_=src), reads=bufs, writes=[B_dbg], dsem=S_dbg)
        prog.add("sp", None, reads=[B_dbg])
        if stage < 7:
            pass

    block = es.enter_context(nc.Block())
    prog.emit(block, engsem)
    es.close()
    return nc, list(dbg_outs.keys())


_CONSTS = None


def _prepare_inputs(inputs, cores):
    global _CONSTS
    if _CONSTS is None:
        _CONSTS = _const_tables()
    c = _CONSTS
    f = lambda a: np.ascontiguousarray(np.asarray(a, dtype=np.float32))
    x = f(inputs["x"])
    shared = {
        "w_in": f(inputs["w_in"]), "w_up_swa": f(inputs["w_up_swa"]), "w_up_moba": f(inputs["w_up_moba"]),
        "w_out": f(inputs["w_out"]),
        "w_r": np.ascontiguousarray(np.concatenate(
            [f(inputs["w_router_group"]), f(inputs["w_router_expert"]).transpose(1, 0, 2).reshape(D, 16)], axis=1)),
        "w_gate_e": f(inputs["w_gate_e"]), "w_up_e": f(inputs["w_up_e"]), "w_down_e": f(inputs["w_down_e"]),
        "gmix_bc": np.ascontiguousarray(np.broadcast_to(f(inputs["g_mix"])[None, :], (128, D))),
        "gffn_bc": np.ascontiguousarray(np.broadcast_to(f(inputs["g_ffn"])[None, :], (128, D))),
        "gcols": np.ascontiguousarray(np.stack(
            [np.tile(f(inputs[k]), 2) for k in ("q_norm_swa", "k_norm_swa", "q_norm_moba", "k_norm_moba")], axis=1)),
        "sinks_bc": np.ascontiguousarray(np.broadcast_to(f(inputs["sinks"])[None, :], (128, 16))),
        "ident_bf": c["ident_bf"], "ident_f": c["ident_f"], "bd64": c["bd64"], "dsw": c["dsw"], "mneg": c["mneg"],
        "tabq": c["tabq"], "tabk": c["tabk"],
    }
    in_maps = []
    for cid in cores:
        b, hf = cid // 2, cid % 2
        if hf == 1:
            xs_ = x[b]
        else:
            xs_ = np.concatenate([x[b, NOWN:], x[b, :NOWN]], axis=0)
        m = dict(shared)
        m["xs"] = np.ascontiguousarray(xs_)
        m.update(_percore_tables(hf))
        in_maps.append(m)
    return in_maps


_NC_CACHE = {}


def kernel(**inputs):
    if "full" not in _NC_CACHE:
        _NC_CACHE["full"] = build_program(stage=99, debug=False)[0]
    nc = _NC_CACHE["full"]
    cores = list(range(8))
    in_maps = _prepare_inputs(inputs, cores)
    res = run_bass_kernel_spmd(nc, in_maps, core_ids=cores)
    out = np.empty((4, 2048, D), np.float32)
    for cid in cores:
        b, hf = cid // 2, cid % 2
        out[b, hf * NOWN:(hf + 1) * NOWN, :] = res.results[cid]["y"]
    return out
```

```python
import numpy as np
import ml_dtypes
from contextlib import ExitStack
import concourse.bass as bass
import concourse.mybir as mybir
from concourse.bass_utils import run_bass_kernel_spmd

F32 = mybir.dt.float32
BF16 = mybir.dt.bfloat16
U8 = mybir.dt.uint8
ALU = mybir.AluOpType
AF = mybir.ActivationFunctionType
AX = mybir.AxisListType
NPBF = ml_dtypes.bfloat16

D = 2048
NOWN = 1024
NKV = 2048
EPS = 1e-6
SCALE = 0.125
BIG = 32768.0
IN_COLS = 8448
C_QA, C_KA, C_VA, C_QB, C_KB, C_VB, C_GA, C_GB = 0, 1024, 1152, 1280, 2304, 3328, 4352, 6400
SLOPES = [2.0 ** (-(h + 1) / 2.0) for h in range(16)]
ARENA = 211968


class Buf:
    __slots__ = ("name", "lw", "lr", "excl")

    def __init__(self, name, excl=False):
        self.name = name
        self.lw = None
        self.lr = {}
        self.excl = excl


class DSem:
    def __init__(self, h):
        self.h = h
        self.count = 0


class Op:
    __slots__ = ("eng", "fn", "deps", "signal", "dsem", "val", "idx")


ENGS = ("pe", "act", "dve", "pool", "sp")


class Prog:
    def __init__(self):
        self.ops = []

    @staticmethod
    def _need(p, eng, is_dma, raw):
        if p.dsem is not None or is_dma:
            return True
        if p.eng != eng:
            return True
        if eng == "pe":
            return False
        return raw

    def add(self, eng, fn, reads=(), writes=(), dsem=None, ndma=1):
        idx = len(self.ops)
        op = Op()
        op.eng, op.fn, op.signal, op.dsem, op.idx, op.val = eng, fn, False, dsem, idx, None
        is_dma = dsem is not None
        key = ("d", idx) if is_dma else eng
        deps = set()
        for b in reads:
            w = b.lw
            if w is not None and self._need(self.ops[w], eng, is_dma, True):
                deps.add(w)
            if b.excl:
                for k2, r in b.lr.items():
                    if k2 != key:
                        deps.add(r)
        for b in writes:
            w = b.lw
            if w is not None and self._need(self.ops[w], eng, is_dma, False):
                deps.add(w)
            for r in b.lr.values():
                if self._need(self.ops[r], eng, is_dma, False):
                    deps.add(r)
        for b in reads:
            b.lr[key] = idx
        for b in writes:
            b.lw = idx
            b.lr = {}
        for d in deps:
            self.ops[d].signal = True
        op.deps = sorted(deps)
        if is_dma:
            dsem.count += 16 * ndma
            op.val = dsem.count
        self.ops.append(op)
        return op

    def emit(self, block, engsem):
        cnt = {}
        for op in self.ops:
            if op.dsem is None and op.signal:
                cnt[op.eng] = cnt.get(op.eng, 0) + 1
                op.val = cnt[op.eng]
        by = {e: [] for e in ENGS}
        for op in self.ops:
            by[op.eng].append(op)
        ops = self.ops

        def run(name):
            def body(e):
                waited = {}
                for op in by[name]:
                    for d in op.deps:
                        p = ops[d]
                        if p.dsem is not None:
                            sem, k = p.dsem.h, ("d", id(p.dsem))
                        else:
                            sem, k = engsem[p.eng], p.eng
                        if waited.get(k, 0) < p.val:
                            e.wait_ge(sem, p.val)
                            waited[k] = p.val
                    if op.fn is None:
                        continue
                    r = op.fn(e)
                    if op.dsem is not None:
                        for ins in (r if isinstance(r, (list, tuple)) else [r]):
                            ins.then_inc(op.dsem.h, 16)
                    elif op.signal:
                        r.then_inc(engsem[op.eng], 1)
            return body

        block.tensor(run("pe"))
        block.scalar(run("act"))
        block.vector(run("dve"))
        block.gpsimd(run("pool"))
        block.sync(run("sp"))


def _split3(a):
    a = a.astype(np.float64)
    hi = a.astype(NPBF)
    r = a - hi.astype(np.float64)
    mid = r.astype(NPBF)
    r = r - mid.astype(np.float64)
    lo = r.astype(NPBF)
    return hi, mid, lo


def _const_tables():
    c = {}
    c["ident_bf"] = np.eye(128, dtype=np.float32).astype(NPBF)
    c["ident_f"] = np.eye(128, dtype=np.float32)
    bd = np.zeros((128, 128), np.float32)
    bd[:64, :64] = 1.0 / 64
    bd[64:, 64:] = 1.0 / 64
    c["bd64"] = bd.astype(NPBF)
    k = np.arange(128)[:, None].astype(np.float64)
    q = np.arange(128)[None, :].astype(np.float64)
    da = q - k
    da = np.where(da >= 0, da, 1e9)
    db = q + 128 - k
    db = np.where(db < 128, db, 1e9)
    c["dsw"] = np.concatenate([db, da, db, da], axis=1).astype(np.float32)
    q2 = np.arange(256)[None, :]
    m0 = np.where(q2 >= np.arange(128)[:, None], 0.0, -1e9)
    m1 = np.where(q2 >= (np.arange(128)[:, None] + 128), 0.0, -1e9)
    c["mneg"] = np.concatenate([m0, m1], axis=1).astype(np.float32)
    tq = np.arange(NOWN).astype(np.float64)
    tk = (np.arange(NKV) - 1024).astype(np.float64)
    tabq = np.zeros((16, 6, NOWN), NPBF)
    tabk = np.zeros((16, 14, NKV), NPBF)
    ind = (np.arange(NKV)[None, :] // 256 == np.arange(8)[:, None]).astype(np.float32)
    for h in range(16):
        s = SLOPES[h]
        a = _split3(-s * tq / SCALE)
        b = _split3(s * tk / SCALE)
        for i in range(3):
            tabq[h, i] = a[i]
            tabq[h, 3 + i] = 1.0
            tabk[h, 8 + i] = 1.0
            tabk[h, 11 + i] = b[i]
        tabk[h, 0:8] = ind.astype(NPBF)
    c["tabq"] = tabq
    c["tabk"] = tabk
    return c


def _percore_tables(hf):
    pb = np.full((128, 8, 8), -1e30, np.float32)
    for t in range(8):
        own = 4 + t // 2
        for n in range(8):
            if n == own:
                pb[:, t, n] = 1e30
            elif n < own and (hf == 1 or n >= 4):
                pb[:, t, n] = 0.0
    pv = np.full((128, 1), float(hf), np.float32)
    return {"pastbias": pb.reshape(128, 64), "pvcol": pv}


def build_program(stage=99, debug=False):
    nc = bass.Bass("TRN2", target_bir_lowering=False)
    es = ExitStack()
    prog = Prog()
    dbg_outs = {}

    def din(name, shape, dt):
        return nc.dram_tensor(name, list(shape), dt, kind="ExternalInput").ap()

    xs = din("xs", [NKV, D], F32)
    w_in = din("w_in", [D, IN_COLS], F32)
    w_up_swa = din("w_up_swa", [1024, D], F32)
    w_up_moba = din("w_up_moba", [1024, D], F32)
    w_out = din("w_out", [D, D], F32)
    w_r = din("w_r", [D, 20], F32)
    w_gate_e = din("w_gate_e", [16, D, 512], F32)
    w_up_e = din("w_up_e", [16, D, 512], F32)
    w_down_e = din("w_down_e", [16, 512, D], F32)
    gmix_bc = din("gmix_bc", [128, D], F32)
    gffn_bc = din("gffn_bc", [128, D], F32)
    gcols_d = din("gcols", [128, 4], F32)
    sinks_bc = din("sinks_bc", [128, 16], F32)
    ident_bf_d = din("ident_bf", [128, 128], BF16)
    ident_f_d = din("ident_f", [128, 128], F32)
    bd64_d = din("bd64", [128, 128], BF16)
    dsw_d = din("dsw", [128, 512], F32)
    mneg_d = din("mneg", [128, 512], F32)
    tabq_d = din("tabq", [16, 6, NOWN], BF16)
    tabk_d = din("tabk", [16, 14, NKV], BF16)
    pastbias_d = din("pastbias", [128, 64], F32)
    pvcol_d = din("pvcol", [128, 1], F32)
    y = nc.dram_tensor("y", [NOWN, D], F32, kind="ExternalOutput").ap()

    arena = es.enter_context(nc.sbuf_tensor("arena", [128, ARENA], U8))
    psum = es.enter_context(nc.psum_tensor("psum", [128, 4096], F32))

    def PS(b, lo=0, hi=512):
        return psum[:, b * 512 + lo:b * 512 + hi]

    def PSB(b, lo=0, hi=1024):
        return psum[:, b * 512:(b + 1) * 512].bitcast(BF16)[:, lo:hi]

    B_ps = [Buf("ps%d" % i, excl=True) for i in range(8)]

    class Region:
        def __init__(self, base, size):
            self.base, self.size, self.off = base, size, 0

        def reset(self, off=0):
            self.off = off

        def alloc(self, shape, dt, at=None):
            nb = {F32: 4, BF16: 2}[dt]
            n = int(np.prod(shape[1:])) * nb
            n32 = (n + 31) // 32 * 32
            off = self.off if at is None else at
            assert off + n32 <= self.size, (off, n32, self.size)
            if at is None:
                self.off += n32
            v = arena[:, self.base + off:self.base + off + n].bitcast(dt)
            if len(shape) == 3:
                v = v.rearrange("p (a b) -> p a b", b=shape[2])
            return v

    CONST_SZ = 6144
    R1_SZ = 65536
    R2_SZ = 32768
    RC = Region(0, CONST_SZ)
    R1 = Region(CONST_SZ, R1_SZ)
    R2 = Region(CONST_SZ + R1_SZ, R2_SZ)
    R3 = Region(CONST_SZ + R1_SZ + R2_SZ, ARENA - CONST_SZ - R1_SZ - R2_SZ)

    sem_id = [0]

    def new_sem():
        sem_id[0] += 1
        return DSem(es.enter_context(nc.semaphore("s%d" % sem_id[0])))

    engsem = {e: es.enter_context(nc.semaphore("eng_" + e)) for e in ENGS}

    ident_bf = RC.alloc([128, 128], BF16)
    ident_f = RC.alloc([128, 128], F32)
    bd64 = RC.alloc([128, 128], BF16)
    gcols = RC.alloc([128, 4], F32)
    epscol = RC.alloc([128, 1], F32)
    expsink = RC.alloc([128, 16], F32)
    pvcol = RC.alloc([128, 1], F32)
    pastbias = RC.alloc([128, 64], F32)
    dsw = RC.alloc([128, 512], F32)
    mneg = RC.alloc([128, 512], F32)
    ss1 = RC.alloc([128, 16], F32)
    ln1 = RC.alloc([128, 16], F32)
    rs1 = RC.alloc([128, 16], F32)
    B_const = Buf("const")
    S_const = new_sem()
    const_loads = [(ident_bf, ident_bf_d), (ident_f, ident_f_d), (bd64, bd64_d), (gcols, gcols_d),
                   (expsink, sinks_bc), (pvcol, pvcol_d), (pastbias, pastbias_d), (dsw, dsw_d), (mneg, mneg_d)]

    def _ld_consts(e):
        return [e.dma_start(out=o, in_=i[:, :]) for o, i in const_loads]
    prog.add("sp", _ld_consts, writes=[B_const], dsem=S_const, ndma=len(const_loads))
    B_eps = Buf("eps")
    prog.add("dve", lambda e: e.memset(epscol, EPS), writes=[B_eps])
    B_ss1 = [Buf("ss1_%d" % t) for t in range(16)]
    prog.add("dve", lambda e: e.memset(ss1, 0.0), writes=B_ss1)
    B_expsink = Buf("expsink")
    prog.add("act", lambda e: e.activation(out=expsink, in_=expsink, func=AF.Exp), reads=[B_const], writes=[B_expsink])

    hTp = R1.alloc([128, 16, 1024], BF16)
    hTo = R1.alloc([128, 16, 1024], BF16)
    B_hTp, B_hTo = Buf("hTp"), Buf("hTo")
    attn_swa = R2.alloc([128, 8, 1024], BF16)
    attn_moba = R2.alloc([128, 8, 1024], BF16)
    B_attn_swa = [Buf("attn_swa%d" % i) for i in range(8)]
    B_attn_moba = [Buf("attn_moba%d" % i) for i in range(8)]

    NW = 6
    wslot = [R3.alloc([128, 16, 128], BF16) for _ in range(NW)]
    B_w = [Buf("w%d" % i) for i in range(NW)]
    S_w = [new_sem() for _ in range(NW)]
    QA = [[R3.alloc([128, 1024], BF16) for _ in range(2)] for _ in range(2)]
    B_QA = [[Buf("QA%d%d" % (i, j)) for j in range(2)] for i in range(2)]
    B_QAaug = [[Buf("QAaug%d%d" % (i, j)) for j in range(2)] for i in range(2)]
    S_QAaug = [[new_sem() for j in range(2)] for i in range(2)]
    off_KA = R3.off
    KA = [[R3.alloc([128, 2048], BF16) for _ in range(2)] for _ in range(2)]
    B_KA = [[Buf("KA%d%d" % (i, j)) for j in range(2)] for i in range(2)]
    B_KAaug = [[Buf("KAaug%d%d" % (i, j)) for j in range(2)] for i in range(2)]
    S_KAaug = [[new_sem() for j in range(2)] for i in range(2)]
    VA = [[R3.alloc([128, 16, 128], BF16) for _ in range(2)] for _ in range(2)]
    B_VA = [[Buf("VA%d%d" % (i, j)) for j in range(2)] for i in range(2)]
    KS = [R3.alloc([128, 1152], BF16) for _ in range(2)]
    B_KS = [Buf("KS%d" % i) for i in range(2)]
    VS = [R3.alloc([128, 9, 128], BF16) for _ in range(2)]
    B_VS = [Buf("VS%d" % i) for i in range(2)]
    sqb = [R3.alloc([128, 512], BF16) for _ in range(2)]
    B_sqb = [Buf("sqb%d" % i) for i in range(2)]
    lnb = [R3.alloc([128, 512], F32) for _ in range(2)]
    B_lnb = [Buf("lnb%d" % i) for i in range(2)]
    rsb = [R3.alloc([128, 512], F32) for _ in range(2)]
    B_rsb = [Buf("rsb%d" % i) for i in range(2)]
    tmpB = [R3.alloc([128, 512], BF16) for _ in range(2)]
    B_tmpB = [Buf("tmpB%d" % i) for i in range(2)]
    off_Sp = R3.off
    Sp = [R3.alloc([128, 512], F32) for _ in range(3)]
    B_Sp = [Buf("Sp%d" % i) for i in range(3)]
    NPT = 4
    Pt = [R3.alloc([128, 512], BF16) for _ in range(NPT)]
    B_Pt = [Buf("Pt%d" % i) for i in range(NPT)]
    Rt = [R3.alloc([128, 256], F32) for _ in range(2)]
    B_Rt = [Buf("Rt%d" % i) for i in range(2)]
    Rt2 = [R3.alloc([128, 256], F32) for _ in range(2)]
    tmpo = [R3.alloc([128, 256], BF16) for _ in range(2)]
    B_tmpo = [Buf("tmpo%d" % i) for i in range(2)]
    gm = R3.alloc([128, 8, 8], F32)
    m8 = R3.alloc([128, 8, 8], F32)
    selt = R3.alloc([128, 8, 8], F32)
    kmf = R3.alloc([128, 8], F32)
    kmb = R3.alloc([128, 8], BF16)
    B_gm, B_m8, B_selt, B_kmf, B_kmb = Buf("gm"), Buf("m8"), Buf("selt"), Buf("kmf"), Buf("kmb")
    stage_t = [R3.alloc([128, 8, 72], BF16) for _ in range(2)]
    B_stage = [Buf("stage%d" % i) for i in range(2)]
    sId = [R3.alloc([128, 128], F32) for _ in range(2)]
    B_sId = [Buf("sId%d" % i) for i in range(2)]
    attn_end = R3.off
    xt = [R3.alloc([128, D], F32, at=off_KA + i * 8192) for i in range(3)]
    B_xt = [Buf("xt%d" % i) for i in range(3)]
    S_xt = [new_sem() for _ in range(3)]
    gbc = R3.alloc([128, D], F32, at=off_KA + 3 * 8192)
    xn = [R3.alloc([128, D], BF16, at=off_KA + 4 * 8192 + i * 4096) for i in range(2)]
    B_xn = [Buf("xn%d" % i) for i in range(2)]
    junk = R3.alloc([128, D], BF16, at=off_Sp)
    B_junk = Buf("junk")
    B_gbc = Buf("gbc")
    S_gbc = new_sem()
    bar_scr = RC.alloc([128, 8], F32)

    def barrier(old, new):
        prog.add("dve", lambda e: e.memset(bar_scr, 0.0), writes=list(old) + list(new))

    prog.add("sp", lambda e: e.dma_start(out=gbc, in_=gmix_bc[:, :]), writes=[B_gbc], dsem=S_gbc)
    for t in range(16):
        sl = t % 3
        x2 = t % 2
        prog.add("sp", lambda e, t=t, sl=sl: e.dma_start(out=xt[sl], in_=xs[t * 128:(t + 1) * 128, :]),
                 writes=[B_xt[sl]], dsem=S_xt[sl])
        prog.add("act", lambda e, t=t, sl=sl: e.activation(out=junk, in_=xt[sl], func=AF.Square,
                                                           accum_out=ss1[:, t:t + 1]),
                 reads=[B_xt[sl], B_ss1[t]], writes=[B_junk, B_ss1[t]])
        prog.add("act", lambda e, t=t: e.activation(out=ln1[:, t:t + 1], in_=ss1[:, t:t + 1], func=AF.Ln,
                                                    scale=1.0 / D, bias=epscol[:, 0:1]),
                 reads=[B_ss1[t], B_eps], writes=[B_ss1[t]])
        prog.add("act", lambda e, t=t: e.activation(out=rs1[:, t:t + 1], in_=ln1[:, t:t + 1], func=AF.Exp, scale=-0.5),
                 reads=[B_ss1[t]], writes=[B_ss1[t]])
        prog.add("dve", lambda e, t=t, sl=sl, x2=x2: e.scalar_tensor_tensor(
            out=xn[x2], in0=xt[sl], scalar=rs1[:, t:t + 1], op0=ALU.mult, in1=gbc, op1=ALU.mult),
            reads=[B_xt[sl], B_ss1[t], B_gbc], writes=[B_xn[x2]])
        dst = hTp if t < 8 else hTo
        Bdst = B_hTp if t < 8 else B_hTo
        tc = (t % 8) * 128
        for k in range(16):
            bank = 6 + k // 8
            prog.add("pe", lambda e, k=k, x2=x2, bank=bank: e.transpose(
                out=PSB(bank, (k % 8) * 128, (k % 8 + 1) * 128), in_=xn[x2][:, k * 128:(k + 1) * 128],
                identity=ident_bf), reads=[B_xn[x2], B_const], writes=[B_ps[bank]])
        prog.add("act", lambda e, dst=dst, tc=tc: e.activation(
            out=dst[:, 0:8, tc:tc + 128], in_=PSB(6).rearrange("p (a b) -> p a b", b=128), func=AF.Copy),
            reads=[B_ps[6]], writes=[Bdst])
        prog.add("dve", lambda e, dst=dst, tc=tc: e.tensor_copy(
            out=dst[:, 8:16, tc:tc + 128], in_=PSB(7).rearrange("p (a b) -> p a b", b=128)),
            reads=[B_ps[7]], writes=[Bdst])

    if debug and stage == 1:
        dbg_outs["hTp"] = (hTp, [128, 16 * 1024], BF16, [B_hTp])
        dbg_outs["hTo"] = (hTo, [128, 16 * 1024], BF16, [B_hTo])

    w_in_v = w_in.rearrange("(k p) n -> p k n", p=128)
    wring = [0]

    def load_wcols(c0):
        s = wring[0] % NW
        wring[0] += 1
        prog.add("pool", lambda e: e.dma_start(out=wslot[s], in_=w_in_v[:, :, c0:c0 + 128]),
                 writes=[B_w[s]], dsem=S_w[s])
        return s

    pbank = [0]
    nrm = [0]

    def proj_fm(s, src, Bsrc, lo, n):
        bank = pbank[0] % 2
        pbank[0] += 1
        for k in range(16):
            prog.add("pe", lambda e, k=k: e.matmul(PS(bank, 0, n), lhsT=wslot[s][:, k, :], rhs=src[:, k, lo:lo + n],
                                                   start=(k == 0), stop=(k == 15)),
                     reads=[B_w[s], Bsrc], writes=[B_ps[bank]])
        return bank

    def headnorm(bank, n, gidx, dstA, BA, dstB, BB):
        headnorm_b(headnorm_a(bank, n), bank, n, gidx, dstA, BA, dstB, BB)

    def headnorm_a(bank, n):
        i = nrm[0] % 2
        nrm[0] += 1
        prog.add("act", lambda e: e.activation(out=sqb[i][:, 0:n], in_=PS(bank, 0, n), func=AF.Square),
                 reads=[B_ps[bank]], writes=[B_sqb[i]])
        return i

    def headnorm_b(i, bank, n, gidx, dstA, BA, dstB, BB):
        prog.add("pe", lambda e: e.matmul(PS(2, 0, n), lhsT=bd64, rhs=sqb[i][:, 0:n], start=True, stop=True),
                 reads=[B_sqb[i], B_const], writes=[B_ps[2]])
        prog.add("act", lambda e: e.activation(out=lnb[i][:, 0:n], in_=PS(2, 0, n), func=AF.Ln, bias=epscol[:, 0:1]),
                 reads=[B_ps[2], B_eps], writes=[B_lnb[i]])
        prog.add("act", lambda e: e.activation(out=rsb[i][:, 0:n], in_=lnb[i][:, 0:n], func=AF.Exp, scale=-0.5),
                 reads=[B_lnb[i]], writes=[B_rsb[i]])
        prog.add("dve", lambda e: e.scalar_tensor_tensor(
            out=dstA, in0=PS(bank, 0, n)[0:64, :], scalar=gcols[0:64, gidx:gidx + 1], op0=ALU.mult,
            in1=rsb[i][0:64, 0:n], op1=ALU.mult), reads=[B_ps[bank], B_rsb[i], B_const], writes=[BA])
        prog.add("dve", lambda e: e.scalar_tensor_tensor(
            out=tmpB[i][64:128, 0:n], in0=PS(bank, 0, n)[64:128, :], scalar=gcols[64:128, gidx:gidx + 1], op0=ALU.mult,
            in1=rsb[i][64:128, 0:n], op1=ALU.mult), reads=[B_ps[bank], B_rsb[i], B_const], writes=[B_tmpB[i]])
        prog.add("dve", lambda e: e.tensor_copy(out=dstB, in_=tmpB[i][64:128, 0:n]),
                 reads=[B_tmpB[i]], writes=[BB])

    sbank = [0]
    swa_sb = [0]
    obank = [0]
    ptc = [0]
    rtc = [0]

    def finish_block(ob, nq, dst_pair, Bdst, parity, qlo, sink_h=None):
        r = rtc[0] % 2
        rtc[0] += 1
        if sink_h is not None:
            prog.add("act", lambda e: e.activation(out=Rt2[r][64:128, 0:nq], in_=PS(ob, 0, nq)[64:128, :], func=AF.Ln,
                                                   bias=expsink[64:128, sink_h:sink_h + 1]),
                     reads=[B_ps[ob], B_expsink], writes=[B_Rt[r]])
        else:
            prog.add("act", lambda e: e.activation(out=Rt2[r][64:128, 0:nq], in_=PS(ob, 0, nq)[64:128, :], func=AF.Ln),
                     reads=[B_ps[ob]], writes=[B_Rt[r]])
        prog.add("act", lambda e: e.activation(out=Rt2[r][64:128, 0:nq], in_=Rt2[r][64:128, 0:nq], func=AF.Exp, scale=-1.0),
                 reads=[B_Rt[r]], writes=[B_Rt[r]])
        prog.add("dve", lambda e: e.tensor_copy(out=Rt[r][0:64, 0:nq], in_=Rt2[r][64:128, 0:nq]),
                 reads=[B_Rt[r]], writes=[B_Rt[r]])
        if parity == 0:
            prog.add("dve", lambda e: e.tensor_tensor(out=dst_pair[0:64, qlo:qlo + nq], in0=PS(ob, 0, nq)[0:64, :],
                                                      in1=Rt[r][0:64, 0:nq], op=ALU.mult),
                     reads=[B_ps[ob], B_Rt[r]], writes=[Bdst])
        else:
            prog.add("dve", lambda e: e.tensor_tensor(out=tmpo[r][0:64, 0:nq], in0=PS(ob, 0, nq)[0:64, :],
                                                      in1=Rt[r][0:64, 0:nq], op=ALU.mult),
                     reads=[B_ps[ob], B_Rt[r]], writes=[B_tmpo[r]])
            prog.add("dve", lambda e: e.tensor_copy(out=dst_pair[64:128, qlo:qlo + nq], in_=tmpo[r][0:64, 0:nq]),
                     reads=[B_tmpo[r]], writes=[Bdst])

    def drain(g):
        if g is not None:
            for _ in g:
                pass

    def run_units(units, filler=None, every=3, depth=2):
        n = len(units)
        if not n:
            drain(filler)
            return
        for j in range(min(depth, n)):
            units[j]["S"]()
        for j in range(min(depth - 1, n)):
            units[j]["E"]()
        for i, u in enumerate(units):
            u["PV"]()
            if i + depth < n:
                units[i + depth]["S"]()
            if filler is not None and i % every == every - 1:
                next(filler, None)
            if i + depth - 1 < n:
                units[i + depth - 1]["E"]()
            if u.get("F") is not None:
                u["F"]()
        drain(filler)

    def swa_kv():
        for g in range(2):
            prog.add("dve", lambda e, g=g: e.memset(VS[g][:, :, 64:128], 1.0), writes=[B_VS[g]])
            prog.add("dve", lambda e, g=g: e.tensor_scalar(out=VS[g][:, 0:1, 64:128], in0=VS[g][:, 0:1, 64:128],
                                                           scalar1=pvcol[:, 0:1], scalar2=None, op0=ALU.mult),
                     reads=[B_const, B_VS[g]], writes=[B_VS[g]])
        sk = load_wcols(C_KA)
        sv = load_wcols(C_VA)
        for (src, Bsrc, lo, n, dlo) in ((hTp, B_hTp, 896, 128, 0), (hTo, B_hTo, 0, 512, 128), (hTo, B_hTo, 512, 512, 640)):
            bank = proj_fm(sk, src, Bsrc, lo, n)
            headnorm(bank, n, 1, KS[0][0:64, dlo:dlo + n], B_KS[0], KS[1][0:64, dlo:dlo + n], B_KS[1])
        for grp in ((7, 8, 9, 10), (11, 12, 13, 14), (15,)):
            bank = pbank[0] % 2
            pbank[0] += 1
            for j, t in enumerate(grp):
                src, Bsrc, tc = (hTp, B_hTp, t * 128) if t < 8 else (hTo, B_hTo, (t - 8) * 128)
                for k in range(16):
                    prog.add("pe", lambda e, k=k, j=j, src=src, tc=tc, bank=bank: e.matmul(
                        PS(bank, j * 128, (j + 1) * 128), lhsT=src[:, k, tc:tc + 128], rhs=wslot[sv][:, k, :],
                        start=(k == 0), stop=(k == 15)), reads=[B_w[sv], Bsrc], writes=[B_ps[bank]])
            for g in range(2):
                for j, t in enumerate(grp):
                    if t < 8:
                        prog.add("act", lambda e, g=g, j=j, t=t, bank=bank: e.activation(
                            out=VS[g][:, t - 7, 0:64], in_=PS(bank, j * 128 + g * 64, j * 128 + g * 64 + 64),
                            func=AF.Copy, scale=pvcol[:, 0:1]), reads=[B_ps[bank], B_const], writes=[B_VS[g]])
                    else:
                        prog.add("act", lambda e, g=g, j=j, t=t, bank=bank: e.activation(
                            out=VS[g][:, t - 7, 0:64], in_=PS(bank, j * 128 + g * 64, j * 128 + g * 64 + 64),
                            func=AF.Copy), reads=[B_ps[bank]], writes=[B_VS[g]])

    def swa_qproj(p, buf):
        s = load_wcols(C_QA + p * 128)
        pend = None
        for n in range(2):
            if pend is not None:
                pend()
            bank = proj_fm(s, hTo, B_hTo, n * 512, 512)
            pend = (lambda bank=bank, n=n: headnorm(
                bank, 512, 0, QA[buf][0][0:64, n * 512:(n + 1) * 512], B_QA[buf][0],
                QA[buf][1][0:64, n * 512:(n + 1) * 512], B_QA[buf][1]))
            yield
        pend()
        yield

    def swa_attn(p, buf, filler=None, every=3, depth=3):
        banks = (3, 4, 7) if depth == 3 else (3, 4)
        units = []
        for i in range(2):
            h = 2 * p + i
            g = h // 8
            q = QA[buf][i]
            Bq = B_QA[buf][i]
            xi = h % 2
            prog.add("act", lambda e, h=h, xi=xi: e.activation(out=sId[xi], in_=ident_f, func=AF.Copy,
                                                               scale=-SLOPES[h] / SCALE),
                     reads=[B_const], writes=[B_sId[xi]])
            for c in range(4):
                i0 = 8 + 2 * c - 7
                sb = banks[swa_sb[0] % len(banks)]
                swa_sb[0] += 1
                ob = 5 + obank[0] % 2
                obank[0] += 1
                pi = ptc[0] % NPT
                ptc[0] += 1

                def S(q=q, Bq=Bq, g=g, c=c, i0=i0, sb=sb, xi=xi):
                    qa = 2 * c * 128
                    prog.add("pe", lambda e: e.matmul(PS(sb), lhsT=sId[xi], rhs=dsw, start=True, stop=False),
                             reads=[B_sId[xi], B_const], writes=[B_ps[sb]])
                    for (olo, ohi, kt, qlo, qhi, lastmm) in ((0, 128, i0 - 1, qa, qa + 128, False),
                                                             (128, 384, i0, qa, qa + 256, False),
                                                             (384, 512, i0 + 1, qa + 128, qa + 256, True)):
                        prog.add("pe", lambda e, olo=olo, ohi=ohi, kt=kt, qlo=qlo, qhi=qhi, lastmm=lastmm: e.matmul(
                            PS(sb, olo, ohi), lhsT=KS[g][0:64, kt * 128:(kt + 1) * 128], rhs=q[0:64, qlo:qhi],
                            start=False, stop=lastmm), reads=[B_KS[g], Bq], writes=[B_ps[sb]])

                def E(h=h, sb=sb, pi=pi):
                    prog.add("act", lambda e: e.activation(out=Pt[pi], in_=PS(sb), func=AF.Exp, scale=SCALE),
                             reads=[B_ps[sb]], writes=[B_Pt[pi]])

                def PV(h=h, p=p, i=i, g=g, c=c, i0=i0, ob=ob, pi=pi):
                    for (olo, kt, plo, st, sp_) in ((0, i0 - 1, 0, True, False), (0, i0, 128, False, True),
                                                    (128, i0, 256, True, False), (128, i0 + 1, 384, False, True)):
                        prog.add("pe", lambda e, olo=olo, kt=kt, plo=plo, st=st, sp_=sp_: e.matmul(
                            PS(ob, olo, olo + 128), lhsT=VS[g][:, kt, :], rhs=Pt[pi][:, plo:plo + 128],
                            start=st, stop=sp_), reads=[B_VS[g], B_Pt[pi]], writes=[B_ps[ob]])

                def F(h=h, p=p, i=i, c=c, ob=ob):
                    finish_block(ob, 256, attn_swa[:, p, :], B_attn_swa[p], i, c * 256, sink_h=h)

                units.append({"S": S, "E": E, "PV": PV, "F": F})
        run_units(units, filler, every, depth=depth)

    def moba_init():
        for b in range(2):
            for i in range(2):
                prog.add("dve", lambda e, b=b, i=i: e.memset(VA[b][i][:, :, 64:128], 1.0),
                         writes=[B_VA[b][i]])
                prog.add("dve", lambda e, b=b, i=i: e.tensor_scalar(
                    out=VA[b][i][:, 0:8, 64:128], in0=VA[b][i][:, 0:8, 64:128], scalar1=pvcol[:, 0:1], scalar2=None,
                    op0=ALU.mult), reads=[B_const, B_VA[b][i]], writes=[B_VA[b][i]])
            prog.add("dve", lambda e, b=b: e.memset(stage_t[b], 0.0), writes=[B_stage[b]])

    def moba_proj(p, buf):
        sk = load_wcols(C_KB + p * 128)
        sv = load_wcols(C_VB + p * 128)
        sq = load_wcols(C_QB + p * 128)
        for i in range(2):
            h = 2 * p + i
            prog.add("sp", lambda e, i=i, h=h: e.dma_start(out=KA[buf][i][64:78, :], in_=tabk_d[h]),
                     writes=[B_KAaug[buf][i]], dsem=S_KAaug[buf][i])
            prog.add("sp", lambda e, i=i, h=h: e.dma_start(out=QA[buf][i][72:78, :], in_=tabq_d[h]),
                     writes=[B_QAaug[buf][i]], dsem=S_QAaug[buf][i])
        pend = None
        for (src, Bsrc, lo, dlo) in ((hTp, B_hTp, 0, 0), (hTp, B_hTp, 512, 512), (hTo, B_hTo, 0, 1024), (hTo, B_hTo, 512, 1536)):
            if pend is not None:
                pend()
            bank = proj_fm(sk, src, Bsrc, lo, 512)
            pend = (lambda bank=bank, dlo=dlo: headnorm(
                bank, 512, 3, KA[buf][0][0:64, dlo:dlo + 512], B_KA[buf][0], KA[buf][1][0:64, dlo:dlo + 512], B_KA[buf][1]))
            cmd = yield
            if cmd == "flush" and pend is not None:
                pend()
                pend = None
                yield
            elif pend is not None:
                ia = headnorm_a(bank, 512)
                pend = (lambda ia=ia, bank=bank, dlo=dlo: headnorm_b(
                    ia, bank, 512, 3, KA[buf][0][0:64, dlo:dlo + 512], B_KA[buf][0], KA[buf][1][0:64, dlo:dlo + 512],
                    B_KA[buf][1]))
                cmd = yield
                if cmd == "flush":
                    pend()
                    pend = None
                    yield
        for t0 in range(0, 16, 4):
            if pend is not None:
                pend()
            bank = pbank[0] % 2
            pbank[0] += 1
            for j in range(4):
                t = t0 + j
                src, Bsrc, tc = (hTp, B_hTp, t * 128) if t < 8 else (hTo, B_hTo, (t - 8) * 128)
                for k in range(16):
                    prog.add("pe", lambda e, k=k, j=j, src=src, tc=tc, bank=bank: e.matmul(
                        PS(bank, j * 128, (j + 1) * 128), lhsT=src[:, k, tc:tc + 128], rhs=wslot[sv][:, k, :],
                        start=(k == 0), stop=(k == 15)), reads=[B_w[sv], Bsrc], writes=[B_ps[bank]])

            def vcopy(bank=bank, t0=t0):
                for i in range(2):
                    src_ps = PS(bank).rearrange("p (a b) -> p a b", b=128)[:, :, i * 64:(i + 1) * 64]
                    if t0 < 8:
                        prog.add("act", lambda e, i=i, src_ps=src_ps: e.activation(
                            out=VA[buf][i][:, t0:t0 + 4, 0:64], in_=src_ps, func=AF.Copy, scale=pvcol[:, 0:1]),
                            reads=[B_ps[bank], B_const], writes=[B_VA[buf][i]])
                    else:
                        prog.add("act", lambda e, i=i, src_ps=src_ps: e.activation(
                            out=VA[buf][i][:, t0:t0 + 4, 0:64], in_=src_ps, func=AF.Copy),
                            reads=[B_ps[bank]], writes=[B_VA[buf][i]])
            pend = vcopy
            cmd = yield
            if cmd == "flush" and pend is not None:
                pend()
                pend = None
                yield
        for n in range(2):
            if pend is not None:
                pend()
            bank = proj_fm(sq, hTo, B_hTo, n * 512, 512)
            pend = (lambda bank=bank, n=n: headnorm(
                bank, 512, 2, QA[buf][0][0:64, n * 512:(n + 1) * 512], B_QA[buf][0],
                QA[buf][1][0:64, n * 512:(n + 1) * 512], B_QA[buf][1]))
            cmd = yield
            if cmd == "flush" and pend is not None:
                pend()
                pend = None
                yield
            elif pend is not None:
                ia = headnorm_a(bank, 512)
                pend = (lambda ia=ia, bank=bank, n=n: headnorm_b(
                    ia, bank, 512, 2, QA[buf][0][0:64, n * 512:(n + 1) * 512], B_QA[buf][0],
                    QA[buf][1][0:64, n * 512:(n + 1) * 512], B_QA[buf][1]))
                cmd = yield
                if cmd == "flush":
                    pend()
                    pend = None
                    yield
        if pend is not None:
            pend()
            pend = None
        yield
        for i in range(2):
            K_, Q_ = KA[buf][i], QA[buf][i]
            prog.add("dve", lambda e, K_=K_: e.tensor_reduce(
                out=kmf[0:64, :], in_=K_[0:64, :].rearrange("p (n l) -> p n l", l=256), axis=AX.X, op=ALU.add),
                reads=[B_KA[buf][i]], writes=[B_kmf])
            yield
            prog.add("dve", lambda e: e.tensor_copy(out=kmb[0:64, :], in_=kmf[0:64, :]), reads=[B_kmf], writes=[B_kmb])
            for t in range(8):
                prog.add("pe", lambda e, t=t, Q_=Q_: e.matmul(PS(2, 256 + t * 8, 256 + t * 8 + 8),
                                                              lhsT=Q_[0:64, t * 128:(t + 1) * 128], rhs=kmb[0:64, :],
                                                              start=True, stop=True),
                         reads=[B_QA[buf][i], B_kmb], writes=[B_ps[2]])
            prog.add("dve", lambda e: e.tensor_tensor(out=gm.rearrange("p a b -> p (a b)"), in0=PS(2, 256, 320),
                                                      in1=pastbias, op=ALU.add),
                     reads=[B_ps[2], B_const], writes=[B_gm])
            yield
            for t in range(8):
                prog.add("dve", lambda e, t=t: e.max(out=m8[:, t, :], in_=gm[:, t, :]), reads=[B_gm], writes=[B_m8])
            prog.add("dve", lambda e: e.tensor_tensor(out=selt, in0=gm, in1=m8[:, :, 3:4].to_broadcast([128, 8, 8]),
                                                      op=ALU.is_ge), reads=[B_gm, B_m8], writes=[B_selt])
            prog.add("dve", lambda e: e.tensor_scalar(out=stage_t[buf][:, :, 64:72], in0=selt, scalar1=BIG,
                                                      scalar2=-BIG, op0=ALU.mult, op1=ALU.add),
                     reads=[B_selt], writes=[B_stage[buf]])
            yield
            for r in range(2):
                for t4 in range(4):
                    t = r * 4 + t4
                    prog.add("pe", lambda e, t=t, t4=t4: e.transpose(
                        out=PSB(2, t4 * 128, (t4 + 1) * 128)[0:72, :], in_=stage_t[buf][:, t, :], identity=ident_bf),
                        reads=[B_stage[buf], B_const], writes=[B_ps[2]])
                prog.add("dve", lambda e, r=r, Q_=Q_: e.tensor_copy(out=Q_[64:72, r * 512:(r + 1) * 512],
                                                                    in_=PSB(2, 0, 512)[64:72, :]),
                         reads=[B_ps[2]], writes=[B_QA[buf][i]])
            yield

    def moba_attn(p, buf, filler=None, every=3):
        units = []
        for i in range(2):
            h = 2 * p + i
            K_, Q_, V_ = KA[buf][i], QA[buf][i], VA[buf][i]
            rd = [B_KA[buf][i], B_KAaug[buf][i], B_QA[buf][i], B_QAaug[buf][i]]
            for j in range(4):
                nkt = 8 + 2 * (j + 1)
                ob = 5 + obank[0] % 2
                obank[0] += 1
                for kt in range(0, nkt, 2):
                    sb = (3, 4, 7)[sbank[0] % 3]
                    si = sbank[0] % 3
                    sbank[0] += 1
                    pi = ptc[0] % NPT
                    ptc[0] += 1
                    diag = (kt == 8 + 2 * j)
                    last = (kt == nkt - 2)

                    def S(K_=K_, Q_=Q_, rd=rd, j=j, kt=kt, sb=sb):
                        for u in range(2):
                            prog.add("pe", lambda e, u=u: e.matmul(
                                PS(sb, u * 256, (u + 1) * 256), lhsT=K_[0:78, (kt + u) * 128:(kt + u + 1) * 128],
                                rhs=Q_[0:78, j * 256:(j + 1) * 256], start=True, stop=True), reads=rd, writes=[B_ps[sb]])

                    def E(diag=diag, sb=sb, si=si, pi=pi):
                        if diag:
                            prog.add("dve", lambda e: e.tensor_tensor(out=Sp[si], in0=PS(sb), in1=mneg, op=ALU.add),
                                     reads=[B_ps[sb], B_const], writes=[B_Sp[si]])
                            prog.add("act", lambda e: e.activation(out=Pt[pi], in_=Sp[si], func=AF.Exp, scale=SCALE),
                                     reads=[B_Sp[si]], writes=[B_Pt[pi]])
                        else:
                            prog.add("act", lambda e: e.activation(out=Pt[pi], in_=PS(sb), func=AF.Exp, scale=SCALE),
                                     reads=[B_ps[sb]], writes=[B_Pt[pi]])

                    def PV(V_=V_, i=i, p=p, j=j, kt=kt, ob=ob, pi=pi, last=last, buf=buf):
                        for u in range(2):
                            prog.add("pe", lambda e, u=u: e.matmul(
                                PS(ob, 0, 256), lhsT=V_[:, kt + u, :], rhs=Pt[pi][:, u * 256:(u + 1) * 256],
                                start=(kt == 0 and u == 0), stop=(last and u == 1)),
                                reads=[B_VA[buf][i], B_Pt[pi]], writes=[B_ps[ob]])

                    F = None
                    if last:
                        def F(i=i, p=p, j=j, ob=ob):
                            finish_block(ob, 256, attn_moba[:, p, :], B_attn_moba[p], i, j * 256)

                    units.append({"S": S, "E": E, "PV": PV, "F": F})
        run_units(units, filler, every, depth=3)

    if stage >= 2:
        barrier(B_xt + B_xn + [B_gbc, B_junk],
                [b for bb in B_KA for b in bb] + [b for bb in B_KAaug for b in bb] + [b for bb in B_VA for b in bb] +
                B_KS + B_VS + B_Sp + B_Pt)
        swa_kv()
        moba_init()
        drain(swa_qproj(0, 0))
        moba0 = moba_proj(0, 0) if stage >= 3 else None

        def chain2(a, b, nb):
            for _ in a:
                yield
            for _ in range(nb):
                if next(b, "end") == "end":
                    return
                yield
            try:
                b.send("flush")
            except StopIteration:
                pass

        for p in range(8):
            if p + 1 < 8:
                if moba0 is not None and p >= 4:
                    filler, every = chain2(swa_qproj(p + 1, (p + 1) % 2), moba0, 3), 1
                else:
                    filler, every = swa_qproj(p + 1, (p + 1) % 2), 2
            elif moba0 is not None:
                filler, every = moba0, 1
            else:
                filler, every = None, 1
            swa_attn(p, p % 2, filler, every, depth=3)
        if debug and stage == 2:
            dbg_outs["KS0"] = (KS[0], [128, 1152], BF16, [B_KS[0]])
            dbg_outs["KS1"] = (KS[1], [128, 1152], BF16, [B_KS[1]])
            dbg_outs["VS0"] = (VS[0], [128, 9 * 128], BF16, [B_VS[0]])
            dbg_outs["attn_swa"] = (attn_swa, [128, 8 * 1024], BF16, B_attn_swa)
    if stage >= 3:
        for p in range(8):
            filler = moba_proj(p + 1, (p + 1) % 2) if p + 1 < 8 else None
            moba_attn(p, p % 2, filler, 2)
        if debug and stage == 3:
            dbg_outs["attn_moba"] = (attn_moba, [128, 8 * 1024], BF16, B_attn_moba)
            dbg_outs["QA10"] = (QA[1][0], [128, 1024], BF16, [B_QA[1][0], B_QAaug[1][0]])
            dbg_outs["KA10"] = (KA[1][0], [128, 2048], BF16, [B_KA[1][0], B_KAaug[1][0]])
            dbg_outs["VA10"] = (VA[1][0], [128, 2048], BF16, [B_VA[1][0]])

    all_attn_bufs = (B_w + [b for bb in B_QA for b in bb] + [b for bb in B_QAaug for b in bb] +
                     [b for bb in B_KA for b in bb] + [b for bb in B_KAaug for b in bb] +
                     [b for bb in B_VA for b in bb] + B_KS + B_VS + B_sqb + B_lnb + B_rsb + B_tmpB + B_Sp + B_Pt +
                     B_Rt + B_tmpo + [B_gm, B_m8, B_selt, B_kmf, B_kmb] + B_stage + B_xt + B_xn + [B_junk, B_gbc])
    if stage >= 4:
        R3.reset()
        gslot = [[R3.alloc([128, 16, 128], BF16) for _ in range(2)] for _ in range(2)]
        uslot = [[R3.alloc([128, 8, 128], BF16) for _ in range(2)] for _ in range(2)]
        B_gs = [Buf("gs%d" % i) for i in range(2)]
        S_gs = [new_sem() for _ in range(2)]
        mergedT = R3.alloc([128, 16, 1024], BF16)
        B_merged = [Buf("merged%d" % i) for i in range(16)]
        sga = [R3.alloc([128, 512], F32) for _ in range(2)]
        sgb = [R3.alloc([128, 512], F32) for _ in range(2)]
        tmul = [R3.alloc([128, 512], F32) for _ in range(2)]
        B_sga = [Buf("sga%d" % i) for i in range(2)]
        B_sgb = [Buf("sgb%d" % i) for i in range(2)]
        B_tmul = [Buf("tmul%d" % i) for i in range(2)]
        NWO = 3
        woslot = [R3.alloc([128, 16, 256], BF16) for _ in range(NWO)]
        B_wo = [Buf("wo%d" % i) for i in range(NWO)]
        S_wo = [new_sem() for _ in range(NWO)]
        assert R3.off <= R3.size
        barrier(all_attn_bufs, B_gs + B_merged + B_sga + B_sgb + B_tmul + B_wo)
        first = [True]
        wus_v = w_up_swa.rearrange("(k p) n -> p k n", p=128)
        wum_v = w_up_moba.rearrange("(k p) n -> p k n", p=128)
        wo_v = w_out.rearrange("(k p) n -> p k n", p=128)
        cnt4 = [0]
        for c in range(16):
            st_ = c % 2
            extra = []

            def _ld(e, c=c, st_=st_):
                return [e.dma_start(out=gslot[st_][0], in_=w_in_v[:, :, C_GA + c * 128:C_GA + (c + 1) * 128]),
                        e.dma_start(out=gslot[st_][1], in_=w_in_v[:, :, C_GB + c * 128:C_GB + (c + 1) * 128]),
                        e.dma_start(out=uslot[st_][0], in_=wus_v[:, :, c * 128:(c + 1) * 128]),
                        e.dma_start(out=uslot[st_][1], in_=wum_v[:, :, c * 128:(c + 1) * 128])]
            prog.add("pool", _ld, writes=[B_gs[st_]] + extra, dsem=S_gs[st_], ndma=4)
            for n in range(2):
                alt = cnt4[0] % 2
                cnt4[0] += 1
                bga, bgb, bya, byb = ((0, 1, 3, 4), (2, 5, 6, 7))[alt]
                tl = n * 512
                for (bank, wt) in ((bga, gslot[st_][0]), (bgb, gslot[st_][1])):
                    for k in range(16):
                        prog.add("pe", lambda e, k=k, bank=bank, wt=wt, tl=tl: e.matmul(
                            PS(bank), lhsT=wt[:, k, :], rhs=hTo[:, k, tl:tl + 512], start=(k == 0), stop=(k == 15)),
                            reads=[B_gs[st_], B_hTo], writes=[B_ps[bank]])
                for (bank, wt, at_, Bat) in ((bya, uslot[st_][0], attn_swa, B_attn_swa), (byb, uslot[st_][1], attn_moba, B_attn_moba)):
                    for k in range(8):
                        prog.add("pe", lambda e, k=k, bank=bank, wt=wt, at_=at_, tl=tl: e.matmul(
                            PS(bank), lhsT=wt[:, k, :], rhs=at_[:, k, tl:tl + 512], start=(k == 0), stop=(k == 7)),
                            reads=[B_gs[st_]] + Bat, writes=[B_ps[bank]])
                prog.add("act", lambda e, bga=bga, alt=alt: e.activation(out=sga[alt], in_=PS(bga), func=AF.Sigmoid),
                         reads=[B_ps[bga]], writes=[B_sga[alt]])
                prog.add("act", lambda e, bgb=bgb, alt=alt: e.activation(out=sgb[alt], in_=PS(bgb), func=AF.Sigmoid),
                         reads=[B_ps[bgb]], writes=[B_sgb[alt]])
                prog.add("dve", lambda e, bya=bya, alt=alt: e.tensor_tensor(out=tmul[alt], in0=PS(bya), in1=sga[alt], op=ALU.mult),
                         reads=[B_ps[bya], B_sga[alt]], writes=[B_tmul[alt]])
                prog.add("dve", lambda e, byb=byb, alt=alt: e.tensor_tensor(out=sgb[alt], in0=PS(byb), in1=sgb[alt], op=ALU.mult),
                         reads=[B_ps[byb], B_sgb[alt]], writes=[B_sgb[alt]])
                prog.add("dve", lambda e, c=c, tl=tl, alt=alt: e.tensor_tensor(out=mergedT[:, c, tl:tl + 512], in0=tmul[alt],
                                                                               in1=sgb[alt], op=ALU.add),
                         reads=[B_tmul[alt], B_sgb[alt]], writes=[B_merged[c]])
        if debug and stage == 4:
            dbg_outs["mergedT"] = (mergedT, [128, 16 * 1024], BF16, B_merged)

    if stage >= 5:
        R1.reset()
        x1 = R1.alloc([128, 8, D], F32)
        B_x1 = [Buf("x1_%d" % t) for t in range(8)]
        S_x1 = new_sem()
        xs_own = xs[NOWN:NKV, :].rearrange("(t p) d -> p t d", p=128)

        def _ldx(e):
            return [e.dma_start(out=x1[:, t, :], in_=xs_own[:, t, :]) for t in range(8)]
        prog.add("sp", _ldx, writes=B_x1 + [B_hTp, B_hTo], dsem=S_x1, ndma=8)
        ob2 = [0]
        for cg in range(8):
            s = cg % NWO
            prog.add("pool", lambda e, cg=cg, s=s: e.dma_start(out=woslot[s], in_=wo_v[:, :, cg * 256:(cg + 1) * 256]),
                     writes=[B_wo[s]], dsem=S_wo[s])
            for t in range(8):
                bank = ob2[0] % 8
                ob2[0] += 1
                for k in range(16):
                    prog.add("pe", lambda e, k=k, t=t, s=s, bank=bank: e.matmul(
                        PS(bank, 0, 256), lhsT=mergedT[:, k, t * 128:(t + 1) * 128], rhs=woslot[s][:, k, :],
                        start=(k == 0), stop=(k == 15)), reads=[B_wo[s]] + B_merged, writes=[B_ps[bank]])
                prog.add("dve", lambda e, t=t, cg=cg, bank=bank: e.tensor_tensor(
                    out=x1[:, t, cg * 256:(cg + 1) * 256], in0=PS(bank, 0, 256), in1=x1[:, t, cg * 256:(cg + 1) * 256],
                    op=ALU.add), reads=[B_ps[bank], B_x1[t]], writes=[B_x1[t]])
        if debug and stage == 5:
            dbg_outs["x1"] = (x1, [128, 8 * D], F32, B_x1)

    if stage >= 6:
        R2.reset()
        h2T = R2.alloc([128, 16, 1024], BF16)
        B_h2T = Buf("h2T")
        phaseB_bufs = B_gs + B_merged + B_sga + B_sgb + B_tmul + B_wo
        R3.reset()
        NWE = 4
        ering = [R3.alloc([128, 8192], BF16) for _ in range(NWE)]
        B_er = [Buf("er%d" % i) for i in range(NWE)]
        S_er = [new_sem() for _ in range(NWE)]
        hidT = [R3.alloc([128, 4, 1024], BF16) for _ in range(2)]
        B_hid = [[Buf("hid%d_%d" % (i, n)) for n in range(2)] for i in range(2)]
        sg = [R3.alloc([128, 512], F32) for _ in range(2)]
        B_sg = [Buf("sg%d" % i) for i in range(2)]
        comb = R3.alloc([128, 8, 16], F32)
        B_comb = Buf("comb")
        wr_sb = R3.alloc([128, 16, 20], F32)
        B_wr = Buf("wr")
        S_wr = new_sem()
        L_all = R3.alloc([128, 8, 20], F32)
        B_L = Buf("L")
        ss2 = R3.alloc([128, 8], F32)
        ln2 = R3.alloc([128, 8], F32)
        rs2 = R3.alloc([128, 8], F32)
        B_ss2l = [Buf("ss2_%d" % t) for t in range(8)]
        B_ss2 = Buf("ss2")
        rt_small = [R3.alloc([128, 8, 16], F32) for _ in range(8)]
        B_rts = Buf("rts")
        moe_end = R3.off
        assert R3.off <= R3.size, (R3.off, R3.size)
        gbc2 = R3.alloc([128, D], F32, at=16384)
        h2f = [R3.alloc([128, D], F32, at=16384 + 8192 + i * 8192) for i in range(2)]
        B_h2f = [Buf("h2f%d" % i) for i in range(2)]
        h2Tf = [R3.alloc([128, 16, 128], F32, at=16384 + 3 * 8192 + i * 8192) for i in range(2)]
        B_h2Tf = [Buf("h2Tf%d" % i) for i in range(2)]
        junk2 = R3.alloc([128, D], BF16, at=16384 + 5 * 8192)
        B_junk2 = Buf("junk2")
        B_gbc2 = Buf("gbc2")
        S_gbc2 = new_sem()
        B_scrC = [B_er[1], B_er[2], B_er[3]]

        barrier(phaseB_bufs, B_er + [b for bb in B_hid for b in bb] + B_sg + [B_comb, B_wr, B_L, B_ss2, B_rts, B_gbc2, B_junk2] +
                B_h2f + B_h2Tf)
        prog.add("sp", lambda e: e.dma_start(out=gbc2, in_=gffn_bc[:, :]), writes=[B_gbc2], dsem=S_gbc2)
        prog.add("sp", lambda e: e.dma_start(out=wr_sb, in_=w_r.rearrange("(k p) n -> p k n", p=128)),
                 writes=[B_wr], dsem=S_wr)
        prog.add("dve", lambda e: e.memset(ss2, 0.0), writes=[B_ss2] + B_ss2l)
        pend_router = [None]
        for t in range(8):
            i2 = t % 2
            prog.add("act", lambda e, t=t: e.activation(out=junk2, in_=x1[:, t, :], func=AF.Square, accum_out=ss2[:, t:t + 1]),
                     reads=[B_x1[t], B_ss2l[t], B_gbc2], writes=[B_junk2, B_ss2l[t]])
            prog.add("act", lambda e, t=t: e.activation(out=ln2[:, t:t + 1], in_=ss2[:, t:t + 1], func=AF.Ln, scale=1.0 / D,
                                                        bias=epscol[:, 0:1]), reads=[B_ss2l[t], B_eps], writes=[B_ss2l[t]])
            prog.add("act", lambda e, t=t: e.activation(out=rs2[:, t:t + 1], in_=ln2[:, t:t + 1], func=AF.Exp, scale=-0.5),
                     reads=[B_ss2l[t]], writes=[B_ss2l[t]])
            prog.add("dve", lambda e, t=t, i2=i2: e.scalar_tensor_tensor(
                out=h2f[i2], in0=x1[:, t, :], scalar=rs2[:, t:t + 1], op0=ALU.mult, in1=gbc2, op1=ALU.mult),
                reads=[B_x1[t], B_ss2l[t], B_gbc2], writes=[B_h2f[i2]])
            for q4 in range(4):
                bank = (t * 4 + q4) % 4
                for kk in range(4):
                    k = q4 * 4 + kk
                    prog.add("pe", lambda e, k=k, kk=kk, bank=bank, i2=i2: e.transpose(
                        out=PS(bank, kk * 128, (kk + 1) * 128), in_=h2f[i2][:, k * 128:(k + 1) * 128], identity=ident_f),
                        reads=[B_h2f[i2], B_const], writes=[B_ps[bank]])
                prog.add("act", lambda e, q4=q4, bank=bank, i2=i2: e.activation(
                    out=h2Tf[i2][:, q4 * 4:(q4 + 1) * 4, :], in_=PS(bank).rearrange("p (a b) -> p a b", b=128), func=AF.Copy),
                    reads=[B_ps[bank]], writes=[B_h2Tf[i2]])
                prog.add("dve", lambda e, q4=q4, i2=i2, t=t: e.tensor_copy(
                    out=h2T[:, q4 * 4:(q4 + 1) * 4, t * 128:(t + 1) * 128], in_=h2Tf[i2][:, q4 * 4:(q4 + 1) * 4, :]),
                    reads=[B_h2Tf[i2]], writes=[B_h2T] + (B_attn_swa + B_attn_moba if (t == 0 and q4 == 0) else []))
            def router(t=t, i2=i2):
                for k in range(16):
                    prog.add("pe", lambda e, k=k: e.matmul(PS(4 + t % 2, 0, 20), lhsT=h2Tf[i2][:, k, :], rhs=wr_sb[:, k, :],
                                                           start=(k == 0), stop=(k == 15)),
                             reads=[B_h2Tf[i2], B_wr], writes=[B_ps[4 + t % 2]])
                prog.add("dve", lambda e: e.tensor_copy(out=L_all[:, t, :], in_=PS(4 + t % 2, 0, 20)),
                         reads=[B_ps[4 + t % 2]], writes=[B_L])
            if pend_router[0] is not None:
                pend_router[0]()
            pend_router[0] = router
        pend_router[0]()
        lg = L_all[:, :, 0:4]
        le = L_all[:, :, 4:20]
        mg, ohg, tmp16, sl, l1, msk, l2, ex, exm, den, gp, sumg, wexp, junk8 = (None,) * 14
        mg = rt_small[0][:, :, 0:1]
        ohg = rt_small[0][:, :, 4:8]
        sumg = rt_small[0][:, :, 8:9]
        gp = rt_small[0][:, :, 9:10]
        l1 = rt_small[0][:, :, 10:11]
        l2 = rt_small[0][:, :, 11:12]
        den = rt_small[0][:, :, 12:13]
        fac = rt_small[0][:, :, 13:14]
        tmp16 = rt_small[1]
        sl = rt_small[2][:, :, 0:4]
        msk = rt_small[2][:, :, 4:8]
        sl2 = rt_small[2][:, :, 8:12]
        ex = rt_small[3][:, :, 0:4]
        exm = rt_small[3][:, :, 4:8]
        wexp = rt_small[3][:, :, 8:12]
        eg = rt_small[4][:, :, 0:4]
        dgl = rt_small[4][:, :, 4:8]
        dsl = rt_small[4][:, :, 8:12]

        def dv(fn, r=(B_L, B_rts), w=(B_rts,)):
            prog.add("dve", fn, reads=list(r), writes=list(w))
        dv(lambda e: e.tensor_reduce(out=mg, in_=lg, axis=AX.X, op=ALU.max))
        dv(lambda e: e.tensor_tensor(out=ohg, in0=lg, in1=mg.to_broadcast([128, 8, 4]), op=ALU.is_ge))
        dv(lambda e: e.tensor_tensor(out=dgl, in0=lg, in1=mg.to_broadcast([128, 8, 4]), op=ALU.subtract))
        prog.add("act", lambda e: e.activation(out=eg, in_=dgl, func=AF.Exp), reads=[B_rts], writes=[B_rts])
        dv(lambda e: e.tensor_reduce(out=sumg, in_=eg, axis=AX.X, op=ALU.add))
        dv(lambda e: e.reciprocal(out=gp, in_=sumg))
        dv(lambda e: e.tensor_tensor(out=tmp16.rearrange("p t (g e) -> p t g e", e=4),
                                     in0=le.rearrange("p t (g e) -> p t g e", e=4),
                                     in1=ohg.unsqueeze(3).to_broadcast([128, 8, 4, 4]), op=ALU.mult))
        dv(lambda e: e.tensor_reduce(out=sl, in_=tmp16.rearrange("p t (g e) -> p t e g", e=4), axis=AX.X, op=ALU.add))
        dv(lambda e: e.tensor_reduce(out=l1, in_=sl, axis=AX.X, op=ALU.max))
        dv(lambda e: e.tensor_tensor(out=msk, in0=sl, in1=l1.to_broadcast([128, 8, 4]), op=ALU.is_ge))
        dv(lambda e: e.scalar_tensor_tensor(out=sl2, in0=msk, scalar=-1e30, op0=ALU.mult, in1=sl, op1=ALU.add))
        dv(lambda e: e.tensor_reduce(out=l2, in_=sl2, axis=AX.X, op=ALU.max))
        dv(lambda e: e.tensor_tensor(out=msk, in0=sl, in1=l2.to_broadcast([128, 8, 4]), op=ALU.is_ge))
        dv(lambda e: e.tensor_tensor(out=dsl, in0=sl, in1=l1.to_broadcast([128, 8, 4]), op=ALU.subtract))
        prog.add("act", lambda e: e.activation(out=ex, in_=dsl, func=AF.Exp), reads=[B_rts], writes=[B_rts])
        dv(lambda e: e.tensor_tensor(out=exm, in0=ex, in1=msk, op=ALU.mult))
        dv(lambda e: e.tensor_reduce(out=den, in_=exm, axis=AX.X, op=ALU.add))
        dv(lambda e: e.reciprocal(out=fac, in_=den))
        dv(lambda e: e.tensor_tensor(out=fac, in0=fac, in1=gp, op=ALU.mult))
        dv(lambda e: e.tensor_tensor(out=wexp, in0=exm, in1=fac.to_broadcast([128, 8, 4]), op=ALU.mult))
        dv(lambda e: e.tensor_tensor(out=comb.rearrange("p t (g e) -> p t g e", e=4),
                                     in0=ohg.unsqueeze(3).to_broadcast([128, 8, 4, 4]),
                                     in1=wexp.unsqueeze(2).to_broadcast([128, 8, 4, 4]), op=ALU.mult),
           w=(B_rts, B_comb))
        if debug and stage == 6:
            dbg_outs["h2T"] = (h2T, [128, 16 * 1024], BF16, [B_h2T])
            dbg_outs["L_all"] = (L_all, [128, 8 * 20], F32, [B_L])
            dbg_outs["comb"] = (comb, [128, 8 * 16], F32, [B_comb])

    if stage >= 7:
        er = [0]

        def load_e(dram_ap, shape3):
            s = er[0] % NWE
            er[0] += 1
            view = ering[s].rearrange("p (a b) -> p a b", b=shape3[2])
            extra = B_scrC_users if s in (1, 2, 3) and er[0] <= NWE else []
            prog.add("pool", lambda e: e.dma_start(out=view, in_=dram_ap), writes=[B_er[s]] + extra, dsem=S_er[s])
            return s, view
        B_scrC_users = [B_gbc2, B_junk2] + B_h2f + B_h2Tf
        gu = [0]
        yb = [0]
        for ex_i in range(16):
            hb = ex_i % 2
            s_g, Wg = load_e(w_gate_e[ex_i].rearrange("(k p) f -> p k f", p=128), [128, 16, 512])
            s_u, Wu = load_e(w_up_e[ex_i].rearrange("(k p) f -> p k f", p=128), [128, 16, 512])
            s_d, Wd = load_e(w_down_e[ex_i].rearrange("(k p) d -> p k d", p=128), [128, 4, 2048])
            for n in range(2):
                for fc in range(4):
                    alt = gu[0] % 2
                    gu[0] += 1
                    bg, bu = (0, 1) if alt == 0 else (2, 3)
                    for (bank, W_, s_) in ((bg, Wg, s_g), (bu, Wu, s_u)):
                        for k in range(16):
                            prog.add("pe", lambda e, k=k, bank=bank, W_=W_, fc=fc, n=n: e.matmul(
                                PS(bank), lhsT=W_[:, k, fc * 128:(fc + 1) * 128], rhs=h2T[:, k, n * 512:(n + 1) * 512],
                                start=(k == 0), stop=(k == 15)), reads=[B_er[s_], B_h2T], writes=[B_ps[bank]])
                    prog.add("act", lambda e, bg=bg, alt=alt: e.activation(out=sg[alt], in_=PS(bg), func=AF.Silu),
                             reads=[B_ps[bg]], writes=[B_sg[alt]])
                    prog.add("dve", lambda e, bu=bu, alt=alt, hb=hb, fc=fc, n=n: e.tensor_tensor(
                        out=hidT[hb][:, fc, n * 512:(n + 1) * 512], in0=PS(bu), in1=sg[alt], op=ALU.mult),
                        reads=[B_ps[bu], B_sg[alt]], writes=[B_hid[hb][n]])
            for t in range(8):
                for half in range(2):
                    b0 = 4 + 2 * (yb[0] % 2)
                    yb[0] += 1
                    for bb in range(2):
                        col = half * 1024 + bb * 512
                        for fc in range(4):
                            prog.add("pe", lambda e, fc=fc, t=t, col=col, bank=b0 + bb, hb=hb, Wd=Wd: e.matmul(
                                PS(bank), lhsT=hidT[hb][:, fc, t * 128:(t + 1) * 128], rhs=Wd[:, fc, col:col + 512],
                                start=(fc == 0), stop=(fc == 3)), reads=[B_er[s_d], B_hid[hb][t // 4]],
                                writes=[B_ps[b0 + bb]])
                    prog.add("dve", lambda e, t=t, half=half, b0=b0, ex_i=ex_i: e.scalar_tensor_tensor(
                        out=x1[:, t, half * 1024:(half + 1) * 1024], in0=psum[:, b0 * 512:(b0 + 2) * 512],
                        scalar=comb[:, t, ex_i:ex_i + 1], op0=ALU.mult, in1=x1[:, t, half * 1024:(half + 1) * 1024],
                        op1=ALU.add), reads=[B_ps[b0], B_ps[b0 + 1], B_comb, B_x1[t]], writes=[B_x1[t]])
        B_y = Buf("y")
        S_y = new_sem()
        y_v = y.rearrange("(t p) d -> p t d", p=128)
        for t in range(8):
            prog.add("sp", lambda e, t=t: e.dma_start(out=y_v[:, t, :], in_=x1[:, t, :]), reads=[B_x1[t]], writes=[B_y],
                     dsem=S_y)
        prog.add("sp", None, reads=[B_y])

    if debug:
        B_dbg = Buf("dbg")
        S_dbg = new_sem()
        for name, (ap, shape, dt, bufs) in dbg_outs.items():
            o = nc.dram_tensor("dbg_" + name, list(shape), dt, kind="ExternalOutput").ap()
            src = ap
            if len(ap.shape) == 3:
                src = ap.rearrange("p a b -> p (a b)")
            prog.add("sp", lambda e, o=o, src=src: e.dma_start(out=o[:, :], in_=src), reads=bufs, writes=[B_dbg], dsem=S_dbg)
        prog.add("sp", None, reads=[B_dbg])
        if stage < 7:
            pass

    block = es.enter_context(nc.Block())
    prog.emit(block, engsem)
    es.close()
    return nc, list(dbg_outs.keys())


_CONSTS = None


def _prepare_inputs(inputs, cores):
    global _CONSTS
    if _CONSTS is None:
        _CONSTS = _const_tables()
    c = _CONSTS
    f = lambda a: np.ascontiguousarray(np.asarray(a, dtype=np.float32))
    x = f(inputs["x"])
    shared = {
        "w_in": f(inputs["w_in"]), "w_up_swa": f(inputs["w_up_swa"]), "w_up_moba": f(inputs["w_up_moba"]),
        "w_out": f(inputs["w_out"]),
        "w_r": np.ascontiguousarray(np.concatenate(
            [f(inputs["w_router_group"]), f(inputs["w_router_expert"]).transpose(1, 0, 2).reshape(D, 16)], axis=1)),
        "w_gate_e": f(inputs["w_gate_e"]), "w_up_e": f(inputs["w_up_e"]), "w_down_e": f(inputs["w_down_e"]),
        "gmix_bc": np.ascontiguousarray(np.broadcast_to(f(inputs["g_mix"])[None, :], (128, D))),
        "gffn_bc": np.ascontiguousarray(np.broadcast_to(f(inputs["g_ffn"])[None, :], (128, D))),
        "gcols": np.ascontiguousarray(np.stack(
            [np.tile(f(inputs[k]), 2) for k in ("q_norm_swa", "k_norm_swa", "q_norm_moba", "k_norm_moba")], axis=1)),
        "sinks_bc": np.ascontiguousarray(np.broadcast_to(f(inputs["sinks"])[None, :], (128, 16))),
        "ident_bf": c["ident_bf"], "ident_f": c["ident_f"], "bd64": c["bd64"], "dsw": c["dsw"], "mneg": c["mneg"],
        "tabq": c["tabq"], "tabk": c["tabk"],
    }
    in_maps = []
    for cid in cores:
        b, hf = cid // 2, cid % 2
        if hf == 1:
            xs_ = x[b]
        else:
            xs_ = np.concatenate([x[b, NOWN:], x[b, :NOWN]], axis=0)
        m = dict(shared)
        m["xs"] = np.ascontiguousarray(xs_)
        m.update(_percore_tables(hf))
        in_maps.append(m)
    return in_maps


_NC_CACHE = {}


def kernel(**inputs):
    if "full" not in _NC_CACHE:
        _NC_CACHE["full"] = build_program(stage=99, debug=False)[0]
    nc = _NC_CACHE["full"]
    cores = list(range(8))
    in_maps = _prepare_inputs(inputs, cores)
    res = run_bass_kernel_spmd(nc, in_maps, core_ids=cores)
    out = np.empty((4, 2048, D), np.float32)
    for cid in cores:
        b, hf = cid // 2, cid % 2
        out[b, hf * NOWN:(hf + 1) * NOWN, :] = res.results[cid]["y"]
    return out
```

```python
import numpy as np
import ml_dtypes
from contextlib import ExitStack
import concourse.bass as bass
import concourse.mybir as mybir
from concourse.bass_utils import run_bass_kernel_spmd

F32 = mybir.dt.float32
BF16 = mybir.dt.bfloat16
U8 = mybir.dt.uint8
ALU = mybir.AluOpType
AF = mybir.ActivationFunctionType
AX = mybir.AxisListType
NPBF = ml_dtypes.bfloat16

D = 2048
NOWN = 1024
NKV = 2048
EPS = 1e-6
SCALE = 0.125
BIG = 32768.0
IN_COLS = 8448
C_QA, C_KA, C_VA, C_QB, C_KB, C_VB, C_GA, C_GB = 0, 1024, 1152, 1280, 2304, 3328, 4352, 6400
SLOPES = [2.0 ** (-(h + 1) / 2.0) for h in range(16)]
ARENA = 211968


class Buf:
    __slots__ = ("name", "lw", "lr", "excl")

    def __init__(self, name, excl=False):
        self.name = name
        self.lw = None
        self.lr = {}
        self.excl = excl


class DSem:
    def __init__(self, h):
        self.h = h
        self.count = 0


class Op:
    __slots__ = ("eng", "fn", "deps", "signal", "dsem", "val", "idx")


ENGS = ("pe", "act", "dve", "pool", "sp")


class Prog:
    def __init__(self):
        self.ops = []

    @staticmethod
    def _need(p, eng, is_dma, raw):
        if p.dsem is not None or is_dma:
            return True
        if p.eng != eng:
            return True
        if eng == "pe":
            return False
        return raw

    def add(self, eng, fn, reads=(), writes=(), dsem=None, ndma=1):
        idx = len(self.ops)
        op = Op()
        op.eng, op.fn, op.signal, op.dsem, op.idx, op.val = eng, fn, False, dsem, idx, None
        is_dma = dsem is not None
        key = ("d", idx) if is_dma else eng
        deps = set()
        for b in reads:
            w = b.lw
            if w is not None and self._need(self.ops[w], eng, is_dma, True):
                deps.add(w)
            if b.excl:
                for k2, r in b.lr.items():
                    if k2 != key:
                        deps.add(r)
        for b in writes:
            w = b.lw
            if w is not None and self._need(self.ops[w], eng, is_dma, False):
                deps.add(w)
            for r in b.lr.values():
                if self._need(self.ops[r], eng, is_dma, False):
                    deps.add(r)
        for b in reads:
            b.lr[key] = idx
        for b in writes:
            b.lw = idx
            b.lr = {}
        for d in deps:
            self.ops[d].signal = True
        op.deps = sorted(deps)
        if is_dma:
            dsem.count += 16 * ndma
            op.val = dsem.count
        self.ops.append(op)
        return op

    def emit(self, block, engsem):
        cnt = {}
        for op in self.ops:
            if op.dsem is None and op.signal:
                cnt[op.eng] = cnt.get(op.eng, 0) + 1
                op.val = cnt[op.eng]
        by = {e: [] for e in ENGS}
        for op in self.ops:
            by[op.eng].append(op)
        ops = self.ops

        def run(name):
            def body(e):
                waited = {}
                for op in by[name]:
                    for d in op.deps:
                        p = ops[d]
                        if p.dsem is not None:
                            sem, k = p.dsem.h, ("d", id(p.dsem))
                        else:
                            sem, k = engsem[p.eng], p.eng
                        if waited.get(k, 0) < p.val:
                            e.wait_ge(sem, p.val)
                            waited[k] = p.val
                    if op.fn is None:
                        continue
                    r = op.fn(e)
                    if op.dsem is not None:
                        for ins in (r if isinstance(r, (list, tuple)) else [r]):
                            ins.then_inc(op.dsem.h, 16)
                    elif op.signal:
                        r.then_inc(engsem[op.eng], 1)
            return body

        block.tensor(run("pe"))
        block.scalar(run("act"))
        block.vector(run("dve"))
        block.gpsimd(run("pool"))
        block.sync(run("sp"))


def _split3(a):
    a = a.astype(np.float64)
    hi = a.astype(NPBF)
    r = a - hi.astype(np.float64)
    mid = r.astype(NPBF)
    r = r - mid.astype(np.float64)
    lo = r.astype(NPBF)
    return hi, mid, lo


def _const_tables():
    c = {}
    c["ident_bf"] = np.eye(128, dtype=np.float32).astype(NPBF)
    c["ident_f"] = np.eye(128, dtype=np.float32)
    bd = np.zeros((128, 128), np.float32)
    bd[:64, :64] = 1.0 / 64
    bd[64:, 64:] = 1.0 / 64
    c["bd64"] = bd.astype(NPBF)
    k = np.arange(128)[:, None].astype(np.float64)
    q = np.arange(128)[None, :].astype(np.float64)
    da = q - k
    da = np.where(da >= 0, da, 1e9)
    db = q + 128 - k
    db = np.where(db < 128, db, 1e9)
    c["dsw"] = np.concatenate([db, da, db, da], axis=1).astype(np.float32)
    q2 = np.arange(256)[None, :]
    m0 = np.where(q2 >= np.arange(128)[:, None], 0.0, -1e9)
    m1 = np.where(q2 >= (np.arange(128)[:, None] + 128), 0.0, -1e9)
    c["mneg"] = np.concatenate([m0, m1], axis=1).astype(np.float32)
    tq = np.arange(NOWN).astype(np.float64)
    tk = (np.arange(NKV) - 1024).astype(np.float64)
    tabq = np.zeros((16, 6, NOWN), NPBF)
    tabk = np.zeros((16, 14, NKV), NPBF)
    ind = (np.arange(NKV)[None, :] // 256 == np.arange(8)[:, None]).astype(np.float32)
    for h in range(16):
        s = SLOPES[h]
        a = _split3(-s * tq / SCALE)
        b = _split3(s * tk / SCALE)
        for i in range(3):
            tabq[h, i] = a[i]
            tabq[h, 3 + i] = 1.0
            tabk[h, 8 + i] = 1.0
            tabk[h, 11 + i] = b[i]
        tabk[h, 0:8] = ind.astype(NPBF)
    c["tabq"] = tabq
    c["tabk"] = tabk
    return c


def _percore_tables(hf):
    pb = np.full((128, 8, 8), -1e30, np.float32)
    for t in range(8):
        own = 4 + t // 2
        for n in range(8):
            if n == own:
                pb[:, t, n] = 1e30
            elif n < own and (hf == 1 or n >= 4):
                pb[:, t, n] = 0.0
    pv = np.full((128, 1), float(hf), np.float32)
    return {"pastbias": pb.reshape(128, 64), "pvcol": pv}


def build_program(stage=99, debug=False):
    nc = bass.Bass("TRN2", target_bir_lowering=False)
    es = ExitStack()
    prog = Prog()
    dbg_outs = {}

    def din(name, shape, dt):
        return nc.dram_tensor(name, list(shape), dt, kind="ExternalInput").ap()

    xs = din("xs", [NKV, D], F32)
    w_in = din("w_in", [D, IN_COLS], F32)
    w_up_swa = din("w_up_swa", [1024, D], F32)
    w_up_moba = din("w_up_moba", [1024, D], F32)
    w_out = din("w_out", [D, D], F32)
    w_r = din("w_r", [D, 20], F32)
    w_gate_e = din("w_gate_e", [16, D, 512], F32)
    w_up_e = din("w_up_e", [16, D, 512], F32)
    w_down_e = din("w_down_e", [16, 512, D], F32)
    gmix_bc = din("gmix_bc", [128, D], F32)
    gffn_bc = din("gffn_bc", [128, D], F32)
    gcols_d = din("gcols", [128, 4], F32)
    sinks_bc = din("sinks_bc", [128, 16], F32)
    ident_bf_d = din("ident_bf", [128, 128], BF16)
    ident_f_d = din("ident_f", [128, 128], F32)
    bd64_d = din("bd64", [128, 128], BF16)
    dsw_d = din("dsw", [128, 512], F32)
    mneg_d = din("mneg", [128, 512], F32)
    tabq_d = din("tabq", [16, 6, NOWN], BF16)
    tabk_d = din("tabk", [16, 14, NKV], BF16)
    pastbias_d = din("pastbias", [128, 64], F32)
    pvcol_d = din("pvcol", [128, 1], F32)
    y = nc.dram_tensor("y", [NOWN, D], F32, kind="ExternalOutput").ap()

    arena = es.enter_context(nc.sbuf_tensor("arena", [128, ARENA], U8))
    psum = es.enter_context(nc.psum_tensor("psum", [128, 4096], F32))

    def PS(b, lo=0, hi=512):
        return psum[:, b * 512 + lo:b * 512 + hi]

    def PSB(b, lo=0, hi=1024):
        return psum[:, b * 512:(b + 1) * 512].bitcast(BF16)[:, lo:hi]

    B_ps = [Buf("ps%d" % i, excl=True) for i in range(8)]

    class Region:
        def __init__(self, base, size):
            self.base, self.size, self.off = base, size, 0

        def reset(self, off=0):
            self.off = off

        def alloc(self, shape, dt, at=None):
            nb = {F32: 4, BF16: 2}[dt]
            n = int(np.prod(shape[1:])) * nb
            n32 = (n + 31) // 32 * 32
            off = self.off if at is None else at
            assert off + n32 <= self.size, (off, n32, self.size)
            if at is None:
                self.off += n32
            v = arena[:, self.base + off:self.base + off + n].bitcast(dt)
            if len(shape) == 3:
                v = v.rearrange("p (a b) -> p a b", b=shape[2])
            return v

    CONST_SZ = 6144
    R1_SZ = 65536
    R2_SZ = 32768
    RC = Region(0, CONST_SZ)
    R1 = Region(CONST_SZ, R1_SZ)
    R2 = Region(CONST_SZ + R1_SZ, R2_SZ)
    R3 = Region(CONST_SZ + R1_SZ + R2_SZ, ARENA - CONST_SZ - R1_SZ - R2_SZ)

    sem_id = [0]

    def new_sem():
        sem_id[0] += 1
        return DSem(es.enter_context(nc.semaphore("s%d" % sem_id[0])))

    engsem = {e: es.enter_context(nc.semaphore("eng_" + e)) for e in ENGS}

    ident_bf = RC.alloc([128, 128], BF16)
    ident_f = RC.alloc([128, 128], F32)
    bd64 = RC.alloc([128, 128], BF16)
    gcols = RC.alloc([128, 4], F32)
    epscol = RC.alloc([128, 1], F32)
    expsink = RC.alloc([128, 16], F32)
    pvcol = RC.alloc([128, 1], F32)
    pastbias = RC.alloc([128, 64], F32)
    dsw = RC.alloc([128, 512], F32)
    mneg = RC.alloc([128, 512], F32)
    ss1 = RC.alloc([128, 16], F32)
    ln1 = RC.alloc([128, 16], F32)
    rs1 = RC.alloc([128, 16], F32)
    B_const = Buf("const")
    S_const = new_sem()
    const_loads = [(ident_bf, ident_bf_d), (ident_f, ident_f_d), (bd64, bd64_d), (gcols, gcols_d),
                   (expsink, sinks_bc), (pvcol, pvcol_d), (pastbias, pastbias_d), (dsw, dsw_d), (mneg, mneg_d)]

    def _ld_consts(e):
        return [e.dma_start(out=o, in_=i[:, :]) for o, i in const_loads]
    prog.add("sp", _ld_consts, writes=[B_const], dsem=S_const, ndma=len(const_loads))
    B_eps = Buf("eps")
    prog.add("dve", lambda e: e.memset(epscol, EPS), writes=[B_eps])
    B_ss1 = [Buf("ss1_%d" % t) for t in range(16)]
    prog.add("dve", lambda e: e.memset(ss1, 0.0), writes=B_ss1)
    B_expsink = Buf("expsink")
    prog.add("act", lambda e: e.activation(out=expsink, in_=expsink, func=AF.Exp), reads=[B_const], writes=[B_expsink])

    hTp = R1.alloc([128, 16, 1024], BF16)
    hTo = R1.alloc([128, 16, 1024], BF16)
    B_hTp, B_hTo = Buf("hTp"), Buf("hTo")
    attn_swa = R2.alloc([128, 8, 1024], BF16)
    attn_moba = R2.alloc([128, 8, 1024], BF16)
    B_attn_swa = [Buf("attn_swa%d" % i) for i in range(8)]
    B_attn_moba = [Buf("attn_moba%d" % i) for i in range(8)]

    NW = 6
    wslot = [R3.alloc([128, 16, 128], BF16) for _ in range(NW)]
    B_w = [Buf("w%d" % i) for i in range(NW)]
    S_w = [new_sem() for _ in range(NW)]
    QA = [[R3.alloc([128, 1024], BF16) for _ in range(2)] for _ in range(2)]
    B_QA = [[Buf("QA%d%d" % (i, j)) for j in range(2)] for i in range(2)]
    B_QAaug = [[Buf("QAaug%d%d" % (i, j)) for j in range(2)] for i in range(2)]
    S_QAaug = [[new_sem() for j in range(2)] for i in range(2)]
    off_KA = R3.off
    KA = [[R3.alloc([128, 2048], BF16) for _ in range(2)] for _ in range(2)]
    B_KA = [[Buf("KA%d%d" % (i, j)) for j in range(2)] for i in range(2)]
    B_KAaug = [[Buf("KAaug%d%d" % (i, j)) for j in range(2)] for i in range(2)]
    S_KAaug = [[new_sem() for j in range(2)] for i in range(2)]
    VA = [[R3.alloc([128, 16, 128], BF16) for _ in range(2)] for _ in range(2)]
    B_VA = [[Buf("VA%d%d" % (i, j)) for j in range(2)] for i in range(2)]
    KS = [R3.alloc([128, 1152], BF16) for _ in range(2)]
    B_KS = [Buf("KS%d" % i) for i in range(2)]
    VS = [R3.alloc([128, 9, 128], BF16) for _ in range(2)]
    B_VS = [Buf("VS%d" % i) for i in range(2)]
    sqb = [R3.alloc([128, 512], BF16) for _ in range(2)]
    B_sqb = [Buf("sqb%d" % i) for i in range(2)]
    lnb = [R3.alloc([128, 512], F32) for _ in range(2)]
    B_lnb = [Buf("lnb%d" % i) for i in range(2)]
    rsb = [R3.alloc([128, 512], F32) for _ in range(2)]
    B_rsb = [Buf("rsb%d" % i) for i in range(2)]
    tmpB = [R3.alloc([128, 512], BF16) for _ in range(2)]
    B_tmpB = [Buf("tmpB%d" % i) for i in range(2)]
    off_Sp = R3.off
    Sp = [R3.alloc([128, 512], F32) for _ in range(3)]
    B_Sp = [Buf("Sp%d" % i) for i in range(3)]
    NPT = 4
    Pt = [R3.alloc([128, 512], BF16) for _ in range(NPT)]
    B_Pt = [Buf("Pt%d" % i) for i in range(NPT)]
    Rt = [R3.alloc([128, 256], F32) for _ in range(2)]
    B_Rt = [Buf("Rt%d" % i) for i in range(2)]
    Rt2 = [R3.alloc([128, 256], F32) for _ in range(2)]
    tmpo = [R3.alloc([128, 256], BF16) for _ in range(2)]
    B_tmpo = [Buf("tmpo%d" % i) for i in range(2)]
    gm = R3.alloc([128, 8, 8], F32)
    m8 = R3.alloc([128, 8, 8], F32)
    selt = R3.alloc([128, 8, 8], F32)
    kmf = R3.alloc([128, 8], F32)
    kmb = R3.alloc([128, 8], BF16)
    B_gm, B_m8, B_selt, B_kmf, B_kmb = Buf("gm"), Buf("m8"), Buf("selt"), Buf("kmf"), Buf("kmb")
    stage_t = [R3.alloc([128, 8, 72], BF16) for _ in range(2)]
    B_stage = [Buf("stage%d" % i) for i in range(2)]
    sId = [R3.alloc([128, 128], F32) for _ in range(2)]
    B_sId = [Buf("sId%d" % i) for i in range(2)]
    attn_end = R3.off
    xt = [R3.alloc([128, D], F32, at=off_KA + i * 8192) for i in range(3)]
    B_xt = [Buf("xt%d" % i) for i in range(3)]
    S_xt = [new_sem() for _ in range(3)]
    gbc = R3.alloc([128, D], F32, at=off_KA + 3 * 8192)
    xn = [R3.alloc([128, D], BF16, at=off_KA + 4 * 8192 + i * 4096) for i in range(2)]
    B_xn = [Buf("xn%d" % i) for i in range(2)]
    junk = R3.alloc([128, D], BF16, at=off_Sp)
    B_junk = Buf("junk")
    B_gbc = Buf("gbc")
    S_gbc = new_sem()
    bar_scr = RC.alloc([128, 8], F32)

    def barrier(old, new):
        prog.add("dve", lambda e: e.memset(bar_scr, 0.0), writes=list(old) + list(new))

    prog.add("sp", lambda e: e.dma_start(out=gbc, in_=gmix_bc[:, :]), writes=[B_gbc], dsem=S_gbc)
    for t in range(16):
        sl = t % 3
        x2 = t % 2
        prog.add("sp", lambda e, t=t, sl=sl: e.dma_start(out=xt[sl], in_=xs[t * 128:(t + 1) * 128, :]),
                 writes=[B_xt[sl]], dsem=S_xt[sl])
        prog.add("act", lambda e, t=t, sl=sl: e.activation(out=junk, in_=xt[sl], func=AF.Square,
                                                           accum_out=ss1[:, t:t + 1]),
                 reads=[B_xt[sl], B_ss1[t]], writes=[B_junk, B_ss1[t]])
        prog.add("act", lambda e, t=t: e.activation(out=ln1[:, t:t + 1], in_=ss1[:, t:t + 1], func=AF.Ln,
                                                    scale=1.0 / D, bias=epscol[:, 0:1]),
                 reads=[B_ss1[t], B_eps], writes=[B_ss1[t]])
        prog.add("act", lambda e, t=t: e.activation(out=rs1[:, t:t + 1], in_=ln1[:, t:t + 1], func=AF.Exp, scale=-0.5),
                 reads=[B_ss1[t]], writes=[B_ss1[t]])
        prog.add("dve", lambda e, t=t, sl=sl, x2=x2: e.scalar_tensor_tensor(
            out=xn[x2], in0=xt[sl], scalar=rs1[:, t:t + 1], op0=ALU.mult, in1=gbc, op1=ALU.mult),
            reads=[B_xt[sl], B_ss1[t], B_gbc], writes=[B_xn[x2]])
        dst = hTp if t < 8 else hTo
        Bdst = B_hTp if t < 8 else B_hTo
        tc = (t % 8) * 128
        for k in range(16):
            bank = 6 + k // 8
            prog.add("pe", lambda e, k=k, x2=x2, bank=bank: e.transpose(
                out=PSB(bank, (k % 8) * 128, (k % 8 + 1) * 128), in_=xn[x2][:, k * 128:(k + 1) * 128],
                identity=ident_bf), reads=[B_xn[x2], B_const], writes=[B_ps[bank]])
        prog.add("act", lambda e, dst=dst, tc=tc: e.activation(
            out=dst[:, 0:8, tc:tc + 128], in_=PSB(6).rearrange("p (a b) -> p a b", b=128), func=AF.Copy),
            reads=[B_ps[6]], writes=[Bdst])
        prog.add("dve", lambda e, dst=dst, tc=tc: e.tensor_copy(
            out=dst[:, 8:16, tc:tc + 128], in_=PSB(7).rearrange("p (a b) -> p a b", b=128)),
            reads=[B_ps[7]], writes=[Bdst])

    if debug and stage == 1:
        dbg_outs["hTp"] = (hTp, [128, 16 * 1024], BF16, [B_hTp])
        dbg_outs["hTo"] = (hTo, [128, 16 * 1024], BF16, [B_hTo])

    w_in_v = w_in.rearrange("(k p) n -> p k n", p=128)
    wring = [0]

    def load_wcols(c0):
        s = wring[0] % NW
        wring[0] += 1
        prog.add("pool", lambda e: e.dma_start(out=wslot[s], in_=w_in_v[:, :, c0:c0 + 128]),
                 writes=[B_w[s]], dsem=S_w[s])
        return s

    pbank = [0]
    nrm = [0]

    def proj_fm(s, src, Bsrc, lo, n):
        bank = pbank[0] % 2
        pbank[0] += 1
        for k in range(16):
            prog.add("pe", lambda e, k=k: e.matmul(PS(bank, 0, n), lhsT=wslot[s][:, k, :], rhs=src[:, k, lo:lo + n],
                                                   start=(k == 0), stop=(k == 15)),
                     reads=[B_w[s], Bsrc], writes=[B_ps[bank]])
        return bank

    def headnorm(bank, n, gidx, dstA, BA, dstB, BB):
        headnorm_b(headnorm_a(bank, n), bank, n, gidx, dstA, BA, dstB, BB)

    def headnorm_a(bank, n):
        i = nrm[0] % 2
        nrm[0] += 1
        prog.add("act", lambda e: e.activation(out=sqb[i][:, 0:n], in_=PS(bank, 0, n), func=AF.Square),
                 reads=[B_ps[bank]], writes=[B_sqb[i]])
        return i

    def headnorm_b(i, bank, n, gidx, dstA, BA, dstB, BB):
        prog.add("pe", lambda e: e.matmul(PS(2, 0, n), lhsT=bd64, rhs=sqb[i][:, 0:n], start=True, stop=True),
                 reads=[B_sqb[i], B_const], writes=[B_ps[2]])
        prog.add("act", lambda e: e.activation(out=lnb[i][:, 0:n], in_=PS(2, 0, n), func=AF.Ln, bias=epscol[:, 0:1]),
                 reads=[B_ps[2], B_eps], writes=[B_lnb[i]])
        prog.add("act", lambda e: e.activation(out=rsb[i][:, 0:n], in_=lnb[i][:, 0:n], func=AF.Exp, scale=-0.5),
                 reads=[B_lnb[i]], writes=[B_rsb[i]])
        prog.add("dve", lambda e: e.scalar_tensor_tensor(
            out=dstA, in0=PS(bank, 0, n)[0:64, :], scalar=gcols[0:64, gidx:gidx + 1], op0=ALU.mult,
            in1=rsb[i][0:64, 0:n], op1=ALU.mult), reads=[B_ps[bank], B_rsb[i], B_const], writes=[BA])
        prog.add("dve", lambda e: e.scalar_tensor_tensor(
            out=tmpB[i][64:128, 0:n], in0=PS(bank, 0, n)[64:128, :], scalar=gcols[64:128, gidx:gidx + 1], op0=ALU.mult,
            in1=rsb[i][64:128, 0:n], op1=ALU.mult), reads=[B_ps[bank], B_rsb[i], B_const], writes=[B_tmpB[i]])
        prog.add("dve", lambda e: e.tensor_copy(out=dstB, in_=tmpB[i][64:128, 0:n]),
                 reads=[B_tmpB[i]], writes=[BB])

    sbank = [0]
    swa_sb = [0]
    obank = [0]
    ptc = [0]
    rtc = [0]

    def finish_block(ob, nq, dst_pair, Bdst, parity, qlo, sink_h=None):
        r = rtc[0] % 2
        rtc[0] += 1
        if sink_h is not None:
            prog.add("act", lambda e: e.activation(out=Rt2[r][64:128, 0:nq], in_=PS(ob, 0, nq)[64:128, :], func=AF.Ln,
                                                   bias=expsink[64:128, sink_h:sink_h + 1]),
                     reads=[B_ps[ob], B_expsink], writes=[B_Rt[r]])
        else:
            prog.add("act", lambda e: e.activation(out=Rt2[r][64:128, 0:nq], in_=PS(ob, 0, nq)[64:128, :], func=AF.Ln),
                     reads=[B_ps[ob]], writes=[B_Rt[r]])
        prog.add("act", lambda e: e.activation(out=Rt2[r][64:128, 0:nq], in_=Rt2[r][64:128, 0:nq], func=AF.Exp, scale=-1.0),
                 reads=[B_Rt[r]], writes=[B_Rt[r]])
        prog.add("dve", lambda e: e.tensor_copy(out=Rt[r][0:64, 0:nq], in_=Rt2[r][64:128, 0:nq]),
                 reads=[B_Rt[r]], writes=[B_Rt[r]])
        if parity == 0:
            prog.add("dve", lambda e: e.tensor_tensor(out=dst_pair[0:64, qlo:qlo + nq], in0=PS(ob, 0, nq)[0:64, :],
                                                      in1=Rt[r][0:64, 0:nq], op=ALU.mult),
                     reads=[B_ps[ob], B_Rt[r]], writes=[Bdst])
        else:
            prog.add("dve", lambda e: e.tensor_tensor(out=tmpo[r][0:64, 0:nq], in0=PS(ob, 0, nq)[0:64, :],
                                                      in1=Rt[r][0:64, 0:nq], op=ALU.mult),
                     reads=[B_ps[ob], B_Rt[r]], writes=[B_tmpo[r]])
            prog.add("dve", lambda e: e.tensor_copy(out=dst_pair[64:128, qlo:qlo + nq], in_=tmpo[r][0:64, 0:nq]),
                     reads=[B_tmpo[r]], writes=[Bdst])

    def drain(g):
        if g is not None:
            for _ in g:
                pass

    def run_units(units, filler=None, every=3, depth=2):
        n = len(units)
        if not n:
            drain(filler)
            return
        for j in range(min(depth, n)):
            units[j]["S"]()
        for j in range(min(depth - 1, n)):
            units[j]["E"]()
        for i, u in enumerate(units):
            u["PV"]()
            if i + depth < n:
                units[i + depth]["S"]()
            if filler is not None and i % every == every - 1:
                next(filler, None)
            if i + depth - 1 < n:
                units[i + depth - 1]["E"]()
            if u.get("F") is not None:
                u["F"]()
        drain(filler)

    def swa_kv():
        for g in range(2):
            prog.add("dve", lambda e, g=g: e.memset(VS[g][:, :, 64:128], 1.0), writes=[B_VS[g]])
            prog.add("dve", lambda e, g=g: e.tensor_scalar(out=VS[g][:, 0:1, 64:128], in0=VS[g][:, 0:1, 64:128],
                                                           scalar1=pvcol[:, 0:1], scalar2=None, op0=ALU.mult),
                     reads=[B_const, B_VS[g]], writes=[B_VS[g]])
        sk = load_wcols(C_KA)
        sv = load_wcols(C_VA)
        for (src, Bsrc, lo, n, dlo) in ((hTp, B_hTp, 896, 128, 0), (hTo, B_hTo, 0, 512, 128), (hTo, B_hTo, 512, 512, 640)):
            bank = proj_fm(sk, src, Bsrc, lo, n)
            headnorm(bank, n, 1, KS[0][0:64, dlo:dlo + n], B_KS[0], KS[1][0:64, dlo:dlo + n], B_KS[1])
        for grp in ((7, 8, 9, 10), (11, 12, 13, 14), (15,)):
            bank = pbank[0] % 2
            pbank[0] += 1
            for j, t in enumerate(grp):
                src, Bsrc, tc = (hTp, B_hTp, t * 128) if t < 8 else (hTo, B_hTo, (t - 8) * 128)
                for k in range(16):
                    prog.add("pe", lambda e, k=k, j=j, src=src, tc=tc, bank=bank: e.matmul(
                        PS(bank, j * 128, (j + 1) * 128), lhsT=src[:, k, tc:tc + 128], rhs=wslot[sv][:, k, :],
                        start=(k == 0), stop=(k == 15)), reads=[B_w[sv], Bsrc], writes=[B_ps[bank]])
            for g in range(2):
                for j, t in enumerate(grp):
                    if t < 8:
                        prog.add("act", lambda e, g=g, j=j, t=t, bank=bank: e.activation(
                            out=VS[g][:, t - 7, 0:64], in_=PS(bank, j * 128 + g * 64, j * 128 + g * 64 + 64),
                            func=AF.Copy, scale=pvcol[:, 0:1]), reads=[B_ps[bank], B_const], writes=[B_VS[g]])
                    else:
                        prog.add("act", lambda e, g=g, j=j, t=t, bank=bank: e.activation(
                            out=VS[g][:, t - 7, 0:64], in_=PS(bank, j * 128 + g * 64, j * 128 + g * 64 + 64),
                            func=AF.Copy), reads=[B_ps[bank]], writes=[B_VS[g]])

    def swa_qproj(p, buf):
        s = load_wcols(C_QA + p * 128)
        pend = None
        for n in range(2):
            if pend is not None:
                pend()
            bank = proj_fm(s, hTo, B_hTo, n * 512, 512)
            pend = (lambda bank=bank, n=n: headnorm(
                bank, 512, 0, QA[buf][0][0:64, n * 512:(n + 1) * 512], B_QA[buf][0],
                QA[buf][1][0:64, n * 512:(n + 1) * 512], B_QA[buf][1]))
            yield
        pend()
        yield

    def swa_attn(p, buf, filler=None, every=3, depth=3):
        banks = (3, 4, 7) if depth == 3 else (3, 4)
        units = []
        for i in range(2):
            h = 2 * p + i
            g = h // 8
            q = QA[buf][i]
            Bq = B_QA[buf][i]
            xi = h % 2
            prog.add("act", lambda e, h=h, xi=xi: e.activation(out=sId[xi], in_=ident_f, func=AF.Copy,
                                                               scale=-SLOPES[h] / SCALE),
                     reads=[B_const], writes=[B_sId[xi]])
            for c in range(4):
                i0 = 8 + 2 * c - 7
                sb = banks[swa_sb[0] % len(banks)]
                swa_sb[0] += 1
                ob = 5 + obank[0] % 2
                obank[0] += 1
                pi = ptc[0] % NPT
                ptc[0] += 1

                def S(q=q, Bq=Bq, g=g, c=c, i0=i0, sb=sb, xi=xi):
                    qa = 2 * c * 128
                    prog.add("pe", lambda e: e.matmul(PS(sb), lhsT=sId[xi], rhs=dsw, start=True, stop=False),
                             reads=[B_sId[xi], B_const], writes=[B_ps[sb]])
                    for (olo, ohi, kt, qlo, qhi, lastmm) in ((0, 128, i0 - 1, qa, qa + 128, False),
                                                             (128, 384, i0, qa, qa + 256, False),
                                                             (384, 512, i0 + 1, qa + 128, qa + 256, True)):
                        prog.add("pe", lambda e, olo=olo, ohi=ohi, kt=kt, qlo=qlo, qhi=qhi, lastmm=lastmm: e.matmul(
                            PS(sb, olo, ohi), lhsT=KS[g][0:64, kt * 128:(kt + 1) * 128], rhs=q[0:64, qlo:qhi],
                            start=False, stop=lastmm), reads=[B_KS[g], Bq], writes=[B_ps[sb]])

                def E(h=h, sb=sb, pi=pi):
                    prog.add("act", lambda e: e.activation(out=Pt[pi], in_=PS(sb), func=AF.Exp, scale=SCALE),
                             reads=[B_ps[sb]], writes=[B_Pt[pi]])

                def PV(h=h, p=p, i=i, g=g, c=c, i0=i0, ob=ob, pi=pi):
                    for (olo, kt, plo, st, sp_) in ((0, i0 - 1, 0, True, False), (0, i0, 128, False, True),
                                                    (128, i0, 256, True, False), (128, i0 + 1, 384, False, True)):
                        prog.add("pe", lambda e, olo=olo, kt=kt, plo=plo, st=st, sp_=sp_: e.matmul(
                            PS(ob, olo, olo + 128), lhsT=VS[g][:, kt, :], rhs=Pt[pi][:, plo:plo + 128],
                            start=st, stop=sp_), reads=[B_VS[g], B_Pt[pi]], writes=[B_ps[ob]])

                def F(h=h, p=p, i=i, c=c, ob=ob):
                    finish_block(ob, 256, attn_swa[:, p, :], B_attn_swa[p], i, c * 256, sink_h=h)

                units.append({"S": S, "E": E, "PV": PV, "F": F})
        run_units(units, filler, every, depth=depth)

    def moba_init():
        for b in range(2):
            for i in range(2):
                prog.add("dve", lambda e, b=b, i=i: e.memset(VA[b][i][:, :, 64:128], 1.0),
                         writes=[B_VA[b][i]])
                prog.add("dve", lambda e, b=b, i=i: e.tensor_scalar(
                    out=VA[b][i][:, 0:8, 64:128], in0=VA[b][i][:, 0:8, 64:128], scalar1=pvcol[:, 0:1], scalar2=None,
                    op0=ALU.mult), reads=[B_const, B_VA[b][i]], writes=[B_VA[b][i]])
            prog.add("dve", lambda e, b=b: e.memset(stage_t[b], 0.0), writes=[B_stage[b]])

    def moba_proj(p, buf):
        sk = load_wcols(C_KB + p * 128)
        sv = load_wcols(C_VB + p * 128)
        sq = load_wcols(C_QB + p * 128)
        for i in range(2):
            h = 2 * p + i
            prog.add("sp", lambda e, i=i, h=h: e.dma_start(out=KA[buf][i][64:78, :], in_=tabk_d[h]),
                     writes=[B_KAaug[buf][i]], dsem=S_KAaug[buf][i])
            prog.add("sp", lambda e, i=i, h=h: e.dma_start(out=QA[buf][i][72:78, :], in_=tabq_d[h]),
                     writes=[B_QAaug[buf][i]], dsem=S_QAaug[buf][i])
        pend = None
        for (src, Bsrc, lo, dlo) in ((hTp, B_hTp, 0, 0), (hTp, B_hTp, 512, 512), (hTo, B_hTo, 0, 1024), (hTo, B_hTo, 512, 1536)):
            if pend is not None:
                pend()
            bank = proj_fm(sk, src, Bsrc, lo, 512)
            pend = (lambda bank=bank, dlo=dlo: headnorm(
                bank, 512, 3, KA[buf][0][0:64, dlo:dlo + 512], B_KA[buf][0], KA[buf][1][0:64, dlo:dlo + 512], B_KA[buf][1]))
            cmd = yield
            if cmd == "flush" and pend is not None:
                pend()
                pend = None
                yield
            elif pend is not None:
                ia = headnorm_a(bank, 512)
                pend = (lambda ia=ia, bank=bank, dlo=dlo: headnorm_b(
                    ia, bank, 512, 3, KA[buf][0][0:64, dlo:dlo + 512], B_KA[buf][0], KA[buf][1][0:64, dlo:dlo + 512],
                    B_KA[buf][1]))
                cmd = yield
                if cmd == "flush":
                    pend()
                    pend = None
                    yield
        for t0 in range(0, 16, 4):
            if pend is not None:
                pend()
            bank = pbank[0] % 2
            pbank[0] += 1
            for j in range(4):
                t = t0 + j
                src, Bsrc, tc = (hTp, B_hTp, t * 128) if t < 8 else (hTo, B_hTo, (t - 8) * 128)
                for k in range(16):
                    prog.add("pe", lambda e, k=k, j=j, src=src, tc=tc, bank=bank: e.matmul(
                        PS(bank, j * 128, (j + 1) * 128), lhsT=src[:, k, tc:tc + 128], rhs=wslot[sv][:, k, :],
                        start=(k == 0), stop=(k == 15)), reads=[B_w[sv], Bsrc], writes=[B_ps[bank]])

            def vcopy(bank=bank, t0=t0):
                for i in range(2):
                    src_ps = PS(bank).rearrange("p (a b) -> p a b", b=128)[:, :, i * 64:(i + 1) * 64]
                    if t0 < 8:
                        prog.add("act", lambda e, i=i, src_ps=src_ps: e.activation(
                            out=VA[buf][i][:, t0:t0 + 4, 0:64], in_=src_ps, func=AF.Copy, scale=pvcol[:, 0:1]),
                            reads=[B_ps[bank], B_const], writes=[B_VA[buf][i]])
                    else:
                        prog.add("act", lambda e, i=i, src_ps=src_ps: e.activation(
                            out=VA[buf][i][:, t0:t0 + 4, 0:64], in_=src_ps, func=AF.Copy),
                            reads=[B_ps[bank]], writes=[B_VA[buf][i]])
            pend = vcopy
            cmd = yield
            if cmd == "flush" and pend is not None:
                pend()
                pend = None
                yield
        for n in range(2):
            if pend is not None:
                pend()
            bank = proj_fm(sq, hTo, B_hTo, n * 512, 512)
            pend = (lambda bank=bank, n=n: headnorm(
                bank, 512, 2, QA[buf][0][0:64, n * 512:(n + 1) * 512], B_QA[buf][0],
                QA[buf][1][0:64, n * 512:(n + 1) * 512], B_QA[buf][1]))
            cmd = yield
            if cmd == "flush" and pend is not None:
                pend()
                pend = None
                yield
            elif pend is not None:
                ia = headnorm_a(bank, 512)
                pend = (lambda ia=ia, bank=bank, n=n: headnorm_b(
                    ia, bank, 512, 2, QA[buf][0][0:64, n * 512:(n + 1) * 512], B_QA[buf][0],
                    QA[buf][1][0:64, n * 512:(n + 1) * 512], B_QA[buf][1]))
                cmd = yield
                if cmd == "flush":
                    pend()
                    pend = None
                    yield
        if pend is not None:
            pend()
            pend = None
        yield
        for i in range(2):
            K_, Q_ = KA[buf][i], QA[buf][i]
            prog.add("dve", lambda e, K_=K_: e.tensor_reduce(
                out=kmf[0:64, :], in_=K_[0:64, :].rearrange("p (n l) -> p n l", l=256), axis=AX.X, op=ALU.add),
                reads=[B_KA[buf][i]], writes=[B_kmf])
            yield
            prog.add("dve", lambda e: e.tensor_copy(out=kmb[0:64, :], in_=kmf[0:64, :]), reads=[B_kmf], writes=[B_kmb])
            for t in range(8):
                prog.add("pe", lambda e, t=t, Q_=Q_: e.matmul(PS(2, 256 + t * 8, 256 + t * 8 + 8),
                                                              lhsT=Q_[0:64, t * 128:(t + 1) * 128], rhs=kmb[0:64, :],
                                                              start=True, stop=True),
                         reads=[B_QA[buf][i], B_kmb], writes=[B_ps[2]])
            prog.add("dve", lambda e: e.tensor_tensor(out=gm.rearrange("p a b -> p (a b)"), in0=PS(2, 256, 320),
                                                      in1=pastbias, op=ALU.add),
                     reads=[B_ps[2], B_const], writes=[B_gm])
            yield
            for t in range(8):
                prog.add("dve", lambda e, t=t: e.max(out=m8[:, t, :], in_=gm[:, t, :]), reads=[B_gm], writes=[B_m8])
            prog.add("dve", lambda e: e.tensor_tensor(out=selt, in0=gm, in1=m8[:, :, 3:4].to_broadcast([128, 8, 8]),
                                                      op=ALU.is_ge), reads=[B_gm, B_m8], writes=[B_selt])
            prog.add("dve", lambda e: e.tensor_scalar(out=stage_t[buf][:, :, 64:72], in0=selt, scalar1=BIG,
                                                      scalar2=-BIG, op0=ALU.mult, op1=ALU.add),
                     reads=[B_selt], writes=[B_stage[buf]])
            yield
            for r in range(2):
                for t4 in range(4):
                    t = r * 4 + t4
                    prog.add("pe", lambda e, t=t, t4=t4: e.transpose(
                        out=PSB(2, t4 * 128, (t4 + 1) * 128)[0:72, :], in_=stage_t[buf][:, t, :], identity=ident_bf),
                        reads=[B_stage[buf], B_const], writes=[B_ps[2]])
                prog.add("dve", lambda e, r=r, Q_=Q_: e.tensor_copy(out=Q_[64:72, r * 512:(r + 1) * 512],
                                                                    in_=PSB(2, 0, 512)[64:72, :]),
                         reads=[B_ps[2]], writes=[B_QA[buf][i]])
            yield

    def moba_attn(p, buf, filler=None, every=3):
        units = []
        for i in range(2):
            h = 2 * p + i
            K_, Q_, V_ = KA[buf][i], QA[buf][i], VA[buf][i]
            rd = [B_KA[buf][i], B_KAaug[buf][i], B_QA[buf][i], B_QAaug[buf][i]]
            for j in range(4):
                nkt = 8 + 2 * (j + 1)
                ob = 5 + obank[0] % 2
                obank[0] += 1
                for kt in range(0, nkt, 2):
                    sb = (3, 4, 7)[sbank[0] % 3]
                    si = sbank[0] % 3
                    sbank[0] += 1
                    pi = ptc[0] % NPT
                    ptc[0] += 1
                    diag = (kt == 8 + 2 * j)
                    last = (kt == nkt - 2)

                    def S(K_=K_, Q_=Q_, rd=rd, j=j, kt=kt, sb=sb):
                        for u in range(2):
                            prog.add("pe", lambda e, u=u: e.matmul(
                                PS(sb, u * 256, (u + 1) * 256), lhsT=K_[0:78, (kt + u) * 128:(kt + u + 1) * 128],
                                rhs=Q_[0:78, j * 256:(j + 1) * 256], start=True, stop=True), reads=rd, writes=[B_ps[sb]])

                    def E(diag=diag, sb=sb, si=si, pi=pi):
                        if diag:
                            prog.add("dve", lambda e: e.tensor_tensor(out=Sp[si], in0=PS(sb), in1=mneg, op=ALU.add),
                                     reads=[B_ps[sb], B_const], writes=[B_Sp[si]])
                            prog.add("act", lambda e: e.activation(out=Pt[pi], in_=Sp[si], func=AF.Exp, scale=SCALE),
                                     reads=[B_Sp[si]], writes=[B_Pt[pi]])
                        else:
                            prog.add("act", lambda e: e.activation(out=Pt[pi], in_=PS(sb), func=AF.Exp, scale=SCALE),
                                     reads=[B_ps[sb]], writes=[B_Pt[pi]])

                    def PV(V_=V_, i=i, p=p, j=j, kt=kt, ob=ob, pi=pi, last=last, buf=buf):
                        for u in range(2):
                            prog.add("pe", lambda e, u=u: e.matmul(
                                PS(ob, 0, 256), lhsT=V_[:, kt + u, :], rhs=Pt[pi][:, u * 256:(u + 1) * 256],
                                start=(kt == 0 and u == 0), stop=(last and u == 1)),
                                reads=[B_VA[buf][i], B_Pt[pi]], writes=[B_ps[ob]])

                    F = None
                    if last:
                        def F(i=i, p=p, j=j, ob=ob):
                            finish_block(ob, 256, attn_moba[:, p, :], B_attn_moba[p], i, j * 256)

                    units.append({"S": S, "E": E, "PV": PV, "F": F})
        run_units(units, filler, every, depth=3)

    if stage >= 2:
        barrier(B_xt + B_xn + [B_gbc, B_junk],
                [b for bb in B_KA for b in bb] + [b for bb in B_KAaug for b in bb] + [b for bb in B_VA for b in bb] +
                B_KS + B_VS + B_Sp + B_Pt)
        swa_kv()
        moba_init()
        drain(swa_qproj(0, 0))
        moba0 = moba_proj(0, 0) if stage >= 3 else None

        def chain2(a, b, nb):
            for _ in a:
                yield
            for _ in range(nb):
                if next(b, "end") == "end":
                    return
                yield
            try:
                b.send("flush")
            except StopIteration:
                pass

        for p in range(8):
            if p + 1 < 8:
                if moba0 is not None and p >= 4:
                    filler, every = chain2(swa_qproj(p + 1, (p + 1) % 2), moba0, 3), 1
                else:
                    filler, every = swa_qproj(p + 1, (p + 1) % 2), 2
            elif moba0 is not None:
                filler, every = moba0, 1
            else:
                filler, every = None, 1
            swa_attn(p, p % 2, filler, every, depth=3)
        if debug and stage == 2:
            dbg_outs["KS0"] = (KS[0], [128, 1152], BF16, [B_KS[0]])
            dbg_outs["KS1"] = (KS[1], [128, 1152], BF16, [B_KS[1]])
            dbg_outs["VS0"] = (VS[0], [128, 9 * 128], BF16, [B_VS[0]])
            dbg_outs["attn_swa"] = (attn_swa, [128, 8 * 1024], BF16, B_attn_swa)
    if stage >= 3:
        for p in range(8):
            filler = moba_proj(p + 1, (p + 1) % 2) if p + 1 < 8 else None
            moba_attn(p, p % 2, filler, 2)
        if debug and stage == 3:
            dbg_outs["attn_moba"] = (attn_moba, [128, 8 * 1024], BF16, B_attn_moba)
            dbg_outs["QA10"] = (QA[1][0], [128, 1024], BF16, [B_QA[1][0], B_QAaug[1][0]])
            dbg_outs["KA10"] = (KA[1][0], [128, 2048], BF16, [B_KA[1][0], B_KAaug[1][0]])
            dbg_outs["VA10"] = (VA[1][0], [128, 2048], BF16, [B_VA[1][0]])

    all_attn_bufs = (B_w + [b for bb in B_QA for b in bb] + [b for bb in B_QAaug for b in bb] +
                     [b for bb in B_KA for b in bb] + [b for bb in B_KAaug for b in bb] +
                     [b for bb in B_VA for b in bb] + B_KS + B_VS + B_sqb + B_lnb + B_rsb + B_tmpB + B_Sp + B_Pt +
                     B_Rt + B_tmpo + [B_gm, B_m8, B_selt, B_kmf, B_kmb] + B_stage + B_xt + B_xn + [B_junk, B_gbc])
    if stage >= 4:
        R3.reset()
        gslot = [[R3.alloc([128, 16, 128], BF16) for _ in range(2)] for _ in range(2)]
        uslot = [[R3.alloc([128, 8, 128], BF16) for _ in range(2)] for _ in range(2)]
        B_gs = [Buf("gs%d" % i) for i in range(2)]
        S_gs = [new_sem() for _ in range(2)]
        mergedT = R3.alloc([128, 16, 1024], BF16)
        B_merged = [Buf("merged%d" % i) for i in range(16)]
        sga = [R3.alloc([128, 512], F32) for _ in range(2)]
        sgb = [R3.alloc([128, 512], F32) for _ in range(2)]
        tmul = [R3.alloc([128, 512], F32) for _ in range(2)]
        B_sga = [Buf("sga%d" % i) for i in range(2)]
        B_sgb = [Buf("sgb%d" % i) for i in range(2)]
        B_tmul = [Buf("tmul%d" % i) for i in range(2)]
        NWO = 3
        woslot = [R3.alloc([128, 16, 256], BF16) for _ in range(NWO)]
        B_wo = [Buf("wo%d" % i) for i in range(NWO)]
        S_wo = [new_sem() for _ in range(NWO)]
        assert R3.off <= R3.size
        barrier(all_attn_bufs, B_gs + B_merged + B_sga + B_sgb + B_tmul + B_wo)
        first = [True]
        wus_v = w_up_swa.rearrange("(k p) n -> p k n", p=128)
        wum_v = w_up_moba.rearrange("(k p) n -> p k n", p=128)
        wo_v = w_out.rearrange("(k p) n -> p k n", p=128)
        cnt4 = [0]
        for c in range(16):
            st_ = c % 2
            extra = []

            def _ld(e, c=c, st_=st_):
                return [e.dma_start(out=gslot[st_][0], in_=w_in_v[:, :, C_GA + c * 128:C_GA + (c + 1) * 128]),
                        e.dma_start(out=gslot[st_][1], in_=w_in_v[:, :, C_GB + c * 128:C_GB + (c + 1) * 128]),
                        e.dma_start(out=uslot[st_][0], in_=wus_v[:, :, c * 128:(c + 1) * 128]),
                        e.dma_start(out=uslot[st_][1], in_=wum_v[:, :, c * 128:(c + 1) * 128])]
            prog.add("pool", _ld, writes=[B_gs[st_]] + extra, dsem=S_gs[st_], ndma=4)
            for n in range(2):
                alt = cnt4[0] % 2
                cnt4[0] += 1
                bga, bgb, bya, byb = ((0, 1, 3, 4), (2, 5, 6, 7))[alt]
                tl = n * 512
                for (bank, wt) in ((bga, gslot[st_][0]), (bgb, gslot[st_][1])):
                    for k in range(16):
                        prog.add("pe", lambda e, k=k, bank=bank, wt=wt, tl=tl: e.matmul(
                            PS(bank), lhsT=wt[:, k, :], rhs=hTo[:, k, tl:tl + 512], start=(k == 0), stop=(k == 15)),
                            reads=[B_gs[st_], B_hTo], writes=[B_ps[bank]])
                for (bank, wt, at_, Bat) in ((bya, uslot[st_][0], attn_swa, B_attn_swa), (byb, uslot[st_][1], attn_moba, B_attn_moba)):
                    for k in range(8):
                        prog.add("pe", lambda e, k=k, bank=bank, wt=wt, at_=at_, tl=tl: e.matmul(
                            PS(bank), lhsT=wt[:, k, :], rhs=at_[:, k, tl:tl + 512], start=(k == 0), stop=(k == 7)),
                            reads=[B_gs[st_]] + Bat, writes=[B_ps[bank]])
                prog.add("act", lambda e, bga=bga, alt=alt: e.activation(out=sga[alt], in_=PS(bga), func=AF.Sigmoid),
                         reads=[B_ps[bga]], writes=[B_sga[alt]])
                prog.add("act", lambda e, bgb=bgb, alt=alt: e.activation(out=sgb[alt], in_=PS(bgb), func=AF.Sigmoid),
                         reads=[B_ps[bgb]], writes=[B_sgb[alt]])
                prog.add("dve", lambda e, bya=bya, alt=alt: e.tensor_tensor(out=tmul[alt], in0=PS(bya), in1=sga[alt], op=ALU.mult),
                         reads=[B_ps[bya], B_sga[alt]], writes=[B_tmul[alt]])
                prog.add("dve", lambda e, byb=byb, alt=alt: e.tensor_tensor(out=sgb[alt], in0=PS(byb), in1=sgb[alt], op=ALU.mult),
                         reads=[B_ps[byb], B_sgb[alt]], writes=[B_sgb[alt]])
                prog.add("dve", lambda e, c=c, tl=tl, alt=alt: e.tensor_tensor(out=mergedT[:, c, tl:tl + 512], in0=tmul[alt],
                                                                               in1=sgb[alt], op=ALU.add),
                         reads=[B_tmul[alt], B_sgb[alt]], writes=[B_merged[c]])
        if debug and stage == 4:
            dbg_outs["mergedT"] = (mergedT, [128, 16 * 1024], BF16, B_merged)

    if stage >= 5:
        R1.reset()
        x1 = R1.alloc([128, 8, D], F32)
        B_x1 = [Buf("x1_%d" % t) for t in range(8)]
        S_x1 = new_sem()
        xs_own = xs[NOWN:NKV, :].rearrange("(t p) d -> p t d", p=128)

        def _ldx(e):
            return [e.dma_start(out=x1[:, t, :], in_=xs_own[:, t, :]) for t in range(8)]
        prog.add("sp", _ldx, writes=B_x1 + [B_hTp, B_hTo], dsem=S_x1, ndma=8)
        ob2 = [0]
        for cg in range(8):
            s = cg % NWO
            prog.add("pool", lambda e, cg=cg, s=s: e.dma_start(out=woslot[s], in_=wo_v[:, :, cg * 256:(cg + 1) * 256]),
                     writes=[B_wo[s]], dsem=S_wo[s])
            for t in range(8):
                bank = ob2[0] % 8
                ob2[0] += 1
                for k in range(16):
                    prog.add("pe", lambda e, k=k, t=t, s=s, bank=bank: e.matmul(
                        PS(bank, 0, 256), lhsT=mergedT[:, k, t * 128:(t + 1) * 128], rhs=woslot[s][:, k, :],
                        start=(k == 0), stop=(k == 15)), reads=[B_wo[s]] + B_merged, writes=[B_ps[bank]])
                prog.add("dve", lambda e, t=t, cg=cg, bank=bank: e.tensor_tensor(
                    out=x1[:, t, cg * 256:(cg + 1) * 256], in0=PS(bank, 0, 256), in1=x1[:, t, cg * 256:(cg + 1) * 256],
                    op=ALU.add), reads=[B_ps[bank], B_x1[t]], writes=[B_x1[t]])
        if debug and stage == 5:
            dbg_outs["x1"] = (x1, [128, 8 * D], F32, B_x1)

    if stage >= 6:
        R2.reset()
        h2T = R2.alloc([128, 16, 1024], BF16)
        B_h2T = Buf("h2T")
        phaseB_bufs = B_gs + B_merged + B_sga + B_sgb + B_tmul + B_wo
        R3.reset()
        NWE = 4
        ering = [R3.alloc([128, 8192], BF16) for _ in range(NWE)]
        B_er = [Buf("er%d" % i) for i in range(NWE)]
        S_er = [new_sem() for _ in range(NWE)]
        hidT = [R3.alloc([128, 4, 1024], BF16) for _ in range(2)]
        B_hid = [[Buf("hid%d_%d" % (i, n)) for n in range(2)] for i in range(2)]
        sg = [R3.alloc([128, 512], F32) for _ in range(2)]
        B_sg = [Buf("sg%d" % i) for i in range(2)]
        comb = R3.alloc([128, 8, 16], F32)
        B_comb = Buf("comb")
        wr_sb = R3.alloc([128, 16, 20], F32)
        B_wr = Buf("wr")
        S_wr = new_sem()
        L_all = R3.alloc([128, 8, 20], F32)
        B_L = Buf("L")
        ss2 = R3.alloc([128, 8], F32)
        ln2 = R3.alloc([128, 8], F32)
        rs2 = R3.alloc([128, 8], F32)
        B_ss2l = [Buf("ss2_%d" % t) for t in range(8)]
        B_ss2 = Buf("ss2")
        rt_small = [R3.alloc([128, 8, 16], F32) for _ in range(8)]
        B_rts = Buf("rts")
        moe_end = R3.off
        assert R3.off <= R3.size, (R3.off, R3.size)
        gbc2 = R3.alloc([128, D], F32, at=16384)
        h2f = [R3.alloc([128, D], F32, at=16384 + 8192 + i * 8192) for i in range(2)]
        B_h2f = [Buf("h2f%d" % i) for i in range(2)]
        h2Tf = [R3.alloc([128, 16, 128], F32, at=16384 + 3 * 8192 + i * 8192) for i in range(2)]
        B_h2Tf = [Buf("h2Tf%d" % i) for i in range(2)]
        junk2 = R3.alloc([128, D], BF16, at=16384 + 5 * 8192)
        B_junk2 = Buf("junk2")
        B_gbc2 = Buf("gbc2")
        S_gbc2 = new_sem()
        B_scrC = [B_er[1], B_er[2], B_er[3]]

        barrier(phaseB_bufs, B_er + [b for bb in B_hid for b in bb] + B_sg + [B_comb, B_wr, B_L, B_ss2, B_rts, B_gbc2, B_junk2] +
                B_h2f + B_h2Tf)
        prog.add("sp", lambda e: e.dma_start(out=gbc2, in_=gffn_bc[:, :]), writes=[B_gbc2], dsem=S_gbc2)
        prog.add("sp", lambda e: e.dma_start(out=wr_sb, in_=w_r.rearrange("(k p) n -> p k n", p=128)),
                 writes=[B_wr], dsem=S_wr)
        prog.add("dve", lambda e: e.memset(ss2, 0.0), writes=[B_ss2] + B_ss2l)
        pend_router = [None]
        def part1(t):
            i2 = t % 2
            prog.add("act", lambda e, t=t: e.activation(out=junk2, in_=x1[:, t, :], func=AF.Square, accum_out=ss2[:, t:t + 1]),
                     reads=[B_x1[t], B_ss2l[t], B_gbc2], writes=[B_junk2, B_ss2l[t]])
            prog.add("act", lambda e, t=t: e.activation(out=ln2[:, t:t + 1], in_=ss2[:, t:t + 1], func=AF.Ln, scale=1.0 / D,
                                                        bias=epscol[:, 0:1]), reads=[B_ss2l[t], B_eps], writes=[B_ss2l[t]])
            prog.add("act", lambda e, t=t: e.activation(out=rs2[:, t:t + 1], in_=ln2[:, t:t + 1], func=AF.Exp, scale=-0.5),
                     reads=[B_ss2l[t]], writes=[B_ss2l[t]])
            prog.add("dve", lambda e, t=t, i2=i2: e.scalar_tensor_tensor(
                out=h2f[i2], in0=x1[:, t, :], scalar=rs2[:, t:t + 1], op0=ALU.mult, in1=gbc2, op1=ALU.mult),
                reads=[B_x1[t], B_ss2l[t], B_gbc2], writes=[B_h2f[i2]])
        def part2(t):
            i2 = t % 2
            for q4 in range(4):
                bank = (t * 4 + q4) % 4
                for kk in range(4):
                    k = q4 * 4 + kk
                    prog.add("pe", lambda e, k=k, kk=kk, bank=bank, i2=i2: e.transpose(
                        out=PS(bank, kk * 128, (kk + 1) * 128), in_=h2f[i2][:, k * 128:(k + 1) * 128], identity=ident_f),
                        reads=[B_h2f[i2], B_const], writes=[B_ps[bank]])
                prog.add("act", lambda e, q4=q4, bank=bank, i2=i2: e.activation(
                    out=h2Tf[i2][:, q4 * 4:(q4 + 1) * 4, :], in_=PS(bank).rearrange("p (a b) -> p a b", b=128), func=AF.Copy),
                    reads=[B_ps[bank]], writes=[B_h2Tf[i2]])
                prog.add("dve", lambda e, q4=q4, i2=i2, t=t: e.tensor_copy(
                    out=h2T[:, q4 * 4:(q4 + 1) * 4, t * 128:(t + 1) * 128], in_=h2Tf[i2][:, q4 * 4:(q4 + 1) * 4, :]),
                    reads=[B_h2Tf[i2]], writes=[B_h2T] + (B_attn_swa + B_attn_moba if (t == 0 and q4 == 0) else []))
            def router(t=t, i2=i2):
                for k in range(16):
                    prog.add("pe", lambda e, k=k: e.matmul(PS(4 + t % 2, 0, 20), lhsT=h2Tf[i2][:, k, :], rhs=wr_sb[:, k, :],
                                                           start=(k == 0), stop=(k == 15)),
                             reads=[B_h2Tf[i2], B_wr], writes=[B_ps[4 + t % 2]])
                prog.add("dve", lambda e: e.tensor_copy(out=L_all[:, t, :], in_=PS(4 + t % 2, 0, 20)),
                         reads=[B_ps[4 + t % 2]], writes=[B_L])
            if pend_router[0] is not None:
                pend_router[0]()
            pend_router[0] = router
        part1(0)
        for t in range(8):
            if t + 1 < 8:
                part1(t + 1)
            part2(t)
        pend_router[0]()
        lg = L_all[:, :, 0:4]
        le = L_all[:, :, 4:20]
        mg, ohg, tmp16, sl, l1, msk, l2, ex, exm, den, gp, sumg, wexp, junk8 = (None,) * 14
        mg = rt_small[0][:, :, 0:1]
        ohg = rt_small[0][:, :, 4:8]
        sumg = rt_small[0][:, :, 8:9]
        gp = rt_small[0][:, :, 9:10]
        l1 = rt_small[0][:, :, 10:11]
        l2 = rt_small[0][:, :, 11:12]
        den = rt_small[0][:, :, 12:13]
        fac = rt_small[0][:, :, 13:14]
        tmp16 = rt_small[1]
        sl = rt_small[2][:, :, 0:4]
        msk = rt_small[2][:, :, 4:8]
        sl2 = rt_small[2][:, :, 8:12]
        ex = rt_small[3][:, :, 0:4]
        exm = rt_small[3][:, :, 4:8]
        wexp = rt_small[3][:, :, 8:12]
        eg = rt_small[4][:, :, 0:4]
        dgl = rt_small[4][:, :, 4:8]
        dsl = rt_small[4][:, :, 8:12]

        def dv(fn, r=(B_L, B_rts), w=(B_rts,)):
            prog.add("dve", fn, reads=list(r), writes=list(w))
        dv(lambda e: e.tensor_reduce(out=mg, in_=lg, axis=AX.X, op=ALU.max))
        dv(lambda e: e.tensor_tensor(out=ohg, in0=lg, in1=mg.to_broadcast([128, 8, 4]), op=ALU.is_ge))
        dv(lambda e: e.tensor_tensor(out=dgl, in0=lg, in1=mg.to_broadcast([128, 8, 4]), op=ALU.subtract))
        prog.add("act", lambda e: e.activation(out=eg, in_=dgl, func=AF.Exp), reads=[B_rts], writes=[B_rts])
        dv(lambda e: e.tensor_reduce(out=sumg, in_=eg, axis=AX.X, op=ALU.add))
        dv(lambda e: e.reciprocal(out=gp, in_=sumg))
        dv(lambda e: e.tensor_tensor(out=tmp16.rearrange("p t (g e) -> p t g e", e=4),
                                     in0=le.rearrange("p t (g e) -> p t g e", e=4),
                                     in1=ohg.unsqueeze(3).to_broadcast([128, 8, 4, 4]), op=ALU.mult))
        dv(lambda e: e.tensor_reduce(out=sl, in_=tmp16.rearrange("p t (g e) -> p t e g", e=4), axis=AX.X, op=ALU.add))
        dv(lambda e: e.tensor_reduce(out=l1, in_=sl, axis=AX.X, op=ALU.max))
        dv(lambda e: e.tensor_tensor(out=msk, in0=sl, in1=l1.to_broadcast([128, 8, 4]), op=ALU.is_ge))
        dv(lambda e: e.scalar_tensor_tensor(out=sl2, in0=msk, scalar=-1e30, op0=ALU.mult, in1=sl, op1=ALU.add))
        dv(lambda e: e.tensor_reduce(out=l2, in_=sl2, axis=AX.X, op=ALU.max))
        dv(lambda e: e.tensor_tensor(out=msk, in0=sl, in1=l2.to_broadcast([128, 8, 4]), op=ALU.is_ge))
        dv(lambda e: e.tensor_tensor(out=dsl, in0=sl, in1=l1.to_broadcast([128, 8, 4]), op=ALU.subtract))
        prog.add("act", lambda e: e.activation(out=ex, in_=dsl, func=AF.Exp), reads=[B_rts], writes=[B_rts])
        dv(lambda e: e.tensor_tensor(out=exm, in0=ex, in1=msk, op=ALU.mult))
        dv(lambda e: e.tensor_reduce(out=den, in_=exm, axis=AX.X, op=ALU.add))
        dv(lambda e: e.reciprocal(out=fac, in_=den))
        dv(lambda e: e.tensor_tensor(out=fac, in0=fac, in1=gp, op=ALU.mult))
        dv(lambda e: e.tensor_tensor(out=wexp, in0=exm, in1=fac.to_broadcast([128, 8, 4]), op=ALU.mult))
        dv(lambda e: e.tensor_tensor(out=comb.rearrange("p t (g e) -> p t g e", e=4),
                                     in0=ohg.unsqueeze(3).to_broadcast([128, 8, 4, 4]),
                                     in1=wexp.unsqueeze(2).to_broadcast([128, 8, 4, 4]), op=ALU.mult),
           w=(B_rts, B_comb))
        if debug and stage == 6:
            dbg_outs["h2T"] = (h2T, [128, 16 * 1024], BF16, [B_h2T])
            dbg_outs["L_all"] = (L_all, [128, 8 * 20], F32, [B_L])
            dbg_outs["comb"] = (comb, [128, 8 * 16], F32, [B_comb])

    if stage >= 7:
        er = [0]

        def load_e(dram_ap, shape3):
            s = er[0] % NWE
            er[0] += 1
            view = ering[s].rearrange("p (a b) -> p a b", b=shape3[2])
            extra = B_scrC_users if s in (1, 2, 3) and er[0] <= NWE else []
            prog.add("pool", lambda e: e.dma_start(out=view, in_=dram_ap), writes=[B_er[s]] + extra, dsem=S_er[s])
            return s, view
        B_scrC_users = [B_gbc2, B_junk2] + B_h2f + B_h2Tf
        gu = [0]
        yb = [0]
        for ex_i in range(16):
            hb = ex_i % 2
            s_g, Wg = load_e(w_gate_e[ex_i].rearrange("(k p) f -> p k f", p=128), [128, 16, 512])
            s_u, Wu = load_e(w_up_e[ex_i].rearrange("(k p) f -> p k f", p=128), [128, 16, 512])
            s_d, Wd = load_e(w_down_e[ex_i].rearrange("(k p) d -> p k d", p=128), [128, 4, 2048])
            for n in range(2):
                for fc in range(4):
                    alt = gu[0] % 2
                    gu[0] += 1
                    bg, bu = (0, 1) if alt == 0 else (2, 3)
                    for (bank, W_, s_) in ((bg, Wg, s_g), (bu, Wu, s_u)):
                        for k in range(16):
                            prog.add("pe", lambda e, k=k, bank=bank, W_=W_, fc=fc, n=n: e.matmul(
                                PS(bank), lhsT=W_[:, k, fc * 128:(fc + 1) * 128], rhs=h2T[:, k, n * 512:(n + 1) * 512],
                                start=(k == 0), stop=(k == 15)), reads=[B_er[s_], B_h2T], writes=[B_ps[bank]])
                    prog.add("act", lambda e, bg=bg, alt=alt: e.activation(out=sg[alt], in_=PS(bg), func=AF.Silu),
                             reads=[B_ps[bg]], writes=[B_sg[alt]])
                    prog.add("dve", lambda e, bu=bu, alt=alt, hb=hb, fc=fc, n=n: e.tensor_tensor(
                        out=hidT[hb][:, fc, n * 512:(n + 1) * 512], in0=PS(bu), in1=sg[alt], op=ALU.mult),
                        reads=[B_ps[bu], B_sg[alt]], writes=[B_hid[hb][n]])
            for t in range(8):
                for half in range(2):
                    b0 = 4 + 2 * (yb[0] % 2)
                    yb[0] += 1
                    for bb in range(2):
                        col = half * 1024 + bb * 512
                        for fc in range(4):
                            prog.add("pe", lambda e, fc=fc, t=t, col=col, bank=b0 + bb, hb=hb, Wd=Wd: e.matmul(
                                PS(bank), lhsT=hidT[hb][:, fc, t * 128:(t + 1) * 128], rhs=Wd[:, fc, col:col + 512],
                                start=(fc == 0), stop=(fc == 3)), reads=[B_er[s_d], B_hid[hb][t // 4]],
                                writes=[B_ps[b0 + bb]])
                    prog.add("dve", lambda e, t=t, half=half, b0=b0, ex_i=ex_i: e.scalar_tensor_tensor(
                        out=x1[:, t, half * 1024:(half + 1) * 1024], in0=psum[:, b0 * 512:(b0 + 2) * 512],
                        scalar=comb[:, t, ex_i:ex_i + 1], op0=ALU.mult, in1=x1[:, t, half * 1024:(half + 1) * 1024],
                        op1=ALU.add), reads=[B_ps[b0], B_ps[b0 + 1], B_comb, B_x1[t]], writes=[B_x1[t]])
        B_y = Buf("y")
        S_y = new_sem()
        y_v = y.rearrange("(t p) d -> p t d", p=128)
        for t in range(8):
            prog.add("sp", lambda e, t=t: e.dma_start(out=y_v[:, t, :], in_=x1[:, t, :]), reads=[B_x1[t]], writes=[B_y],
                     dsem=S_y)
        prog.add("sp", None, reads=[B_y])

    if debug:
        B_dbg = Buf("dbg")
        S_dbg = new_sem()
        for name, (ap, shape, dt, bufs) in dbg_outs.items():
            o = nc.dram_tensor("dbg_" + name, list(shape), dt, kind="ExternalOutput").ap()
            src = ap
            if len(ap.shape) == 3:
                src = ap.rearrange("p a b -> p (a b)")
            prog.add("sp", lambda e, o=o, src=src: e.dma_start(out=o[:, :], in_=src), reads=bufs, writes=[B_dbg], dsem=S_dbg)
        prog.add("sp", None, reads=[B_dbg])
        if stage < 7:
            pass

    block = es.enter_context(nc.Block())
    prog.emit(block, engsem)
    es.close()
    return nc, list(dbg_outs.keys())


_CONSTS = None


def _prepare_inputs(inputs, cores):
    global _CONSTS
    if _CONSTS is None:
        _CONSTS = _const_tables()
    c = _CONSTS
    f = lambda a: np.ascontiguousarray(np.asarray(a, dtype=np.float32))
    x = f(inputs["x"])
    shared = {
        "w_in": f(inputs["w_in"]), "w_up_swa": f(inputs["w_up_swa"]), "w_up_moba": f(inputs["w_up_moba"]),
        "w_out": f(inputs["w_out"]),
        "w_r": np.ascontiguousarray(np.concatenate(
            [f(inputs["w_router_group"]), f(inputs["w_router_expert"]).transpose(1, 0, 2).reshape(D, 16)], axis=1)),
        "w_gate_e": f(inputs["w_gate_e"]), "w_up_e": f(inputs["w_up_e"]), "w_down_e": f(inputs["w_down_e"]),
        "gmix_bc": np.ascontiguousarray(np.broadcast_to(f(inputs["g_mix"])[None, :], (128, D))),
        "gffn_bc": np.ascontiguousarray(np.broadcast_to(f(inputs["g_ffn"])[None, :], (128, D))),
        "gcols": np.ascontiguousarray(np.stack(
            [np.tile(f(inputs[k]), 2) for k in ("q_norm_swa", "k_norm_swa", "q_norm_moba", "k_norm_moba")], axis=1)),
        "sinks_bc": np.ascontiguousarray(np.broadcast_to(f(inputs["sinks"])[None, :], (128, 16))),
        "ident_bf": c["ident_bf"], "ident_f": c["ident_f"], "bd64": c["bd64"], "dsw": c["dsw"], "mneg": c["mneg"],
        "tabq": c["tabq"], "tabk": c["tabk"],
    }
    in_maps = []
    for cid in cores:
        b, hf = cid // 2, cid % 2
        if hf == 1:
            xs_ = x[b]
        else:
            xs_ = np.concatenate([x[b, NOWN:], x[b, :NOWN]], axis=0)
        m = dict(shared)
        m["xs"] = np.ascontiguousarray(xs_)
        m.update(_percore_tables(hf))
        in_maps.append(m)
    return in_maps


_NC_CACHE = {}


def kernel(**inputs):
    if "full" not in _NC_CACHE:
        _NC_CACHE["full"] = build_program(stage=99, debug=False)[0]
    nc = _NC_CACHE["full"]
    cores = list(range(8))
    in_maps = _prepare_inputs(inputs, cores)
    res = run_bass_kernel_spmd(nc, in_maps, core_ids=cores)
    out = np.empty((4, 2048, D), np.float32)
    for cid in cores:
        b, hf = cid // 2, cid % 2
        out[b, hf * NOWN:(hf + 1) * NOWN, :] = res.results[cid]["y"]
    return out
```

```python
import numpy as np
import ml_dtypes
from contextlib import ExitStack
import concourse.bass as bass
import concourse.mybir as mybir
from concourse.bass_utils import run_bass_kernel_spmd

F32 = mybir.dt.float32
BF16 = mybir.dt.bfloat16
U8 = mybir.dt.uint8
ALU = mybir.AluOpType
AF = mybir.ActivationFunctionType
AX = mybir.AxisListType
NPBF = ml_dtypes.bfloat16

D = 2048
NOWN = 1024
NKV = 2048
EPS = 1e-6
SCALE = 0.125
BIG = 32768.0
IN_COLS = 8448
C_QA, C_KA, C_VA, C_QB, C_KB, C_VB, C_GA, C_GB = 0, 1024, 1152, 1280, 2304, 3328, 4352, 6400
SLOPES = [2.0 ** (-(h + 1) / 2.0) for h in range(16)]
ARENA = 211968


class Buf:
    __slots__ = ("name", "lw", "lr", "excl")

    def __init__(self, name, excl=False):
        self.name = name
        self.lw = None
        self.lr = {}
        self.excl = excl


class DSem:
    def __init__(self, h):
        self.h = h
        self.count = 0


class Op:
    __slots__ = ("eng", "fn", "deps", "signal", "dsem", "val", "idx")


ENGS = ("pe", "act", "dve", "pool", "sp")


class Prog:
    def __init__(self):
        self.ops = []

    @staticmethod
    def _need(p, eng, is_dma, raw):
        if p.dsem is not None or is_dma:
            return True
        if p.eng != eng:
            return True
        if eng == "pe":
            return False
        return raw

    def add(self, eng, fn, reads=(), writes=(), dsem=None, ndma=1):
        idx = len(self.ops)
        op = Op()
        op.eng, op.fn, op.signal, op.dsem, op.idx, op.val = eng, fn, False, dsem, idx, None
        is_dma = dsem is not None
        key = ("d", idx) if is_dma else eng
        deps = set()
        for b in reads:
            w = b.lw
            if w is not None and self._need(self.ops[w], eng, is_dma, True):
                deps.add(w)
            if b.excl:
                for k2, r in b.lr.items():
                    if k2 != key:
                        deps.add(r)
        for b in writes:
            w = b.lw
            if w is not None and self._need(self.ops[w], eng, is_dma, False):
                deps.add(w)
            for r in b.lr.values():
                if self._need(self.ops[r], eng, is_dma, False):
                    deps.add(r)
        for b in reads:
            b.lr[key] = idx
        for b in writes:
            b.lw = idx
            b.lr = {}
        for d in deps:
            self.ops[d].signal = True
        op.deps = sorted(deps)
        if is_dma:
            dsem.count += 16 * ndma
            op.val = dsem.count
        self.ops.append(op)
        return op

    def emit(self, block, engsem):
        cnt = {}
        for op in self.ops:
            if op.dsem is None and op.signal:
                cnt[op.eng] = cnt.get(op.eng, 0) + 1
                op.val = cnt[op.eng]
        by = {e: [] for e in ENGS}
        for op in self.ops:
            by[op.eng].append(op)
        ops = self.ops

        def run(name):
            def body(e):
                waited = {}
                for op in by[name]:
                    for d in op.deps:
                        p = ops[d]
                        if p.dsem is not None:
                            sem, k = p.dsem.h, ("d", id(p.dsem))
                        else:
                            sem, k = engsem[p.eng], p.eng
                        if waited.get(k, 0) < p.val:
                            e.wait_ge(sem, p.val)
                            waited[k] = p.val
                    if op.fn is None:
                        continue
                    r = op.fn(e)
                    if op.dsem is not None:
                        for ins in (r if isinstance(r, (list, tuple)) else [r]):
                            ins.then_inc(op.dsem.h, 16)
                    elif op.signal:
                        r.then_inc(engsem[op.eng], 1)
            return body

        block.tensor(run("pe"))
        block.scalar(run("act"))
        block.vector(run("dve"))
        block.gpsimd(run("pool"))
        block.sync(run("sp"))


def _split3(a):
    a = a.astype(np.float64)
    hi = a.astype(NPBF)
    r = a - hi.astype(np.float64)
    mid = r.astype(NPBF)
    r = r - mid.astype(np.float64)
    lo = r.astype(NPBF)
    return hi, mid, lo


def _const_tables():
    c = {}
    c["ident_bf"] = np.eye(128, dtype=np.float32).astype(NPBF)
    c["ident_f"] = np.eye(128, dtype=np.float32)
    bd = np.zeros((128, 128), np.float32)
    bd[:64, :64] = 1.0 / 64
    bd[64:, 64:] = 1.0 / 64
    c["bd64"] = bd.astype(NPBF)
    k = np.arange(128)[:, None].astype(np.float64)
    q = np.arange(128)[None, :].astype(np.float64)
    da = q - k
    da = np.where(da >= 0, da, 1e9)
    db = q + 128 - k
    db = np.where(db < 128, db, 1e9)
    c["dsw"] = np.concatenate([db, da, db, da], axis=1).astype(np.float32)
    q2 = np.arange(256)[None, :]
    m0 = np.where(q2 >= np.arange(128)[:, None], 0.0, -1e9)
    m1 = np.where(q2 >= (np.arange(128)[:, None] + 128), 0.0, -1e9)
    c["mneg"] = np.concatenate([m0, m1], axis=1).astype(np.float32)
    tq = np.arange(NOWN).astype(np.float64)
    tk = (np.arange(NKV) - 1024).astype(np.float64)
    tabq = np.zeros((16, 6, NOWN), NPBF)
    tabk = np.zeros((16, 14, NKV), NPBF)
    ind = (np.arange(NKV)[None, :] // 256 == np.arange(8)[:, None]).astype(np.float32)
    for h in range(16):
        s = SLOPES[h]
        a = _split3(-s * tq / SCALE)
        b = _split3(s * tk / SCALE)
        for i in range(3):
            tabq[h, i] = a[i]
            tabq[h, 3 + i] = 1.0
            tabk[h, 8 + i] = 1.0
            tabk[h, 11 + i] = b[i]
        tabk[h, 0:8] = ind.astype(NPBF)
    c["tabq"] = tabq
    c["tabk"] = tabk
    return c


def _percore_tables(hf):
    pb = np.full((128, 8, 8), -1e30, np.float32)
    for t in range(8):
        own = 4 + t // 2
        for n in range(8):
            if n == own:
                pb[:, t, n] = 1e30
            elif n < own and (hf == 1 or n >= 4):
                pb[:, t, n] = 0.0
    pv = np.full((128, 1), float(hf), np.float32)
    return {"pastbias": pb.reshape(128, 64), "pvcol": pv}


def build_program(stage=99, debug=False):
    nc = bass.Bass("TRN2", target_bir_lowering=False)
    es = ExitStack()
    prog = Prog()
    dbg_outs = {}

    def din(name, shape, dt):
        return nc.dram_tensor(name, list(shape), dt, kind="ExternalInput").ap()

    xs = din("xs", [NKV, D], F32)
    w_in = din("w_in", [D, IN_COLS], F32)
    w_up_swa = din("w_up_swa", [1024, D], F32)
    w_up_moba = din("w_up_moba", [1024, D], F32)
    w_out = din("w_out", [D, D], F32)
    w_r = din("w_r", [D, 20], F32)
    w_gate_e = din("w_gate_e", [16, D, 512], F32)
    w_up_e = din("w_up_e", [16, D, 512], F32)
    w_down_e = din("w_down_e", [16, 512, D], F32)
    gmix_bc = din("gmix_bc", [128, D], F32)
    gffn_bc = din("gffn_bc", [128, D], F32)
    gcols_d = din("gcols", [128, 4], F32)
    sinks_bc = din("sinks_bc", [128, 16], F32)
    ident_bf_d = din("ident_bf", [128, 128], BF16)
    ident_f_d = din("ident_f", [128, 128], F32)
    bd64_d = din("bd64", [128, 128], BF16)
    dsw_d = din("dsw", [128, 512], F32)
    mneg_d = din("mneg", [128, 512], F32)
    tabq_d = din("tabq", [16, 6, NOWN], BF16)
    tabk_d = din("tabk", [16, 14, NKV], BF16)
    pastbias_d = din("pastbias", [128, 64], F32)
    pvcol_d = din("pvcol", [128, 1], F32)
    y = nc.dram_tensor("y", [NOWN, D], F32, kind="ExternalOutput").ap()

    arena = es.enter_context(nc.sbuf_tensor("arena", [128, ARENA], U8))
    psum = es.enter_context(nc.psum_tensor("psum", [128, 4096], F32))

    def PS(b, lo=0, hi=512):
        return psum[:, b * 512 + lo:b * 512 + hi]

    def PSB(b, lo=0, hi=1024):
        return psum[:, b * 512:(b + 1) * 512].bitcast(BF16)[:, lo:hi]

    B_ps = [Buf("ps%d" % i, excl=True) for i in range(8)]

    class Region:
        def __init__(self, base, size):
            self.base, self.size, self.off = base, size, 0

        def reset(self, off=0):
            self.off = off

        def alloc(self, shape, dt, at=None):
            nb = {F32: 4, BF16: 2}[dt]
            n = int(np.prod(shape[1:])) * nb
            n32 = (n + 31) // 32 * 32
            off = self.off if at is None else at
            assert off + n32 <= self.size, (off, n32, self.size)
            if at is None:
                self.off += n32
            v = arena[:, self.base + off:self.base + off + n].bitcast(dt)
            if len(shape) == 3:
                v = v.rearrange("p (a b) -> p a b", b=shape[2])
            return v

    CONST_SZ = 6144
    R1_SZ = 65536
    R2_SZ = 32768
    RC = Region(0, CONST_SZ)
    R1 = Region(CONST_SZ, R1_SZ)
    R2 = Region(CONST_SZ + R1_SZ, R2_SZ)
    R3 = Region(CONST_SZ + R1_SZ + R2_SZ, ARENA - CONST_SZ - R1_SZ - R2_SZ)

    sem_id = [0]

    def new_sem():
        sem_id[0] += 1
        return DSem(es.enter_context(nc.semaphore("s%d" % sem_id[0])))

    engsem = {e: es.enter_context(nc.semaphore("eng_" + e)) for e in ENGS}

    ident_bf = RC.alloc([128, 128], BF16)
    ident_f = RC.alloc([128, 128], F32)
    bd64 = RC.alloc([128, 128], BF16)
    gcols = RC.alloc([128, 4], F32)
    epscol = RC.alloc([128, 1], F32)
    expsink = RC.alloc([128, 16], F32)
    pvcol = RC.alloc([128, 1], F32)
    pastbias = RC.alloc([128, 64], F32)
    dsw = RC.alloc([128, 512], F32)
    mneg = RC.alloc([128, 512], F32)
    ss1 = RC.alloc([128, 16], F32)
    ln1 = RC.alloc([128, 16], F32)
    rs1 = RC.alloc([128, 16], F32)
    B_const = Buf("const")
    S_const = new_sem()
    const_loads = [(ident_bf, ident_bf_d), (ident_f, ident_f_d), (bd64, bd64_d), (gcols, gcols_d),
                   (expsink, sinks_bc), (pvcol, pvcol_d), (pastbias, pastbias_d), (dsw, dsw_d), (mneg, mneg_d)]

    def _ld_consts(e):
        return [e.dma_start(out=o, in_=i[:, :]) for o, i in const_loads]
    prog.add("sp", _ld_consts, writes=[B_const], dsem=S_const, ndma=len(const_loads))
    B_eps = Buf("eps")
    prog.add("dve", lambda e: e.memset(epscol, EPS), writes=[B_eps])
    B_ss1 = [Buf("ss1_%d" % t) for t in range(16)]
    prog.add("dve", lambda e: e.memset(ss1, 0.0), writes=B_ss1)
    B_expsink = Buf("expsink")
    prog.add("act", lambda e: e.activation(out=expsink, in_=expsink, func=AF.Exp), reads=[B_const], writes=[B_expsink])

    hTp = R1.alloc([128, 16, 1024], BF16)
    hTo = R1.alloc([128, 16, 1024], BF16)
    B_hTp, B_hTo = Buf("hTp"), Buf("hTo")
    attn_swa = R2.alloc([128, 8, 1024], BF16)
    attn_moba = R2.alloc([128, 8, 1024], BF16)
    B_attn_swa = [Buf("attn_swa%d" % i) for i in range(8)]
    B_attn_moba = [Buf("attn_moba%d" % i) for i in range(8)]

    NW = 6
    wslot = [R3.alloc([128, 16, 128], BF16) for _ in range(NW)]
    B_w = [Buf("w%d" % i) for i in range(NW)]
    S_w = [new_sem() for _ in range(NW)]
    QA = [[R3.alloc([128, 1024], BF16) for _ in range(2)] for _ in range(2)]
    B_QA = [[Buf("QA%d%d" % (i, j)) for j in range(2)] for i in range(2)]
    B_QAaug = [[Buf("QAaug%d%d" % (i, j)) for j in range(2)] for i in range(2)]
    S_QAaug = [[new_sem() for j in range(2)] for i in range(2)]
    off_KA = R3.off
    KA = [[R3.alloc([128, 2048], BF16) for _ in range(2)] for _ in range(2)]
    B_KA = [[Buf("KA%d%d" % (i, j)) for j in range(2)] for i in range(2)]
    B_KAaug = [[Buf("KAaug%d%d" % (i, j)) for j in range(2)] for i in range(2)]
    S_KAaug = [[new_sem() for j in range(2)] for i in range(2)]
    VA = [[R3.alloc([128, 16, 128], BF16) for _ in range(2)] for _ in range(2)]
    B_VA = [[Buf("VA%d%d" % (i, j)) for j in range(2)] for i in range(2)]
    KS = [R3.alloc([128, 1152], BF16) for _ in range(2)]
    B_KS = [Buf("KS%d" % i) for i in range(2)]
    VS = [R3.alloc([128, 9, 128], BF16) for _ in range(2)]
    B_VS = [Buf("VS%d" % i) for i in range(2)]
    sqb = [R3.alloc([128, 512], BF16) for _ in range(2)]
    B_sqb = [Buf("sqb%d" % i) for i in range(2)]
    lnb = [R3.alloc([128, 512], F32) for _ in range(2)]
    B_lnb = [Buf("lnb%d" % i) for i in range(2)]
    rsb = [R3.alloc([128, 512], F32) for _ in range(2)]
    B_rsb = [Buf("rsb%d" % i) for i in range(2)]
    tmpB = [R3.alloc([128, 512], BF16) for _ in range(2)]
    B_tmpB = [Buf("tmpB%d" % i) for i in range(2)]
    off_Sp = R3.off
    Sp = [R3.alloc([128, 512], F32) for _ in range(3)]
    B_Sp = [Buf("Sp%d" % i) for i in range(3)]
    NPT = 4
    Pt = [R3.alloc([128, 512], BF16) for _ in range(NPT)]
    B_Pt = [Buf("Pt%d" % i) for i in range(NPT)]
    Rt = [R3.alloc([128, 256], F32) for _ in range(2)]
    B_Rt = [Buf("Rt%d" % i) for i in range(2)]
    Rt2 = [R3.alloc([128, 256], F32) for _ in range(2)]
    tmpo = [R3.alloc([128, 256], BF16) for _ in range(2)]
    B_tmpo = [Buf("tmpo%d" % i) for i in range(2)]
    gm = R3.alloc([128, 8, 8], F32)
    m8 = R3.alloc([128, 8, 8], F32)
    selt = R3.alloc([128, 8, 8], F32)
    kmf = R3.alloc([128, 8], F32)
    kmb = R3.alloc([128, 8], BF16)
    B_gm, B_m8, B_selt, B_kmf, B_kmb = Buf("gm"), Buf("m8"), Buf("selt"), Buf("kmf"), Buf("kmb")
    stage_t = [R3.alloc([128, 8, 72], BF16) for _ in range(2)]
    B_stage = [Buf("stage%d" % i) for i in range(2)]
    sId = [R3.alloc([128, 128], F32) for _ in range(2)]
    B_sId = [Buf("sId%d" % i) for i in range(2)]
    attn_end = R3.off
    xt = [R3.alloc([128, D], F32, at=off_KA + i * 8192) for i in range(3)]
    B_xt = [Buf("xt%d" % i) for i in range(3)]
    S_xt = [new_sem() for _ in range(3)]
    gbc = R3.alloc([128, D], F32, at=off_KA + 3 * 8192)
    xn = [R3.alloc([128, D], BF16, at=off_KA + 4 * 8192 + i * 4096) for i in range(2)]
    B_xn = [Buf("xn%d" % i) for i in range(2)]
    junk = R3.alloc([128, D], BF16, at=off_Sp)
    B_junk = Buf("junk")
    B_gbc = Buf("gbc")
    S_gbc = new_sem()
    bar_scr = RC.alloc([128, 8], F32)

    def barrier(old, new):
        prog.add("dve", lambda e: e.memset(bar_scr, 0.0), writes=list(old) + list(new))

    prog.add("sp", lambda e: e.dma_start(out=gbc, in_=gmix_bc[:, :]), writes=[B_gbc], dsem=S_gbc)
    def partA1(t):
        sl = t % 3
        x2 = t % 2
        prog.add("sp", lambda e, t=t, sl=sl: e.dma_start(out=xt[sl], in_=xs[t * 128:(t + 1) * 128, :]),
                 writes=[B_xt[sl]], dsem=S_xt[sl])
        prog.add("act", lambda e, t=t, sl=sl: e.activation(out=junk, in_=xt[sl], func=AF.Square,
                                                           accum_out=ss1[:, t:t + 1]),
                 reads=[B_xt[sl], B_ss1[t]], writes=[B_junk, B_ss1[t]])
        prog.add("act", lambda e, t=t: e.activation(out=ln1[:, t:t + 1], in_=ss1[:, t:t + 1], func=AF.Ln,
                                                    scale=1.0 / D, bias=epscol[:, 0:1]),
                 reads=[B_ss1[t], B_eps], writes=[B_ss1[t]])
        prog.add("act", lambda e, t=t: e.activation(out=rs1[:, t:t + 1], in_=ln1[:, t:t + 1], func=AF.Exp, scale=-0.5),
                 reads=[B_ss1[t]], writes=[B_ss1[t]])
        prog.add("dve", lambda e, t=t, sl=sl, x2=x2: e.scalar_tensor_tensor(
            out=xn[x2], in0=xt[sl], scalar=rs1[:, t:t + 1], op0=ALU.mult, in1=gbc, op1=ALU.mult),
            reads=[B_xt[sl], B_ss1[t], B_gbc], writes=[B_xn[x2]])
    def partA2(t):
        x2 = t % 2
        dst = hTp if t < 8 else hTo
        Bdst = B_hTp if t < 8 else B_hTo
        tc = (t % 8) * 128
        for k in range(16):
            bank = 6 + k // 8
            prog.add("pe", lambda e, k=k, x2=x2, bank=bank: e.transpose(
                out=PSB(bank, (k % 8) * 128, (k % 8 + 1) * 128), in_=xn[x2][:, k * 128:(k + 1) * 128],
                identity=ident_bf), reads=[B_xn[x2], B_const], writes=[B_ps[bank]])
        prog.add("act", lambda e, dst=dst, tc=tc: e.activation(
            out=dst[:, 0:8, tc:tc + 128], in_=PSB(6).rearrange("p (a b) -> p a b", b=128), func=AF.Copy),
            reads=[B_ps[6]], writes=[Bdst])
        prog.add("dve", lambda e, dst=dst, tc=tc: e.tensor_copy(
            out=dst[:, 8:16, tc:tc + 128], in_=PSB(7).rearrange("p (a b) -> p a b", b=128)),
            reads=[B_ps[7]], writes=[Bdst])

    partA1(0)
    for t in range(16):
        if t + 1 < 16:
            partA1(t + 1)
        partA2(t)

    if debug and stage == 1:
        dbg_outs["hTp"] = (hTp, [128, 16 * 1024], BF16, [B_hTp])
        dbg_outs["hTo"] = (hTo, [128, 16 * 1024], BF16, [B_hTo])

    w_in_v = w_in.rearrange("(k p) n -> p k n", p=128)
    wring = [0]

    def load_wcols(c0):
        s = wring[0] % NW
        wring[0] += 1
        prog.add("pool", lambda e: e.dma_start(out=wslot[s], in_=w_in_v[:, :, c0:c0 + 128]),
                 writes=[B_w[s]], dsem=S_w[s])
        return s

    pbank = [0]
    nrm = [0]

    def proj_fm(s, src, Bsrc, lo, n):
        bank = pbank[0] % 2
        pbank[0] += 1
        for k in range(16):
            prog.add("pe", lambda e, k=k: e.matmul(PS(bank, 0, n), lhsT=wslot[s][:, k, :], rhs=src[:, k, lo:lo + n],
                                                   start=(k == 0), stop=(k == 15)),
                     reads=[B_w[s], Bsrc], writes=[B_ps[bank]])
        return bank

    def headnorm(bank, n, gidx, dstA, BA, dstB, BB):
        headnorm_b(headnorm_a(bank, n), bank, n, gidx, dstA, BA, dstB, BB)

    def headnorm_a(bank, n):
        i = nrm[0] % 2
        nrm[0] += 1
        prog.add("act", lambda e: e.activation(out=sqb[i][:, 0:n], in_=PS(bank, 0, n), func=AF.Square),
                 reads=[B_ps[bank]], writes=[B_sqb[i]])
        return i

    def headnorm_b(i, bank, n, gidx, dstA, BA, dstB, BB):
        prog.add("pe", lambda e: e.matmul(PS(2, 0, n), lhsT=bd64, rhs=sqb[i][:, 0:n], start=True, stop=True),
                 reads=[B_sqb[i], B_const], writes=[B_ps[2]])
        prog.add("act", lambda e: e.activation(out=lnb[i][:, 0:n], in_=PS(2, 0, n), func=AF.Ln, bias=epscol[:, 0:1]),
                 reads=[B_ps[2], B_eps], writes=[B_lnb[i]])
        prog.add("act", lambda e: e.activation(out=rsb[i][:, 0:n], in_=lnb[i][:, 0:n], func=AF.Exp, scale=-0.5),
                 reads=[B_lnb[i]], writes=[B_rsb[i]])
        prog.add("dve", lambda e: e.scalar_tensor_tensor(
            out=dstA, in0=PS(bank, 0, n)[0:64, :], scalar=gcols[0:64, gidx:gidx + 1], op0=ALU.mult,
            in1=rsb[i][0:64, 0:n], op1=ALU.mult), reads=[B_ps[bank], B_rsb[i], B_const], writes=[BA])
        prog.add("dve", lambda e: e.scalar_tensor_tensor(
            out=tmpB[i][64:128, 0:n], in0=PS(bank, 0, n)[64:128, :], scalar=gcols[64:128, gidx:gidx + 1], op0=ALU.mult,
            in1=rsb[i][64:128, 0:n], op1=ALU.mult), reads=[B_ps[bank], B_rsb[i], B_const], writes=[B_tmpB[i]])
        prog.add("dve", lambda e: e.tensor_copy(out=dstB, in_=tmpB[i][64:128, 0:n]),
                 reads=[B_tmpB[i]], writes=[BB])

    sbank = [0]
    swa_sb = [0]
    obank = [0]
    ptc = [0]
    rtc = [0]

    def finish_block(ob, nq, dst_pair, Bdst, parity, qlo, sink_h=None):
        r = rtc[0] % 2
        rtc[0] += 1
        if sink_h is not None:
            prog.add("act", lambda e: e.activation(out=Rt2[r][64:128, 0:nq], in_=PS(ob, 0, nq)[64:128, :], func=AF.Ln,
                                                   bias=expsink[64:128, sink_h:sink_h + 1]),
                     reads=[B_ps[ob], B_expsink], writes=[B_Rt[r]])
        else:
            prog.add("act", lambda e: e.activation(out=Rt2[r][64:128, 0:nq], in_=PS(ob, 0, nq)[64:128, :], func=AF.Ln),
                     reads=[B_ps[ob]], writes=[B_Rt[r]])
        prog.add("act", lambda e: e.activation(out=Rt2[r][64:128, 0:nq], in_=Rt2[r][64:128, 0:nq], func=AF.Exp, scale=-1.0),
                 reads=[B_Rt[r]], writes=[B_Rt[r]])
        prog.add("dve", lambda e: e.tensor_copy(out=Rt[r][0:64, 0:nq], in_=Rt2[r][64:128, 0:nq]),
                 reads=[B_Rt[r]], writes=[B_Rt[r]])
        if parity == 0:
            prog.add("dve", lambda e: e.tensor_tensor(out=dst_pair[0:64, qlo:qlo + nq], in0=PS(ob, 0, nq)[0:64, :],
                                                      in1=Rt[r][0:64, 0:nq], op=ALU.mult),
                     reads=[B_ps[ob], B_Rt[r]], writes=[Bdst])
        else:
            prog.add("dve", lambda e: e.tensor_tensor(out=tmpo[r][0:64, 0:nq], in0=PS(ob, 0, nq)[0:64, :],
                                                      in1=Rt[r][0:64, 0:nq], op=ALU.mult),
                     reads=[B_ps[ob], B_Rt[r]], writes=[B_tmpo[r]])
            prog.add("dve", lambda e: e.tensor_copy(out=dst_pair[64:128, qlo:qlo + nq], in_=tmpo[r][0:64, 0:nq]),
                     reads=[B_tmpo[r]], writes=[Bdst])

    def drain(g):
        if g is not None:
            for _ in g:
                pass

    def run_units(units, filler=None, every=3, depth=2):
        n = len(units)
        if not n:
            drain(filler)
            return
        for j in range(min(depth, n)):
            units[j]["S"]()
        for j in range(min(depth - 1, n)):
            units[j]["E"]()
        for i, u in enumerate(units):
            u["PV"]()
            if i + depth < n:
                units[i + depth]["S"]()
            if filler is not None and i % every == every - 1:
                next(filler, None)
            if i + depth - 1 < n:
                units[i + depth - 1]["E"]()
            if u.get("F") is not None:
                u["F"]()
        drain(filler)

    def swa_kv():
        for g in range(2):
            prog.add("dve", lambda e, g=g: e.memset(VS[g][:, :, 64:128], 1.0), writes=[B_VS[g]])
            prog.add("dve", lambda e, g=g: e.tensor_scalar(out=VS[g][:, 0:1, 64:128], in0=VS[g][:, 0:1, 64:128],
                                                           scalar1=pvcol[:, 0:1], scalar2=None, op0=ALU.mult),
                     reads=[B_const, B_VS[g]], writes=[B_VS[g]])
        sk = load_wcols(C_KA)
        sv = load_wcols(C_VA)
        for (src, Bsrc, lo, n, dlo) in ((hTp, B_hTp, 896, 128, 0), (hTo, B_hTo, 0, 512, 128), (hTo, B_hTo, 512, 512, 640)):
            bank = proj_fm(sk, src, Bsrc, lo, n)
            headnorm(bank, n, 1, KS[0][0:64, dlo:dlo + n], B_KS[0], KS[1][0:64, dlo:dlo + n], B_KS[1])
        for grp in ((7, 8, 9, 10), (11, 12, 13, 14), (15,)):
            bank = pbank[0] % 2
            pbank[0] += 1
            for j, t in enumerate(grp):
                src, Bsrc, tc = (hTp, B_hTp, t * 128) if t < 8 else (hTo, B_hTo, (t - 8) * 128)
                for k in range(16):
                    prog.add("pe", lambda e, k=k, j=j, src=src, tc=tc, bank=bank: e.matmul(
                        PS(bank, j * 128, (j + 1) * 128), lhsT=src[:, k, tc:tc + 128], rhs=wslot[sv][:, k, :],
                        start=(k == 0), stop=(k == 15)), reads=[B_w[sv], Bsrc], writes=[B_ps[bank]])
            for g in range(2):
                for j, t in enumerate(grp):
                    if t < 8:
                        prog.add("act", lambda e, g=g, j=j, t=t, bank=bank: e.activation(
                            out=VS[g][:, t - 7, 0:64], in_=PS(bank, j * 128 + g * 64, j * 128 + g * 64 + 64),
                            func=AF.Copy, scale=pvcol[:, 0:1]), reads=[B_ps[bank], B_const], writes=[B_VS[g]])
                    else:
                        prog.add("act", lambda e, g=g, j=j, t=t, bank=bank: e.activation(
                            out=VS[g][:, t - 7, 0:64], in_=PS(bank, j * 128 + g * 64, j * 128 + g * 64 + 64),
                            func=AF.Copy), reads=[B_ps[bank]], writes=[B_VS[g]])

    def swa_qproj(p, buf):
        s = load_wcols(C_QA + p * 128)
        pend = None
        for n in range(2):
            if pend is not None:
                pend()
            bank = proj_fm(s, hTo, B_hTo, n * 512, 512)
            pend = (lambda bank=bank, n=n: headnorm(
                bank, 512, 0, QA[buf][0][0:64, n * 512:(n + 1) * 512], B_QA[buf][0],
                QA[buf][1][0:64, n * 512:(n + 1) * 512], B_QA[buf][1]))
            yield
        pend()
        yield

    def swa_attn(p, buf, filler=None, every=3, depth=3):
        banks = (3, 4, 7) if depth == 3 else (3, 4)
        units = []
        for i in range(2):
            h = 2 * p + i
            g = h // 8
            q = QA[buf][i]
            Bq = B_QA[buf][i]
            xi = h % 2
            prog.add("act", lambda e, h=h, xi=xi: e.activation(out=sId[xi], in_=ident_f, func=AF.Copy,
                                                               scale=-SLOPES[h] / SCALE),
                     reads=[B_const], writes=[B_sId[xi]])
            for c in range(4):
                i0 = 8 + 2 * c - 7
                sb = banks[swa_sb[0] % len(banks)]
                swa_sb[0] += 1
                ob = 5 + obank[0] % 2
                obank[0] += 1
                pi = ptc[0] % NPT
                ptc[0] += 1

                def S(q=q, Bq=Bq, g=g, c=c, i0=i0, sb=sb, xi=xi):
                    qa = 2 * c * 128
                    prog.add("pe", lambda e: e.matmul(PS(sb), lhsT=sId[xi], rhs=dsw, start=True, stop=False),
                             reads=[B_sId[xi], B_const], writes=[B_ps[sb]])
                    for (olo, ohi, kt, qlo, qhi, lastmm) in ((0, 128, i0 - 1, qa, qa + 128, False),
                                                             (128, 384, i0, qa, qa + 256, False),
                                                             (384, 512, i0 + 1, qa + 128, qa + 256, True)):
                        prog.add("pe", lambda e, olo=olo, ohi=ohi, kt=kt, qlo=qlo, qhi=qhi, lastmm=lastmm: e.matmul(
                            PS(sb, olo, ohi), lhsT=KS[g][0:64, kt * 128:(kt + 1) * 128], rhs=q[0:64, qlo:qhi],
                            start=False, stop=lastmm), reads=[B_KS[g], Bq], writes=[B_ps[sb]])

                def E(h=h, sb=sb, pi=pi):
                    prog.add("act", lambda e: e.activation(out=Pt[pi], in_=PS(sb), func=AF.Exp, scale=SCALE),
                             reads=[B_ps[sb]], writes=[B_Pt[pi]])

                def PV(h=h, p=p, i=i, g=g, c=c, i0=i0, ob=ob, pi=pi):
                    for (olo, kt, plo, st, sp_) in ((0, i0 - 1, 0, True, False), (0, i0, 128, False, True),
                                                    (128, i0, 256, True, False), (128, i0 + 1, 384, False, True)):
                        prog.add("pe", lambda e, olo=olo, kt=kt, plo=plo, st=st, sp_=sp_: e.matmul(
                            PS(ob, olo, olo + 128), lhsT=VS[g][:, kt, :], rhs=Pt[pi][:, plo:plo + 128],
                            start=st, stop=sp_), reads=[B_VS[g], B_Pt[pi]], writes=[B_ps[ob]])

                def F(h=h, p=p, i=i, c=c, ob=ob):
                    finish_block(ob, 256, attn_swa[:, p, :], B_attn_swa[p], i, c * 256, sink_h=h)

                units.append({"S": S, "E": E, "PV": PV, "F": F})
        run_units(units, filler, every, depth=depth)

    def moba_init():
        for b in range(2):
            for i in range(2):
                prog.add("dve", lambda e, b=b, i=i: e.memset(VA[b][i][:, :, 64:128], 1.0),
                         writes=[B_VA[b][i]])
                prog.add("dve", lambda e, b=b, i=i: e.tensor_scalar(
                    out=VA[b][i][:, 0:8, 64:128], in0=VA[b][i][:, 0:8, 64:128], scalar1=pvcol[:, 0:1], scalar2=None,
                    op0=ALU.mult), reads=[B_const, B_VA[b][i]], writes=[B_VA[b][i]])
            prog.add("dve", lambda e, b=b: e.memset(stage_t[b], 0.0), writes=[B_stage[b]])

    def moba_proj(p, buf):
        sk = load_wcols(C_KB + p * 128)
        sv = load_wcols(C_VB + p * 128)
        sq = load_wcols(C_QB + p * 128)
        for i in range(2):
            h = 2 * p + i
            prog.add("sp", lambda e, i=i, h=h: e.dma_start(out=KA[buf][i][64:78, :], in_=tabk_d[h]),
                     writes=[B_KAaug[buf][i]], dsem=S_KAaug[buf][i])
            prog.add("sp", lambda e, i=i, h=h: e.dma_start(out=QA[buf][i][72:78, :], in_=tabq_d[h]),
                     writes=[B_QAaug[buf][i]], dsem=S_QAaug[buf][i])
        pend = None
        for (src, Bsrc, lo, dlo) in ((hTp, B_hTp, 0, 0), (hTp, B_hTp, 512, 512), (hTo, B_hTo, 0, 1024), (hTo, B_hTo, 512, 1536)):
            if pend is not None:
                pend()
            bank = proj_fm(sk, src, Bsrc, lo, 512)
            pend = (lambda bank=bank, dlo=dlo: headnorm(
                bank, 512, 3, KA[buf][0][0:64, dlo:dlo + 512], B_KA[buf][0], KA[buf][1][0:64, dlo:dlo + 512], B_KA[buf][1]))
            cmd = yield
            if cmd == "flush" and pend is not None:
                pend()
                pend = None
                yield
            elif pend is not None:
                ia = headnorm_a(bank, 512)
                pend = (lambda ia=ia, bank=bank, dlo=dlo: headnorm_b(
                    ia, bank, 512, 3, KA[buf][0][0:64, dlo:dlo + 512], B_KA[buf][0], KA[buf][1][0:64, dlo:dlo + 512],
                    B_KA[buf][1]))
                cmd = yield
                if cmd == "flush":
                    pend()
                    pend = None
                    yield
        for t0 in range(0, 16, 4):
            if pend is not None:
                pend()
            bank = pbank[0] % 2
            pbank[0] += 1
            for j in range(4):
                t = t0 + j
                src, Bsrc, tc = (hTp, B_hTp, t * 128) if t < 8 else (hTo, B_hTo, (t - 8) * 128)
                for k in range(16):
                    prog.add("pe", lambda e, k=k, j=j, src=src, tc=tc, bank=bank: e.matmul(
                        PS(bank, j * 128, (j + 1) * 128), lhsT=src[:, k, tc:tc + 128], rhs=wslot[sv][:, k, :],
                        start=(k == 0), stop=(k == 15)), reads=[B_w[sv], Bsrc], writes=[B_ps[bank]])

            def vcopy(bank=bank, t0=t0):
                for i in range(2):
                    src_ps = PS(bank).rearrange("p (a b) -> p a b", b=128)[:, :, i * 64:(i + 1) * 64]
                    if t0 < 8:
                        prog.add("act", lambda e, i=i, src_ps=src_ps: e.activation(
                            out=VA[buf][i][:, t0:t0 + 4, 0:64], in_=src_ps, func=AF.Copy, scale=pvcol[:, 0:1]),
                            reads=[B_ps[bank], B_const], writes=[B_VA[buf][i]])
                    else:
                        prog.add("act", lambda e, i=i, src_ps=src_ps: e.activation(
                            out=VA[buf][i][:, t0:t0 + 4, 0:64], in_=src_ps, func=AF.Copy),
                            reads=[B_ps[bank]], writes=[B_VA[buf][i]])
            pend = vcopy
            cmd = yield
            if cmd == "flush" and pend is not None:
                pend()
                pend = None
                yield
        for n in range(2):
            if pend is not None:
                pend()
            bank = proj_fm(sq, hTo, B_hTo, n * 512, 512)
            pend = (lambda bank=bank, n=n: headnorm(
                bank, 512, 2, QA[buf][0][0:64, n * 512:(n + 1) * 512], B_QA[buf][0],
                QA[buf][1][0:64, n * 512:(n + 1) * 512], B_QA[buf][1]))
            cmd = yield
            if cmd == "flush" and pend is not None:
                pend()
                pend = None
                yield
            elif pend is not None:
                ia = headnorm_a(bank, 512)
                pend = (lambda ia=ia, bank=bank, n=n: headnorm_b(
                    ia, bank, 512, 2, QA[buf][0][0:64, n * 512:(n + 1) * 512], B_QA[buf][0],
                    QA[buf][1][0:64, n * 512:(n + 1) * 512], B_QA[buf][1]))
                cmd = yield
                if cmd == "flush":
                    pend()
                    pend = None
                    yield
        if pend is not None:
            pend()
            pend = None
        yield
        for i in range(2):
            K_, Q_ = KA[buf][i], QA[buf][i]
            prog.add("dve", lambda e, K_=K_: e.tensor_reduce(
                out=kmf[0:64, :], in_=K_[0:64, :].rearrange("p (n l) -> p n l", l=256), axis=AX.X, op=ALU.add),
                reads=[B_KA[buf][i]], writes=[B_kmf])
            yield
            prog.add("dve", lambda e: e.tensor_copy(out=kmb[0:64, :], in_=kmf[0:64, :]), reads=[B_kmf], writes=[B_kmb])
            for t in range(8):
                prog.add("pe", lambda e, t=t, Q_=Q_: e.matmul(PS(2, 256 + t * 8, 256 + t * 8 + 8),
                                                              lhsT=Q_[0:64, t * 128:(t + 1) * 128], rhs=kmb[0:64, :],
                                                              start=True, stop=True),
                         reads=[B_QA[buf][i], B_kmb], writes=[B_ps[2]])
            prog.add("dve", lambda e: e.tensor_tensor(out=gm.rearrange("p a b -> p (a b)"), in0=PS(2, 256, 320),
                                                      in1=pastbias, op=ALU.add),
                     reads=[B_ps[2], B_const], writes=[B_gm])
            yield
            for t in range(8):
                prog.add("dve", lambda e, t=t: e.max(out=m8[:, t, :], in_=gm[:, t, :]), reads=[B_gm], writes=[B_m8])
            prog.add("dve", lambda e: e.tensor_tensor(out=selt, in0=gm, in1=m8[:, :, 3:4].to_broadcast([128, 8, 8]),
                                                      op=ALU.is_ge), reads=[B_gm, B_m8], writes=[B_selt])
            prog.add("dve", lambda e: e.tensor_scalar(out=stage_t[buf][:, :, 64:72], in0=selt, scalar1=BIG,
                                                      scalar2=-BIG, op0=ALU.mult, op1=ALU.add),
                     reads=[B_selt], writes=[B_stage[buf]])
            yield
            for r in range(2):
                for t4 in range(4):
                    t = r * 4 + t4
                    prog.add("pe", lambda e, t=t, t4=t4: e.transpose(
                        out=PSB(2, t4 * 128, (t4 + 1) * 128)[0:72, :], in_=stage_t[buf][:, t, :], identity=ident_bf),
                        reads=[B_stage[buf], B_const], writes=[B_ps[2]])
                prog.add("dve", lambda e, r=r, Q_=Q_: e.tensor_copy(out=Q_[64:72, r * 512:(r + 1) * 512],
                                                                    in_=PSB(2, 0, 512)[64:72, :]),
                         reads=[B_ps[2]], writes=[B_QA[buf][i]])
            yield

    def moba_attn(p, buf, filler=None, every=3):
        units = []
        for i in range(2):
            h = 2 * p + i
            K_, Q_, V_ = KA[buf][i], QA[buf][i], VA[buf][i]
            rd = [B_KA[buf][i], B_KAaug[buf][i], B_QA[buf][i], B_QAaug[buf][i]]
            for j in range(4):
                nkt = 8 + 2 * (j + 1)
                ob = 5 + obank[0] % 2
                obank[0] += 1
                for kt in range(0, nkt, 2):
                    sb = (3, 4, 7)[sbank[0] % 3]
                    si = sbank[0] % 3
                    sbank[0] += 1
                    pi = ptc[0] % NPT
                    ptc[0] += 1
                    diag = (kt == 8 + 2 * j)
                    last = (kt == nkt - 2)

                    def S(K_=K_, Q_=Q_, rd=rd, j=j, kt=kt, sb=sb):
                        for u in range(2):
                            prog.add("pe", lambda e, u=u: e.matmul(
                                PS(sb, u * 256, (u + 1) * 256), lhsT=K_[0:78, (kt + u) * 128:(kt + u + 1) * 128],
                                rhs=Q_[0:78, j * 256:(j + 1) * 256], start=True, stop=True), reads=rd, writes=[B_ps[sb]])

                    def E(diag=diag, sb=sb, si=si, pi=pi):
                        if diag:
                            prog.add("dve", lambda e: e.tensor_tensor(out=Sp[si], in0=PS(sb), in1=mneg, op=ALU.add),
                                     reads=[B_ps[sb], B_const], writes=[B_Sp[si]])
                            prog.add("act", lambda e: e.activation(out=Pt[pi], in_=Sp[si], func=AF.Exp, scale=SCALE),
                                     reads=[B_Sp[si]], writes=[B_Pt[pi]])
                        else:
                            prog.add("act", lambda e: e.activation(out=Pt[pi], in_=PS(sb), func=AF.Exp, scale=SCALE),
                                     reads=[B_ps[sb]], writes=[B_Pt[pi]])

                    def PV(V_=V_, i=i, p=p, j=j, kt=kt, ob=ob, pi=pi, last=last, buf=buf):
                        for u in range(2):
                            prog.add("pe", lambda e, u=u: e.matmul(
                                PS(ob, 0, 256), lhsT=V_[:, kt + u, :], rhs=Pt[pi][:, u * 256:(u + 1) * 256],
                                start=(kt == 0 and u == 0), stop=(last and u == 1)),
                                reads=[B_VA[buf][i], B_Pt[pi]], writes=[B_ps[ob]])

                    F = None
                    if last:
                        def F(i=i, p=p, j=j, ob=ob):
                            finish_block(ob, 256, attn_moba[:, p, :], B_attn_moba[p], i, j * 256)

                    units.append({"S": S, "E": E, "PV": PV, "F": F})
        run_units(units, filler, every, depth=3)

    if stage >= 2:
        barrier(B_xt + B_xn + [B_gbc, B_junk],
                [b for bb in B_KA for b in bb] + [b for bb in B_KAaug for b in bb] + [b for bb in B_VA for b in bb] +
                B_KS + B_VS + B_Sp + B_Pt)
        swa_kv()
        moba_init()
        drain(swa_qproj(0, 0))
        moba0 = moba_proj(0, 0) if stage >= 3 else None

        def chain2(a, b, nb):
            for _ in a:
                yield
            for _ in range(nb):
                if next(b, "end") == "end":
                    return
                yield
            try:
                b.send("flush")
            except StopIteration:
                pass

        for p in range(8):
            if p + 1 < 8:
                if moba0 is not None and p >= 4:
                    filler, every = chain2(swa_qproj(p + 1, (p + 1) % 2), moba0, 3), 1
                else:
                    filler, every = swa_qproj(p + 1, (p + 1) % 2), 2
            elif moba0 is not None:
                filler, every = moba0, 1
            else:
                filler, every = None, 1
            swa_attn(p, p % 2, filler, every, depth=3)
        if debug and stage == 2:
            dbg_outs["KS0"] = (KS[0], [128, 1152], BF16, [B_KS[0]])
            dbg_outs["KS1"] = (KS[1], [128, 1152], BF16, [B_KS[1]])
            dbg_outs["VS0"] = (VS[0], [128, 9 * 128], BF16, [B_VS[0]])
            dbg_outs["attn_swa"] = (attn_swa, [128, 8 * 1024], BF16, B_attn_swa)
    if stage >= 3:
        for p in range(8):
            filler = moba_proj(p + 1, (p + 1) % 2) if p + 1 < 8 else None
            moba_attn(p, p % 2, filler, 1)
        if debug and stage == 3:
            dbg_outs["attn_moba"] = (attn_moba, [128, 8 * 1024], BF16, B_attn_moba)
            dbg_outs["QA10"] = (QA[1][0], [128, 1024], BF16, [B_QA[1][0], B_QAaug[1][0]])
            dbg_outs["KA10"] = (KA[1][0], [128, 2048], BF16, [B_KA[1][0], B_KAaug[1][0]])
            dbg_outs["VA10"] = (VA[1][0], [128, 2048], BF16, [B_VA[1][0]])

    all_attn_bufs = (B_w + [b for bb in B_QA for b in bb] + [b for bb in B_QAaug for b in bb] +
                     [b for bb in B_KA for b in bb] + [b for bb in B_KAaug for b in bb] +
                     [b for bb in B_VA for b in bb] + B_KS + B_VS + B_sqb + B_lnb + B_rsb + B_tmpB + B_Sp + B_Pt +
                     B_Rt + B_tmpo + [B_gm, B_m8, B_selt, B_kmf, B_kmb] + B_stage + B_xt + B_xn + [B_junk, B_gbc])
    if stage >= 4:
        R3.reset()
        gslot = [[R3.alloc([128, 16, 128], BF16) for _ in range(2)] for _ in range(2)]
        uslot = [[R3.alloc([128, 8, 128], BF16) for _ in range(2)] for _ in range(2)]
        B_gs = [Buf("gs%d" % i) for i in range(2)]
        S_gs = [new_sem() for _ in range(2)]
        mergedT = R3.alloc([128, 16, 1024], BF16)
        B_merged = [Buf("merged%d" % i) for i in range(16)]
        sga = [R3.alloc([128, 512], F32) for _ in range(2)]
        sgb = [R3.alloc([128, 512], F32) for _ in range(2)]
        tmul = [R3.alloc([128, 512], F32) for _ in range(2)]
        B_sga = [Buf("sga%d" % i) for i in range(2)]
        B_sgb = [Buf("sgb%d" % i) for i in range(2)]
        B_tmul = [Buf("tmul%d" % i) for i in range(2)]
        NWO = 3
        woslot = [R3.alloc([128, 16, 256], BF16) for _ in range(NWO)]
        B_wo = [Buf("wo%d" % i) for i in range(NWO)]
        S_wo = [new_sem() for _ in range(NWO)]
        assert R3.off <= R3.size
        barrier(all_attn_bufs, B_gs + B_merged + B_sga + B_sgb + B_tmul + B_wo)
        first = [True]
        wus_v = w_up_swa.rearrange("(k p) n -> p k n", p=128)
        wum_v = w_up_moba.rearrange("(k p) n -> p k n", p=128)
        wo_v = w_out.rearrange("(k p) n -> p k n", p=128)
        cnt4 = [0]
        for c in range(16):
            st_ = c % 2
            extra = []

            def _ld(e, c=c, st_=st_):
                return [e.dma_start(out=gslot[st_][0], in_=w_in_v[:, :, C_GA + c * 128:C_GA + (c + 1) * 128]),
                        e.dma_start(out=gslot[st_][1], in_=w_in_v[:, :, C_GB + c * 128:C_GB + (c + 1) * 128]),
                        e.dma_start(out=uslot[st_][0], in_=wus_v[:, :, c * 128:(c + 1) * 128]),
                        e.dma_start(out=uslot[st_][1], in_=wum_v[:, :, c * 128:(c + 1) * 128])]
            prog.add("pool", _ld, writes=[B_gs[st_]] + extra, dsem=S_gs[st_], ndma=4)
            for n in range(2):
                alt = cnt4[0] % 2
                cnt4[0] += 1
                bga, bgb, bya, byb = ((0, 1, 3, 4), (2, 5, 6, 7))[alt]
                tl = n * 512
                for (bank, wt) in ((bga, gslot[st_][0]), (bgb, gslot[st_][1])):
                    for k in range(16):
                        prog.add("pe", lambda e, k=k, bank=bank, wt=wt, tl=tl: e.matmul(
                            PS(bank), lhsT=wt[:, k, :], rhs=hTo[:, k, tl:tl + 512], start=(k == 0), stop=(k == 15)),
                            reads=[B_gs[st_], B_hTo], writes=[B_ps[bank]])
                for (bank, wt, at_, Bat) in ((bya, uslot[st_][0], attn_swa, B_attn_swa), (byb, uslot[st_][1], attn_moba, B_attn_moba)):
                    for k in range(8):
                        prog.add("pe", lambda e, k=k, bank=bank, wt=wt, at_=at_, tl=tl: e.matmul(
                            PS(bank), lhsT=wt[:, k, :], rhs=at_[:, k, tl:tl + 512], start=(k == 0), stop=(k == 7)),
                            reads=[B_gs[st_]] + Bat, writes=[B_ps[bank]])
                prog.add("act", lambda e, bga=bga, alt=alt: e.activation(out=sga[alt], in_=PS(bga), func=AF.Sigmoid),
                         reads=[B_ps[bga]], writes=[B_sga[alt]])
                prog.add("act", lambda e, bgb=bgb, alt=alt: e.activation(out=sgb[alt], in_=PS(bgb), func=AF.Sigmoid),
                         reads=[B_ps[bgb]], writes=[B_sgb[alt]])
                prog.add("dve", lambda e, bya=bya, alt=alt: e.tensor_tensor(out=tmul[alt], in0=PS(bya), in1=sga[alt], op=ALU.mult),
                         reads=[B_ps[bya], B_sga[alt]], writes=[B_tmul[alt]])
                prog.add("dve", lambda e, byb=byb, alt=alt: e.tensor_tensor(out=sgb[alt], in0=PS(byb), in1=sgb[alt], op=ALU.mult),
                         reads=[B_ps[byb], B_sgb[alt]], writes=[B_sgb[alt]])
                prog.add("dve", lambda e, c=c, tl=tl, alt=alt: e.tensor_tensor(out=mergedT[:, c, tl:tl + 512], in0=tmul[alt],
                                                                               in1=sgb[alt], op=ALU.add),
                         reads=[B_tmul[alt], B_sgb[alt]], writes=[B_merged[c]])
        if debug and stage == 4:
            dbg_outs["mergedT"] = (mergedT, [128, 16 * 1024], BF16, B_merged)

    if stage >= 5:
        R1.reset()
        x1 = R1.alloc([128, 8, D], F32)
        B_x1 = [Buf("x1_%d" % t) for t in range(8)]
        S_x1 = new_sem()
        xs_own = xs[NOWN:NKV, :].rearrange("(t p) d -> p t d", p=128)

        def _ldx(e):
            return [e.dma_start(out=x1[:, t, :], in_=xs_own[:, t, :]) for t in range(8)]
        prog.add("sp", _ldx, writes=B_x1 + [B_hTp, B_hTo], dsem=S_x1, ndma=8)
        ob2 = [0]
        for cg in range(8):
            s = cg % NWO
            prog.add("pool", lambda e, cg=cg, s=s: e.dma_start(out=woslot[s], in_=wo_v[:, :, cg * 256:(cg + 1) * 256]),
                     writes=[B_wo[s]], dsem=S_wo[s])
            for t in range(8):
                bank = ob2[0] % 8
                ob2[0] += 1
                for k in range(16):
                    prog.add("pe", lambda e, k=k, t=t, s=s, bank=bank: e.matmul(
                        PS(bank, 0, 256), lhsT=mergedT[:, k, t * 128:(t + 1) * 128], rhs=woslot[s][:, k, :],
                        start=(k == 0), stop=(k == 15)), reads=[B_wo[s]] + B_merged, writes=[B_ps[bank]])
                prog.add("dve", lambda e, t=t, cg=cg, bank=bank: e.tensor_tensor(
                    out=x1[:, t, cg * 256:(cg + 1) * 256], in0=PS(bank, 0, 256), in1=x1[:, t, cg * 256:(cg + 1) * 256],
                    op=ALU.add), reads=[B_ps[bank], B_x1[t]], writes=[B_x1[t]])
        if debug and stage == 5:
            dbg_outs["x1"] = (x1, [128, 8 * D], F32, B_x1)

    if stage >= 6:
        R2.reset()
        h2T = R2.alloc([128, 16, 1024], BF16)
        B_h2T = Buf("h2T")
        phaseB_bufs = B_gs + B_merged + B_sga + B_sgb + B_tmul + B_wo
        R3.reset()
        NWE = 4
        ering = [R3.alloc([128, 8192], BF16) for _ in range(NWE)]
        B_er = [Buf("er%d" % i) for i in range(NWE)]
        S_er = [new_sem() for _ in range(NWE)]
        hidT = [R3.alloc([128, 4, 1024], BF16) for _ in range(2)]
        B_hid = [[Buf("hid%d_%d" % (i, n)) for n in range(2)] for i in range(2)]
        sg = [R3.alloc([128, 512], F32) for _ in range(2)]
        B_sg = [Buf("sg%d" % i) for i in range(2)]
        comb = R3.alloc([128, 8, 16], F32)
        B_comb = Buf("comb")
        wr_sb = R3.alloc([128, 16, 20], F32)
        B_wr = Buf("wr")
        S_wr = new_sem()
        L_all = R3.alloc([128, 8, 20], F32)
        B_L = Buf("L")
        ss2 = R3.alloc([128, 8], F32)
        ln2 = R3.alloc([128, 8], F32)
        rs2 = R3.alloc([128, 8], F32)
        B_ss2l = [Buf("ss2_%d" % t) for t in range(8)]
        B_ss2 = Buf("ss2")
        rt_small = [R3.alloc([128, 8, 16], F32) for _ in range(8)]
        B_rts = Buf("rts")
        moe_end = R3.off
        assert R3.off <= R3.size, (R3.off, R3.size)
        gbc2 = R3.alloc([128, D], F32, at=16384)
        h2f = [R3.alloc([128, D], F32, at=16384 + 8192 + i * 8192) for i in range(2)]
        B_h2f = [Buf("h2f%d" % i) for i in range(2)]
        h2Tf = [R3.alloc([128, 16, 128], F32, at=16384 + 3 * 8192 + i * 8192) for i in range(2)]
        B_h2Tf = [Buf("h2Tf%d" % i) for i in range(2)]
        junk2 = R3.alloc([128, D], BF16, at=16384 + 5 * 8192)
        B_junk2 = Buf("junk2")
        B_gbc2 = Buf("gbc2")
        S_gbc2 = new_sem()
        B_scrC = [B_er[1], B_er[2], B_er[3]]

        barrier(phaseB_bufs, B_er + [b for bb in B_hid for b in bb] + B_sg + [B_comb, B_wr, B_L, B_ss2, B_rts, B_gbc2, B_junk2] +
                B_h2f + B_h2Tf)
        prog.add("sp", lambda e: e.dma_start(out=gbc2, in_=gffn_bc[:, :]), writes=[B_gbc2], dsem=S_gbc2)
        prog.add("sp", lambda e: e.dma_start(out=wr_sb, in_=w_r.rearrange("(k p) n -> p k n", p=128)),
                 writes=[B_wr], dsem=S_wr)
        prog.add("dve", lambda e: e.memset(ss2, 0.0), writes=[B_ss2] + B_ss2l)
        pend_router = [None]
        def part1(t):
            i2 = t % 2
            prog.add("act", lambda e, t=t: e.activation(out=junk2, in_=x1[:, t, :], func=AF.Square, accum_out=ss2[:, t:t + 1]),
                     reads=[B_x1[t], B_ss2l[t], B_gbc2], writes=[B_junk2, B_ss2l[t]])
            prog.add("act", lambda e, t=t: e.activation(out=ln2[:, t:t + 1], in_=ss2[:, t:t + 1], func=AF.Ln, scale=1.0 / D,
                                                        bias=epscol[:, 0:1]), reads=[B_ss2l[t], B_eps], writes=[B_ss2l[t]])
            prog.add("act", lambda e, t=t: e.activation(out=rs2[:, t:t + 1], in_=ln2[:, t:t + 1], func=AF.Exp, scale=-0.5),
                     reads=[B_ss2l[t]], writes=[B_ss2l[t]])
            prog.add("dve", lambda e, t=t, i2=i2: e.scalar_tensor_tensor(
                out=h2f[i2], in0=x1[:, t, :], scalar=rs2[:, t:t + 1], op0=ALU.mult, in1=gbc2, op1=ALU.mult),
                reads=[B_x1[t], B_ss2l[t], B_gbc2], writes=[B_h2f[i2]])
        def part2(t):
            i2 = t % 2
            for q4 in range(4):
                bank = (t * 4 + q4) % 4
                for kk in range(4):
                    k = q4 * 4 + kk
                    prog.add("pe", lambda e, k=k, kk=kk, bank=bank, i2=i2: e.transpose(
                        out=PS(bank, kk * 128, (kk + 1) * 128), in_=h2f[i2][:, k * 128:(k + 1) * 128], identity=ident_f),
                        reads=[B_h2f[i2], B_const], writes=[B_ps[bank]])
                prog.add("act", lambda e, q4=q4, bank=bank, i2=i2: e.activation(
                    out=h2Tf[i2][:, q4 * 4:(q4 + 1) * 4, :], in_=PS(bank).rearrange("p (a b) -> p a b", b=128), func=AF.Copy),
                    reads=[B_ps[bank]], writes=[B_h2Tf[i2]])
                prog.add("dve", lambda e, q4=q4, i2=i2, t=t: e.tensor_copy(
                    out=h2T[:, q4 * 4:(q4 + 1) * 4, t * 128:(t + 1) * 128], in_=h2Tf[i2][:, q4 * 4:(q4 + 1) * 4, :]),
                    reads=[B_h2Tf[i2]], writes=[B_h2T] + (B_attn_swa + B_attn_moba if (t == 0 and q4 == 0) else []))
            def router(t=t, i2=i2):
                for k in range(16):
                    prog.add("pe", lambda e, k=k: e.matmul(PS(4 + t % 2, 0, 20), lhsT=h2Tf[i2][:, k, :], rhs=wr_sb[:, k, :],
                                                           start=(k == 0), stop=(k == 15)),
                             reads=[B_h2Tf[i2], B_wr], writes=[B_ps[4 + t % 2]])
                prog.add("dve", lambda e: e.tensor_copy(out=L_all[:, t, :], in_=PS(4 + t % 2, 0, 20)),
                         reads=[B_ps[4 + t % 2]], writes=[B_L])
            if pend_router[0] is not None:
                pend_router[0]()
            pend_router[0] = router
        part1(0)
        for t in range(8):
            if t + 1 < 8:
                part1(t + 1)
            part2(t)
        pend_router[0]()
        lg = L_all[:, :, 0:4]
        le = L_all[:, :, 4:20]
        mg, ohg, tmp16, sl, l1, msk, l2, ex, exm, den, gp, sumg, wexp, junk8 = (None,) * 14
        mg = rt_small[0][:, :, 0:1]
        ohg = rt_small[0][:, :, 4:8]
        sumg = rt_small[0][:, :, 8:9]
        gp = rt_small[0][:, :, 9:10]
        l1 = rt_small[0][:, :, 10:11]
        l2 = rt_small[0][:, :, 11:12]
        den = rt_small[0][:, :, 12:13]
        fac = rt_small[0][:, :, 13:14]
        tmp16 = rt_small[1]
        sl = rt_small[2][:, :, 0:4]
        msk = rt_small[2][:, :, 4:8]
        sl2 = rt_small[2][:, :, 8:12]
        ex = rt_small[3][:, :, 0:4]
        exm = rt_small[3][:, :, 4:8]
        wexp = rt_small[3][:, :, 8:12]
        eg = rt_small[4][:, :, 0:4]
        dgl = rt_small[4][:, :, 4:8]
        dsl = rt_small[4][:, :, 8:12]

        def dv(fn, r=(B_L, B_rts), w=(B_rts,)):
            prog.add("dve", fn, reads=list(r), writes=list(w))
        dv(lambda e: e.tensor_reduce(out=mg, in_=lg, axis=AX.X, op=ALU.max))
        dv(lambda e: e.tensor_tensor(out=ohg, in0=lg, in1=mg.to_broadcast([128, 8, 4]), op=ALU.is_ge))
        dv(lambda e: e.tensor_tensor(out=dgl, in0=lg, in1=mg.to_broadcast([128, 8, 4]), op=ALU.subtract))
        prog.add("act", lambda e: e.activation(out=eg, in_=dgl, func=AF.Exp), reads=[B_rts], writes=[B_rts])
        dv(lambda e: e.tensor_reduce(out=sumg, in_=eg, axis=AX.X, op=ALU.add))
        dv(lambda e: e.reciprocal(out=gp, in_=sumg))
        dv(lambda e: e.tensor_tensor(out=tmp16.rearrange("p t (g e) -> p t g e", e=4),
                                     in0=le.rearrange("p t (g e) -> p t g e", e=4),
                                     in1=ohg.unsqueeze(3).to_broadcast([128, 8, 4, 4]), op=ALU.mult))
        dv(lambda e: e.tensor_reduce(out=sl, in_=tmp16.rearrange("p t (g e) -> p t e g", e=4), axis=AX.X, op=ALU.add))
        dv(lambda e: e.tensor_reduce(out=l1, in_=sl, axis=AX.X, op=ALU.max))
        dv(lambda e: e.tensor_tensor(out=msk, in0=sl, in1=l1.to_broadcast([128, 8, 4]), op=ALU.is_ge))
        dv(lambda e: e.scalar_tensor_tensor(out=sl2, in0=msk, scalar=-1e30, op0=ALU.mult, in1=sl, op1=ALU.add))
        dv(lambda e: e.tensor_reduce(out=l2, in_=sl2, axis=AX.X, op=ALU.max))
        dv(lambda e: e.tensor_tensor(out=msk, in0=sl, in1=l2.to_broadcast([128, 8, 4]), op=ALU.is_ge))
        dv(lambda e: e.tensor_tensor(out=dsl, in0=sl, in1=l1.to_broadcast([128, 8, 4]), op=ALU.subtract))
        prog.add("act", lambda e: e.activation(out=ex, in_=dsl, func=AF.Exp), reads=[B_rts], writes=[B_rts])
        dv(lambda e: e.tensor_tensor(out=exm, in0=ex, in1=msk, op=ALU.mult))
        dv(lambda e: e.tensor_reduce(out=den, in_=exm, axis=AX.X, op=ALU.add))
        dv(lambda e: e.reciprocal(out=fac, in_=den))
        dv(lambda e: e.tensor_tensor(out=fac, in0=fac, in1=gp, op=ALU.mult))
        dv(lambda e: e.tensor_tensor(out=wexp, in0=exm, in1=fac.to_broadcast([128, 8, 4]), op=ALU.mult))
        dv(lambda e: e.tensor_tensor(out=comb.rearrange("p t (g e) -> p t g e", e=4),
                                     in0=ohg.unsqueeze(3).to_broadcast([128, 8, 4, 4]),
                                     in1=wexp.unsqueeze(2).to_broadcast([128, 8, 4, 4]), op=ALU.mult),
           w=(B_rts, B_comb))
        if debug and stage == 6:
            dbg_outs["h2T"] = (h2T, [128, 16 * 1024], BF16, [B_h2T])
            dbg_outs["L_all"] = (L_all, [128, 8 * 20], F32, [B_L])
            dbg_outs["comb"] = (comb, [128, 8 * 16], F32, [B_comb])

    if stage >= 7:
        er = [0]

        def load_e(dram_ap, shape3):
            s = er[0] % NWE
            er[0] += 1
            view = ering[s].rearrange("p (a b) -> p a b", b=shape3[2])
            extra = B_scrC_users if s in (1, 2, 3) and er[0] <= NWE else []
            prog.add("pool", lambda e: e.dma_start(out=view, in_=dram_ap), writes=[B_er[s]] + extra, dsem=S_er[s])
            return s, view
        B_scrC_users = [B_gbc2, B_junk2] + B_h2f + B_h2Tf
        gu = [0]
        yb = [0]
        for ex_i in range(16):
            hb = ex_i % 2
            s_g, Wg = load_e(w_gate_e[ex_i].rearrange("(k p) f -> p k f", p=128), [128, 16, 512])
            s_u, Wu = load_e(w_up_e[ex_i].rearrange("(k p) f -> p k f", p=128), [128, 16, 512])
            s_d, Wd = load_e(w_down_e[ex_i].rearrange("(k p) d -> p k d", p=128), [128, 4, 2048])
            for n in range(2):
                for fc in range(4):
                    alt = gu[0] % 2
                    gu[0] += 1
                    bg, bu = (0, 1) if alt == 0 else (2, 3)
                    for (bank, W_, s_) in ((bg, Wg, s_g), (bu, Wu, s_u)):
                        for k in range(16):
                            prog.add("pe", lambda e, k=k, bank=bank, W_=W_, fc=fc, n=n: e.matmul(
                                PS(bank), lhsT=W_[:, k, fc * 128:(fc + 1) * 128], rhs=h2T[:, k, n * 512:(n + 1) * 512],
                                start=(k == 0), stop=(k == 15)), reads=[B_er[s_], B_h2T], writes=[B_ps[bank]])
                    prog.add("act", lambda e, bg=bg, alt=alt: e.activation(out=sg[alt], in_=PS(bg), func=AF.Silu),
                             reads=[B_ps[bg]], writes=[B_sg[alt]])
                    prog.add("dve", lambda e, bu=bu, alt=alt, hb=hb, fc=fc, n=n: e.tensor_tensor(
                        out=hidT[hb][:, fc, n * 512:(n + 1) * 512], in0=PS(bu), in1=sg[alt], op=ALU.mult),
                        reads=[B_ps[bu], B_sg[alt]], writes=[B_hid[hb][n]])
            for t in range(8):
                for half in range(2):
                    b0 = 4 + 2 * (yb[0] % 2)
                    yb[0] += 1
                    for bb in range(2):
                        col = half * 1024 + bb * 512
                        for fc in range(4):
                            prog.add("pe", lambda e, fc=fc, t=t, col=col, bank=b0 + bb, hb=hb, Wd=Wd: e.matmul(
                                PS(bank), lhsT=hidT[hb][:, fc, t * 128:(t + 1) * 128], rhs=Wd[:, fc, col:col + 512],
                                start=(fc == 0), stop=(fc == 3)), reads=[B_er[s_d], B_hid[hb][t // 4]],
                                writes=[B_ps[b0 + bb]])
                    prog.add("dve", lambda e, t=t, half=half, b0=b0, ex_i=ex_i: e.scalar_tensor_tensor(
                        out=x1[:, t, half * 1024:(half + 1) * 1024], in0=psum[:, b0 * 512:(b0 + 2) * 512],
                        scalar=comb[:, t, ex_i:ex_i + 1], op0=ALU.mult, in1=x1[:, t, half * 1024:(half + 1) * 1024],
                        op1=ALU.add), reads=[B_ps[b0], B_ps[b0 + 1], B_comb, B_x1[t]], writes=[B_x1[t]])
        B_y = Buf("y")
        S_y = new_sem()
        y_v = y.rearrange("(t p) d -> p t d", p=128)
        for t in range(8):
            prog.add("sp", lambda e, t=t: e.dma_start(out=y_v[:, t, :], in_=x1[:, t, :]), reads=[B_x1[t]], writes=[B_y],
                     dsem=S_y)
        prog.add("sp", None, reads=[B_y])

    if debug:
        B_dbg = Buf("dbg")
        S_dbg = new_sem()
        for name, (ap, shape, dt, bufs) in dbg_outs.items():
            o = nc.dram_tensor("dbg_" + name, list(shape), dt, kind="ExternalOutput").ap()
            src = ap
            if len(ap.shape) == 3:
                src = ap.rearrange("p a b -> p (a b)")
            prog.add("sp", lambda e, o=o, src=src: e.dma_start(out=o[:, :], in_=src), reads=bufs, writes=[B_dbg], dsem=S_dbg)
        prog.add("sp", None, reads=[B_dbg])
        if stage < 7:
            pass

    block = es.enter_context(nc.Block())
    prog.emit(block, engsem)
    es.close()
    return nc, list(dbg_outs.keys())


_CONSTS = None


def _prepare_inputs(inputs, cores):
    global _CONSTS
    if _CONSTS is None:
        _CONSTS = _const_tables()
    c = _CONSTS
    f = lambda a: np.ascontiguousarray(np.asarray(a, dtype=np.float32))
    x = f(inputs["x"])
    shared = {
        "w_in": f(inputs["w_in"]), "w_up_swa": f(inputs["w_up_swa"]), "w_up_moba": f(inputs["w_up_moba"]),
        "w_out": f(inputs["w_out"]),
        "w_r": np.ascontiguousarray(np.concatenate(
            [f(inputs["w_router_group"]), f(inputs["w_router_expert"]).transpose(1, 0, 2).reshape(D, 16)], axis=1)),
        "w_gate_e": f(inputs["w_gate_e"]), "w_up_e": f(inputs["w_up_e"]), "w_down_e": f(inputs["w_down_e"]),
        "gmix_bc": np.ascontiguousarray(np.broadcast_to(f(inputs["g_mix"])[None, :], (128, D))),
        "gffn_bc": np.ascontiguousarray(np.broadcast_to(f(inputs["g_ffn"])[None, :], (128, D))),
        "gcols": np.ascontiguousarray(np.stack(
            [np.tile(f(inputs[k]), 2) for k in ("q_norm_swa", "k_norm_swa", "q_norm_moba", "k_norm_moba")], axis=1)),
        "sinks_bc": np.ascontiguousarray(np.broadcast_to(f(inputs["sinks"])[None, :], (128, 16))),
        "ident_bf": c["ident_bf"], "ident_f": c["ident_f"], "bd64": c["bd64"], "dsw": c["dsw"], "mneg": c["mneg"],
        "tabq": c["tabq"], "tabk": c["tabk"],
    }
    in_maps = []
    for cid in cores:
        b, hf = cid // 2, cid % 2
        if hf == 1:
            xs_ = x[b]
        else:
            xs_ = np.concatenate([x[b, NOWN:], x[b, :NOWN]], axis=0)
        m = dict(shared)
        m["xs"] = np.ascontiguousarray(xs_)
        m.update(_percore_tables(hf))
        in_maps.append(m)
    return in_maps


_NC_CACHE = {}


def kernel(**inputs):
    if "full" not in _NC_CACHE:
        _NC_CACHE["full"] = build_program(stage=99, debug=False)[0]
    nc = _NC_CACHE["full"]
    cores = list(range(8))
    in_maps = _prepare_inputs(inputs, cores)
    res = run_bass_kernel_spmd(nc, in_maps, core_ids=cores)
    out = np.empty((4, 2048, D), np.float32)
    for cid in cores:
        b, hf = cid // 2, cid % 2
        out[b, hf * NOWN:(hf + 1) * NOWN, :] = res.results[cid]["y"]
    return out
```
